# Optimizing a Trainium2 kernel written in Bass

```python
import math
import jax
import jax.numpy as jnp
from jax import lax
import numpy as np

D_MODEL = 1024
BATCH = 4
SEQ = 4096
DEPTH = 1

GRID_W = 64
CTX_LEN = 256
EPS = 1e-6
SSM_WIDTH = 512
SSM_GROUP = 16
SSM_GROUPS = SSM_WIDTH // SSM_GROUP
SSM_STATE = 64
DT_MIN = 1e-3
DT_MAX = 1e-1
MLA_HEADS = 8
QK_NOPE = 64
QK_ROPE = 32
QK_DIM = QK_NOPE + QK_ROPE
V_DIM = 64
Q_LORA = 384
KV_LORA = 256
ROPE_THETA = 10000.0
Q_BLOCK = 128
IN_SPLITS = (SSM_WIDTH, SSM_WIDTH + Q_LORA, SSM_WIDTH + Q_LORA + KV_LORA, SSM_WIDTH + Q_LORA + KV_LORA + QK_ROPE)
IN_COLS = IN_SPLITS[-1] + 2 * D_MODEL
N_EXPERTS = 16
EXPERT_FF = 2816
CAPACITY_FACTOR = 2

kernel_name = "hybrid_s5_mla_ec_moe_diffusion_block"


def rmsnorm(x, g):
    xf = x.astype(jnp.float32)
    y = xf * lax.rsqrt(jnp.mean(xf * xf, axis=-1, keepdims=True) + EPS)
    return (y * g.astype(jnp.float32)).astype(x.dtype)


def modulate(h, shift, scale):
    return h * (1 + scale) + shift


def axial_rope(n):
    rows = n // GRID_W
    row = jnp.repeat(jnp.arange(rows, dtype=jnp.float32), GRID_W)
    col = jnp.tile(jnp.arange(GRID_W, dtype=jnp.float32), rows)
    pairs = QK_ROPE // 4
    inv_freq = ROPE_THETA ** (-jnp.arange(pairs, dtype=jnp.float32) / pairs)
    ang = jnp.stack([row[:, None] * inv_freq, col[:, None] * inv_freq], axis=1)
    return jnp.cos(ang), jnp.sin(ang)


def apply_rope(x, cos, sin):
    xf = x.astype(jnp.float32).reshape(x.shape[:-1] + (2, QK_ROPE // 2))
    x1, x2 = jnp.split(xf, 2, axis=-1)
    cs, sn = cos[None, :, None], sin[None, :, None]
    out = jnp.concatenate([x1 * cs - x2 * sn, x2 * cs + x1 * sn], axis=-1)
    return out.reshape(x.shape).astype(x.dtype)


def mla_q(q_a, q_a_g, w_qb, q_norm_g, rope):
    b, n, _ = q_a.shape
    q = (rmsnorm(q_a, q_a_g) @ w_qb).reshape(b, n, MLA_HEADS, QK_DIM)
    q = rmsnorm(q, q_norm_g)
    if rope is not None:
        q = jnp.concatenate([q[..., :QK_NOPE], apply_rope(q[..., QK_NOPE:], *rope)], axis=-1)
    return q


def mla_kv(kv_a, k_r, kv_a_g, w_kvb, k_norm_g, rope):
    b, n, _ = kv_a.shape
    kv = (rmsnorm(kv_a, kv_a_g) @ w_kvb).reshape(b, n, MLA_HEADS, QK_NOPE + V_DIM)
    k_nope, v = jnp.split(kv, [QK_NOPE], axis=-1)
    k_rope = jnp.broadcast_to(k_r[:, :, None, :], (b, n, MLA_HEADS, QK_ROPE))
    k = rmsnorm(jnp.concatenate([k_nope, k_rope], axis=-1), k_norm_g)
    if rope is not None:
        k = jnp.concatenate([k[..., :QK_NOPE], apply_rope(k[..., QK_NOPE:], *rope)], axis=-1)
    return k, v


def attend(q, k, v):
    s = jnp.einsum('bqhd,bkhd->bhqk', q, k).astype(jnp.float32) * (QK_DIM ** -0.5)
    p = jax.nn.softmax(s, axis=-1).astype(v.dtype)
    out = jnp.einsum('bhqk,bkhd->bqhd', p, v)
    return out.reshape(out.shape[:2] + (MLA_HEADS * V_DIM,))


def attend_blocked(q, k, v):
    b, n = q.shape[:2]
    qb = q.reshape(b, n // Q_BLOCK, Q_BLOCK, MLA_HEADS, QK_DIM).transpose(1, 0, 2, 3, 4)
    out = lax.map(lambda qi: attend(qi, k, v), qb)
    return out.transpose(1, 0, 2, 3).reshape(b, n, MLA_HEADS * V_DIM)


def s5_discretise(lam_re, lam_im, log_dt, b_re, b_im):
    lam = lax.complex(lam_re.astype(jnp.float32), lam_im.astype(jnp.float32))
    dt = jnp.exp(log_dt.astype(jnp.float32))[:, None]
    lam_bar = jnp.exp(lam * dt)
    b_bar = ((lam_bar - 1) / lam)[..., None] * lax.complex(b_re.astype(jnp.float32), b_im.astype(jnp.float32))
    return lam_bar, b_bar


def s5_drive(u, b_bar):
    b, n, _ = u.shape
    ug = u.reshape(b, n, SSM_GROUPS, SSM_GROUP).astype(jnp.complex64)
    return jnp.einsum('bngh,gph->bngp', ug, b_bar)


def _linrec(e1, e2):
    a1, b1 = e1
    a2, b2 = e2
    return a1 * a2, a2 * b1 + b2


def s5_scan(bu, lam_bar, reverse, h0=None):
    n = bu.shape[1]
    a = jnp.broadcast_to(lam_bar, (1, n) + lam_bar.shape)
    a_cum, xs = lax.associative_scan(_linrec, (a, bu), reverse=reverse, axis=1)
    if h0 is not None:
        xs = xs + a_cum * h0[:, None]
    return xs


def s5_readout(xs, c_out):
    b, n = xs.shape[:2]
    return jnp.einsum('bngp,ghp->bngh', xs, c_out).real.reshape(b, n, SSM_WIDTH)


def s5_branch_out(y, w_glu, b_glu, w_ssm_o):
    y = jax.nn.gelu(y)
    y = y * jax.nn.sigmoid(y @ w_glu + b_glu)
    return y @ w_ssm_o


def merge_branches(ssm_out, attn_out, gates, w_out):
    g_ssm, g_attn = jnp.split(gates, 2, axis=-1)
    return (jax.nn.sigmoid(g_ssm) * ssm_out + jax.nn.sigmoid(g_attn) * attn_out) @ w_out


def expert_choice_ffn(h, w_router, w_gate, w_up, w_down):
    b, n, _ = h.shape
    cap = CAPACITY_FACTOR * n // N_EXPERTS
    aff = jax.nn.softmax((h @ w_router).astype(jnp.float32), axis=-1)
    gate, idx = lax.top_k(aff.transpose(0, 2, 1), cap)
    bidx = jnp.arange(b)[:, None, None]
    xs = h[bidx, idx]
    hid = jax.nn.silu(jnp.einsum('becd,edf->becf', xs, w_gate)) * jnp.einsum('becd,edf->becf', xs, w_up)
    ys = jnp.einsum('becf,efd->becd', hid, w_down) * gate[..., None].astype(h.dtype)
    return jnp.zeros_like(h).at[bidx, idx].add(ys)


def setup_inputs(seed: int = 0) -> dict:
    key = jax.random.key(seed)
    ks = iter(jax.random.split(key, 40))
    f32 = jnp.float32

    def nrm(shape, scale):
        return scale * jax.random.normal(next(ks), shape, f32)

    L, G, P, H = DEPTH, SSM_GROUPS, SSM_STATE, MLA_HEADS
    x = nrm((BATCH, SEQ, D_MODEL), 1.0)
    c = nrm((BATCH, D_MODEL), 1.0)
    ctx = nrm((BATCH, CTX_LEN, D_MODEL), 1.0)
    c_ctx = nrm((D_MODEL,), 1.0)
    w_ada = nrm((L, D_MODEL, 6 * D_MODEL), 0.5 * D_MODEL ** -0.5)
    b_ada = nrm((L, 6 * D_MODEL), 0.02)
    norm1_g = 1.0 + nrm((L, D_MODEL), 0.02)
    norm2_g = 1.0 + nrm((L, D_MODEL), 0.02)
    w_in = nrm((L, D_MODEL, IN_COLS), D_MODEL ** -0.5)
    q_a_g = 1.0 + nrm((L, Q_LORA), 0.02)
    w_qb = nrm((L, Q_LORA, H * QK_DIM), Q_LORA ** -0.5)
    kv_a_g = 1.0 + nrm((L, KV_LORA), 0.02)
    w_kvb = nrm((L, KV_LORA, H * (QK_NOPE + V_DIM)), KV_LORA ** -0.5)
    q_norm_g = 1.0 + nrm((L, QK_DIM), 0.02)
    k_norm_g = 1.0 + nrm((L, QK_DIM), 0.02)
    w_mla_o = nrm((L, H * V_DIM, D_MODEL), (H * V_DIM) ** -0.5)
    ssm_lam_re = -0.5 + nrm((L, 2, G, P), 0.01)
    ssm_lam_im = jnp.pi * jnp.arange(P, dtype=f32) + nrm((L, 2, G, P), 0.01)
    ssm_log_dt = jax.random.uniform(next(ks), (L, 2, G), f32, math.log(DT_MIN), math.log(DT_MAX))
    ssm_b_re = nrm((L, 2, G, P, SSM_GROUP), (2 * SSM_GROUP) ** -0.5)
    ssm_b_im = nrm((L, 2, G, P, SSM_GROUP), (2 * SSM_GROUP) ** -0.5)
    ssm_c_re = nrm((L, 2, G, SSM_GROUP, P), P ** -0.5)
    ssm_c_im = nrm((L, 2, G, SSM_GROUP, P), P ** -0.5)
    ssm_d = nrm((L, SSM_WIDTH), 1.0)
    w_glu = nrm((L, SSM_WIDTH, SSM_WIDTH), SSM_WIDTH ** -0.5)
    b_glu = nrm((L, SSM_WIDTH), 0.02)
    w_ssm_o = nrm((L, SSM_WIDTH, D_MODEL), SSM_WIDTH ** -0.5)
    w_out = nrm((L, D_MODEL, D_MODEL), D_MODEL ** -0.5)
    w_router = nrm((L, D_MODEL, N_EXPERTS), D_MODEL ** -0.5)
    w_e_gate = nrm((L, N_EXPERTS, D_MODEL, EXPERT_FF), D_MODEL ** -0.5)
    w_e_up = nrm((L, N_EXPERTS, D_MODEL, EXPERT_FF), D_MODEL ** -0.5)
    w_e_down = nrm((L, N_EXPERTS, EXPERT_FF, D_MODEL), EXPERT_FF ** -0.5)
    return {"x": x, "c": c, "ctx": ctx, "c_ctx": c_ctx, "w_ada": w_ada, "b_ada": b_ada,
            "norm1_g": norm1_g, "norm2_g": norm2_g, "w_in": w_in, "q_a_g": q_a_g, "w_qb": w_qb,
            "kv_a_g": kv_a_g, "w_kvb": w_kvb, "q_norm_g": q_norm_g, "k_norm_g": k_norm_g,
            "w_mla_o": w_mla_o, "ssm_lam_re": ssm_lam_re, "ssm_lam_im": ssm_lam_im,
            "ssm_log_dt": ssm_log_dt, "ssm_b_re": ssm_b_re, "ssm_b_im": ssm_b_im,
            "ssm_c_re": ssm_c_re, "ssm_c_im": ssm_c_im, "ssm_d": ssm_d, "w_glu": w_glu,
            "b_glu": b_glu, "w_ssm_o": w_ssm_o, "w_out": w_out, "w_router": w_router,
            "w_e_gate": w_e_gate, "w_e_up": w_e_up, "w_e_down": w_e_down}


def reference(x, c, ctx, c_ctx, w_ada, b_ada, norm1_g, norm2_g, w_in, q_a_g, w_qb, kv_a_g, w_kvb,
              q_norm_g, k_norm_g, w_mla_o, ssm_lam_re, ssm_lam_im, ssm_log_dt, ssm_b_re, ssm_b_im,
              ssm_c_re, ssm_c_im, ssm_d, w_glu, b_glu, w_ssm_o, w_out, w_router, w_e_gate, w_e_up,
              w_e_down):
    n = x.shape[1]
    rope = axial_rope(n)
    for i in range(DEPTH):
        last = i == DEPTH - 1
        mod_x = (jax.nn.silu(c) @ w_ada[i] + b_ada[i])[:, None, :]
        mod_c = (jax.nn.silu(c_ctx) @ w_ada[i] + b_ada[i])[None, None, :]
        sh1x, sc1x, g1x, sh2x, sc2x, g2x = jnp.split(mod_x, 6, axis=-1)
        sh1c, sc1c, g1c, sh2c, sc2c, g2c = jnp.split(mod_c, 6, axis=-1)

        hx = modulate(rmsnorm(x, norm1_g[i]), sh1x, sc1x)
        hc = modulate(rmsnorm(ctx, norm1_g[i]), sh1c, sc1c)
        ux, qax, kvax, krx, gatex = jnp.split(hx @ w_in[i], IN_SPLITS, axis=-1)
        uc, qac, kvac, krc, gatec = jnp.split(hc @ w_in[i], IN_SPLITS, axis=-1)

        qx = mla_q(qax, q_a_g[i], w_qb[i], q_norm_g[i], rope)
        kx, vx = mla_kv(kvax, krx, kv_a_g[i], w_kvb[i], k_norm_g[i], rope)
        kc, vc = mla_kv(kvac, krc, kv_a_g[i], w_kvb[i], k_norm_g[i], None)
        attn_x = attend_blocked(qx, jnp.concatenate([kc, kx], axis=1), jnp.concatenate([vc, vx], axis=1)) @ w_mla_o[i]

        ux32 = ux.astype(jnp.float32)
        uc32 = uc.astype(jnp.float32)
        d32 = ssm_d[i].astype(jnp.float32)
        y_x = d32 * ux32
        y_c = d32 * uc32 if not last else None
        for d, rev in enumerate((False, True)):
            lam_bar, b_bar = s5_discretise(ssm_lam_re[i, d], ssm_lam_im[i, d], ssm_log_dt[i, d],
                                           ssm_b_re[i, d], ssm_b_im[i, d])
            c_out = lax.complex(ssm_c_re[i, d].astype(jnp.float32), ssm_c_im[i, d].astype(jnp.float32))
            xs_c = s5_scan(s5_drive(uc32, b_bar), lam_bar, rev)
            h0 = xs_c[:, 0] if rev else xs_c[:, -1]
            xs_x = s5_scan(s5_drive(ux32, b_bar), lam_bar, rev, h0)
            y_x = y_x + s5_readout(xs_x, c_out)
            if not last:
                y_c = y_c + s5_readout(xs_c, c_out)
        ssm_x = s5_branch_out(y_x.astype(x.dtype), w_glu[i], b_glu[i], w_ssm_o[i])
        x_mid = x + g1x * merge_branches(ssm_x, attn_x, gatex, w_out[i])

        if not last:
            qc = mla_q(qac, q_a_g[i], w_qb[i], q_norm_g[i], None)
            attn_c = attend(qc, kc, vc) @ w_mla_o[i]
            ssm_c = s5_branch_out(y_c.astype(ctx.dtype), w_glu[i], b_glu[i], w_ssm_o[i])
            ctx_mid = ctx + g1c * merge_branches(ssm_c, attn_c, gatec, w_out[i])
            hc2 = modulate(rmsnorm(ctx_mid, norm2_g[i]), sh2c, sc2c)
            ctx = ctx_mid + g2c * expert_choice_ffn(hc2, w_router[i], w_e_gate[i], w_e_up[i], w_e_down[i])

        hx2 = modulate(rmsnorm(x_mid, norm2_g[i]), sh2x, sc2x)
        x = x_mid + g2x * expert_choice_ffn(hx2, w_router[i], w_e_gate[i], w_e_up[i], w_e_down[i])
    return x
```

```python
import math
import os
from contextlib import ExitStack

import numpy as np
import concourse.bass as bass
import concourse.mybir as mybir
from concourse.bass_utils import run_bass_kernel_spmd

F32 = mybir.dt.float32
F32R = mybir.dt.float32r
BF16 = mybir.dt.bfloat16
U32 = mybir.dt.uint32
I32 = mybir.dt.int32
ALU = mybir.AluOpType
AF = mybir.ActivationFunctionType
AX = mybir.AxisListType

D = 1024
NX = 4096
NCTX = 256
NT = NX + NCTX
NTILE = NT // 128
NCH = NT // 8
NCH_C = NCTX // 8
H = 8
QK = 96
NE = 16
FF = 2816
CAP = 512
EPS = 1e-6
IN_COLS = 3232
NCORES = 4

ENG = {"pe": "tensor", "act": "scalar", "dve": "vector", "pool": "gpsimd", "sp": "sync"}


class Prog:
    def __init__(self, nc, sems, dsems):
        self.nc = nc
        self.ops = []
        self.lastw = {}
        self.readers = {}
        self.sems = sems
        self.dsems = dsems
        self.sig = {e: 0 for e in ENG}
        self.dcnt = {e: [0] * len(dsems[e]) for e in dsems}
        self.dnext = {e: 0 for e in dsems}
        self.seen = {e: {} for e in ENG}
        self.emitted = 0

    def op(self, eng, fn, r=(), w=(), dma=False):
        i = len(self.ops)
        deps = set()
        for k in list(r) + list(w):
            if k in self.lastw:
                deps.add(self.lastw[k])
        for k in w:
            for j in self.readers.get(k, ()):
                deps.add(j)
        deps.discard(i)
        o = dict(eng=eng, fn=fn, deps=deps, dma=dma, need=False, val=None, sem=None, idx=i)
        self.ops.append(o)
        for k in w:
            self.lastw[k] = i
            self.readers[k] = []
        for k in r:
            if k not in w:
                self.readers.setdefault(k, []).append(i)
        return i

    def dmaop(self, eng, fn, r=(), w=()):
        return self.op(eng, fn, r, w, dma=True)

    def flush(self, final_wait_eng="sp"):
        nc = self.nc
        ops = self.ops[self.emitted:]
        pos = {}
        cnt = {e: 0 for e in ENG}
        for o in self.ops[:self.emitted]:
            pass
        for o in ops:
            pos[o["idx"]] = cnt[o["eng"]]
            cnt[o["eng"]] += 1
        for o in ops:
            real = []
            for d in o["deps"]:
                if d < self.emitted:
                    continue
                p = self.ops[d]
                if not p["dma"] and p["eng"] == o["eng"]:
                    if o["eng"] == "pe":
                        continue
                    if o["dma"]:
                        continue
                real.append(d)
                p["need"] = True
            o["real"] = real
        for o in ops:
            if o["dma"]:
                for d in o["deps"]:
                    if d >= self.emitted:
                        p = self.ops[d]
                        if not p["dma"] and p["eng"] == o["eng"] and d not in o["real"]:
                            o["real"].append(d)
                            p["need"] = True
        for o in ops:
            if o["dma"]:
                e = o["eng"]
                k = self.dnext[e]
                self.dnext[e] = (k + 1) % len(self.dsems[e])
                o["prev"] = self.dcnt[e][k]
                self.dcnt[e][k] += 16
                o["sem"] = self.dsems[e][k]
                o["val"] = self.dcnt[e][k]
            elif o["need"]:
                self.sig[o["eng"]] += 1
                o["sem"] = self.sems[o["eng"]]
                o["val"] = self.sig[o["eng"]]
        byeng = {e: [o for o in ops if o["eng"] == e] for e in ENG}
        seen = self.seen
        allops = self.ops
        dsems = self.dsems
        dcnt = self.dcnt

        def body(e):
            def f(engine):
                sn = seen[e]

                def wait(sem, val):
                    key = id(sem)
                    if sn.get(key, 0) >= val:
                        return
                    sn[key] = val
                    engine.wait_ge(sem, val)

                for o in byeng[e]:
                    for d in o["real"]:
                        p = allops[d]
                        wait(p["sem"], p["val"])
                    if o["dma"]:
                        if o["prev"] > 0:
                            wait(o["sem"], o["prev"])
                        ins = o["fn"](engine)
                        ins.then_inc(o["sem"], 16)
                    else:
                        ins = o["fn"](engine)
                        if o["need"]:
                            ins.then_inc(o["sem"], 1)
                if e in dsems:
                    for k, s in enumerate(dsems[e]):
                        if dcnt[e][k] > 0:
                            wait(s, dcnt[e][k])
            return f

        with nc.Block() as block:
            for e in ENG:
                getattr(block, ENG[e])(body(e))
        self.emitted = len(self.ops)
        self.lastw = {}
        self.readers = {}


class Keyed:
    def __init__(self, P, slot, shared, alias=None):
        self.P, self.slot, self.shared, self.alias = P, slot, set(shared), (alias or {})
        self.cap = []

    def _k(self, keys):
        out = []
        for k in keys:
            k = self.alias.get(k, k)
            base = k[0] if isinstance(k, tuple) else k
            out.append(k if base in self.shared else ("slot", self.slot, k))
        return out

    def op(self, eng, fn, r=(), w=(), dma=False):
        self.cap.append((eng, fn, self._k(r), self._k(w), dma))

    def dmaop(self, eng, fn, r=(), w=()):
        self.op(eng, fn, r, w, dma=True)


def interleave(P, caps, chunk=3):
    pos = [0] * len(caps)
    live = True
    while live:
        live = False
        for i, c in enumerate(caps):
            n = 0
            while pos[i] < len(c) and n < chunk:
                eng, fn, r, w, dma = c[pos[i]]
                P.op(eng, fn, r, w, dma=dma)
                pos[i] += 1
                n += 1
            if pos[i] < len(c):
                live = True


def r32(ap):
    return ap.bitcast(F32R)


def build(debug=0):
    nc = bass.Bass("TRN2", target_bir_lowering=False)
    es = ExitStack()
    DONE = []

    def din(name, shape, dt=F32):
        return nc.dram_tensor(name, list(shape), dt, kind="ExternalInput").ap()

    def dscr(name, shape, dt=F32):
        return nc.dram_tensor(name, list(shape), dt, kind="Internal").ap()

    xc = din("xc", [NT, D])
    cb = din("cb", [D])
    cctx = din("c_ctx", [D])
    w_ada = din("w_ada", [D, 6 * D])
    b_ada = din("b_ada", [6 * D])
    norm1_g = din("norm1_g", [D])
    norm2_g = din("norm2_g", [D])
    w_in = din("w_in", [D, IN_COLS])
    q_a_g = din("q_a_g", [384])
    w_qb = din("w_qb", [384, 768])
    kv_a_g = din("kv_a_g", [256])
    w_kvb = din("w_kvb", [256, 1024])
    q_norm_g = din("q_norm_g", [96])
    k_norm_g = din("k_norm_g", [96])
    w_mla_o = din("w_mla_o", [512, D])
    lam_re = din("ssm_lam_re", [2, 32, 64])
    lam_im = din("ssm_lam_im", [2, 32, 64])
    log_dt = din("ssm_log_dt", [2, 32])
    b_re = din("ssm_b_re", [2, 32, 64, 16])
    b_im = din("ssm_b_im", [2, 32, 64, 16])
    c_re = din("ssm_c_re", [2, 32, 16, 64])
    c_im = din("ssm_c_im", [2, 32, 16, 64])
    ssm_d = din("ssm_d", [512])
    w_glu = din("w_glu", [512, 512])
    b_glu = din("b_glu", [512])
    w_ssm_o = din("w_ssm_o", [512, D])
    w_out = din("w_out", [D, D])
    w_router = din("w_router", [D, NE])
    if debug in (0, 6):
        w_e_gate = din("w_e_gate", [NE, D, FF])
        w_e_up = din("w_e_up", [NE, D, FF])
        w_e_down = din("w_e_down", [NE, FF, D])
    ident_d = din("ident", [128, 128])
    rope_d = din("rope", [NT, 32])
    cmask_d = din("cmask", [128, 256])
    out = nc.dram_tensor("out", [NX, D], F32, kind="ExternalOutput").ap()

    u_s = dscr("u_s", [NT, 512])
    gates_s = dscr("gates_s", [NX, 2048])
    qT_s = dscr("qT_s", [H, QK, NX], BF16)
    kT_s = dscr("kT_s", [H, QK, NT], BF16)
    v_s = dscr("v_s", [NT, 512], BF16)
    attnT_s = dscr("attnT_s", [H, 64, NX])
    y_s = dscr("y_s", [NX // 8, 8 * 512], BF16)
    h2_s = dscr("h2_s", [NX, D])

    sems = {e: es.enter_context(nc.semaphore("s_" + e)) for e in ENG}
    dsems = {e: [es.enter_context(nc.semaphore("d_%s%d" % (e, k))) for k in range(8)]
             for e in ("sp", "act", "pool")}
    P = Prog(nc, sems, dsems)

    def sb(stack, name, shape, dt=F32):
        return stack.enter_context(nc.sbuf_tensor("t_" + name, list(shape), dt))

    def ps(stack, name, shape=(128, 512), dt=F32):
        return stack.enter_context(nc.psum_tensor("p_" + name, list(shape), dt))

    ident = sb(es, "ident", [128, 128])
    modx = sb(es, "modx", [128, 6 * D])
    es1 = ExitStack()
    modc = sb(es1, "modc", [128, 2 * D])
    P.dmaop("sp", lambda e: e.dma_start(out=ident[:], in_=ident_d), w=["ident"])

    with ExitStack() as st:
        cT = sb(st, "cT", [128, 2, 8])
        sc = sb(st, "sc", [128, 2, 8])
        lbc = sb(st, "lbc", [128, 2, 8, 128], BF16)
        wa = sb(st, "wa", [128, 2, 8, 512], BF16)
        bb = sb(st, "bb", [128, 6 * D])
        g1b = sb(st, "g1b", [128, D])
        g2b = sb(st, "g2b", [128, D])
        pm = [ps(st, "pm%d" % i) for i in range(4)]
        P.dmaop("sp", lambda e: e.dma_start(out=cT[:, 0, :], in_=cb.rearrange("(dc p) -> p dc", p=128),
                                            allow_slow_non_contiguous=True), w=["cT0"])
        P.dmaop("sp", lambda e: e.dma_start(out=cT[:, 1, :], in_=cctx.rearrange("(dc p) -> p dc", p=128),
                                            allow_slow_non_contiguous=True), w=["cT1"])
        P.dmaop("act", lambda e: e.dma_start(out=bb[:], in_=b_ada.partition_broadcast(128)), w=["bb"])
        P.dmaop("act", lambda e: e.dma_start(out=g1b[:], in_=norm1_g.partition_broadcast(128)), w=["g1b"])
        P.dmaop("act", lambda e: e.dma_start(out=g2b[:], in_=norm2_g.partition_broadcast(128)), w=["g2b"])
        P.op("act", lambda e: e.activation(out=sc[:], in_=cT[:], func=AF.Silu), r=["cT0", "cT1"], w=["sc"])
        P.op("dve", lambda e: e.tensor_copy(out=lbc[:], in_=sc[:].unsqueeze(3).to_broadcast([128, 2, 8, 128])),
             r=["sc"], w=["lbc"])
        wv = w_ada.rearrange("(dc p) n -> p dc n", p=128)
        for ct in range(12):
            s = ct % 2
            P.dmaop("pool",
                    lambda e, ct=ct, s=s: e.dma_start(out=wa[:, s], in_=wv[:, :, ct * 512:(ct + 1) * 512]),
                    w=[("wa", s)])
            for which in range(2 if ct < 4 else 1):
                pt = pm[(ct * 2 + which) % 4]
                pk = ("pm", (ct * 2 + which) % 4)

                def mm(e, pt=pt, s=s, which=which):
                    ins = None
                    for dc in range(8):
                        ins = e.matmul(pt[:], lhsT=lbc[:, which, dc, :], rhs=wa[:, s, dc, :],
                                       start=(dc == 0), stop=(dc == 7))
                    return ins
                P.op("pe", mm, r=["lbc", ("wa", s)], w=[pk])
                dst = modx if which == 0 else modc
                P.op("dve", lambda e, pt=pt, dst=dst, ct=ct: e.tensor_tensor(
                    out=dst[:, ct * 512:(ct + 1) * 512], in0=pt[:], in1=bb[:, ct * 512:(ct + 1) * 512], op=ALU.add),
                    r=[pk, "bb"], w=["modx" if which == 0 else "modc"])
        P.op("dve", lambda e: e.scalar_tensor_tensor(out=modx[:, D:2 * D], in0=modx[:, D:2 * D], scalar=1.0, in1=g1b[:],
                                                     op0=ALU.add, op1=ALU.mult), r=["modx", "g1b"], w=["modx"])
        P.op("dve", lambda e: e.scalar_tensor_tensor(out=modx[:, 4 * D:5 * D], in0=modx[:, 4 * D:5 * D], scalar=1.0, in1=g2b[:],
                                                     op0=ALU.add, op1=ALU.mult), r=["modx", "g2b"], w=["modx"])
        P.op("dve", lambda e: e.scalar_tensor_tensor(out=modc[:, D:2 * D], in0=modc[:, D:2 * D], scalar=1.0, in1=g1b[:],
                                                     op0=ALU.add, op1=ALU.mult), r=["modc", "g1b"], w=["modc"])
        P.flush()


    with ExitStack() as st:
      if debug != 1:
          TL1 = [dict(), dict()]
          w_in_sb = sb(st, "w_in_sb", [128, 8, IN_COLS], BF16)
          w_qb_sb = sb(st, "w_qb_sb", [128, 3, 768], BF16)
          w_kvb_sb = sb(st, "w_kvb_sb", [128, 2, 1024], BF16)
          qag = sb(st, "qag", [128, 384])
          kvag = sb(st, "kvag", [128, 256])
          qng = sb(st, "qng", [128, 96])
          kng = sb(st, "kng", [128, 96])
          identb = sb(st, "identb", [128, 128], BF16)
          xt = sb(st, "xt", [128, 2, D])
          TL1[0]['junk'] = sb(st, "junk_0", [128, D]); TL1[1]['junk'] = sb(st, "junk_1", [128, D])
          TL1[0]['hh'] = sb(st, "hh_0", [128, D]); TL1[1]['hh'] = sb(st, "hh_1", [128, D])
          TL1[0]['hb'] = sb(st, "hb_0", [128, D], BF16); TL1[1]['hb'] = sb(st, "hb_1", [128, D], BF16)
          TL1[0]['hT'] = sb(st, "hT_0", [128, 8, 128], BF16); TL1[1]['hT'] = sb(st, "hT_1", [128, 8, 128], BF16)
          proj = sb(st, "proj", [128, 2, IN_COLS])
          st8 = sb(st, "st8", [128, 2, 64])
          TL1[0]['qn'] = sb(st, "qn_0", [128, 384], BF16); TL1[1]['qn'] = sb(st, "qn_1", [128, 384], BF16)
          TL1[0]['qnT'] = sb(st, "qnT_0", [128, 3, 128], BF16); TL1[1]['qnT'] = sb(st, "qnT_1", [128, 3, 128], BF16)
          TL1[0]['kvn'] = sb(st, "kvn_0", [128, 256], BF16); TL1[1]['kvn'] = sb(st, "kvn_1", [128, 256], BF16)
          TL1[0]['kvnT'] = sb(st, "kvnT_0", [128, 2, 128], BF16); TL1[1]['kvnT'] = sb(st, "kvnT_1", [128, 2, 128], BF16)
          TL1[0]['qsq'] = sb(st, "qsq_0", [128, 768]); TL1[1]['qsq'] = sb(st, "qsq_1", [128, 768])
          TL1[0]['qf'] = sb(st, "qf_0", [128, 8, 96]); TL1[1]['qf'] = sb(st, "qf_1", [128, 8, 96])
          TL1[0]['qb'] = sb(st, "qb_0", [128, 8, 96], BF16); TL1[1]['qb'] = sb(st, "qb_1", [128, 8, 96], BF16)
          TL1[0]['kf'] = sb(st, "kf_0", [128, 8, 96]); TL1[1]['kf'] = sb(st, "kf_1", [128, 8, 96])
          TL1[0]['kb'] = sb(st, "kb_0", [128, 8, 96], BF16); TL1[1]['kb'] = sb(st, "kb_1", [128, 8, 96], BF16)
          vb = sb(st, "vb", [128, 2, 512], BF16)
          TL1[0]['kvs'] = sb(st, "kvs_0", [128, 1024]); TL1[1]['kvs'] = sb(st, "kvs_1", [128, 1024])
          rp = sb(st, "rp", [128, 2, 32])
          TL1[0]['rt'] = sb(st, "rt_0", [128, 6, 128]); TL1[1]['rt'] = sb(st, "rt_1", [128, 6, 128])
          TL1[0]['krg'] = sb(st, "krg_0", [128, 32]); TL1[1]['krg'] = sb(st, "krg_1", [128, 32])
          TL1[0]['krr'] = sb(st, "krr_0", [128, 32]); TL1[1]['krr'] = sb(st, "krr_1", [128, 32])
          TL1[0]['qTt'] = sb(st, "qTt_0", [128, 8, 128], BF16); TL1[1]['qTt'] = sb(st, "qTt_1", [128, 8, 128], BF16)
          TL1[0]['kTt'] = sb(st, "kTt_0", [128, 8, 128], BF16); TL1[1]['kTt'] = sb(st, "kTt_1", [128, 8, 128], BF16)
          PSALL = [ps(st, "ph1_%d" % i) for i in range(8)]

          P.dmaop("pool", lambda e: e.dma_start(out=w_in_sb[:], in_=w_in.rearrange("(dc p) n -> p dc n", p=128)), w=["w_in_sb"])
          P.dmaop("pool", lambda e: e.dma_start(out=w_qb_sb[:], in_=w_qb.rearrange("(dc p) n -> p dc n", p=128)), w=["w_qb_sb"])
          P.dmaop("pool", lambda e: e.dma_start(out=w_kvb_sb[:], in_=w_kvb.rearrange("(dc p) n -> p dc n", p=128)), w=["w_kvb_sb"])
          P.dmaop("act", lambda e: e.dma_start(out=qag[:], in_=q_a_g.partition_broadcast(128)), w=["qag"])
          P.dmaop("act", lambda e: e.dma_start(out=kvag[:], in_=kv_a_g.partition_broadcast(128)), w=["kvag"])
          P.dmaop("act", lambda e: e.dma_start(out=qng[:], in_=q_norm_g.partition_broadcast(128)), w=["qng"])
          P.dmaop("act", lambda e: e.dma_start(out=kng[:], in_=k_norm_g.partition_broadcast(128)), w=["kng"])
          P.op("dve", lambda e: e.tensor_scalar(out=qng[:], in0=qng[:], scalar1=float(QK ** -0.5), scalar2=None, op0=ALU.mult),
               r=["qng"], w=["qng"])
          P.op("dve", lambda e: e.tensor_copy(out=identb[:], in_=ident[:]), r=["ident"], w=["identb"])

          def rstd(Pq, src_key, src_ap, n, dst_ap, dst_key):
              Pq.op("dve", lambda e: e.tensor_scalar(out=dst_ap, in0=src_ap, scalar1=1.0 / n, scalar2=EPS, op0=ALU.mult, op1=ALU.add),
                   r=[src_key], w=[dst_key])
              Pq.op("act", lambda e: e.activation(out=dst_ap, in_=dst_ap, func=AF.Sqrt), r=[dst_key], w=[dst_key])
              Pq.op("dve", lambda e: e.reciprocal(out=dst_ap, in_=dst_ap), r=[dst_key], w=[dst_key])

          def rope(Pq, rt, src, dst, tab, nh, tag, eng="pool"):
              sv = src.rearrange("p h (a t) -> p h a t", a=2)
              dv = dst.rearrange("p h (a t) -> p h a t", a=2)
              cosb = tab[:, 0:16].rearrange("p (a t) -> p a t", a=2).unsqueeze(1).to_broadcast([128, nh, 2, 8])
              sinb = tab[:, 16:32].rearrange("p (a t) -> p a t", a=2).unsqueeze(1).to_broadcast([128, nh, 2, 8])
              v0 = sv[:, :, :, 0:8]
              v1 = sv[:, :, :, 8:16]
              n = nh * 16
              T = [rt[:, k, 0:n].rearrange("p (h a t) -> p h a t", h=nh, a=2) for k in range(4)]
              rk = ("rt", tag)
              Pq.op(eng, lambda e: e.tensor_tensor(out=T[0], in0=v0, in1=cosb, op=ALU.mult), r=[tag + "_src", tag + "_tab"], w=[rk + (0,)])
              Pq.op(eng, lambda e: e.tensor_tensor(out=T[1], in0=v1, in1=sinb, op=ALU.mult), r=[tag + "_src", tag + "_tab"], w=[rk + (1,)])
              Pq.op(eng, lambda e: e.tensor_tensor(out=T[2], in0=v1, in1=cosb, op=ALU.mult), r=[tag + "_src", tag + "_tab"], w=[rk + (2,)])
              Pq.op(eng, lambda e: e.tensor_tensor(out=T[3], in0=v0, in1=sinb, op=ALU.mult), r=[tag + "_src", tag + "_tab"], w=[rk + (3,)])
              Pq.op(eng, lambda e: e.tensor_tensor(out=dv[:, :, :, 0:8], in0=T[0], in1=T[1], op=ALU.subtract),
                   r=[rk + (0,), rk + (1,)], w=[tag + "_dst"])
              Pq.op(eng, lambda e: e.tensor_tensor(out=dv[:, :, :, 8:16], in0=T[2], in1=T[3], op=ALU.add),
                   r=[rk + (2,), rk + (3,)], w=[tag + "_dst"])

          ntile1 = 4 if debug in (2, 3, 4, 5, 6) else NTILE
          import os
          STG = int(os.environ.get('PH1_STAGE', '9'))
          SHARED1 = ["w_in_sb", "w_qb_sb", "w_kvb_sb", "qag", "kvag", "qng", "kng", "identb", "ident", "modx", "modc", "u_s", "gates_s", "qT_s", "kT_s", "v_s"]
          ALIAS1 = {("ps", 1): ("ps", 0), "ps3": "ps2", ("ps", 6): ("ps", 4), ("ps", 7): ("ps", 5)}

          def tile1(i, s):
              Pq = Keyed(P, s, SHARED1, ALIAS1)
              junk = TL1[s]['junk']
              hh = TL1[s]['hh']
              hb = TL1[s]['hb']
              hT = TL1[s]['hT']
              qn = TL1[s]['qn']
              qnT = TL1[s]['qnT']
              kvn = TL1[s]['kvn']
              kvnT = TL1[s]['kvnT']
              qsq = TL1[s]['qsq']
              qf = TL1[s]['qf']
              qb = TL1[s]['qb']
              kf = TL1[s]['kf']
              kb = TL1[s]['kb']
              kvs = TL1[s]['kvs']
              rt = TL1[s]['rt']
              krg = TL1[s]['krg']
              krr = TL1[s]['krr']
              qTt = TL1[s]['qTt']
              kTt = TL1[s]['kTt']
              bk = PSALL[4 * s:4 * s + 4]
              PS = [bk[0], bk[0], bk[1], bk[1], bk[2], bk[3], bk[2], bk[3]]
              PSb2 = PS[2][:].bitcast(BF16)
              PSb3 = PS[3][:].bitcast(BF16)
              isx = i >= 2
              t0 = i * 128
              xq = t0 - NCTX
              G = (modx if isx else modc)[:, D:2 * D]
              SH = (modx if isx else modc)[:, 0:D]
              mk = "modx" if isx else "modc"
              Pq.dmaop("sp", lambda e, s=s, t0=t0: e.dma_start(out=xt[:, s, :], in_=xc[t0:t0 + 128, :]), w=[("xt", s)])
              Pq.dmaop("sp", lambda e, s=s, t0=t0: e.dma_start(out=rp[:, s, :], in_=rope_d[t0:t0 + 128, :]), w=[("rp", s)])
              Pq.op("act", lambda e, s=s: e.activation(out=junk[:], in_=xt[:, s, :], func=AF.Square, accum_out=st8[:, s, 0:1]),
                   r=[("xt", s)], w=["junk", ("st", s, 0)])
              rstd(Pq, ("st", s, 0), st8[:, s, 0:1], D, st8[:, s, 1:2], ("st", s, 1))
              Pq.op("dve", lambda e, s=s, G=G: e.scalar_tensor_tensor(out=hh[:], in0=xt[:, s, :], scalar=st8[:, s, 1:2], in1=G,
                                                                  op0=ALU.mult, op1=ALU.mult),
                   r=[("xt", s), ("st", s, 1), mk], w=["hh"])
              Pq.op("pool", lambda e, SH=SH: e.tensor_tensor(out=hb[:], in0=hh[:], in1=SH, op=ALU.add), r=["hh", mk], w=["hb"])

              def tr_h(e):
                  ins = None
                  for dc in range(8):
                      ins = e.transpose(out=PSb2[:, dc * 128:(dc + 1) * 128], in_=hb[:, dc * 128:(dc + 1) * 128], identity=identb[:])
                  return ins
              Pq.op("pe", tr_h, r=["hb", "identb"], w=["ps2"])
              Pq.op("act", lambda e: e.copy(out=hT[:].rearrange("p a b -> p (a b)"), in_=PSb2[:, 0:1024]), r=["ps2"], w=["hT"])
              for ctile in range(7):
                  c0 = ctile * 512
                  n = min(512, IN_COLS - c0)
                  pk = ctile % 2

                  def mmin(e, c0=c0, n=n, pk=pk):
                      ins = None
                      for dc in range(8):
                          ins = e.matmul(PS[pk][:, 0:n], lhsT=hT[:, dc, :], rhs=w_in_sb[:, dc, c0:c0 + n], start=(dc == 0), stop=(dc == 7))
                      return ins
                  Pq.op("pe", mmin, r=["hT", "w_in_sb"], w=[("ps", pk)])
                  if ctile % 2 == 0:
                      Pq.op("dve", lambda e, c0=c0, n=n, pk=pk, s=s: e.tensor_copy(out=proj[:, s, c0:c0 + n], in_=PS[pk][:, 0:n]),
                           r=[("ps", pk)], w=[("proj", s, ctile)])
                  else:
                      Pq.op("act", lambda e, c0=c0, n=n, pk=pk, s=s: e.copy(out=proj[:, s, c0:c0 + n], in_=PS[pk][:, 0:n]),
                           r=[("ps", pk)], w=[("proj", s, ctile)])
              pj = [("proj", s, c) for c in range(7)]
              Pq.dmaop("sp", lambda e, s=s, t0=t0: e.dma_start(out=u_s[t0:t0 + 128, :], in_=proj[:, s, 0:512]), r=[pj[0]], w=["u_s"])
              if isx:
                  Pq.op("act", lambda e, s=s: e.activation(out=proj[:, s, 1184:3232], in_=proj[:, s, 1184:3232], func=AF.Sigmoid),
                        r=pj[2:], w=pj[2:])
                  Pq.dmaop("sp", lambda e, xq=xq, s=s: e.dma_start(out=gates_s[xq:xq + 128, :], in_=proj[:, s, 1184:3232]), r=pj[2:], w=["gates_s"])
                  Pq.op("act", lambda e, s=s: e.activation(out=junk[:, 0:384], in_=proj[:, s, 512:896], func=AF.Square,
                                                          accum_out=st8[:, s, 2:3]), r=[pj[1]], w=["junk", ("st", s, 2)])
                  rstd(Pq, ("st", s, 2), st8[:, s, 2:3], 384, st8[:, s, 3:4], ("st", s, 3))
                  Pq.op("dve", lambda e, s=s: e.scalar_tensor_tensor(out=qn[:], in0=proj[:, s, 512:896], scalar=st8[:, s, 3:4], in1=qag[:],
                                                                    op0=ALU.mult, op1=ALU.mult), r=[pj[1], ("st", s, 3), "qag"], w=["qn"])

                  def tr_q(e):
                      ins = None
                      for k in range(3):
                          ins = e.transpose(out=PSb2[:, k * 128:(k + 1) * 128], in_=qn[:, k * 128:(k + 1) * 128], identity=identb[:])
                      return ins
                  Pq.op("pe", tr_q, r=["qn", "identb"], w=["ps2"])
                  Pq.op("dve", lambda e: e.tensor_copy(out=qnT[:].rearrange("p a b -> p (a b)"), in_=PSb2[:, 0:384]), r=["ps2"], w=["qnT"])

                  def mmq(e):
                      ins = None
                      for (pi, c0, n) in ((4, 0, 512), (5, 512, 256)):
                          for k in range(3):
                              ins = e.matmul(PS[pi][:, 0:n], lhsT=qnT[:, k, :], rhs=w_qb_sb[:, k, c0:c0 + n], start=(k == 0), stop=(k == 2))
                      return ins
                  Pq.op("pe", mmq, r=["qnT", "w_qb_sb"], w=[("ps", 4), ("ps", 5)])
                  qfl = qf[:].rearrange("p h d -> p (h d)")
                  Pq.op("act", lambda e: e.copy(out=qfl[:, 0:512], in_=PS[4][:, 0:512]), r=[("ps", 4)], w=["qf"])
                  Pq.op("act", lambda e: e.copy(out=qfl[:, 512:768], in_=PS[5][:, 0:256]), r=[("ps", 5)], w=["qf"])
                  Pq.op("pool", lambda e: e.tensor_tensor(out=qsq[:], in0=qfl, in1=qfl, op=ALU.mult), r=["qf"], w=["qsq"])
                  Pq.op("dve", lambda e, s=s: e.tensor_reduce(out=st8[:, s, 8:16], in_=qsq[:].rearrange("p (h d) -> p h d", h=8),
                                                             axis=AX.X, op=ALU.add), r=["qsq"], w=[("st", s, 8)])
                  rstd(Pq, ("st", s, 8), st8[:, s, 8:16], QK, st8[:, s, 16:24], ("st", s, 16))
                  Pq.op("dve", lambda e, s=s: e.tensor_tensor(out=qf[:], in0=qf[:], in1=st8[:, s, 16:24].unsqueeze(2).to_broadcast([128, 8, 96]),
                                                             op=ALU.mult), r=["qf", ("st", s, 16)], w=["qf"])
                  Pq.op("dve", lambda e: e.tensor_tensor(out=qf[:], in0=qf[:], in1=qng[:].unsqueeze(1).to_broadcast([128, 8, 96]),
                                                        op=ALU.mult), r=["qf", "qng"], w=["qf", "q_src"])
                  Pq.op("act", lambda e: e.copy(out=qb[:, :, 0:64], in_=qf[:, :, 0:64]), r=["qf"], w=["qb"])
                  Pq.op("pool", lambda e, s=s: e.tensor_copy(out=rt[:, 5, 0:32], in_=rp[:, s, :]), r=[("rp", s)], w=["q_tab"])
                  rope(Pq, rt, qf[:, :, 64:96], qb[:, :, 64:96], rt[:, 5, 0:32], 8, "q")

                  def tr_qh(e):
                      ins = None
                      for h in range(8):
                          ins = e.transpose(out=PSb3[0:96, h * 128:(h + 1) * 128], in_=qb[:, h, :], identity=identb[:])
                      return ins
                  Pq.op("pe", tr_qh, r=["qb", "q_dst", "identb"], w=["ps3"])
                  Pq.op("act", lambda e: e.copy(out=qTt[0:96].rearrange("p a b -> p (a b)"), in_=PSb3[0:96, 0:1024]), r=["ps3"], w=["qTt"])
                  Pq.dmaop("act", lambda e, xq=xq: e.dma_start(out=qT_s[:, :, xq:xq + 128].rearrange("h d t -> d h t"), in_=qTt[0:96]),
                          r=["qTt"], w=["qT_s"])
              Pq.op("act", lambda e, s=s: e.activation(out=junk[:, 0:256], in_=proj[:, s, 896:1152], func=AF.Square,
                                                      accum_out=st8[:, s, 4:5]), r=[pj[1], pj[2]], w=["junk", ("st", s, 4)])
              rstd(Pq, ("st", s, 4), st8[:, s, 4:5], 256, st8[:, s, 5:6], ("st", s, 5))
              Pq.op("dve", lambda e, s=s: e.scalar_tensor_tensor(out=kvn[:], in0=proj[:, s, 896:1152], scalar=st8[:, s, 5:6], in1=kvag[:],
                                                                op0=ALU.mult, op1=ALU.mult), r=[pj[1], pj[2], ("st", s, 5), "kvag"], w=["kvn"])

              def tr_kv(e):
                  ins = None
                  for k in range(2):
                      ins = e.transpose(out=PSb2[:, k * 128:(k + 1) * 128], in_=kvn[:, k * 128:(k + 1) * 128], identity=identb[:])
                  return ins
              Pq.op("pe", tr_kv, r=["kvn", "identb"], w=["ps2"])
              Pq.op("dve", lambda e: e.tensor_copy(out=kvnT[:].rearrange("p a b -> p (a b)"), in_=PSb2[:, 0:256]), r=["ps2"], w=["kvnT"])

              def mmkv(e):
                  ins = None
                  for (pi, c0) in ((6, 0), (7, 512)):
                      for k in range(2):
                          ins = e.matmul(PS[pi][:, 0:512], lhsT=kvnT[:, k, :], rhs=w_kvb_sb[:, k, c0:c0 + 512], start=(k == 0), stop=(k == 1))
                  return ins
              Pq.op("pe", mmkv, r=["kvnT", "w_kvb_sb"], w=[("ps", 6), ("ps", 7)])
              for half in range(2):
                  (Pq.op("act", lambda e, half=half: e.copy(out=kvs[:, half * 512:(half + 1) * 512], in_=PS[6 + half][:, 0:512]),
                        r=[("ps", 6 + half)], w=[("kvs", half)]) if half == 0 else
                   Pq.op("dve", lambda e, half=half: e.tensor_copy(out=kvs[:, half * 512:(half + 1) * 512], in_=PS[6 + half][:, 0:512]),
                        r=[("ps", 6 + half)], w=[("kvs", half)]))
              kvs3 = kvs[:].rearrange("p (h d) -> p h d", h=8)
              kvk = [("kvs", 0), ("kvs", 1)]
              Pq.op("pool", lambda e, s=s: e.tensor_copy(out=vb[:, s].rearrange("p (h d) -> p h d", h=8), in_=kvs3[:, :, 64:128]),
                   r=kvk, w=[("vb", s)])
              Pq.dmaop("sp", lambda e, s=s, t0=t0: e.dma_start(out=v_s[t0:t0 + 128, :], in_=vb[:, s]),
                      r=[("vb", s)], w=["v_s"])
              Pq.op("act", lambda e: e.copy(out=kf[:, :, 0:64], in_=kvs3[:, :, 0:64]), r=kvk, w=[("kf", 0), ("kf", 1)])
              kfk = [("kf", 0), ("kf", 1)]
              Pq.op("pool", lambda e: e.tensor_tensor(out=qsq[:, 0:512].rearrange("p (h d) -> p h d", h=8), in0=kf[:, :, 0:64], in1=kf[:, :, 0:64],
                                                     op=ALU.mult), r=kfk, w=["qsq"])
              Pq.op("dve", lambda e, s=s: e.tensor_reduce(out=st8[:, s, 24:32], in_=qsq[:, 0:512].rearrange("p (h d) -> p h d", h=8),
                                                         axis=AX.X, op=ALU.add), r=["qsq"], w=[("st", s, 24)])
              Pq.op("act", lambda e, s=s: e.activation(out=junk[:, 0:32], in_=proj[:, s, 1152:1184], func=AF.Square,
                                                      accum_out=st8[:, s, 6:7]), r=[pj[2]], w=["junk", ("st", s, 6)])
              Pq.op("dve", lambda e, s=s: e.tensor_scalar(out=st8[:, s, 24:32], in0=st8[:, s, 24:32], scalar1=st8[:, s, 6:7], scalar2=None,
                                                         op0=ALU.add), r=[("st", s, 24), ("st", s, 6)], w=[("st", s, 24)])
              rstd(Pq, ("st", s, 24), st8[:, s, 24:32], QK, st8[:, s, 32:40], ("st", s, 32))
              Pq.op("dve", lambda e, s=s: e.tensor_tensor(out=kf[:, :, 0:64], in0=kf[:, :, 0:64],
                                                         in1=st8[:, s, 32:40].unsqueeze(2).to_broadcast([128, 8, 64]), op=ALU.mult),
                   r=kfk + [("st", s, 32)], w=kfk)
              Pq.op("dve", lambda e: e.tensor_tensor(out=kb[:, :, 0:64], in0=kf[:, :, 0:64],
                                                    in1=kng[:, 0:64].unsqueeze(1).to_broadcast([128, 8, 64]), op=ALU.mult),
                   r=kfk + ["kng"], w=["kb"])
              Pq.op("pool", lambda e, s=s: e.tensor_tensor(out=krg[:], in0=proj[:, s, 1152:1184], in1=kng[:, 64:96], op=ALU.mult),
                   r=[pj[2], "kng"], w=["krg", "k_src"])
              Pq.op("pool", lambda e, s=s: e.tensor_copy(out=rt[:, 4, 0:32], in_=rp[:, s, :]), r=[("rp", s)], w=["k_tab"])
              rope(Pq, rt, krg[:].unsqueeze(1), krr[:].unsqueeze(1), rt[:, 4, 0:32], 1, "k")
              Pq.op("dve", lambda e, s=s: e.tensor_tensor(out=kb[:, :, 64:96], in0=krr[:].unsqueeze(1).to_broadcast([128, 8, 32]),
                                                         in1=st8[:, s, 32:40].unsqueeze(2).to_broadcast([128, 8, 32]), op=ALU.mult),
                   r=["k_dst", ("st", s, 32)], w=["kb"])

              def tr_kh(e):
                  ins = None
                  for h in range(8):
                      ins = e.transpose(out=PSb3[0:96, h * 128:(h + 1) * 128], in_=kb[:, h, :], identity=identb[:])
                  return ins
              Pq.op("pe", tr_kh, r=["kb", "identb"], w=["ps3"])
              Pq.op("dve", lambda e: e.tensor_copy(out=kTt[0:96].rearrange("p a b -> p (a b)"), in_=PSb3[0:96, 0:1024]), r=["ps3"], w=["kTt"])
              Pq.dmaop("act", lambda e, t0=t0: e.dma_start(out=kT_s[:, :, t0:t0 + 128].rearrange("h d t -> d h t"), in_=kTt[0:96]),
                      r=["kTt"], w=["kT_s"])
              return Pq.cap
          for i in range(0, ntile1, 2):
              caps = [tile1(i, 0)] + ([tile1(i + 1, 1)] if i + 1 < ntile1 else [])
              interleave(P, caps, chunk=int(os.environ.get("ILV", "2")))
          P.flush()

    small = debug in (2, 3, 4, 5, 6)
    NTe = 4 if small else NTILE
    NXe = (NTe - 2) * 128
    QG = min(512, NXe)
    NQG = NXe // QG

    es1.close()
    NCX = NXe // 8
    NCC = NCTX // 8
    NCHe = NCX + NCC
    HW = NCHe + 2
    if debug in (0, 4, 5, 6):
      with ExitStack() as st:
        BendT = sb(st, "BendT", [128, 2, 32, 2, 64], BF16)
        Dm = sb(st, "Dm", [128, 2, 16, 2, 128], BF16)
        Tloc = sb(st, "Tloc", [128, 32, 128], BF16)
        mu3 = sb(st, "mu3", [128, 2, 2, 16, 2])
        mu16 = sb(st, "mu16", [128, 2, 17, 2, 16, 2])
        PS2 = [ps(st, "ph2_%d" % i) for i in range(8)]
        with ExitStack() as tt:
            lre = sb(tt, "lre", [128, 32]); lim = sb(tt, "lim", [128, 32]); ldt = sb(tt, "ldt", [128, 32])
            bre = sb(tt, "bre", [128, 32, 16]); bim = sb(tt, "bim", [128, 32, 16])
            cre = sb(tt, "cre", [128, 32, 16]); cim = sb(tt, "cim", [128, 32, 16])
            dsk = sb(tt, "dsk", [128, 32])
            cmk = sb(tt, "cmk", [128, 256])
            tm = sb(tt, "tm", [128, 12, 32])
            ti = sb(tt, "ti", [128, 32], I32)
            pwr = sb(tt, "pwr", [128, 9, 32]); pwi = sb(tt, "pwi", [128, 9, 32])
            nwr = sb(tt, "nwr", [128, 9, 32]); nwi = sb(tt, "nwi", [128, 9, 32])
            Bbr = sb(tt, "Bbr", [128, 32, 16]); Bbi = sb(tt, "Bbi", [128, 32, 16])
            MX = [sb(tt, "MX%d" % i, [128, 16, 8, 16]) for i in range(4)]
            MT = [sb(tt, "MT%d" % i, [128, 16, 8, 16]) for i in range(4)]
            TL = sb(tt, "TL", [128, 2, 128])
            for gh in range(2):
                rows = slice(64 * gh, 64 * gh + 64)
                gs = slice(16 * gh, 16 * gh + 16)
                for d in range(2):
                    for (dst, src, nm) in ((lre, lam_re, "lre"), (lim, lam_im, "lim")):
                        P.dmaop("sp", lambda e, dst=dst, src=src, rows=rows, gs=gs, d=d: e.dma_start(
                            out=dst[rows, 16 * d:16 * d + 16], in_=src[d, gs, :].rearrange("g p -> p g"),
                            allow_slow_non_contiguous=True), w=[nm])
                    P.dmaop("sp", lambda e, rows=rows, gs=gs, d=d: e.dma_start(
                        out=ldt[rows, 16 * d:16 * d + 16], in_=log_dt[d, gs].partition_broadcast(64)), w=["ldt"])
                for (dst, src, nm) in ((bre, b_re, "bre"), (bim, b_im, "bim")):
                    for d in range(2):
                        P.dmaop("act", lambda e, dst=dst, src=src, rows=rows, gs=gs, d=d: e.dma_start(
                            out=dst[rows, 16 * d:16 * d + 16, :], in_=src[d, gs, :, :].rearrange("g p h -> p g h")), w=[nm])
                for (dst, src, nm) in ((cre, c_re, "cre"), (cim, c_im, "cim")):
                    for d in range(2):
                        P.dmaop("sp" if d == 0 else "act", lambda e, dst=dst, src=src, rows=rows, gs=gs, d=d: e.dma_start(
                            out=dst[rows, 16 * d:16 * d + 16, :], in_=src[d, gs, :, :].rearrange("g o p -> p g o"),
                            allow_slow_non_contiguous=True), w=[nm])
            for j in range(8):
                P.dmaop("sp", lambda e, j=j: e.dma_start(out=dsk[16 * j:16 * j + 16, :], in_=ssm_d.rearrange("(g h) -> h g", h=16),
                                                        allow_slow_non_contiguous=True), w=["dsk"])
            P.dmaop("sp", lambda e: e.dma_start(out=cmk[:], in_=cmask_d), w=["cmk"])

            K = [0]

            def T_(i):
                return tm[:, i, :]

            def dv(fn, r, w, eng="dve"):
                P.op(eng, fn, r=r, w=w)

            def tt2(out, a, b, op, r, w, eng="dve"):
                P.op(eng, lambda e: e.tensor_tensor(out=out, in0=a, in1=b, op=op), r=r, w=w)

            def ts(out, a, s1, op0, s2=None, op1=None, r=(), w=(), eng="dve"):
                if op1 is None:
                    P.op(eng, lambda e: e.tensor_scalar(out=out, in0=a, scalar1=s1, scalar2=None, op0=op0), r=r, w=w)
                else:
                    P.op(eng, lambda e: e.tensor_scalar(out=out, in0=a, scalar1=s1, scalar2=s2, op0=op0, op1=op1), r=r, w=w)
            PI = math.pi
            P.op("act", lambda e: e.activation(out=T_(0), in_=ldt[:], func=AF.Exp), r=["ldt"], w=["t0"])
            tt2(T_(1), lre[:], T_(0), ALU.mult, ["lre", "t0"], ["t1"])
            tt2(T_(2), lim[:], T_(0), ALU.mult, ["lim", "t0"], ["t2"])
            P.op("act", lambda e: e.activation(out=T_(3), in_=T_(1), func=AF.Exp), r=["t1"], w=["t3"])
            ts(T_(4), T_(2), 1.0 / (2 * PI), ALU.mult, r=["t2"], w=["t4"])
            P.op("dve", lambda e: e.tensor_copy(out=ti[:], in_=T_(4)), r=["t4"], w=["ti"])
            P.op("dve", lambda e: e.tensor_copy(out=T_(4), in_=ti[:]), r=["ti"], w=["t4"])
            P.op("dve", lambda e: e.scalar_tensor_tensor(out=T_(5), in0=T_(4), scalar=-2 * PI, in1=T_(2), op0=ALU.mult, op1=ALU.add),
                 r=["t4", "t2"], w=["t5"])
            for (src_i, dst_i) in ((5, 5),):
                ts(T_(6), T_(5), PI, ALU.is_gt, -2 * PI, ALU.mult, r=["t5"], w=["t6"])
                tt2(T_(5), T_(5), T_(6), ALU.add, ["t5", "t6"], ["t5"])
                ts(T_(6), T_(5), -PI, ALU.is_lt, 2 * PI, ALU.mult, r=["t5"], w=["t6"])
                tt2(T_(5), T_(5), T_(6), ALU.add, ["t5", "t6"], ["t5"])
            ts(T_(7), T_(5), PI / 2, ALU.add, r=["t5"], w=["t7"])
            ts(T_(6), T_(7), PI, ALU.is_gt, -2 * PI, ALU.mult, r=["t7"], w=["t6"])
            tt2(T_(7), T_(7), T_(6), ALU.add, ["t7", "t6"], ["t7"])
            P.op("act", lambda e: e.activation(out=T_(8), in_=T_(5), func=AF.Sin), r=["t5"], w=["t8"])
            P.op("act", lambda e: e.activation(out=T_(9), in_=T_(7), func=AF.Sin), r=["t7"], w=["t9"])
            P.op("pool", lambda e: e.memset(pwr[:, 0, :], 1.0), w=[("pw", 0)])
            P.op("pool", lambda e: e.memset(pwi[:, 0, :], 0.0), w=[("pw", 0)])
            tt2(pwr[:, 1, :], T_(3), T_(9), ALU.mult, ["t3", "t9"], [("pw", 1)])
            tt2(pwi[:, 1, :], T_(3), T_(8), ALU.mult, ["t3", "t8"], [("pw", 1)])
            for k in range(2, 9):
                a_r, a_i = pwr[:, k - 1, :], pwi[:, k - 1, :]
                tt2(T_(10), a_r, pwr[:, 1, :], ALU.mult, [("pw", k - 1), ("pw", 1)], ["t10"])
                tt2(T_(11), a_i, pwi[:, 1, :], ALU.mult, [("pw", k - 1), ("pw", 1)], ["t11"])
                tt2(pwr[:, k, :], T_(10), T_(11), ALU.subtract, ["t10", "t11"], [("pw", k)])
                tt2(T_(10), a_r, pwi[:, 1, :], ALU.mult, [("pw", k - 1), ("pw", 1)], ["t10"])
                tt2(T_(11), a_i, pwr[:, 1, :], ALU.mult, [("pw", k - 1), ("pw", 1)], ["t11"])
                tt2(pwi[:, k, :], T_(10), T_(11), ALU.add, ["t10", "t11"], [("pw", k)])
            pwk = [("pw", k) for k in range(9)]
            tt2(nwr[:], pwr[:], pwr[:], ALU.mult, pwk, ["nwr"])
            tt2(nwi[:], pwi[:], pwi[:], ALU.mult, pwk, ["nwi"])
            tt2(nwr[:], nwr[:], nwi[:], ALU.add, ["nwr", "nwi"], ["nwr"])
            P.op("dve", lambda e: e.reciprocal(out=nwr[:], in_=nwr[:]), r=["nwr"], w=["nwr"])
            P.op("dve", lambda e: e.scalar_tensor_tensor(out=nwi[:], in0=pwi[:], scalar=-1.0, in1=nwr[:], op0=ALU.mult, op1=ALU.mult),
                 r=pwk + ["nwr"], w=["nwi"])
            tt2(nwr[:], pwr[:], nwr[:], ALU.mult, pwk + ["nwr", "nwi"], ["nwr"])
            for d in range(2):
                dsl = slice(16 * d, 16 * d + 16)
                for pl in range(2):
                    P.op("dve", lambda e, d=d, pl=pl, dsl=dsl: e.tensor_copy(out=mu3[:, d, 0, :, pl], in_=pwr[:, 8, dsl]), r=pwk, w=["mu3"])
                P.op("dve", lambda e, d=d, dsl=dsl: e.tensor_scalar(out=mu3[:, d, 1, :, 0], in0=pwi[:, 8, dsl], scalar1=-1.0, scalar2=None, op0=ALU.mult),
                     r=pwk, w=["mu3"])
                P.op("dve", lambda e, d=d, dsl=dsl: e.tensor_copy(out=mu3[:, d, 1, :, 1], in_=pwi[:, 8, dsl]), r=pwk, w=["mu3"])
            q16r = sb(tt, "q16r", [128, 17, 32]); q16i = sb(tt, "q16i", [128, 17, 32])
            P.op("pool", lambda e: e.memset(q16r[:, 0, :], 1.0), w=[("q16", 0)])
            P.op("pool", lambda e: e.memset(q16i[:, 0, :], 0.0), w=[("q16", 0)])
            P.op("pool", lambda e: e.tensor_copy(out=q16r[:, 1, :], in_=pwr[:, 8, :]), r=pwk, w=[("q16", 1)])
            P.op("pool", lambda e: e.tensor_copy(out=q16i[:, 1, :], in_=pwi[:, 8, :]), r=pwk, w=[("q16", 1)])
            for k in range(2, 17):
                a_r, a_i = q16r[:, k - 1, :], q16i[:, k - 1, :]
                tt2(T_(10), a_r, q16r[:, 1, :], ALU.mult, [("q16", k - 1), ("q16", 1)], ["t10"])
                tt2(T_(11), a_i, q16i[:, 1, :], ALU.mult, [("q16", k - 1), ("q16", 1)], ["t11"])
                tt2(q16r[:, k, :], T_(10), T_(11), ALU.subtract, ["t10", "t11"], [("q16", k)])
                tt2(T_(10), a_r, q16i[:, 1, :], ALU.mult, [("q16", k - 1), ("q16", 1)], ["t10"])
                tt2(T_(11), a_i, q16r[:, 1, :], ALU.mult, [("q16", k - 1), ("q16", 1)], ["t11"])
                tt2(q16i[:, k, :], T_(10), T_(11), ALU.add, ["t10", "t11"], [("q16", k)])
            q16k = [("q16", k) for k in range(17)]
            for d in range(2):
                dsl = slice(16 * d, 16 * d + 16)
                for pl in range(2):
                    P.op("dve", lambda e, d=d, pl=pl, dsl=dsl: e.tensor_copy(out=mu16[:, d, :, 0, :, pl], in_=q16r[:, :, dsl]), r=q16k, w=["mu16"])
                P.op("dve", lambda e, d=d, dsl=dsl: e.tensor_scalar(out=mu16[:, d, :, 1, :, 0], in0=q16i[:, :, dsl], scalar1=-1.0, scalar2=None, op0=ALU.mult),
                     r=q16k, w=["mu16"])
                P.op("dve", lambda e, d=d, dsl=dsl: e.tensor_copy(out=mu16[:, d, :, 1, :, 1], in_=q16i[:, :, dsl]), r=q16k, w=["mu16"])
            tt2(T_(0), lre[:], lre[:], ALU.mult, ["lre"], ["t0"])
            tt2(T_(1), lim[:], lim[:], ALU.mult, ["lim"], ["t1"])
            tt2(T_(0), T_(0), T_(1), ALU.add, ["t0", "t1"], ["t0"])
            P.op("dve", lambda e: e.reciprocal(out=T_(0), in_=T_(0)), r=["t0"], w=["t0"])
            ts(T_(1), pwr[:, 1, :], -1.0, ALU.add, r=[("pw", 1)], w=["t1"])
            tt2(T_(2), T_(1), lre[:], ALU.mult, ["t1", "lre"], ["t2"])
            tt2(T_(3), pwi[:, 1, :], lim[:], ALU.mult, [("pw", 1), "lim"], ["t3"])
            tt2(T_(2), T_(2), T_(3), ALU.add, ["t2", "t3"], ["t2"])
            tt2(T_(2), T_(2), T_(0), ALU.mult, ["t2", "t0"], ["t2"])
            tt2(T_(3), pwi[:, 1, :], lre[:], ALU.mult, [("pw", 1), "lre"], ["t3"])
            tt2(T_(4), T_(1), lim[:], ALU.mult, ["t1", "lim"], ["t4"])
            tt2(T_(3), T_(3), T_(4), ALU.subtract, ["t3", "t4"], ["t3"])
            tt2(T_(3), T_(3), T_(0), ALU.mult, ["t3", "t0"], ["t3"])
            cfr = T_(2).unsqueeze(2).to_broadcast([128, 32, 16])
            cfi = T_(3).unsqueeze(2).to_broadcast([128, 32, 16])
            tmpA = MT[0][:].rearrange("p a b c -> p (a b c)")[:, 0:512].rearrange("p (g h) -> p g h", h=16)
            tt2(Bbr[:], bre[:], cfr, ALU.mult, ["bre", "t2"], ["Bbr"])
            tt2(tmpA, bim[:], cfi, ALU.mult, ["bim", "t3"], ["MT0"])
            tt2(Bbr[:], Bbr[:], tmpA, ALU.subtract, ["Bbr", "MT0"], ["Bbr"])
            tt2(Bbi[:], bim[:], cfr, ALU.mult, ["bim", "t2"], ["Bbi"])
            tt2(tmpA, bre[:], cfi, ALU.mult, ["bre", "t3"], ["MT0"])
            tt2(Bbi[:], Bbi[:], tmpA, ALU.add, ["Bbi", "MT0"], ["Bbi"])

            def cprod(out_re, out_im, pw_re, pw_im, koff, kstep, vr, vi, d, keys_in, key_out, neg_im=False, eng="dve"):
                dsl = slice(16 * d, 16 * d + 16)
                if kstep == 1:
                    ksl = slice(koff, koff + 8)
                    pr = pw_re[:, ksl, dsl].rearrange("p j g -> p g j").unsqueeze(3).to_broadcast([128, 16, 8, 16])
                    pi = pw_im[:, ksl, dsl].rearrange("p j g -> p g j").unsqueeze(3).to_broadcast([128, 16, 8, 16])
                else:
                    pr = None
                vrb = vr[:, dsl, :].unsqueeze(2).to_broadcast([128, 16, 8, 16])
                vib = vi[:, dsl, :].unsqueeze(2).to_broadcast([128, 16, 8, 16])
                t1 = MT[2][:]
                t2 = MT[3][:]
                tt2(t1, vrb, pr, ALU.mult, keys_in, ["MT2"], eng)
                tt2(t2, vib, pi, ALU.mult, keys_in, ["MT3"], eng)
                tt2(out_re, t1, t2, ALU.subtract, ["MT2", "MT3"], [key_out[0]], eng)
                tt2(t1, vrb, pi, ALU.mult, keys_in, ["MT2"], eng)
                tt2(t2, vib, pr, ALU.mult, keys_in, ["MT3"], eng)
                tt2(out_im, t1, t2, (ALU.add), ["MT2", "MT3"], [key_out[1]], eng)
                if neg_im:
                    ts(out_im, out_im, -1.0, ALU.mult, r=[key_out[1]], w=[key_out[1]], eng=eng)

            dpr = sb(tt, "dpr", [128, 9, 32]); dpi = sb(tt, "dpi", [128, 9, 32])
            for k in range(9):
                P.op("pool", lambda e, k=k: e.tensor_copy(out=dpr[:, k, :], in_=pwr[:, 8 - k, :]), r=pwk, w=["dpr"])
                P.op("pool", lambda e, k=k: e.tensor_copy(out=dpi[:, k, :], in_=pwi[:, 8 - k, :]), r=pwk, w=["dpi"])
            tk = pwk + ["nwr", "nwi", "dpr", "dpi", "Bbr", "Bbi", "cre", "cim"]
            pTL = PS2[0]
            for d in range(2):
                if d == 0:
                    cprod(MX[0][:], MX[1][:], nwr, nwi, 0, 1, Bbr, Bbi, d, tk, ("MX0", "MX1"))
                    cprod(MX[2][:], MX[3][:], pwr, pwi, 0, 1, cre, cim, d, tk, ("MX2", "MX3"), neg_im=True)
                    cprod(MT[0][:], MT[1][:], dpr, dpi, 1, 1, Bbr, Bbi, d, tk, ("MT0", "MT1"))
                else:
                    cprod(MX[0][:], MX[1][:], pwr, pwi, 0, 1, Bbr, Bbi, d, tk, ("MX0", "MX1"))
                    cprod(MX[2][:], MX[3][:], nwr, nwi, 0, 1, cre, cim, d, tk, ("MX2", "MX3"), neg_im=True)
                    P.op("pool", lambda e: e.tensor_copy(out=MT[0][:], in_=MX[0][:]), r=["MX0"], w=["MT0"])
                    P.op("pool", lambda e: e.tensor_copy(out=MT[1][:], in_=MX[1][:]), r=["MX1"], w=["MT1"])
                for pl in range(2):
                    for g in range(32):
                        gh, gp = g // 16, g % 16
                        pb = PS2[2 + (g % 4)]

                        def trb(e, pl=pl, gh=gh, gp=gp, pb=pb):
                            return e.transpose(out=pb[:, 0:64], in_=MT[pl][64 * gh:64 * gh + 64, gp].rearrange("p j h -> p (j h)"),
                                               identity=ident[64 * gh:64 * gh + 64, 64 * gh:64 * gh + 64])
                        P.op("pe", trb, r=["MT0" if pl == 0 else "MT1", "ident"], w=[("p2", 2 + g % 4)])
                        P.op("act" if g % 2 == 0 else "dve",
                             (lambda e, pb=pb, d=d, g=g, pl=pl: e.copy(out=BendT[:, d, g, pl, :], in_=pb[:, 0:64])) if g % 2 == 0 else
                             (lambda e, pb=pb, d=d, g=g, pl=pl: e.tensor_copy(out=BendT[:, d, g, pl, :], in_=pb[:, 0:64])),
                             r=[("p2", 2 + g % 4)], w=["BendT"])
                if d == 0:
                    cprod(MT[0][:], MT[1][:], pwr, pwi, 1, 1, cre, cim, d, tk + ["BendT"], ("MT0", "MT1"), neg_im=True)
                else:
                    cprod(MT[0][:], MT[1][:], dpr, dpi, 0, 1, cre, cim, d, tk + ["BendT"], ("MT0", "MT1"), neg_im=True)
                P.op("act", lambda e, d=d: e.copy(out=Dm[:, d, :, 0, :], in_=MT[0][:].rearrange("p g j o -> p g (j o)")), r=["MT0"], w=["Dm"])
                P.op("act", lambda e, d=d: e.copy(out=Dm[:, d, :, 1, :], in_=MT[1][:].rearrange("p g j o -> p g (j o)")), r=["MT1"], w=["Dm"])
                for g in range(32):
                    gh, gp = g // 16, g % 16
                    rows = slice(64 * gh, 64 * gh + 64)
                    pq = PS2[6 + (g % 2)]

                    def mtl(e, rows=rows, gp=gp, pq=pq):
                        e.matmul(pq[:, 0:128], lhsT=MX[0][rows, gp].rearrange("p j h -> p (j h)"), rhs=MX[2][rows, gp].rearrange("p j h -> p (j h)"),
                                 start=True, stop=False)
                        return e.matmul(pq[:, 0:128], lhsT=MX[1][rows, gp].rearrange("p j h -> p (j h)"),
                                        rhs=MX[3][rows, gp].rearrange("p j h -> p (j h)"), start=False, stop=True)
                    P.op("pe", mtl, r=["MX0", "MX1", "MX2", "MX3"], w=[("p2", 6 + g % 2)])
                    if d == 0:
                        P.op("dve", lambda e, g=g, pq=pq: e.tensor_tensor(out=Tloc[:, g, :], in0=pq[:, 0:128], in1=cmk[:, 0:128], op=ALU.mult),
                             r=[("p2", 6 + g % 2), "cmk"], w=[("Tloc", g)])
                    else:
                        P.op("dve", lambda e, g=g, pq=pq: e.tensor_tensor(out=TL[:, g % 2, :], in0=pq[:, 0:128], in1=cmk[:, 128:256], op=ALU.mult),
                             r=[("p2", 6 + g % 2), "cmk"], w=[("TL", g % 2)])
                        P.op("pool", lambda e, g=g: e.tensor_tensor(out=Tloc[:, g, :], in0=Tloc[:, g, :], in1=TL[:, g % 2, :], op=ALU.add),
                             r=[("Tloc", g), ("TL", g % 2)], w=[("Tloc", g)])
                        P.op("pool", lambda e, g=g: e.scalar_tensor_tensor(out=Tloc[:, g, :], in0=ident[:], scalar=dsk[:, g:g + 1], in1=Tloc[:, g, :],
                                                                          op0=ALU.mult, op1=ALU.add) if False else
                             e.tensor_scalar(out=TL[:, g % 2, :], in0=ident[:], scalar1=dsk[:, g:g + 1], scalar2=None, op0=ALU.mult),
                             r=["ident", "dsk", ("Tloc", g)], w=[("TL", g % 2)])
                        P.op("pool", lambda e, g=g: e.tensor_tensor(out=Tloc[:, g, :], in0=Tloc[:, g, :], in1=TL[:, g % 2, :], op=ALU.add),
                             r=[("Tloc", g), ("TL", g % 2)], w=[("Tloc", g)])
            P.flush()
        U8 = sb(st, "U8", [128, 32, NCHe], BF16)
        Hall = [sb(st, "Hall%d" % d, [128, 16, 2, HW], BF16) for d in range(2)]
        with ExitStack() as tu:
            Uc = sb(tu, "Uc", [128, 1, 4096])
            Ucb = sb(tu, "Ucb", [128, 1, 4096], BF16)
            identb2 = sb(tu, "identb2", [128, 128], BF16)
            P.op("dve", lambda e: e.tensor_copy(out=identb2[:], in_=ident[:]), r=["ident"], w=["identb2"])
            u8v = u_s.rearrange("(c j) f -> c (j f)", j=8)
            nblk = (NCHe + 127) // 128
            for cb in range(nblk):
                c0 = cb * 128
                ncb = min(128, NCHe - c0)
                s = 0
                P.dmaop("sp", lambda e, s=s, c0=c0, ncb=ncb: e.dma_start(out=Uc[0:ncb, s, :], in_=u8v[c0:c0 + ncb, :]), r=["u_s"], w=[("Uc", s)])
                P.op("dve", lambda e, s=s, ncb=ncb: e.tensor_copy(
                    out=Ucb[0:ncb, s, :].rearrange("c (g j h) -> c g j h", g=32, j=8),
                    in_=Uc[0:ncb, s, :].rearrange("c (j g h) -> c g j h", j=8, g=32)), r=[("Uc", s)], w=[("Ucb", s)])
                for g4 in range(8):
                    pb = PS2[g4 % 4]
                    pbb = pb[:].bitcast(BF16)

                    def tru(e, g4=g4, s=s, ncb=ncb, pbb=pbb):
                        ins = None
                        for q in range(4):
                            g = g4 * 4 + q
                            ins = e.transpose(out=pbb[:, q * 128:q * 128 + ncb], in_=Ucb[0:ncb, s, g * 128:(g + 1) * 128],
                                              identity=identb2[0:ncb, 0:ncb])
                        return ins
                    P.op("pe", tru, r=[("Ucb", s), "identb2"], w=[("p2", g4 % 4)])
                    P.op("act" if g4 % 2 == 0 else "dve",
                         (lambda e, g4=g4, c0=c0, ncb=ncb, pbb=pbb: e.copy(
                             out=U8[:, g4 * 4:g4 * 4 + 4, c0:c0 + ncb], in_=pbb[:, 0:512].rearrange("p (q c) -> p q c", q=4)[:, :, 0:ncb]))
                         if g4 % 2 == 0 else
                         (lambda e, g4=g4, c0=c0, ncb=ncb, pbb=pbb: e.tensor_copy(
                             out=U8[:, g4 * 4:g4 * 4 + 4, c0:c0 + ncb], in_=pbb[:, 0:512].rearrange("p (q c) -> p q c", q=4)[:, :, 0:ncb])),
                         r=[("p2", g4 % 4)], w=["U8"])
            P.flush()
        if debug == 4:
            d1 = nc.dram_tensor("dbg_tloc", [128, 32 * 128], BF16, kind="ExternalOutput").ap()
            d2 = nc.dram_tensor("dbg_bendt", [128, 2 * 32 * 2 * 64], BF16, kind="ExternalOutput").ap()
            d3 = nc.dram_tensor("dbg_dm", [128, 2 * 16 * 2 * 128], BF16, kind="ExternalOutput").ap()
            d4 = nc.dram_tensor("dbg_u8", [128, 32 * NCHe], BF16, kind="ExternalOutput").ap()
            d5 = nc.dram_tensor("dbg_mu3", [128, 128], F32, kind="ExternalOutput").ap()
            P.dmaop("sp", lambda e: e.dma_start(out=d1, in_=Tloc[:].rearrange("p a b -> p (a b)")), w=["d1"])
            P.dmaop("sp", lambda e: e.dma_start(out=d2, in_=BendT[:].rearrange("p a b c d -> p (a b c d)")), w=["d2"])
            P.dmaop("sp", lambda e: e.dma_start(out=d3, in_=Dm[:].rearrange("p a b c d -> p (a b c d)")), w=["d3"])
            P.dmaop("sp", lambda e: e.dma_start(out=d4, in_=U8[:].rearrange("p a b -> p (a b)")), w=["d4"])
            P.dmaop("sp", lambda e: e.dma_start(out=d5, in_=mu3[:].rearrange("p a b c d -> p (a b c d)")), w=["d5"])
            P.flush()
            DONE.append(1)

        def hk(d, lo, hi):
            return [("Hall", d, q) for q in range(lo, hi)]
        P.op("pool", lambda e: e.memset(Hall[0][:, :, :, 0:1], 0.0), w=hk(0, 0, 1))
        P.op("pool", lambda e: e.memset(Hall[1][:, :, :, NCHe:NCHe + 1], 0.0), w=hk(1, NCHe, NCHe + 1))
        xlo = [NCC + 1, 0]
        clo = [1, NCX]
        ei = 0
        for d in range(2):
            for pl in range(2):
                pc = PS2[4 + pl]
                for gp in range(16):
                    px = PS2[(d * 32 + pl * 16 + gp) % 4]
                    pk = ("p2", (d * 32 + pl * 16 + gp) % 4)

                    def mms(e, d=d, pl=pl, gp=gp, px=px, pc=pc):
                        ins = None
                        for gh in range(2):
                            g = 16 * gh + gp
                            e.matmul(px[64 * gh:64 * gh + 64, 0:NCX], lhsT=BendT[:, d, g, pl, :], rhs=U8[:, g, NCC:NCC + NCX],
                                     start=True, stop=True, tile_position=(0, 64 * gh))
                            ins = e.matmul(pc[64 * gh:64 * gh + 64, gp * 32:gp * 32 + NCC], lhsT=BendT[:, d, g, pl, :], rhs=U8[:, g, 0:NCC],
                                           start=True, stop=True, tile_position=(0, 64 * gh))
                        return ins
                    P.op("pe", mms, r=["BendT", "U8"], w=[pk, ("p2c", 4 + pl, gp)])
                    dst = Hall[d][:, gp, pl, xlo[d]:xlo[d] + NCX]
                    if ei % 2 == 0:
                        P.op("act", lambda e, dst=dst, px=px: e.copy(out=dst, in_=px[:, 0:NCX]), r=[pk], w=hk(d, xlo[d], xlo[d] + NCX))
                    else:
                        P.op("dve", lambda e, dst=dst, px=px: e.tensor_copy(out=dst, in_=px[:, 0:NCX]), r=[pk], w=hk(d, xlo[d], xlo[d] + NCX))
                    ei += 1
                P.op("act", lambda e, d=d, pl=pl, pc=pc: e.copy(out=Hall[d][:, :, pl, clo[d]:clo[d] + NCC],
                                                                in_=pc[:, 0:512].rearrange("p (g c) -> p g c", g=16)[:, :, 0:NCC]),
                     r=[("p2c", 4 + pl, gp) for gp in range(16)], w=hk(d, clo[d], clo[d] + NCC))
        LB = 16
        NB = NCHe // LB
        assert NB * LB == NCHe
        with ExitStack() as tc:
            Rl = [sb(tc, "Rl_%d" % d, [128, 16, 3, NB + 1]) for d in range(2)]
            TA = [sb(tc, "TA_%d" % d, [128, 16, 2, NB]) for d in range(2)]
            TB = [sb(tc, "TB_%d" % d, [128, 16, 2, NB]) for d in range(2)]
            Ea = Rl
            ET = [sb(tc, "ET_%d" % d, [128, 2, 16, 2]) for d in range(2)]

            def hview(d, i):
                st0 = (1 + i) if d == 0 else (LB - 1 - i)
                return Hall[d][:, :, :, st0:st0 + LB * (NB - 1) + 1:LB]

            def hkeys(d, i):
                st0 = (1 + i) if d == 0 else (LB - 1 - i)
                return [("Hall", d, st0 + LB * m) for m in range(NB)]

            def mub(d, k, which, n):
                return mu16[:, d, k, which].unsqueeze(3).to_broadcast([128, 16, 2, n])
            for d in range(2):
                eng = "dve"
                for i in range(LB):
                    hv = hview(d, i)
                    hk_i = hkeys(d, i)
                    if i == 0:
                        P.op(eng, lambda e, d=d, hv=hv: e.tensor_copy(out=Rl[d][:, :, 0:2, 0:NB], in_=hv), r=hk_i, w=[("Rl", d)])
                    else:
                        P.op(eng, lambda e, d=d: e.tensor_tensor(out=TA[d][:], in0=Rl[d][:, :, 0:2, 0:NB], in1=mub(d, 1, 0, NB), op=ALU.mult),
                             r=[("Rl", d), "mu16"], w=[("TA", d)])
                        P.op(eng, lambda e, d=d: e.tensor_tensor(out=TB[d][:], in0=Rl[d][:, :, 1:3, 0:NB], in1=mub(d, 1, 1, NB), op=ALU.mult),
                             r=[("Rl", d), "mu16"], w=[("TB", d)])
                        P.op(eng, lambda e, d=d: e.tensor_tensor(out=TA[d][:], in0=TA[d][:], in1=TB[d][:], op=ALU.add),
                             r=[("TA", d), ("TB", d)], w=[("TA", d)])
                        P.op(eng, lambda e, d=d, hv=hv: e.tensor_tensor(out=Rl[d][:, :, 0:2, 0:NB], in0=TA[d][:], in1=hv, op=ALU.add),
                             r=[("TA", d)] + hk_i, w=[("Rl", d)])
                        P.op("act", lambda e, d=d, hv=hv: e.copy(out=hv, in_=Rl[d][:, :, 0:2, 0:NB]), r=[("Rl", d)], w=hk_i)
                    if i < LB - 1:
                        P.op(eng, lambda e, d=d: e.tensor_copy(out=Rl[d][:, :, 2, 0:NB], in_=Rl[d][:, :, 0, 0:NB]), r=[("Rl", d)], w=[("Rl", d)])
            for d in range(2):
                eng = "dve" if d == 0 else "pool"
                P.op(eng, lambda e, d=d: e.memset(Ea[d][:], 0.0), w=[("Rl", d)])
                order = list(range(NB - 1)) if d == 0 else list(range(NB - 1, 0, -1))
                for m in order:
                    mn = m + 1 if d == 0 else m - 1
                    pend = (1 + 16 * m + 15) if d == 0 else (16 * m)
                    P.op(eng, lambda e, d=d, m=m: e.tensor_tensor(out=ET[d][:, 0], in0=Ea[d][:, :, 0:2, m], in1=mu16[:, d, 16, 0], op=ALU.mult),
                         r=[("Rl", d), "mu16"], w=[("ET", d, 0)])
                    P.op(eng, lambda e, d=d, m=m: e.tensor_tensor(out=ET[d][:, 1], in0=Ea[d][:, :, 1:3, m], in1=mu16[:, d, 16, 1], op=ALU.mult),
                         r=[("Rl", d), "mu16"], w=[("ET", d, 1)])
                    P.op(eng, lambda e, d=d: e.tensor_tensor(out=ET[d][:, 0], in0=ET[d][:, 0], in1=ET[d][:, 1], op=ALU.add),
                         r=[("ET", d, 0), ("ET", d, 1)], w=[("ET", d, 0)])
                    P.op(eng, lambda e, d=d, mn=mn, pend=pend: e.tensor_tensor(out=Ea[d][:, :, 0:2, mn], in0=ET[d][:, 0], in1=Hall[d][:, :, :, pend], op=ALU.add),
                         r=[("ET", d, 0), ("Hall", d, pend)], w=[("Rl", d)])
                    P.op(eng, lambda e, d=d, mn=mn: e.tensor_copy(out=Ea[d][:, :, 2, mn], in_=Ea[d][:, :, 0, mn]), r=[("Rl", d)], w=[("Rl", d)])
            for d in range(2):
                eng = "dve"
                for i in range(LB):
                    hv = hview(d, i)
                    hk_i = hkeys(d, i)
                    P.op(eng, lambda e, d=d, i=i: e.tensor_tensor(out=TA[d][:], in0=Ea[d][:, :, 0:2, 0:NB], in1=mub(d, i + 1, 0, NB), op=ALU.mult),
                         r=[("Rl", d), "mu16"], w=[("TA", d)])
                    P.op(eng, lambda e, d=d, i=i: e.tensor_tensor(out=TB[d][:], in0=Ea[d][:, :, 1:3, 0:NB], in1=mub(d, i + 1, 1, NB), op=ALU.mult),
                         r=[("Rl", d), "mu16"], w=[("TB", d)])
                    P.op(eng, lambda e, d=d: e.tensor_tensor(out=TA[d][:], in0=TA[d][:], in1=TB[d][:], op=ALU.add),
                         r=[("TA", d), ("TB", d)], w=[("TA", d)])
                    P.op(eng, lambda e, d=d, hv=hv: e.tensor_tensor(out=hv, in0=hv, in1=TA[d][:], op=ALU.add), r=[("TA", d)] + hk_i, w=hk_i)
            P.flush()
        NCB = (NCX + 127) // 128
        NPASS = 2 if NCB >= 2 else 1
        CBP = NCB // NPASS
        NCP = NCX // NPASS
        Yc = sb(st, "Yc", [128, CBP, 4096], BF16)
        Ysb = sb(st, "Ysb", [128, 2, NCP])
        allH = [hk(0, 0, HW), hk(1, 0, HW)]
        for pz in range(NPASS):
            c_lo = pz * NCP
            for g in range(32):
                gh, gp = g // 16, g % 16
                rows = slice(64 * gh, 64 * gh + 64)
                pr = PS2[g % 2]
                prk = ("p2", g % 2)

                def mmy(e, g=g, gp=gp, rows=rows, pr=pr, c_lo=c_lo):
                    e.matmul(pr[:, 0:NCP], lhsT=Tloc[:, g, :], rhs=U8[:, g, NCC + c_lo:NCC + c_lo + NCP], start=True, stop=False)
                    e.matmul(pr[:, 0:NCP], lhsT=Dm[rows, 0, gp, 0, :], rhs=Hall[0][rows, gp, 0, NCC + c_lo:NCC + c_lo + NCP], start=False, stop=False)
                    e.matmul(pr[:, 0:NCP], lhsT=Dm[rows, 0, gp, 1, :], rhs=Hall[0][rows, gp, 1, NCC + c_lo:NCC + c_lo + NCP], start=False, stop=False)
                    e.matmul(pr[:, 0:NCP], lhsT=Dm[rows, 1, gp, 0, :], rhs=Hall[1][rows, gp, 0, 1 + c_lo:1 + c_lo + NCP], start=False, stop=False)
                    return e.matmul(pr[:, 0:NCP], lhsT=Dm[rows, 1, gp, 1, :], rhs=Hall[1][rows, gp, 1, 1 + c_lo:1 + c_lo + NCP], start=False, stop=True)
                P.op("pe", mmy, r=[("Tloc", g), "U8", "Dm"] + allH[0] + allH[1], w=[prk])
                ys = g % 2
                if g % 2 == 0:
                    P.op("act", lambda e, ys=ys, pr=pr: e.copy(out=Ysb[:, ys, :], in_=pr[:, 0:NCP]), r=[prk], w=[("Ysb", ys)])
                else:
                    P.op("dve", lambda e, ys=ys, pr=pr: e.tensor_copy(out=Ysb[:, ys, :], in_=pr[:, 0:NCP]), r=[prk], w=[("Ysb", ys)])
                for cb in range(CBP):
                    ncb = min(128, NCP - cb * 128)
                    pt_ = PS2[2 + (g * CBP + cb) % 4]
                    ptk = ("p2", 2 + (g * CBP + cb) % 4)
                    P.op("pe", lambda e, ys=ys, cb=cb, ncb=ncb, pt_=pt_: e.transpose(out=pt_[0:ncb, 0:128], in_=Ysb[:, ys, cb * 128:cb * 128 + ncb],
                                                                                   identity=ident[:]), r=[("Ysb", ys), "ident"], w=[ptk])
                    oap = Yc[0:ncb, cb, :].rearrange("c (j g o) -> c g j o", j=8, g=32)[:, g]
                    iap = pt_[0:ncb, 0:128].rearrange("c (j o) -> c j o", j=8)
                    if (g + cb) % 2 == 0:
                        P.op("dve", lambda e, oap=oap, iap=iap: e.tensor_copy(out=oap, in_=iap), r=[ptk], w=[("Yc", cb)])
                    else:
                        P.op("act", lambda e, oap=oap, iap=iap: e.copy(out=oap, in_=iap), r=[ptk], w=[("Yc", cb)])
            for cb in range(CBP):
                ncb = min(128, NCP - cb * 128)
                r0 = c_lo + cb * 128
                P.dmaop("sp", lambda e, cb=cb, ncb=ncb, r0=r0: e.dma_start(out=y_s[r0:r0 + ncb, :], in_=Yc[0:ncb, cb, :]),
                        r=[("Yc", cb)], w=["y_s"])
        P.flush()
    if DONE:
        es.close()
        return nc
    if debug == 5:
        dbg = nc.dram_tensor("dbg_y", [NXe // 8, 4096], BF16, kind="ExternalOutput").ap()
        P.dmaop("sp", lambda e: e.dma_start(out=dbg, in_=y_s[0:NXe // 8, :]), w=["dbg"])
        P.flush()
        es.close()
        return nc

    if debug in (0, 3, 6):
      with ExitStack() as st:
        vt = sb(st, "vt", [128, NTe, 8, 80], BF16)
        kTh = sb(st, "kTh", [128, 2, NTe * 128], BF16)
        qTh = sb(st, "qTh", [128, 2, NXe], BF16)
        pT = sb(st, "pT", [128, 5, 512], BF16)
        osb = sb(st, "osb", [128, 2, 512])
        rr = sb(st, "rr", [128, 512])
        ao = sb(st, "ao", [64, 2, 512])
        ones1 = sb(st, "ones1", [128, 64])
        PSs = [ps(st, "ps_s%d" % i) for i in range(5)]
        PSo = [ps(st, "ps_o%d" % i) for i in range(2)]
        PSb = ps(st, "ps_b")
        P.op("pool", lambda e: e.memset(vt[:], 1.0), w=["vt"])
        P.op("pool", lambda e: e.memset(ones1[:], 1.0), w=["ones1"])
        for kt in range(NTe):
            P.dmaop("sp" if kt % 2 == 0 else "act",
                    lambda e, kt=kt: e.dma_start(out=vt[:, kt, :, 0:64],
                                                 in_=v_s[kt * 128:(kt + 1) * 128, :].rearrange("p (h d) -> p h d", h=8)),
                    r=["v_s"], w=["vt"])
        cnt = 0
        for h in range(H):
            hs = h % 2
            P.dmaop("sp", lambda e, h=h, hs=hs: e.dma_start(out=kTh[0:96, hs, :], in_=kT_s[h, :, 0:NTe * 128]), r=["kT_s"], w=[("kTh", hs)])
            P.dmaop("act", lambda e, h=h, hs=hs: e.dma_start(out=qTh[0:96, hs, :], in_=qT_s[h, :, 0:NXe]), r=["qT_s"], w=[("qTh", hs)])
            for g in range(NQG):
                og = (h * NQG + g) % 2

                def smm(e, kt, hs=hs, g=g):
                    return e.matmul(PSs[kt % 5][:, 0:QG], lhsT=kTh[0:96, hs, kt * 128:(kt + 1) * 128],
                                    rhs=qTh[0:96, hs, g * QG:(g + 1) * QG], start=True, stop=True)

                def pvm(e, kt, h=h, og=og):
                    return e.matmul(PSo[og][0:65, 0:QG], lhsT=vt[:, kt, h, 0:65], rhs=pT[:, kt % 5, 0:QG],
                                    start=(kt == 0), stop=(kt == NTe - 1))
                LOOK = 3
                for step in range(NTe + LOOK):
                    if step < NTe:
                        kt = step
                        P.op("pe", lambda e, kt=kt, f=smm: f(e, kt), r=[("kTh", hs), ("qTh", hs)], w=[("pss", kt % 5)])
                        P.op("act", lambda e, kt=kt: e.activation(out=pT[:, kt % 5, 0:QG], in_=PSs[kt % 5][:, 0:QG], func=AF.Exp),
                             r=[("pss", kt % 5)], w=[("pT", kt % 5)])
                    if step >= LOOK:
                        kt = step - LOOK
                        P.op("pe", lambda e, kt=kt, f=pvm: f(e, kt), r=[("pT", kt % 5), "vt"], w=[("pso", og)])
                P.op("dve", lambda e, og=og: e.tensor_copy(out=osb[0:65, og, 0:QG], in_=PSo[og][0:65, 0:QG]), r=[("pso", og)], w=[("osb", og)])
                P.op("dve", lambda e, og=og: e.reciprocal(out=rr[64:65, 0:QG], in_=osb[64:65, og, 0:QG]), r=[("osb", og)], w=["rr"])
                P.op("pe", lambda e: e.matmul(PSb[0:64, 0:QG], lhsT=ones1[64:65, 0:64], rhs=rr[64:65, 0:QG], start=True, stop=True),
                     r=["rr", "ones1"], w=["psb"])
                P.op("dve", lambda e, og=og: e.tensor_tensor(out=ao[:, og, 0:QG], in0=osb[0:64, og, 0:QG], in1=PSb[0:64, 0:QG], op=ALU.mult),
                     r=[("osb", og), "psb"], w=[("ao", og)])
                P.dmaop("sp", lambda e, h=h, g=g, og=og: e.dma_start(out=attnT_s[h, :, g * QG:(g + 1) * QG], in_=ao[:, og, 0:QG]),
                        r=[("ao", og)], w=["attnT_s"])
        P.flush()

    CAPe = 2 * NXe // NE
    SLT = min(128, CAPe)
    NRC = CAPe // SLT
    NXT = NXe // 128
    h2b_s = nc.dram_tensor("h2b_s", [NX, D], BF16, kind="Internal").ap()

    if debug in (0, 6):
      with ExitStack() as sp:
        idxT = sb(sp, "idxT", [128, NRC, 16], I32)
        gateT = sb(sp, "gateT", [128, NRC, 16])
        sp45 = ExitStack()
        affT = sb(sp45, "affT", [16, NXe])
        with ExitStack() as st:
            wglu = sb(st, "wglu", [128, 4, 512], BF16)
            wsso = sb(st, "wsso", [128, 4, D], BF16)
            wmla = sb(st, "wmla", [64, 8, D], BF16)
            wout = sb(st, "wout", [128, 8, D], BF16)
            wrt = sb(st, "wrt", [128, 8, 16])
            bglu = sb(st, "bglu", [128, 512])
            identb = sb(st, "identb4", [128, 128], BF16)
            TL4 = [dict(), dict()]
            for _s in range(2):
                TL4[_s]['yt'] = sb(st, "yt_%d" % _s, [128, 512], BF16)
                TL4[_s]['yg'] = sb(st, "yg_%d" % _s, [128, 512])
                TL4[_s]['t1'] = sb(st, "t1_%d" % _s, [128, 512])
                TL4[_s]['ygb'] = sb(st, "ygb_%d" % _s, [128, 512], BF16)
                TL4[_s]['ygT'] = sb(st, "ygT_%d" % _s, [128, 4, 128], BF16)
                TL4[_s]['sg'] = sb(st, "sg_%d" % _s, [128, 512])
                TL4[_s]['zb'] = sb(st, "zb_%d" % _s, [128, 512], BF16)
                TL4[_s]['zT'] = sb(st, "zT_%d" % _s, [128, 4, 128], BF16)
                TL4[_s]['at32'] = sb(st, "at32_%d" % _s, [64, 8, 128])
                TL4[_s]['atb'] = sb(st, "atb_%d" % _s, [64, 8, 128], BF16)
                TL4[_s]['gt'] = sb(st, "gt_%d" % _s, [128, 2048])
                TL4[_s]['m1'] = sb(st, "m1_%d" % _s, [128, D])
                TL4[_s]['m2'] = sb(st, "m2_%d" % _s, [128, D])
                TL4[_s]['mb'] = sb(st, "mb_%d" % _s, [128, D], BF16)
                TL4[_s]['mT'] = sb(st, "mT_%d" % _s, [128, 8, 128], BF16)
                TL4[_s]['xt4'] = sb(st, "xt4_%d" % _s, [128, D])
                TL4[_s]['xm'] = sb(st, "xm_%d" % _s, [128, D])
                TL4[_s]['jk'] = sb(st, "jk_%d" % _s, [128, D])
                TL4[_s]['h2'] = sb(st, "h2_%d" % _s, [128, D])
                TL4[_s]['h2b'] = sb(st, "h2b_%d" % _s, [128, D], BF16)
                TL4[_s]['h2T'] = sb(st, "h2T_%d" % _s, [128, 8, 128])
                TL4[_s]['s4'] = sb(st, "s4_%d" % _s, [128, 8])
                TL4[_s]['lg'] = sb(st, "lg_%d" % _s, [128, 16])
                TL4[_s]['af'] = sb(st, "af_%d" % _s, [128, 16])
            BALL = [ps(st, "ph4_%d" % i) for i in range(8)]
            P.dmaop("pool", lambda e: e.dma_start(out=wglu[:], in_=w_glu.rearrange("(k p) n -> p k n", p=128)), w=["wglu"])
            P.dmaop("pool", lambda e: e.dma_start(out=wsso[:], in_=w_ssm_o.rearrange("(k p) n -> p k n", p=128)), w=["wsso"])
            P.dmaop("pool", lambda e: e.dma_start(out=wmla[:], in_=w_mla_o.rearrange("(h v) n -> v h n", v=64)), w=["wmla"])
            P.dmaop("pool", lambda e: e.dma_start(out=wout[:], in_=w_out.rearrange("(k p) n -> p k n", p=128)), w=["wout"])
            P.dmaop("sp", lambda e: e.dma_start(out=wrt[:], in_=w_router.rearrange("(k p) n -> p k n", p=128)), w=["wrt"])
            P.dmaop("sp", lambda e: e.dma_start(out=bglu[:], in_=b_glu.partition_broadcast(128)), w=["bglu"])
            P.op("dve", lambda e: e.tensor_copy(out=identb[:], in_=ident[:]), r=["ident"], w=["identb4"])
            ysv = y_s.rearrange("c (j f) -> (c j) f", j=8)
            SHARED4 = ["wglu", "wsso", "wmla", "wout", "wrt", "bglu", "identb4", "ident", "modx", "y_s", "gates_s", "attnT_s", "out", "h2b_s", "affT"]
            ALIAS4 = {"b4": "b2", "b5": "b3", "b6": "b2", "b7": "b3"}

            def tile4(i, s):
                Pq = Keyed(P, s, SHARED4, ALIAS4)
                t0 = i * 128
                yt = TL4[s]['yt']
                yg = TL4[s]['yg']
                t1 = TL4[s]['t1']
                ygb = TL4[s]['ygb']
                ygT = TL4[s]['ygT']
                sg = TL4[s]['sg']
                zb = TL4[s]['zb']
                zT = TL4[s]['zT']
                at32 = TL4[s]['at32']
                atb = TL4[s]['atb']
                gt = TL4[s]['gt']
                m1 = TL4[s]['m1']
                m2 = TL4[s]['m2']
                mb = TL4[s]['mb']
                mT = TL4[s]['mT']
                xt4 = TL4[s]['xt4']
                xm = TL4[s]['xm']
                jk = TL4[s]['jk']
                h2 = TL4[s]['h2']
                h2b = TL4[s]['h2b']
                h2T = TL4[s]['h2T']
                s4 = TL4[s]['s4']
                lg = TL4[s]['lg']
                af = TL4[s]['af']
                bk = BALL[4 * s:4 * s + 4]
                B = [bk[0], bk[1], bk[2], bk[3], bk[2], bk[3], bk[2], bk[3]]
                B0b = B[0][:].bitcast(BF16)
                Pq.dmaop("sp", lambda e, t0=t0: e.dma_start(out=yt[:], in_=ysv[t0:t0 + 128, :]), r=["y_s"], w=["yt"])
                Pq.dmaop("act", lambda e, t0=t0: e.dma_start(out=gt[:], in_=gates_s[t0:t0 + 128, :]), r=["gates_s"], w=["gt"])
                Pq.dmaop("sp", lambda e, t0=t0: e.dma_start(out=at32[:], in_=attnT_s[:, :, t0:t0 + 128].rearrange("h v t -> v h t")),
                        r=["attnT_s"], w=["at32"])
                Pq.dmaop("act", lambda e, t0=t0: e.dma_start(out=xt4[:], in_=xc[NCTX + t0:NCTX + t0 + 128, :]), w=["xt4"])
                Pq.op("pool", lambda e: e.tensor_tensor(out=t1[:], in0=yt[:], in1=yt[:], op=ALU.mult), r=["yt"], w=["t1"])
                Pq.op("dve", lambda e: e.tensor_scalar(out=t1[:], in0=t1[:], scalar1=0.044715, scalar2=1.0, op0=ALU.mult, op1=ALU.add),
                     r=["t1"], w=["t1"])
                Pq.op("dve", lambda e: e.tensor_tensor(out=t1[:], in0=t1[:], in1=yt[:], op=ALU.mult), r=["t1", "yt"], w=["t1"])
                Pq.op("act", lambda e: e.activation(out=t1[:], in_=t1[:], func=AF.Tanh, scale=0.7978845608028654), r=["t1"], w=["t1"])
                Pq.op("dve", lambda e: e.tensor_scalar(out=t1[:], in0=t1[:], scalar1=1.0, scalar2=0.5, op0=ALU.add, op1=ALU.mult),
                     r=["t1"], w=["t1"])
                Pq.op("dve", lambda e: e.tensor_tensor(out=yg[:], in0=t1[:], in1=yt[:], op=ALU.mult), r=["t1", "yt"], w=["yg"])
                Pq.op("pool", lambda e: e.tensor_copy(out=ygb[:], in_=yg[:]), r=["yg"], w=["ygb"])

                def tr4(src, n):
                    def f(e):
                        ins = None
                        for k in range(n):
                            ins = e.transpose(out=B0b[:, k * 128:(k + 1) * 128], in_=src[:, k * 128:(k + 1) * 128], identity=identb[:])
                        return ins
                    return f
                Pq.op("pe", tr4(ygb, 4), r=["ygb", "identb4"], w=["b0"])
                Pq.op("act", lambda e: e.copy(out=ygT[:].rearrange("p a b -> p (a b)"), in_=B0b[:, 0:512]), r=["b0"], w=["ygT"])

                def mmglu(e):
                    ins = None
                    for k in range(4):
                        ins = e.matmul(B[1][:, 0:512], lhsT=ygT[:, k, :], rhs=wglu[:, k, :], start=(k == 0), stop=(k == 3))
                    return ins
                Pq.op("pe", mmglu, r=["ygT", "wglu"], w=["b1"])
                Pq.op("dve", lambda e: e.tensor_tensor(out=sg[:], in0=B[1][:, 0:512], in1=bglu[:], op=ALU.add), r=["b1", "bglu"], w=["sg"])
                Pq.op("act", lambda e: e.activation(out=sg[:], in_=sg[:], func=AF.Sigmoid), r=["sg"], w=["sg"])
                Pq.op("dve", lambda e: e.tensor_tensor(out=zb[:], in0=sg[:], in1=yg[:], op=ALU.mult), r=["sg", "yg"], w=["zb"])
                Pq.op("pe", tr4(zb, 4), r=["zb", "identb4"], w=["b0"])
                Pq.op("act", lambda e: e.copy(out=zT[:].rearrange("p a b -> p (a b)"), in_=B0b[:, 0:512]), r=["b0"], w=["zT"])

                def mmsso(e):
                    ins = None
                    for hf in range(2):
                        for k in range(4):
                            ins = e.matmul(B[2 + hf][:, 0:512], lhsT=zT[:, k, :], rhs=wsso[:, k, hf * 512:(hf + 1) * 512], start=(k == 0), stop=(k == 3))
                    return ins
                Pq.op("pe", mmsso, r=["zT", "wsso"], w=["b2", "b3"])
                for hf in range(2):
                    cs = slice(hf * 512, (hf + 1) * 512)
                    Pq.op("dve", lambda e, hf=hf, cs=cs: e.tensor_tensor(out=m1[:, cs], in0=B[2 + hf][:, 0:512], in1=gt[:, cs], op=ALU.mult),
                          r=["b%d" % (2 + hf), "gt"], w=[("m1", hf)])
                Pq.op("pool", lambda e: e.tensor_copy(out=atb[:], in_=at32[:]), r=["at32"], w=["atb"])

                def mmat(e):
                    ins = None
                    for hf in range(2):
                        for h in range(8):
                            ins = e.matmul(B[4 + hf][:, 0:512], lhsT=atb[:, h, :], rhs=wmla[:, h, hf * 512:(hf + 1) * 512], start=(h == 0), stop=(h == 7))
                    return ins
                Pq.op("pe", mmat, r=["atb", "wmla"], w=["b4", "b5"])
                for hf in range(2):
                    cs = slice(hf * 512, (hf + 1) * 512)
                    cs2 = slice(D + hf * 512, D + (hf + 1) * 512)
                    Pq.op("dve", lambda e, hf=hf, cs=cs, cs2=cs2: e.tensor_tensor(out=m2[:, cs], in0=B[4 + hf][:, 0:512], in1=gt[:, cs2], op=ALU.mult),
                          r=["b%d" % (4 + hf), "gt"], w=[("m2", hf)])
                Pq.op("pool", lambda e: e.tensor_tensor(out=mb[:], in0=m1[:], in1=m2[:], op=ALU.add),
                     r=[("m1", 0), ("m1", 1), ("m2", 0), ("m2", 1)], w=["mb"])
                Pq.op("pe", tr4(mb, 8), r=["mb", "identb4"], w=["b0"])
                Pq.op("act", lambda e: e.copy(out=mT[:].rearrange("p a b -> p (a b)"), in_=B0b[:, 0:1024]), r=["b0"], w=["mT"])

                def mmout(e):
                    ins = None
                    for hf in range(2):
                        for k in range(8):
                            ins = e.matmul(B[6 + hf][:, 0:512], lhsT=mT[:, k, :], rhs=wout[:, k, hf * 512:(hf + 1) * 512], start=(k == 0), stop=(k == 7))
                    return ins
                Pq.op("pe", mmout, r=["mT", "wout"], w=["b6", "b7"])
                for hf in range(2):
                    cs = slice(hf * 512, (hf + 1) * 512)
                    Pq.op("dve", lambda e, hf=hf, cs=cs: e.tensor_tensor(out=xm[:, cs], in0=B[6 + hf][:, 0:512], in1=modx[:, 2 * D + hf * 512:2 * D + (hf + 1) * 512],
                                                                    op=ALU.mult), r=["b%d" % (6 + hf), "modx"], w=[("xm", hf)])
                Pq.op("pool", lambda e: e.tensor_tensor(out=xm[:], in0=xm[:], in1=xt4[:], op=ALU.add), r=[("xm", 0), ("xm", 1), "xt4"], w=[("xm", 0), ("xm", 1)])
                Pq.dmaop("sp", lambda e, t0=t0: e.dma_start(out=out[t0:t0 + 128, :], in_=xm[:]), r=[("xm", 0), ("xm", 1)], w=["out"])
                Pq.op("act", lambda e: e.activation(out=jk[:], in_=xm[:], func=AF.Square, accum_out=s4[:, 0:1]), r=[("xm", 0), ("xm", 1)], w=["jk", "s4a"])
                Pq.op("dve", lambda e: e.tensor_scalar(out=s4[:, 1:2], in0=s4[:, 0:1], scalar1=1.0 / D, scalar2=EPS, op0=ALU.mult, op1=ALU.add), r=["s4a"], w=["s4b"])
                Pq.op("act", lambda e: e.activation(out=s4[:, 1:2], in_=s4[:, 1:2], func=AF.Sqrt), r=["s4b"], w=["s4b"])
                Pq.op("dve", lambda e: e.reciprocal(out=s4[:, 1:2], in_=s4[:, 1:2]), r=["s4b"], w=["s4b"])
                Pq.op("dve", lambda e: e.scalar_tensor_tensor(out=h2[:], in0=xm[:], scalar=s4[:, 1:2], in1=modx[:, 4 * D:5 * D], op0=ALU.mult, op1=ALU.mult),
                     r=[("xm", 0), ("xm", 1), "s4b", "modx"], w=["h2"])
                Pq.op("pool", lambda e: e.tensor_tensor(out=h2[:], in0=h2[:], in1=modx[:, 3 * D:4 * D], op=ALU.add), r=["h2", "modx"], w=["h2"])
                Pq.op("act", lambda e: e.copy(out=h2b[:], in_=h2[:]), r=["h2"], w=["h2b"])
                Pq.dmaop("act", lambda e, t0=t0: e.dma_start(out=h2b_s[t0:t0 + 128, :], in_=h2b[:]), r=["h2b"], w=["h2b_s"])
                def trh2(e):
                    ins = None
                    for k in range(8):
                        ins = e.transpose(out=B[2 + k // 4][:, (k % 4) * 128:(k % 4 + 1) * 128], in_=h2[:, k * 128:(k + 1) * 128], identity=ident[:])
                    return ins
                Pq.op("pe", trh2, r=["h2", "ident", ("m1", 0), ("m1", 1)], w=["b2", "b3"])
                Pq.op("act", lambda e: e.copy(out=h2T[:, 0:4, :].rearrange("p a b -> p (a b)"), in_=B[2][:, 0:512]), r=["b2"], w=[("h2T", 0)])
                Pq.op("dve", lambda e: e.tensor_copy(out=h2T[:, 4:8, :].rearrange("p a b -> p (a b)"), in_=B[3][:, 0:512]), r=["b3"], w=[("h2T", 1)])

                def mmrt(e):
                    ins = None
                    for k in range(8):
                        ins = e.matmul(B[1][:, 0:16], lhsT=h2T[:, k, :], rhs=wrt[:, k, :], start=(k == 0), stop=(k == 7))
                    return ins
                Pq.op("pe", mmrt, r=[("h2T", 0), ("h2T", 1), "wrt", "sg"], w=["b1"])
                Pq.op("dve", lambda e: e.tensor_copy(out=lg[:], in_=B[1][:, 0:16]), r=["b1"], w=["lg"])
                Pq.op("dve", lambda e: e.tensor_reduce(out=s4[:, 2:3], in_=lg[:], axis=AX.X, op=ALU.max), r=["lg"], w=["s4c"])
                Pq.op("dve", lambda e: e.tensor_scalar(out=s4[:, 3:4], in0=s4[:, 2:3], scalar1=-1.0, scalar2=None, op0=ALU.mult), r=["s4c"], w=["s4d"])
                Pq.op("act", lambda e: e.activation(out=af[:], in_=lg[:], func=AF.Exp, bias=s4[:, 3:4], accum_out=s4[:, 4:5]), r=["lg", "s4d"], w=["af", "s4e"])
                Pq.op("dve", lambda e: e.reciprocal(out=s4[:, 5:6], in_=s4[:, 4:5]), r=["s4e"], w=["s4f"])
                Pq.op("dve", lambda e: e.tensor_scalar(out=af[:], in0=af[:], scalar1=s4[:, 5:6], scalar2=None, op0=ALU.mult), r=["af", "s4f"], w=["af"])
                Pq.op("pe", lambda e: e.transpose(out=B[4][0:16, 0:128], in_=af[:], identity=ident[:]), r=["af", "ident", ("m2", 0), ("m2", 1)], w=["b4"])
                Pq.op("act", lambda e, t0=t0: e.copy(out=affT[:, t0:t0 + 128], in_=B[4][0:16, 0:128]), r=["b4"], w=["affT"])
                return Pq.cap
            for i in range(0, NXT, 2):
                caps = [tile4(i, 0)] + ([tile4(i + 1, 1)] if i + 1 < NXT else [])
                interleave(P, caps, chunk=int(os.environ.get("ILV", "2")))
            P.flush()
        with ExitStack() as st:
            wk = sb(st, "wk", [16, NXe])
            vals = sb(st, "vals", [16, CAPe])
            idxu = sb(st, "idxu", [16, CAPe], U32)
            idxf = sb(st, "idxf", [16, CAPe])
            B5 = ps(st, "ph5")
            B5b = ps(st, "ph5b")
            P.op("dve", lambda e: e.tensor_copy(out=wk[:], in_=affT[:]), r=["affT"], w=["wk"])
            for r_ in range(CAPe // 8):
                sl = slice(r_ * 8, r_ * 8 + 8)
                P.op("dve", lambda e, sl=sl: e.max(out=vals[:, sl], in_=wk[:]), r=["wk"], w=[("vals", r_)])
                P.op("dve", lambda e, sl=sl: e.max_index(out=idxu[:, sl], in_max=vals[:, sl], in_values=wk[:]), r=["wk", ("vals", r_)], w=[("idxu", r_)])
                P.op("dve", lambda e, sl=sl: e.match_replace(out=wk[:], in_to_replace=vals[:, sl], in_values=wk[:], imm_value=-1.0),
                     r=["wk", ("vals", r_), ("idxu", r_)], w=["wk"])
            allv = [("vals", r_) for r_ in range(CAPe // 8)]
            alli = [("idxu", r_) for r_ in range(CAPe // 8)]
            P.op("dve", lambda e: e.tensor_copy(out=idxf[:], in_=idxu[:]), r=alli, w=["idxf"])
            for rc in range(NRC):
                cs = slice(rc * SLT, (rc + 1) * SLT)
                P.op("pe", lambda e, cs=cs: e.transpose(out=B5[0:SLT, 0:16], in_=idxf[:, cs], identity=ident[0:16, 0:16]), r=["idxf", "ident"], w=["b5a"])
                P.op("dve", lambda e, rc=rc: e.tensor_copy(out=idxT[0:SLT, rc, :], in_=B5[0:SLT, 0:16]), r=["b5a"], w=["idxT"])
                P.op("pe", lambda e, cs=cs: e.transpose(out=B5b[0:SLT, 0:16], in_=vals[:, cs], identity=ident[0:16, 0:16]), r=allv + ["ident"], w=["b5b"])
                P.op("dve", lambda e, rc=rc: e.tensor_copy(out=gateT[0:SLT, rc, :], in_=B5b[0:SLT, 0:16]), r=["b5b"], w=["gateT"])
            P.flush()
        sp45.close()
        NEe = NE if debug == 0 else int(os.environ.get("NEE", "16"))
        with ExitStack() as st:
            identb = sb(st, "identb6", [128, 128], BF16)
            xs = sb(st, "xs", [128, 1, NRC, D], BF16)
            xsT = sb(st, "xsT", [128, 2, 8, CAPe], BF16)
            wg = sb(st, "wg", [128, 3, 8, 512], BF16)
            wu = sb(st, "wu", [128, 3, 8, 512], BF16)
            wd = sb(st, "wd", [128, 2, 22, 512], BF16)
            sgt = sb(st, "sgt", [128, 2, CAPe])
            hidT = sb(st, "hidT", [128, 22, CAPe], BF16)
            ys = sb(st, "ys", [128, 1, NRC, D])
            B = [ps(st, "ph6_%d" % i) for i in range(8)]
            B0b = B[0][:].bitcast(BF16)
            P.op("dve", lambda e: e.tensor_copy(out=identb[:], in_=ident[:]), r=["ident"], w=["identb6"])
            wgi = 0
            wdi = 0
            def prep(ex):
                sl = ex % 2
                for rc in range(NRC):
                    P.dmaop("pool", lambda e, rc=rc, ex=ex, sl=sl: e.indirect_dma_start(
                        out=xs[0:SLT, 0, rc, :], out_offset=None, in_=h2b_s[0:NXe, :],
                        in_offset=bass.IndirectOffsetOnAxis(ap=idxT[0:SLT, rc, ex:ex + 1], axis=0)),
                        r=["idxT", "h2b_s"], w=[("xs", 0, rc)])

                    def trx(e, rc=rc, sl=sl):
                        ins = None
                        for k in range(8):
                            ins = e.transpose(out=B0b[:, k * 128:k * 128 + SLT], in_=xs[0:SLT, 0, rc, k * 128:(k + 1) * 128], identity=identb[0:SLT, 0:SLT])
                        return ins
                    P.op("pe", trx, r=[("xs", 0, rc), "identb6"], w=["b0"])
                    P.op("act", lambda e, rc=rc, sl=sl: e.copy(out=xsT[:, sl, :, rc * SLT:(rc + 1) * SLT],
                                                               in_=B0b[:, 0:1024].rearrange("p (k c) -> p k c", k=8)[:, :, 0:SLT]), r=["b0"], w=[("xsT", sl)])
            prep(0)
            pending = []
            for ex in range(NEe):
                xsl = ex % 2
                wgv = w_e_gate[ex].rearrange("(dc p) f -> p dc f", p=128)
                wuv = w_e_up[ex].rearrange("(dc p) f -> p dc f", p=128)
                wdv = w_e_down[ex].rearrange("(fc p) d -> p fc d", p=128)
                for grp in range(6):
                    ws = wgi % 3
                    wgi += 1
                    f0 = grp * 512
                    fw = min(512, FF - f0)
                    P.dmaop("pool", lambda e, ws=ws, f0=f0, fw=fw, wgv=wgv: e.dma_start(out=wg[:, ws, :, 0:fw], in_=wgv[:, :, f0:f0 + fw]), w=[("wg", ws)])
                    P.dmaop("pool", lambda e, ws=ws, f0=f0, fw=fw, wuv=wuv: e.dma_start(out=wu[:, ws, :, 0:fw], in_=wuv[:, :, f0:f0 + fw]), w=[("wu", ws)])
                    if grp == 1:
                        for f_ in pending:
                            f_()
                        pending = []
                    for q in range(fw // 128):
                        fc = grp * 4 + q
                        pg = B[1 + fc % 2]
                        pu = B[3 + fc % 2]

                        def mmgu(e, ws=ws, q=q, pg=pg, pu=pu, xsl=xsl):
                            ins = None
                            for k in range(8):
                                e.matmul(pg[:, 0:CAPe], lhsT=wg[:, ws, k, q * 128:(q + 1) * 128], rhs=xsT[:, xsl, k, :], start=(k == 0), stop=(k == 7))
                            for k in range(8):
                                ins = e.matmul(pu[:, 0:CAPe], lhsT=wu[:, ws, k, q * 128:(q + 1) * 128], rhs=xsT[:, xsl, k, :], start=(k == 0), stop=(k == 7))
                            return ins
                        P.op("pe", mmgu, r=[("wg", ws), ("wu", ws), ("xsT", xsl)], w=[("b6", 1 + fc % 2), ("b6", 3 + fc % 2)])
                        P.op("act", lambda e, fc=fc, pg=pg: e.activation(out=sgt[:, fc % 2, :], in_=pg[:, 0:CAPe], func=AF.Silu),
                             r=[("b6", 1 + fc % 2)], w=[("sgt", fc % 2)])
                        P.op("dve", lambda e, fc=fc, pu=pu: e.tensor_tensor(out=hidT[:, fc, :], in0=sgt[:, fc % 2, :], in1=pu[:, 0:CAPe], op=ALU.mult),
                             r=[("sgt", fc % 2), ("b6", 3 + fc % 2)], w=[("hidT", fc)])
                hk_ = [("hidT", fc) for fc in range(22)]
                if ex + 1 < NEe:
                    prep(ex + 1)
                for dq in range(2):
                    ws = wdi % 2
                    wdi += 1
                    P.dmaop("pool", lambda e, ws=ws, dq=dq, wdv=wdv: e.dma_start(out=wd[:, ws], in_=wdv[:, :, dq * 512:(dq + 1) * 512]), w=[("wd", ws)])
                    for rc in range(NRC):
                        py = B[5 + (dq * NRC + rc) % 2]
                        pyk = ("b6", 5 + (dq * NRC + rc) % 2)

                        def mmd(e, ws=ws, rc=rc, py=py):
                            ins = None
                            for fc in range(22):
                                ins = e.matmul(py[0:SLT, 0:512], lhsT=hidT[:, fc, rc * SLT:(rc + 1) * SLT], rhs=wd[:, ws, fc, :], start=(fc == 0), stop=(fc == 21))
                            return ins
                        P.op("pe", mmd, r=hk_ + [("wd", ws)], w=[pyk])
                        P.op("dve", lambda e, rc=rc, dq=dq, py=py, ex=ex, xsl=xsl: e.scalar_tensor_tensor(
                            out=ys[0:SLT, 0, rc, dq * 512:(dq + 1) * 512], in0=py[0:SLT, 0:512], scalar=gateT[0:SLT, rc, ex:ex + 1],
                            in1=modx[0:SLT, 5 * D + dq * 512:5 * D + (dq + 1) * 512], op0=ALU.mult, op1=ALU.mult),
                            r=[pyk, "gateT", "modx"], w=[("ys", 0, rc, dq)])
                def scat(ex=ex):
                    prevk = [("outx", (ex - 1) % 2, rc2) for rc2 in range(NRC)] if ex > 0 else ["out"]
                    for rc in range(NRC):
                        P.dmaop("pool", lambda e, rc=rc, ex=ex: e.indirect_dma_start(
                            out=out[0:NXe, :], out_offset=bass.IndirectOffsetOnAxis(ap=idxT[0:SLT, rc, ex:ex + 1], axis=0),
                            in_=ys[0:SLT, 0, rc, :], in_offset=None, compute_op=ALU.add),
                            r=[("ys", 0, rc, dq) for dq in range(2)] + ["idxT"] + prevk, w=[("outx", ex % 2, rc)])
                pending.append(scat)
            for f_ in pending:
                f_()
            P.flush()
        if debug == 6:
            DONE.append(1)
    if DONE:
        es.close()
        return nc

    if debug == 3:
        dbg = nc.dram_tensor("dbg", [H, 64, 256], F32, kind="ExternalOutput").ap()
        P.dmaop("sp", lambda e: e.dma_start(out=dbg, in_=attnT_s[:, :, 0:256]), w=["dbg"])
        P.flush()
        es.close()
        return nc

    if debug == 2:
        dbg = nc.dram_tensor("dbg", [H, QK, 512], BF16, kind="ExternalOutput").ap()
        dbg2 = nc.dram_tensor("dbg2", [512, 512], F32, kind="ExternalOutput").ap()
        dbg3 = nc.dram_tensor("dbg3", [H, QK, 256], BF16, kind="ExternalOutput").ap()
        P.dmaop("sp", lambda e: e.dma_start(out=dbg, in_=kT_s[:, :, 0:512]), w=["dbg"])
        P.dmaop("sp", lambda e: e.dma_start(out=dbg2, in_=u_s[0:512, :]), w=["dbg2"])
        P.dmaop("sp", lambda e: e.dma_start(out=dbg3, in_=qT_s[:, :, 0:256]), w=["dbg3"])
        P.flush()
        es.close()
        return nc

    if debug == 1:
        dbg = nc.dram_tensor("dbg", [128, 6 * D], F32, kind="ExternalOutput").ap()
        P.dmaop("sp", lambda e: e.dma_start(out=dbg, in_=modx[:]), r=["modx"], w=["dbg"])
        P.flush()
        es.close()
        return nc

    es.close()
    return nc


def _consts():
    ident = np.eye(128, dtype=np.float32)
    n = NX
    rows = n // 64
    row = np.repeat(np.arange(rows, dtype=np.float32), 64)
    col = np.tile(np.arange(64, dtype=np.float32), rows)
    inv = (10000.0 ** (-np.arange(8, dtype=np.float32) / 8)).astype(np.float32)
    ang = np.stack([row[:, None] * inv, col[:, None] * inv], axis=1).astype(np.float32)
    rope = np.zeros((NT, 32), np.float32)
    rope[:NCTX, :16] = 1.0
    rope[NCTX:, :16] = np.cos(ang).reshape(n, 16)
    rope[NCTX:, 16:] = np.sin(ang).reshape(n, 16)
    cm = np.zeros((128, 256), np.float32)
    for jp in range(8):
        for j in range(8):
            if jp <= j:
                cm[jp * 16:(jp + 1) * 16, j * 16:(j + 1) * 16] = 1.0
            if jp >= j:
                cm[jp * 16:(jp + 1) * 16, 128 + j * 16:128 + (j + 1) * 16] = 1.0
    return ident, rope, cm


def make_in_maps(inputs):
    ident, rope, cm = _consts()
    f = lambda a: np.ascontiguousarray(np.asarray(a, dtype=np.float32))
    maps = []
    for b in range(NCORES):
        m = {"xc": f(np.concatenate([inputs["ctx"][b], inputs["x"][b]], axis=0)),
             "cb": f(inputs["c"][b]), "c_ctx": f(inputs["c_ctx"]),
             "ident": ident, "rope": rope, "cmask": cm}
        for k in ["w_ada", "b_ada", "norm1_g", "norm2_g", "w_in", "q_a_g", "w_qb", "kv_a_g", "w_kvb", "q_norm_g",
                  "k_norm_g", "w_mla_o", "ssm_lam_re", "ssm_lam_im", "ssm_log_dt", "ssm_b_re", "ssm_b_im",
                  "ssm_c_re", "ssm_c_im", "ssm_d", "w_glu", "b_glu", "w_ssm_o", "w_out", "w_router",
                  "w_e_gate", "w_e_up", "w_e_down"]:
            m[k] = f(np.asarray(inputs[k])[0])
        maps.append(m)
    return maps


def kernel(**inputs):
    nc = build()
    maps = make_in_maps(inputs)
    res = run_bass_kernel_spmd(nc, maps, core_ids=list(range(NCORES)))
    return np.stack([np.asarray(r["out"], dtype=np.float32) for r in res.results], axis=0)
```

```python
import math
import os
from contextlib import ExitStack

import numpy as np
import concourse.bass as bass
import concourse.mybir as mybir
from concourse.bass_utils import run_bass_kernel_spmd

F32 = mybir.dt.float32
F32R = mybir.dt.float32r
BF16 = mybir.dt.bfloat16
U32 = mybir.dt.uint32
I32 = mybir.dt.int32
ALU = mybir.AluOpType
AF = mybir.ActivationFunctionType
AX = mybir.AxisListType

D = 1024
NX = 4096
NCTX = 256
NT = NX + NCTX
NTILE = NT // 128
NCH = NT // 8
NCH_C = NCTX // 8
H = 8
QK = 96
NE = 16
FF = 2816
CAP = 512
EPS = 1e-6
IN_COLS = 3232
NCORES = 4

ENG = {"pe": "tensor", "act": "scalar", "dve": "vector", "pool": "gpsimd", "sp": "sync"}


class Prog:
    def __init__(self, nc, sems, dsems):
        self.nc = nc
        self.ops = []
        self.lastw = {}
        self.readers = {}
        self.sems = sems
        self.dsems = dsems
        self.sig = {e: 0 for e in ENG}
        self.dcnt = {e: [0] * len(dsems[e]) for e in dsems}
        self.dnext = {e: 0 for e in dsems}
        self.seen = {e: {} for e in ENG}
        self.emitted = 0

    def op(self, eng, fn, r=(), w=(), dma=False):
        i = len(self.ops)
        deps = set()
        for k in list(r) + list(w):
            if k in self.lastw:
                deps.add(self.lastw[k])
        for k in w:
            for j in self.readers.get(k, ()):
                deps.add(j)
        deps.discard(i)
        o = dict(eng=eng, fn=fn, deps=deps, dma=dma, need=False, val=None, sem=None, idx=i)
        self.ops.append(o)
        for k in w:
            self.lastw[k] = i
            self.readers[k] = []
        for k in r:
            if k not in w:
                self.readers.setdefault(k, []).append(i)
        return i

    def dmaop(self, eng, fn, r=(), w=()):
        return self.op(eng, fn, r, w, dma=True)

    def flush(self, final_wait_eng="sp"):
        nc = self.nc
        ops = self.ops[self.emitted:]
        pos = {}
        cnt = {e: 0 for e in ENG}
        for o in self.ops[:self.emitted]:
            pass
        for o in ops:
            pos[o["idx"]] = cnt[o["eng"]]
            cnt[o["eng"]] += 1
        for o in ops:
            real = []
            for d in o["deps"]:
                if d < self.emitted:
                    continue
                p = self.ops[d]
                if not p["dma"] and p["eng"] == o["eng"]:
                    if o["eng"] == "pe":
                        continue
                    if o["dma"]:
                        continue
                real.append(d)
                p["need"] = True
            o["real"] = real
        for o in ops:
            if o["dma"]:
                for d in o["deps"]:
                    if d >= self.emitted:
                        p = self.ops[d]
                        if not p["dma"] and p["eng"] == o["eng"] and d not in o["real"]:
                            o["real"].append(d)
                            p["need"] = True
        for o in ops:
            if o["dma"]:
                e = o["eng"]
                k = self.dnext[e]
                self.dnext[e] = (k + 1) % len(self.dsems[e])
                o["prev"] = self.dcnt[e][k]
                self.dcnt[e][k] += 16
                o["sem"] = self.dsems[e][k]
                o["val"] = self.dcnt[e][k]
            elif o["need"]:
                self.sig[o["eng"]] += 1
                o["sem"] = self.sems[o["eng"]]
                o["val"] = self.sig[o["eng"]]
        byeng = {e: [o for o in ops if o["eng"] == e] for e in ENG}
        seen = self.seen
        allops = self.ops
        dsems = self.dsems
        dcnt = self.dcnt

        def body(e):
            def f(engine):
                sn = seen[e]

                def wait(sem, val):
                    key = id(sem)
                    if sn.get(key, 0) >= val:
                        return
                    sn[key] = val
                    engine.wait_ge(sem, val)

                for o in byeng[e]:
                    for d in o["real"]:
                        p = allops[d]
                        wait(p["sem"], p["val"])
                    if o["dma"]:
                        if o["prev"] > 0:
                            wait(o["sem"], o["prev"])
                        ins = o["fn"](engine)
                        ins.then_inc(o["sem"], 16)
                    else:
                        ins = o["fn"](engine)
                        if o["need"]:
                            ins.then_inc(o["sem"], 1)
                if e in dsems:
                    for k, s in enumerate(dsems[e]):
                        if dcnt[e][k] > 0:
                            wait(s, dcnt[e][k])
            return f

        with nc.Block() as block:
            for e in ENG:
                getattr(block, ENG[e])(body(e))
        self.emitted = len(self.ops)
        self.lastw = {}
        self.readers = {}


class Keyed:
    def __init__(self, P, slot, shared, alias=None):
        self.P, self.slot, self.shared, self.alias = P, slot, set(shared), (alias or {})
        self.cap = []

    def _k(self, keys):
        out = []
        for k in keys:
            k = self.alias.get(k, k)
            base = k[0] if isinstance(k, tuple) else k
            out.append(k if base in self.shared else ("slot", self.slot, k))
        return out

    def op(self, eng, fn, r=(), w=(), dma=False):
        self.cap.append((eng, fn, self._k(r), self._k(w), dma))

    def dmaop(self, eng, fn, r=(), w=()):
        self.op(eng, fn, r, w, dma=True)


def interleave(P, caps, chunk=3):
    pos = [0] * len(caps)
    live = True
    while live:
        live = False
        for i, c in enumerate(caps):
            n = 0
            while pos[i] < len(c) and n < chunk:
                eng, fn, r, w, dma = c[pos[i]]
                P.op(eng, fn, r, w, dma=dma)
                pos[i] += 1
                n += 1
            if pos[i] < len(c):
                live = True


def r32(ap):
    return ap.bitcast(F32R)


def build(debug=0):
    nc = bass.Bass("TRN2", target_bir_lowering=False)
    es = ExitStack()
    DONE = []

    def din(name, shape, dt=F32):
        return nc.dram_tensor(name, list(shape), dt, kind="ExternalInput").ap()

    def dscr(name, shape, dt=F32):
        return nc.dram_tensor(name, list(shape), dt, kind="Internal").ap()

    xc = din("xc", [NT, D])
    cb = din("cb", [D])
    cctx = din("c_ctx", [D])
    w_ada = din("w_ada", [D, 6 * D])
    b_ada = din("b_ada", [6 * D])
    norm1_g = din("norm1_g", [D])
    norm2_g = din("norm2_g", [D])
    w_in = din("w_in", [D, IN_COLS])
    q_a_g = din("q_a_g", [384])
    w_qb = din("w_qb", [384, 768])
    kv_a_g = din("kv_a_g", [256])
    w_kvb = din("w_kvb", [256, 1024])
    q_norm_g = din("q_norm_g", [96])
    k_norm_g = din("k_norm_g", [96])
    w_mla_o = din("w_mla_o", [512, D])
    lam_re = din("ssm_lam_re", [2, 32, 64])
    lam_im = din("ssm_lam_im", [2, 32, 64])
    log_dt = din("ssm_log_dt", [2, 32])
    b_re = din("ssm_b_re", [2, 32, 64, 16])
    b_im = din("ssm_b_im", [2, 32, 64, 16])
    c_re = din("ssm_c_re", [2, 32, 16, 64])
    c_im = din("ssm_c_im", [2, 32, 16, 64])
    ssm_d = din("ssm_d", [512])
    w_glu = din("w_glu", [512, 512])
    b_glu = din("b_glu", [512])
    w_ssm_o = din("w_ssm_o", [512, D])
    w_out = din("w_out", [D, D])
    w_router = din("w_router", [D, NE])
    if debug in (0, 6):
        w_e_gate = din("w_e_gate", [NE, D, FF])
        w_e_up = din("w_e_up", [NE, D, FF])
        w_e_down = din("w_e_down", [NE, FF, D])
    ident_d = din("ident", [128, 128])
    rope_d = din("rope", [NT, 32])
    jrev_d = din("jrev", [128, 128])
    cmask_d = din("cmask", [128, 256])
    out = nc.dram_tensor("out", [NX, D], F32, kind="ExternalOutput").ap()

    u_s = dscr("u_s", [NT, 512])
    gates_s = dscr("gates_s", [NX, 2048])
    qT_s = dscr("qT_s", [H, QK, NX], BF16)
    kT_s = dscr("kT_s", [H, QK, NT], BF16)
    v_s = dscr("v_s", [NT, 512], BF16)
    attnT_s = dscr("attnT_s", [H, 64, NX])
    y_s = dscr("y_s", [NX // 8, 8 * 512], BF16)
    h2_s = dscr("h2_s", [NX, D])

    sems = {e: es.enter_context(nc.semaphore("s_" + e)) for e in ENG}
    dsems = {e: [es.enter_context(nc.semaphore("d_%s%d" % (e, k))) for k in range(8)]
             for e in ("sp", "act", "pool")}
    P = Prog(nc, sems, dsems)

    def sb(stack, name, shape, dt=F32):
        return stack.enter_context(nc.sbuf_tensor("t_" + name, list(shape), dt))

    def ps(stack, name, shape=(128, 512), dt=F32):
        return stack.enter_context(nc.psum_tensor("p_" + name, list(shape), dt))

    ident = sb(es, "ident", [128, 128])
    modx = sb(es, "modx", [128, 6 * D])
    es1 = ExitStack()
    modc = sb(es1, "modc", [128, 2 * D])
    P.dmaop("sp", lambda e: e.dma_start(out=ident[:], in_=ident_d), w=["ident"])

    with ExitStack() as st:
        cT = sb(st, "cT", [128, 2, 8])
        sc = sb(st, "sc", [128, 2, 8])
        lbc = sb(st, "lbc", [128, 2, 8, 128], BF16)
        wa = sb(st, "wa", [128, 2, 8, 512], BF16)
        bb = sb(st, "bb", [128, 6 * D])
        g1b = sb(st, "g1b", [128, D])
        g2b = sb(st, "g2b", [128, D])
        pm = [ps(st, "pm%d" % i) for i in range(4)]
        P.dmaop("sp", lambda e: e.dma_start(out=cT[:, 0, :], in_=cb.rearrange("(dc p) -> p dc", p=128),
                                            allow_slow_non_contiguous=True), w=["cT0"])
        P.dmaop("sp", lambda e: e.dma_start(out=cT[:, 1, :], in_=cctx.rearrange("(dc p) -> p dc", p=128),
                                            allow_slow_non_contiguous=True), w=["cT1"])
        P.dmaop("act", lambda e: e.dma_start(out=bb[:], in_=b_ada.partition_broadcast(128)), w=["bb"])
        P.dmaop("act", lambda e: e.dma_start(out=g1b[:], in_=norm1_g.partition_broadcast(128)), w=["g1b"])
        P.dmaop("act", lambda e: e.dma_start(out=g2b[:], in_=norm2_g.partition_broadcast(128)), w=["g2b"])
        P.op("act", lambda e: e.activation(out=sc[:], in_=cT[:], func=AF.Silu), r=["cT0", "cT1"], w=["sc"])
        P.op("dve", lambda e: e.tensor_copy(out=lbc[:], in_=sc[:].unsqueeze(3).to_broadcast([128, 2, 8, 128])),
             r=["sc"], w=["lbc"])
        wv = w_ada.rearrange("(dc p) n -> p dc n", p=128)
        for ct in range(12):
            s = ct % 2
            P.dmaop("pool",
                    lambda e, ct=ct, s=s: e.dma_start(out=wa[:, s], in_=wv[:, :, ct * 512:(ct + 1) * 512]),
                    w=[("wa", s)])
            for which in range(2 if ct < 4 else 1):
                pt = pm[(ct * 2 + which) % 4]
                pk = ("pm", (ct * 2 + which) % 4)

                def mm(e, pt=pt, s=s, which=which):
                    ins = None
                    for dc in range(8):
                        ins = e.matmul(pt[:], lhsT=lbc[:, which, dc, :], rhs=wa[:, s, dc, :],
                                       start=(dc == 0), stop=(dc == 7))
                    return ins
                P.op("pe", mm, r=["lbc", ("wa", s)], w=[pk])
                dst = modx if which == 0 else modc
                P.op("dve", lambda e, pt=pt, dst=dst, ct=ct: e.tensor_tensor(
                    out=dst[:, ct * 512:(ct + 1) * 512], in0=pt[:], in1=bb[:, ct * 512:(ct + 1) * 512], op=ALU.add),
                    r=[pk, "bb"], w=["modx" if which == 0 else "modc"])
        P.op("dve", lambda e: e.scalar_tensor_tensor(out=modx[:, D:2 * D], in0=modx[:, D:2 * D], scalar=1.0, in1=g1b[:],
                                                     op0=ALU.add, op1=ALU.mult), r=["modx", "g1b"], w=["modx"])
        P.op("dve", lambda e: e.scalar_tensor_tensor(out=modx[:, 4 * D:5 * D], in0=modx[:, 4 * D:5 * D], scalar=1.0, in1=g2b[:],
                                                     op0=ALU.add, op1=ALU.mult), r=["modx", "g2b"], w=["modx"])
        P.op("dve", lambda e: e.scalar_tensor_tensor(out=modc[:, D:2 * D], in0=modc[:, D:2 * D], scalar=1.0, in1=g1b[:],
                                                     op0=ALU.add, op1=ALU.mult), r=["modc", "g1b"], w=["modc"])
        P.flush()


    with ExitStack() as st:
      if debug != 1:
          TL1 = [dict(), dict()]
          w_in_sb = sb(st, "w_in_sb", [128, 8, IN_COLS], BF16)
          w_qb_sb = sb(st, "w_qb_sb", [128, 3, 768], BF16)
          w_kvb_sb = sb(st, "w_kvb_sb", [128, 2, 1024], BF16)
          qag = sb(st, "qag", [128, 384])
          kvag = sb(st, "kvag", [128, 256])
          qng = sb(st, "qng", [128, 96])
          kng = sb(st, "kng", [128, 96])
          identb = sb(st, "identb", [128, 128], BF16)
          xt = sb(st, "xt", [128, 2, D])
          TL1[0]['junk'] = sb(st, "junk_0", [128, D]); TL1[1]['junk'] = sb(st, "junk_1", [128, D])
          TL1[0]['hh'] = sb(st, "hh_0", [128, D]); TL1[1]['hh'] = sb(st, "hh_1", [128, D])
          TL1[0]['hb'] = sb(st, "hb_0", [128, D], BF16); TL1[1]['hb'] = sb(st, "hb_1", [128, D], BF16)
          TL1[0]['hT'] = sb(st, "hT_0", [128, 8, 128], BF16); TL1[1]['hT'] = sb(st, "hT_1", [128, 8, 128], BF16)
          proj = sb(st, "proj", [128, 2, IN_COLS])
          st8 = sb(st, "st8", [128, 2, 64])
          TL1[0]['qn'] = sb(st, "qn_0", [128, 384], BF16); TL1[1]['qn'] = sb(st, "qn_1", [128, 384], BF16)
          TL1[0]['qnT'] = sb(st, "qnT_0", [128, 3, 128], BF16); TL1[1]['qnT'] = sb(st, "qnT_1", [128, 3, 128], BF16)
          TL1[0]['kvn'] = sb(st, "kvn_0", [128, 256], BF16); TL1[1]['kvn'] = sb(st, "kvn_1", [128, 256], BF16)
          TL1[0]['kvnT'] = sb(st, "kvnT_0", [128, 2, 128], BF16); TL1[1]['kvnT'] = sb(st, "kvnT_1", [128, 2, 128], BF16)
          TL1[0]['qsq'] = sb(st, "qsq_0", [128, 768]); TL1[1]['qsq'] = sb(st, "qsq_1", [128, 768])
          TL1[0]['qf'] = sb(st, "qf_0", [128, 8, 96]); TL1[1]['qf'] = sb(st, "qf_1", [128, 8, 96])
          TL1[0]['qb'] = sb(st, "qb_0", [128, 8, 96], BF16); TL1[1]['qb'] = sb(st, "qb_1", [128, 8, 96], BF16)
          TL1[0]['kf'] = sb(st, "kf_0", [128, 8, 96]); TL1[1]['kf'] = sb(st, "kf_1", [128, 8, 96])
          TL1[0]['kb'] = sb(st, "kb_0", [128, 8, 96], BF16); TL1[1]['kb'] = sb(st, "kb_1", [128, 8, 96], BF16)
          vb = sb(st, "vb", [128, 2, 512], BF16)
          TL1[0]['kvs'] = sb(st, "kvs_0", [128, 1024]); TL1[1]['kvs'] = sb(st, "kvs_1", [128, 1024])
          rp = sb(st, "rp", [128, 2, 32])
          TL1[0]['rt'] = sb(st, "rt_0", [128, 6, 128]); TL1[1]['rt'] = sb(st, "rt_1", [128, 6, 128])
          TL1[0]['krg'] = sb(st, "krg_0", [128, 32]); TL1[1]['krg'] = sb(st, "krg_1", [128, 32])
          TL1[0]['krr'] = sb(st, "krr_0", [128, 32]); TL1[1]['krr'] = sb(st, "krr_1", [128, 32])
          TL1[0]['qTt'] = sb(st, "qTt_0", [128, 8, 128], BF16); TL1[1]['qTt'] = sb(st, "qTt_1", [128, 8, 128], BF16)
          TL1[0]['kTt'] = sb(st, "kTt_0", [128, 8, 128], BF16); TL1[1]['kTt'] = sb(st, "kTt_1", [128, 8, 128], BF16)
          PSALL = [ps(st, "ph1_%d" % i) for i in range(8)]

          P.dmaop("pool", lambda e: e.dma_start(out=w_in_sb[:], in_=w_in.rearrange("(dc p) n -> p dc n", p=128)), w=["w_in_sb"])
          P.dmaop("pool", lambda e: e.dma_start(out=w_qb_sb[:], in_=w_qb.rearrange("(dc p) n -> p dc n", p=128)), w=["w_qb_sb"])
          P.dmaop("pool", lambda e: e.dma_start(out=w_kvb_sb[:], in_=w_kvb.rearrange("(dc p) n -> p dc n", p=128)), w=["w_kvb_sb"])
          P.dmaop("act", lambda e: e.dma_start(out=qag[:], in_=q_a_g.partition_broadcast(128)), w=["qag"])
          P.dmaop("act", lambda e: e.dma_start(out=kvag[:], in_=kv_a_g.partition_broadcast(128)), w=["kvag"])
          P.dmaop("act", lambda e: e.dma_start(out=qng[:], in_=q_norm_g.partition_broadcast(128)), w=["qng"])
          P.dmaop("act", lambda e: e.dma_start(out=kng[:], in_=k_norm_g.partition_broadcast(128)), w=["kng"])
          P.op("dve", lambda e: e.tensor_scalar(out=qng[:], in0=qng[:], scalar1=float(QK ** -0.5), scalar2=None, op0=ALU.mult),
               r=["qng"], w=["qng"])
          P.op("dve", lambda e: e.tensor_copy(out=identb[:], in_=ident[:]), r=["ident"], w=["identb"])

          def rstd(Pq, src_key, src_ap, n, dst_ap, dst_key):
              Pq.op("dve", lambda e: e.tensor_scalar(out=dst_ap, in0=src_ap, scalar1=1.0 / n, scalar2=EPS, op0=ALU.mult, op1=ALU.add),
                   r=[src_key], w=[dst_key])
              Pq.op("act", lambda e: e.activation(out=dst_ap, in_=dst_ap, func=AF.Sqrt), r=[dst_key], w=[dst_key])
              Pq.op("dve", lambda e: e.reciprocal(out=dst_ap, in_=dst_ap), r=[dst_key], w=[dst_key])

          def rope(Pq, rt, src, dst, tab, nh, tag, eng="pool"):
              sv = src.rearrange("p h (a t) -> p h a t", a=2)
              dv = dst.rearrange("p h (a t) -> p h a t", a=2)
              cosb = tab[:, 0:16].rearrange("p (a t) -> p a t", a=2).unsqueeze(1).to_broadcast([128, nh, 2, 8])
              sinb = tab[:, 16:32].rearrange("p (a t) -> p a t", a=2).unsqueeze(1).to_broadcast([128, nh, 2, 8])
              v0 = sv[:, :, :, 0:8]
              v1 = sv[:, :, :, 8:16]
              n = nh * 16
              T = [rt[:, k, 0:n].rearrange("p (h a t) -> p h a t", h=nh, a=2) for k in range(4)]
              rk = ("rt", tag)
              Pq.op(eng, lambda e: e.tensor_tensor(out=T[0], in0=v0, in1=cosb, op=ALU.mult), r=[tag + "_src", tag + "_tab"], w=[rk + (0,)])
              Pq.op(eng, lambda e: e.tensor_tensor(out=T[1], in0=v1, in1=sinb, op=ALU.mult), r=[tag + "_src", tag + "_tab"], w=[rk + (1,)])
              Pq.op(eng, lambda e: e.tensor_tensor(out=T[2], in0=v1, in1=cosb, op=ALU.mult), r=[tag + "_src", tag + "_tab"], w=[rk + (2,)])
              Pq.op(eng, lambda e: e.tensor_tensor(out=T[3], in0=v0, in1=sinb, op=ALU.mult), r=[tag + "_src", tag + "_tab"], w=[rk + (3,)])
              Pq.op(eng, lambda e: e.tensor_tensor(out=dv[:, :, :, 0:8], in0=T[0], in1=T[1], op=ALU.subtract),
                   r=[rk + (0,), rk + (1,)], w=[tag + "_dst"])
              Pq.op(eng, lambda e: e.tensor_tensor(out=dv[:, :, :, 8:16], in0=T[2], in1=T[3], op=ALU.add),
                   r=[rk + (2,), rk + (3,)], w=[tag + "_dst"])

          ntile1 = 4 if debug in (2, 3, 4, 5, 6) else NTILE
          import os
          STG = int(os.environ.get('PH1_STAGE', '9'))
          SHARED1 = ["w_in_sb", "w_qb_sb", "w_kvb_sb", "qag", "kvag", "qng", "kng", "identb", "ident", "modx", "modc", "u_s", "gates_s", "qT_s", "kT_s", "v_s"]
          ALIAS1 = {("ps", 1): ("ps", 0), "ps3": "ps2", ("ps", 6): ("ps", 4), ("ps", 7): ("ps", 5)}

          def tile1(i, s):
              Pq = Keyed(P, s, SHARED1, ALIAS1)
              junk = TL1[s]['junk']
              hh = TL1[s]['hh']
              hb = TL1[s]['hb']
              hT = TL1[s]['hT']
              qn = TL1[s]['qn']
              qnT = TL1[s]['qnT']
              kvn = TL1[s]['kvn']
              kvnT = TL1[s]['kvnT']
              qsq = TL1[s]['qsq']
              qf = TL1[s]['qf']
              qb = TL1[s]['qb']
              kf = TL1[s]['kf']
              kb = TL1[s]['kb']
              kvs = TL1[s]['kvs']
              rt = TL1[s]['rt']
              krg = TL1[s]['krg']
              krr = TL1[s]['krr']
              qTt = TL1[s]['qTt']
              kTt = TL1[s]['kTt']
              bk = PSALL[4 * s:4 * s + 4]
              PS = [bk[0], bk[0], bk[1], bk[1], bk[2], bk[3], bk[2], bk[3]]
              PSb2 = PS[2][:].bitcast(BF16)
              PSb3 = PS[3][:].bitcast(BF16)
              isx = i >= 2
              t0 = i * 128
              xq = t0 - NCTX
              G = (modx if isx else modc)[:, D:2 * D]
              SH = (modx if isx else modc)[:, 0:D]
              mk = "modx" if isx else "modc"
              Pq.dmaop("sp", lambda e, s=s, t0=t0: e.dma_start(out=xt[:, s, :], in_=xc[t0:t0 + 128, :]), w=[("xt", s)])
              Pq.dmaop("sp", lambda e, s=s, t0=t0: e.dma_start(out=rp[:, s, :], in_=rope_d[t0:t0 + 128, :]), w=[("rp", s)])
              Pq.op("act", lambda e, s=s: e.activation(out=junk[:], in_=xt[:, s, :], func=AF.Square, accum_out=st8[:, s, 0:1]),
                   r=[("xt", s)], w=["junk", ("st", s, 0)])
              rstd(Pq, ("st", s, 0), st8[:, s, 0:1], D, st8[:, s, 1:2], ("st", s, 1))
              Pq.op("dve", lambda e, s=s, G=G: e.scalar_tensor_tensor(out=hh[:], in0=xt[:, s, :], scalar=st8[:, s, 1:2], in1=G,
                                                                  op0=ALU.mult, op1=ALU.mult),
                   r=[("xt", s), ("st", s, 1), mk], w=["hh"])
              Pq.op("pool", lambda e, SH=SH: e.tensor_tensor(out=hb[:], in0=hh[:], in1=SH, op=ALU.add), r=["hh", mk], w=["hb"])

              def tr_h(e):
                  ins = None
                  for dc in range(8):
                      ins = e.transpose(out=PSb2[:, dc * 128:(dc + 1) * 128], in_=hb[:, dc * 128:(dc + 1) * 128], identity=identb[:])
                  return ins
              Pq.op("pe", tr_h, r=["hb", "identb"], w=["ps2"])
              Pq.op("act", lambda e: e.copy(out=hT[:].rearrange("p a b -> p (a b)"), in_=PSb2[:, 0:1024]), r=["ps2"], w=["hT"])
              for ctile in range(7):
                  c0 = ctile * 512
                  n = min(512, IN_COLS - c0)
                  pk = ctile % 2

                  def mmin(e, c0=c0, n=n, pk=pk):
                      ins = None
                      for dc in range(8):
                          ins = e.matmul(PS[pk][:, 0:n], lhsT=hT[:, dc, :], rhs=w_in_sb[:, dc, c0:c0 + n], start=(dc == 0), stop=(dc == 7))
                      return ins
                  Pq.op("pe", mmin, r=["hT", "w_in_sb"], w=[("ps", pk)])
                  if ctile % 2 == 0:
                      Pq.op("dve", lambda e, c0=c0, n=n, pk=pk, s=s: e.tensor_copy(out=proj[:, s, c0:c0 + n], in_=PS[pk][:, 0:n]),
                           r=[("ps", pk)], w=[("proj", s, ctile)])
                  else:
                      Pq.op("act", lambda e, c0=c0, n=n, pk=pk, s=s: e.copy(out=proj[:, s, c0:c0 + n], in_=PS[pk][:, 0:n]),
                           r=[("ps", pk)], w=[("proj", s, ctile)])
              pj = [("proj", s, c) for c in range(7)]
              Pq.dmaop("sp", lambda e, s=s, t0=t0: e.dma_start(out=u_s[t0:t0 + 128, :], in_=proj[:, s, 0:512]), r=[pj[0]], w=["u_s"])
              if isx:
                  Pq.op("act", lambda e, s=s: e.activation(out=proj[:, s, 1184:3232], in_=proj[:, s, 1184:3232], func=AF.Sigmoid),
                        r=pj[2:], w=pj[2:])
                  Pq.dmaop("sp", lambda e, xq=xq, s=s: e.dma_start(out=gates_s[xq:xq + 128, :], in_=proj[:, s, 1184:3232]), r=pj[2:], w=["gates_s"])
                  Pq.op("act", lambda e, s=s: e.activation(out=junk[:, 0:384], in_=proj[:, s, 512:896], func=AF.Square,
                                                          accum_out=st8[:, s, 2:3]), r=[pj[1]], w=["junk", ("st", s, 2)])
                  rstd(Pq, ("st", s, 2), st8[:, s, 2:3], 384, st8[:, s, 3:4], ("st", s, 3))
                  Pq.op("dve", lambda e, s=s: e.scalar_tensor_tensor(out=qn[:], in0=proj[:, s, 512:896], scalar=st8[:, s, 3:4], in1=qag[:],
                                                                    op0=ALU.mult, op1=ALU.mult), r=[pj[1], ("st", s, 3), "qag"], w=["qn"])

                  def tr_q(e):
                      ins = None
                      for k in range(3):
                          ins = e.transpose(out=PSb2[:, k * 128:(k + 1) * 128], in_=qn[:, k * 128:(k + 1) * 128], identity=identb[:])
                      return ins
                  Pq.op("pe", tr_q, r=["qn", "identb"], w=["ps2"])
                  Pq.op("dve", lambda e: e.tensor_copy(out=qnT[:].rearrange("p a b -> p (a b)"), in_=PSb2[:, 0:384]), r=["ps2"], w=["qnT"])

                  def mmq(e):
                      ins = None
                      for (pi, c0, n) in ((4, 0, 512), (5, 512, 256)):
                          for k in range(3):
                              ins = e.matmul(PS[pi][:, 0:n], lhsT=qnT[:, k, :], rhs=w_qb_sb[:, k, c0:c0 + n], start=(k == 0), stop=(k == 2))
                      return ins
                  Pq.op("pe", mmq, r=["qnT", "w_qb_sb"], w=[("ps", 4), ("ps", 5)])
                  qfl = qf[:].rearrange("p h d -> p (h d)")
                  Pq.op("act", lambda e: e.copy(out=qfl[:, 0:512], in_=PS[4][:, 0:512]), r=[("ps", 4)], w=["qf"])
                  Pq.op("act", lambda e: e.copy(out=qfl[:, 512:768], in_=PS[5][:, 0:256]), r=[("ps", 5)], w=["qf"])
                  Pq.op("pool", lambda e: e.tensor_tensor(out=qsq[:], in0=qfl, in1=qfl, op=ALU.mult), r=["qf"], w=["qsq"])
                  Pq.op("dve", lambda e, s=s: e.tensor_reduce(out=st8[:, s, 8:16], in_=qsq[:].rearrange("p (h d) -> p h d", h=8),
                                                             axis=AX.X, op=ALU.add), r=["qsq"], w=[("st", s, 8)])
                  rstd(Pq, ("st", s, 8), st8[:, s, 8:16], QK, st8[:, s, 16:24], ("st", s, 16))
                  Pq.op("dve", lambda e, s=s: e.tensor_tensor(out=qf[:], in0=qf[:], in1=st8[:, s, 16:24].unsqueeze(2).to_broadcast([128, 8, 96]),
                                                             op=ALU.mult), r=["qf", ("st", s, 16)], w=["qf"])
                  Pq.op("dve", lambda e: e.tensor_tensor(out=qf[:], in0=qf[:], in1=qng[:].unsqueeze(1).to_broadcast([128, 8, 96]),
                                                        op=ALU.mult), r=["qf", "qng"], w=["qf", "q_src"])
                  Pq.op("act", lambda e: e.copy(out=qb[:, :, 0:64], in_=qf[:, :, 0:64]), r=["qf"], w=["qb"])
                  Pq.op("pool", lambda e, s=s: e.tensor_copy(out=rt[:, 5, 0:32], in_=rp[:, s, :]), r=[("rp", s)], w=["q_tab"])
                  rope(Pq, rt, qf[:, :, 64:96], qb[:, :, 64:96], rt[:, 5, 0:32], 8, "q")

                  def tr_qh(e):
                      ins = None
                      for h in range(8):
                          ins = e.transpose(out=PSb3[0:96, h * 128:(h + 1) * 128], in_=qb[:, h, :], identity=identb[:])
                      return ins
                  Pq.op("pe", tr_qh, r=["qb", "q_dst", "identb"], w=["ps3"])
                  Pq.op("act", lambda e: e.copy(out=qTt[0:96].rearrange("p a b -> p (a b)"), in_=PSb3[0:96, 0:1024]), r=["ps3"], w=["qTt"])
                  Pq.dmaop("act", lambda e, xq=xq: e.dma_start(out=qT_s[:, :, xq:xq + 128].rearrange("h d t -> d h t"), in_=qTt[0:96]),
                          r=["qTt"], w=["qT_s"])
              Pq.op("act", lambda e, s=s: e.activation(out=junk[:, 0:256], in_=proj[:, s, 896:1152], func=AF.Square,
                                                      accum_out=st8[:, s, 4:5]), r=[pj[1], pj[2]], w=["junk", ("st", s, 4)])
              rstd(Pq, ("st", s, 4), st8[:, s, 4:5], 256, st8[:, s, 5:6], ("st", s, 5))
              Pq.op("dve", lambda e, s=s: e.scalar_tensor_tensor(out=kvn[:], in0=proj[:, s, 896:1152], scalar=st8[:, s, 5:6], in1=kvag[:],
                                                                op0=ALU.mult, op1=ALU.mult), r=[pj[1], pj[2], ("st", s, 5), "kvag"], w=["kvn"])

              def tr_kv(e):
                  ins = None
                  for k in range(2):
                      ins = e.transpose(out=PSb2[:, k * 128:(k + 1) * 128], in_=kvn[:, k * 128:(k + 1) * 128], identity=identb[:])
                  return ins
              Pq.op("pe", tr_kv, r=["kvn", "identb"], w=["ps2"])
              Pq.op("dve", lambda e: e.tensor_copy(out=kvnT[:].rearrange("p a b -> p (a b)"), in_=PSb2[:, 0:256]), r=["ps2"], w=["kvnT"])

              def mmkv(e):
                  ins = None
                  for (pi, c0) in ((6, 0), (7, 512)):
                      for k in range(2):
                          ins = e.matmul(PS[pi][:, 0:512], lhsT=kvnT[:, k, :], rhs=w_kvb_sb[:, k, c0:c0 + 512], start=(k == 0), stop=(k == 1))
                  return ins
              Pq.op("pe", mmkv, r=["kvnT", "w_kvb_sb"], w=[("ps", 6), ("ps", 7)])
              for half in range(2):
                  (Pq.op("act", lambda e, half=half: e.copy(out=kvs[:, half * 512:(half + 1) * 512], in_=PS[6 + half][:, 0:512]),
                        r=[("ps", 6 + half)], w=[("kvs", half)]) if half == 0 else
                   Pq.op("dve", lambda e, half=half: e.tensor_copy(out=kvs[:, half * 512:(half + 1) * 512], in_=PS[6 + half][:, 0:512]),
                        r=[("ps", 6 + half)], w=[("kvs", half)]))
              kvs3 = kvs[:].rearrange("p (h d) -> p h d", h=8)
              kvk = [("kvs", 0), ("kvs", 1)]
              Pq.op("pool", lambda e, s=s: e.tensor_copy(out=vb[:, s].rearrange("p (h d) -> p h d", h=8), in_=kvs3[:, :, 64:128]),
                   r=kvk, w=[("vb", s)])
              Pq.dmaop("sp", lambda e, s=s, t0=t0: e.dma_start(out=v_s[t0:t0 + 128, :], in_=vb[:, s]),
                      r=[("vb", s)], w=["v_s"])
              Pq.op("act", lambda e: e.copy(out=kf[:, :, 0:64], in_=kvs3[:, :, 0:64]), r=kvk, w=[("kf", 0), ("kf", 1)])
              kfk = [("kf", 0), ("kf", 1)]
              Pq.op("pool", lambda e: e.tensor_tensor(out=qsq[:, 0:512].rearrange("p (h d) -> p h d", h=8), in0=kf[:, :, 0:64], in1=kf[:, :, 0:64],
                                                     op=ALU.mult), r=kfk, w=["qsq"])
              Pq.op("dve", lambda e, s=s: e.tensor_reduce(out=st8[:, s, 24:32], in_=qsq[:, 0:512].rearrange("p (h d) -> p h d", h=8),
                                                         axis=AX.X, op=ALU.add), r=["qsq"], w=[("st", s, 24)])
              Pq.op("act", lambda e, s=s: e.activation(out=junk[:, 0:32], in_=proj[:, s, 1152:1184], func=AF.Square,
                                                      accum_out=st8[:, s, 6:7]), r=[pj[2]], w=["junk", ("st", s, 6)])
              Pq.op("dve", lambda e, s=s: e.tensor_scalar(out=st8[:, s, 24:32], in0=st8[:, s, 24:32], scalar1=st8[:, s, 6:7], scalar2=None,
                                                         op0=ALU.add), r=[("st", s, 24), ("st", s, 6)], w=[("st", s, 24)])
              rstd(Pq, ("st", s, 24), st8[:, s, 24:32], QK, st8[:, s, 32:40], ("st", s, 32))
              Pq.op("dve", lambda e, s=s: e.tensor_tensor(out=kf[:, :, 0:64], in0=kf[:, :, 0:64],
                                                         in1=st8[:, s, 32:40].unsqueeze(2).to_broadcast([128, 8, 64]), op=ALU.mult),
                   r=kfk + [("st", s, 32)], w=kfk)
              Pq.op("dve", lambda e: e.tensor_tensor(out=kb[:, :, 0:64], in0=kf[:, :, 0:64],
                                                    in1=kng[:, 0:64].unsqueeze(1).to_broadcast([128, 8, 64]), op=ALU.mult),
                   r=kfk + ["kng"], w=["kb"])
              Pq.op("pool", lambda e, s=s: e.tensor_tensor(out=krg[:], in0=proj[:, s, 1152:1184], in1=kng[:, 64:96], op=ALU.mult),
                   r=[pj[2], "kng"], w=["krg", "k_src"])
              Pq.op("pool", lambda e, s=s: e.tensor_copy(out=rt[:, 4, 0:32], in_=rp[:, s, :]), r=[("rp", s)], w=["k_tab"])
              rope(Pq, rt, krg[:].unsqueeze(1), krr[:].unsqueeze(1), rt[:, 4, 0:32], 1, "k")
              Pq.op("dve", lambda e, s=s: e.tensor_tensor(out=kb[:, :, 64:96], in0=krr[:].unsqueeze(1).to_broadcast([128, 8, 32]),
                                                         in1=st8[:, s, 32:40].unsqueeze(2).to_broadcast([128, 8, 32]), op=ALU.mult),
                   r=["k_dst", ("st", s, 32)], w=["kb"])

              def tr_kh(e):
                  ins = None
                  for h in range(8):
                      ins = e.transpose(out=PSb3[0:96, h * 128:(h + 1) * 128], in_=kb[:, h, :], identity=identb[:])
                  return ins
              Pq.op("pe", tr_kh, r=["kb", "identb"], w=["ps3"])
              Pq.op("dve", lambda e: e.tensor_copy(out=kTt[0:96].rearrange("p a b -> p (a b)"), in_=PSb3[0:96, 0:1024]), r=["ps3"], w=["kTt"])
              Pq.dmaop("act", lambda e, t0=t0: e.dma_start(out=kT_s[:, :, t0:t0 + 128].rearrange("h d t -> d h t"), in_=kTt[0:96]),
                      r=["kTt"], w=["kT_s"])
              return Pq.cap
          for i in range(0, ntile1, 2):
              caps = [tile1(i, 0)] + ([tile1(i + 1, 1)] if i + 1 < ntile1 else [])
              interleave(P, caps, chunk=int(os.environ.get("ILV", "2")))
          P.flush()

    small = debug in (2, 3, 4, 5, 6)
    NTe = 4 if small else NTILE
    NXe = (NTe - 2) * 128
    QG = min(512, NXe)
    NQG = NXe // QG

    es1.close()
    NCX = NXe // 8
    NCC = NCTX // 8
    NCHe = NCX + NCC
    HW = NCHe + 2
    if debug in (0, 4, 5, 6):
      with ExitStack() as st:
        BendT = sb(st, "BendT", [128, 2, 32, 2, 64], BF16)
        Dm = sb(st, "Dm", [128, 2, 16, 2, 128], BF16)
        Tloc = sb(st, "Tloc", [128, 32, 128], BF16)
        mu3 = sb(st, "mu3", [128, 2, 2, 16, 2])
        mu16 = sb(st, "mu16", [128, 2, 17, 2, 16, 2])
        PS2 = [ps(st, "ph2_%d" % i) for i in range(8)]
        with ExitStack() as tt:
            lre = sb(tt, "lre", [128, 32]); lim = sb(tt, "lim", [128, 32]); ldt = sb(tt, "ldt", [128, 32])
            bre = sb(tt, "bre", [128, 32, 16]); bim = sb(tt, "bim", [128, 32, 16])
            cre = sb(tt, "cre", [128, 32, 16]); cim = sb(tt, "cim", [128, 32, 16])
            dsk = sb(tt, "dsk", [128, 32])
            cmk = sb(tt, "cmk", [128, 256])
            tm = sb(tt, "tm", [128, 12, 32])
            ti = sb(tt, "ti", [128, 32], I32)
            pwr = sb(tt, "pwr", [128, 9, 32]); pwi = sb(tt, "pwi", [128, 9, 32])
            nwr = sb(tt, "nwr", [128, 9, 32]); nwi = sb(tt, "nwi", [128, 9, 32])
            Bbr = sb(tt, "Bbr", [128, 32, 16]); Bbi = sb(tt, "Bbi", [128, 32, 16])
            MX = [sb(tt, "MX%d" % i, [128, 16, 8, 16]) for i in range(4)]
            MT = [sb(tt, "MT%d" % i, [128, 16, 8, 16]) for i in range(4)]
            TL = sb(tt, "TL", [128, 2, 128])
            for gh in range(2):
                rows = slice(64 * gh, 64 * gh + 64)
                gs = slice(16 * gh, 16 * gh + 16)
                for d in range(2):
                    for (dst, src, nm) in ((lre, lam_re, "lre"), (lim, lam_im, "lim")):
                        P.dmaop("sp", lambda e, dst=dst, src=src, rows=rows, gs=gs, d=d: e.dma_start(
                            out=dst[rows, 16 * d:16 * d + 16], in_=src[d, gs, :].rearrange("g p -> p g"),
                            allow_slow_non_contiguous=True), w=[nm])
                    P.dmaop("sp", lambda e, rows=rows, gs=gs, d=d: e.dma_start(
                        out=ldt[rows, 16 * d:16 * d + 16], in_=log_dt[d, gs].partition_broadcast(64)), w=["ldt"])
                for (dst, src, nm) in ((bre, b_re, "bre"), (bim, b_im, "bim")):
                    for d in range(2):
                        P.dmaop("act", lambda e, dst=dst, src=src, rows=rows, gs=gs, d=d: e.dma_start(
                            out=dst[rows, 16 * d:16 * d + 16, :], in_=src[d, gs, :, :].rearrange("g p h -> p g h")), w=[nm])
                for (dst, src, nm) in ((cre, c_re, "cre"), (cim, c_im, "cim")):
                    for d in range(2):
                        P.dmaop("sp" if d == 0 else "act", lambda e, dst=dst, src=src, rows=rows, gs=gs, d=d: e.dma_start(
                            out=dst[rows, 16 * d:16 * d + 16, :], in_=src[d, gs, :, :].rearrange("g o p -> p g o"),
                            allow_slow_non_contiguous=True), w=[nm])
            for j in range(8):
                P.dmaop("sp", lambda e, j=j: e.dma_start(out=dsk[16 * j:16 * j + 16, :], in_=ssm_d.rearrange("(g h) -> h g", h=16),
                                                        allow_slow_non_contiguous=True), w=["dsk"])
            P.dmaop("sp", lambda e: e.dma_start(out=cmk[:], in_=cmask_d), w=["cmk"])

            K = [0]

            def T_(i):
                return tm[:, i, :]

            def dv(fn, r, w, eng="dve"):
                P.op(eng, fn, r=r, w=w)

            def tt2(out, a, b, op, r, w, eng="dve"):
                P.op(eng, lambda e: e.tensor_tensor(out=out, in0=a, in1=b, op=op), r=r, w=w)

            def ts(out, a, s1, op0, s2=None, op1=None, r=(), w=(), eng="dve"):
                if op1 is None:
                    P.op(eng, lambda e: e.tensor_scalar(out=out, in0=a, scalar1=s1, scalar2=None, op0=op0), r=r, w=w)
                else:
                    P.op(eng, lambda e: e.tensor_scalar(out=out, in0=a, scalar1=s1, scalar2=s2, op0=op0, op1=op1), r=r, w=w)
            PI = math.pi
            P.op("act", lambda e: e.activation(out=T_(0), in_=ldt[:], func=AF.Exp), r=["ldt"], w=["t0"])
            tt2(T_(1), lre[:], T_(0), ALU.mult, ["lre", "t0"], ["t1"])
            tt2(T_(2), lim[:], T_(0), ALU.mult, ["lim", "t0"], ["t2"])
            P.op("act", lambda e: e.activation(out=T_(3), in_=T_(1), func=AF.Exp), r=["t1"], w=["t3"])
            ts(T_(4), T_(2), 1.0 / (2 * PI), ALU.mult, r=["t2"], w=["t4"])
            P.op("dve", lambda e: e.tensor_copy(out=ti[:], in_=T_(4)), r=["t4"], w=["ti"])
            P.op("dve", lambda e: e.tensor_copy(out=T_(4), in_=ti[:]), r=["ti"], w=["t4"])
            P.op("dve", lambda e: e.scalar_tensor_tensor(out=T_(5), in0=T_(4), scalar=-2 * PI, in1=T_(2), op0=ALU.mult, op1=ALU.add),
                 r=["t4", "t2"], w=["t5"])
            for (src_i, dst_i) in ((5, 5),):
                ts(T_(6), T_(5), PI, ALU.is_gt, -2 * PI, ALU.mult, r=["t5"], w=["t6"])
                tt2(T_(5), T_(5), T_(6), ALU.add, ["t5", "t6"], ["t5"])
                ts(T_(6), T_(5), -PI, ALU.is_lt, 2 * PI, ALU.mult, r=["t5"], w=["t6"])
                tt2(T_(5), T_(5), T_(6), ALU.add, ["t5", "t6"], ["t5"])
            ts(T_(7), T_(5), PI / 2, ALU.add, r=["t5"], w=["t7"])
            ts(T_(6), T_(7), PI, ALU.is_gt, -2 * PI, ALU.mult, r=["t7"], w=["t6"])
            tt2(T_(7), T_(7), T_(6), ALU.add, ["t7", "t6"], ["t7"])
            P.op("act", lambda e: e.activation(out=T_(8), in_=T_(5), func=AF.Sin), r=["t5"], w=["t8"])
            P.op("act", lambda e: e.activation(out=T_(9), in_=T_(7), func=AF.Sin), r=["t7"], w=["t9"])
            P.op("pool", lambda e: e.memset(pwr[:, 0, :], 1.0), w=[("pw", 0)])
            P.op("pool", lambda e: e.memset(pwi[:, 0, :], 0.0), w=[("pw", 0)])
            tt2(pwr[:, 1, :], T_(3), T_(9), ALU.mult, ["t3", "t9"], [("pw", 1)])
            tt2(pwi[:, 1, :], T_(3), T_(8), ALU.mult, ["t3", "t8"], [("pw", 1)])
            for k in range(2, 9):
                a_r, a_i = pwr[:, k - 1, :], pwi[:, k - 1, :]
                tt2(T_(10), a_r, pwr[:, 1, :], ALU.mult, [("pw", k - 1), ("pw", 1)], ["t10"])
                tt2(T_(11), a_i, pwi[:, 1, :], ALU.mult, [("pw", k - 1), ("pw", 1)], ["t11"])
                tt2(pwr[:, k, :], T_(10), T_(11), ALU.subtract, ["t10", "t11"], [("pw", k)])
                tt2(T_(10), a_r, pwi[:, 1, :], ALU.mult, [("pw", k - 1), ("pw", 1)], ["t10"])
                tt2(T_(11), a_i, pwr[:, 1, :], ALU.mult, [("pw", k - 1), ("pw", 1)], ["t11"])
                tt2(pwi[:, k, :], T_(10), T_(11), ALU.add, ["t10", "t11"], [("pw", k)])
            pwk = [("pw", k) for k in range(9)]
            tt2(nwr[:], pwr[:], pwr[:], ALU.mult, pwk, ["nwr"])
            tt2(nwi[:], pwi[:], pwi[:], ALU.mult, pwk, ["nwi"])
            tt2(nwr[:], nwr[:], nwi[:], ALU.add, ["nwr", "nwi"], ["nwr"])
            P.op("dve", lambda e: e.reciprocal(out=nwr[:], in_=nwr[:]), r=["nwr"], w=["nwr"])
            P.op("dve", lambda e: e.scalar_tensor_tensor(out=nwi[:], in0=pwi[:], scalar=-1.0, in1=nwr[:], op0=ALU.mult, op1=ALU.mult),
                 r=pwk + ["nwr"], w=["nwi"])
            tt2(nwr[:], pwr[:], nwr[:], ALU.mult, pwk + ["nwr", "nwi"], ["nwr"])
            for d in range(2):
                dsl = slice(16 * d, 16 * d + 16)
                for pl in range(2):
                    P.op("dve", lambda e, d=d, pl=pl, dsl=dsl: e.tensor_copy(out=mu3[:, d, 0, :, pl], in_=pwr[:, 8, dsl]), r=pwk, w=["mu3"])
                P.op("dve", lambda e, d=d, dsl=dsl: e.tensor_scalar(out=mu3[:, d, 1, :, 0], in0=pwi[:, 8, dsl], scalar1=-1.0, scalar2=None, op0=ALU.mult),
                     r=pwk, w=["mu3"])
                P.op("dve", lambda e, d=d, dsl=dsl: e.tensor_copy(out=mu3[:, d, 1, :, 1], in_=pwi[:, 8, dsl]), r=pwk, w=["mu3"])
            q16r = sb(tt, "q16r", [128, 17, 32]); q16i = sb(tt, "q16i", [128, 17, 32])
            P.op("pool", lambda e: e.memset(q16r[:, 0, :], 1.0), w=[("q16", 0)])
            P.op("pool", lambda e: e.memset(q16i[:, 0, :], 0.0), w=[("q16", 0)])
            P.op("pool", lambda e: e.tensor_copy(out=q16r[:, 1, :], in_=pwr[:, 8, :]), r=pwk, w=[("q16", 1)])
            P.op("pool", lambda e: e.tensor_copy(out=q16i[:, 1, :], in_=pwi[:, 8, :]), r=pwk, w=[("q16", 1)])
            for k in range(2, 17):
                a_r, a_i = q16r[:, k - 1, :], q16i[:, k - 1, :]
                tt2(T_(10), a_r, q16r[:, 1, :], ALU.mult, [("q16", k - 1), ("q16", 1)], ["t10"])
                tt2(T_(11), a_i, q16i[:, 1, :], ALU.mult, [("q16", k - 1), ("q16", 1)], ["t11"])
                tt2(q16r[:, k, :], T_(10), T_(11), ALU.subtract, ["t10", "t11"], [("q16", k)])
                tt2(T_(10), a_r, q16i[:, 1, :], ALU.mult, [("q16", k - 1), ("q16", 1)], ["t10"])
                tt2(T_(11), a_i, q16r[:, 1, :], ALU.mult, [("q16", k - 1), ("q16", 1)], ["t11"])
                tt2(q16i[:, k, :], T_(10), T_(11), ALU.add, ["t10", "t11"], [("q16", k)])
            q16k = [("q16", k) for k in range(17)]
            for d in range(2):
                dsl = slice(16 * d, 16 * d + 16)
                for pl in range(2):
                    P.op("dve", lambda e, d=d, pl=pl, dsl=dsl: e.tensor_copy(out=mu16[:, d, :, 0, :, pl], in_=q16r[:, :, dsl]), r=q16k, w=["mu16"])
                P.op("dve", lambda e, d=d, dsl=dsl: e.tensor_scalar(out=mu16[:, d, :, 1, :, 0], in0=q16i[:, :, dsl], scalar1=-1.0, scalar2=None, op0=ALU.mult),
                     r=q16k, w=["mu16"])
                P.op("dve", lambda e, d=d, dsl=dsl: e.tensor_copy(out=mu16[:, d, :, 1, :, 1], in_=q16i[:, :, dsl]), r=q16k, w=["mu16"])
            tt2(T_(0), lre[:], lre[:], ALU.mult, ["lre"], ["t0"])
            tt2(T_(1), lim[:], lim[:], ALU.mult, ["lim"], ["t1"])
            tt2(T_(0), T_(0), T_(1), ALU.add, ["t0", "t1"], ["t0"])
            P.op("dve", lambda e: e.reciprocal(out=T_(0), in_=T_(0)), r=["t0"], w=["t0"])
            ts(T_(1), pwr[:, 1, :], -1.0, ALU.add, r=[("pw", 1)], w=["t1"])
            tt2(T_(2), T_(1), lre[:], ALU.mult, ["t1", "lre"], ["t2"])
            tt2(T_(3), pwi[:, 1, :], lim[:], ALU.mult, [("pw", 1), "lim"], ["t3"])
            tt2(T_(2), T_(2), T_(3), ALU.add, ["t2", "t3"], ["t2"])
            tt2(T_(2), T_(2), T_(0), ALU.mult, ["t2", "t0"], ["t2"])
            tt2(T_(3), pwi[:, 1, :], lre[:], ALU.mult, [("pw", 1), "lre"], ["t3"])
            tt2(T_(4), T_(1), lim[:], ALU.mult, ["t1", "lim"], ["t4"])
            tt2(T_(3), T_(3), T_(4), ALU.subtract, ["t3", "t4"], ["t3"])
            tt2(T_(3), T_(3), T_(0), ALU.mult, ["t3", "t0"], ["t3"])
            cfr = T_(2).unsqueeze(2).to_broadcast([128, 32, 16])
            cfi = T_(3).unsqueeze(2).to_broadcast([128, 32, 16])
            tmpA = MT[0][:].rearrange("p a b c -> p (a b c)")[:, 0:512].rearrange("p (g h) -> p g h", h=16)
            tt2(Bbr[:], bre[:], cfr, ALU.mult, ["bre", "t2"], ["Bbr"])
            tt2(tmpA, bim[:], cfi, ALU.mult, ["bim", "t3"], ["MT0"])
            tt2(Bbr[:], Bbr[:], tmpA, ALU.subtract, ["Bbr", "MT0"], ["Bbr"])
            tt2(Bbi[:], bim[:], cfr, ALU.mult, ["bim", "t2"], ["Bbi"])
            tt2(tmpA, bre[:], cfi, ALU.mult, ["bre", "t3"], ["MT0"])
            tt2(Bbi[:], Bbi[:], tmpA, ALU.add, ["Bbi", "MT0"], ["Bbi"])

            def cprod(out_re, out_im, pw_re, pw_im, koff, kstep, vr, vi, d, keys_in, key_out, neg_im=False, eng="dve"):
                dsl = slice(16 * d, 16 * d + 16)
                if kstep == 1:
                    ksl = slice(koff, koff + 8)
                    pr = pw_re[:, ksl, dsl].rearrange("p j g -> p g j").unsqueeze(3).to_broadcast([128, 16, 8, 16])
                    pi = pw_im[:, ksl, dsl].rearrange("p j g -> p g j").unsqueeze(3).to_broadcast([128, 16, 8, 16])
                else:
                    pr = None
                vrb = vr[:, dsl, :].unsqueeze(2).to_broadcast([128, 16, 8, 16])
                vib = vi[:, dsl, :].unsqueeze(2).to_broadcast([128, 16, 8, 16])
                t1 = MT[2][:]
                t2 = MT[3][:]
                tt2(t1, vrb, pr, ALU.mult, keys_in, ["MT2"], eng)
                tt2(t2, vib, pi, ALU.mult, keys_in, ["MT3"], eng)
                tt2(out_re, t1, t2, ALU.subtract, ["MT2", "MT3"], [key_out[0]], eng)
                tt2(t1, vrb, pi, ALU.mult, keys_in, ["MT2"], eng)
                tt2(t2, vib, pr, ALU.mult, keys_in, ["MT3"], eng)
                tt2(out_im, t1, t2, (ALU.add), ["MT2", "MT3"], [key_out[1]], eng)
                if neg_im:
                    ts(out_im, out_im, -1.0, ALU.mult, r=[key_out[1]], w=[key_out[1]], eng=eng)

            dpr = sb(tt, "dpr", [128, 9, 32]); dpi = sb(tt, "dpi", [128, 9, 32])
            for k in range(9):
                P.op("pool", lambda e, k=k: e.tensor_copy(out=dpr[:, k, :], in_=pwr[:, 8 - k, :]), r=pwk, w=["dpr"])
                P.op("pool", lambda e, k=k: e.tensor_copy(out=dpi[:, k, :], in_=pwi[:, 8 - k, :]), r=pwk, w=["dpi"])
            tk = pwk + ["nwr", "nwi", "dpr", "dpi", "Bbr", "Bbi", "cre", "cim"]
            pTL = PS2[0]
            for d in range(2):
                if d == 0:
                    cprod(MX[0][:], MX[1][:], nwr, nwi, 0, 1, Bbr, Bbi, d, tk, ("MX0", "MX1"))
                    cprod(MX[2][:], MX[3][:], pwr, pwi, 0, 1, cre, cim, d, tk, ("MX2", "MX3"), neg_im=True)
                    cprod(MT[0][:], MT[1][:], dpr, dpi, 1, 1, Bbr, Bbi, d, tk, ("MT0", "MT1"))
                else:
                    cprod(MX[0][:], MX[1][:], pwr, pwi, 0, 1, Bbr, Bbi, d, tk, ("MX0", "MX1"))
                    cprod(MX[2][:], MX[3][:], nwr, nwi, 0, 1, cre, cim, d, tk, ("MX2", "MX3"), neg_im=True)
                    P.op("pool", lambda e: e.tensor_copy(out=MT[0][:], in_=MX[0][:]), r=["MX0"], w=["MT0"])
                    P.op("pool", lambda e: e.tensor_copy(out=MT[1][:], in_=MX[1][:]), r=["MX1"], w=["MT1"])
                for pl in range(2):
                    for g in range(32):
                        gh, gp = g // 16, g % 16
                        pb = PS2[2 + (g % 4)]

                        def trb(e, pl=pl, gh=gh, gp=gp, pb=pb):
                            return e.transpose(out=pb[:, 0:64], in_=MT[pl][64 * gh:64 * gh + 64, gp].rearrange("p j h -> p (j h)"),
                                               identity=ident[64 * gh:64 * gh + 64, 64 * gh:64 * gh + 64])
                        P.op("pe", trb, r=["MT0" if pl == 0 else "MT1", "ident"], w=[("p2", 2 + g % 4)])
                        P.op("act" if g % 2 == 0 else "dve",
                             (lambda e, pb=pb, d=d, g=g, pl=pl: e.copy(out=BendT[:, d, g, pl, :], in_=pb[:, 0:64])) if g % 2 == 0 else
                             (lambda e, pb=pb, d=d, g=g, pl=pl: e.tensor_copy(out=BendT[:, d, g, pl, :], in_=pb[:, 0:64])),
                             r=[("p2", 2 + g % 4)], w=["BendT"])
                if d == 0:
                    cprod(MT[0][:], MT[1][:], pwr, pwi, 1, 1, cre, cim, d, tk + ["BendT"], ("MT0", "MT1"), neg_im=True)
                else:
                    cprod(MT[0][:], MT[1][:], dpr, dpi, 0, 1, cre, cim, d, tk + ["BendT"], ("MT0", "MT1"), neg_im=True)
                P.op("act", lambda e, d=d: e.copy(out=Dm[:, d, :, 0, :], in_=MT[0][:].rearrange("p g j o -> p g (j o)")), r=["MT0"], w=["Dm"])
                P.op("act", lambda e, d=d: e.copy(out=Dm[:, d, :, 1, :], in_=MT[1][:].rearrange("p g j o -> p g (j o)")), r=["MT1"], w=["Dm"])
                for g in range(32):
                    gh, gp = g // 16, g % 16
                    rows = slice(64 * gh, 64 * gh + 64)
                    pq = PS2[6 + (g % 2)]

                    def mtl(e, rows=rows, gp=gp, pq=pq):
                        e.matmul(pq[:, 0:128], lhsT=MX[0][rows, gp].rearrange("p j h -> p (j h)"), rhs=MX[2][rows, gp].rearrange("p j h -> p (j h)"),
                                 start=True, stop=False)
                        return e.matmul(pq[:, 0:128], lhsT=MX[1][rows, gp].rearrange("p j h -> p (j h)"),
                                        rhs=MX[3][rows, gp].rearrange("p j h -> p (j h)"), start=False, stop=True)
                    P.op("pe", mtl, r=["MX0", "MX1", "MX2", "MX3"], w=[("p2", 6 + g % 2)])
                    if d == 0:
                        P.op("dve", lambda e, g=g, pq=pq: e.tensor_tensor(out=Tloc[:, g, :], in0=pq[:, 0:128], in1=cmk[:, 0:128], op=ALU.mult),
                             r=[("p2", 6 + g % 2), "cmk"], w=[("Tloc", g)])
                    else:
                        P.op("dve", lambda e, g=g, pq=pq: e.tensor_tensor(out=TL[:, g % 2, :], in0=pq[:, 0:128], in1=cmk[:, 128:256], op=ALU.mult),
                             r=[("p2", 6 + g % 2), "cmk"], w=[("TL", g % 2)])
                        P.op("pool", lambda e, g=g: e.tensor_tensor(out=Tloc[:, g, :], in0=Tloc[:, g, :], in1=TL[:, g % 2, :], op=ALU.add),
                             r=[("Tloc", g), ("TL", g % 2)], w=[("Tloc", g)])
                        P.op("pool", lambda e, g=g: e.scalar_tensor_tensor(out=Tloc[:, g, :], in0=ident[:], scalar=dsk[:, g:g + 1], in1=Tloc[:, g, :],
                                                                          op0=ALU.mult, op1=ALU.add) if False else
                             e.tensor_scalar(out=TL[:, g % 2, :], in0=ident[:], scalar1=dsk[:, g:g + 1], scalar2=None, op0=ALU.mult),
                             r=["ident", "dsk", ("Tloc", g)], w=[("TL", g % 2)])
                        P.op("pool", lambda e, g=g: e.tensor_tensor(out=Tloc[:, g, :], in0=Tloc[:, g, :], in1=TL[:, g % 2, :], op=ALU.add),
                             r=[("Tloc", g), ("TL", g % 2)], w=[("Tloc", g)])
            P.flush()
        U8 = sb(st, "U8", [128, 32, NCHe], BF16)
        Hall = [sb(st, "Hall%d" % d, [128, 16, 2, HW], BF16) for d in range(2)]
        with ExitStack() as tu:
            Uc = sb(tu, "Uc", [128, 1, 4096])
            Ucb = sb(tu, "Ucb", [128, 1, 4096], BF16)
            identb2 = sb(tu, "identb2", [128, 128], BF16)
            P.op("dve", lambda e: e.tensor_copy(out=identb2[:], in_=ident[:]), r=["ident"], w=["identb2"])
            u8v = u_s.rearrange("(c j) f -> c (j f)", j=8)
            nblk = (NCHe + 127) // 128
            for cb in range(nblk):
                c0 = cb * 128
                ncb = min(128, NCHe - c0)
                s = 0
                P.dmaop("sp", lambda e, s=s, c0=c0, ncb=ncb: e.dma_start(out=Uc[0:ncb, s, :], in_=u8v[c0:c0 + ncb, :]), r=["u_s"], w=[("Uc", s)])
                P.op("dve", lambda e, s=s, ncb=ncb: e.tensor_copy(
                    out=Ucb[0:ncb, s, :].rearrange("c (g j h) -> c g j h", g=32, j=8),
                    in_=Uc[0:ncb, s, :].rearrange("c (j g h) -> c g j h", j=8, g=32)), r=[("Uc", s)], w=[("Ucb", s)])
                for g4 in range(8):
                    pb = PS2[g4 % 4]
                    pbb = pb[:].bitcast(BF16)

                    def tru(e, g4=g4, s=s, ncb=ncb, pbb=pbb):
                        ins = None
                        for q in range(4):
                            g = g4 * 4 + q
                            ins = e.transpose(out=pbb[:, q * 128:q * 128 + ncb], in_=Ucb[0:ncb, s, g * 128:(g + 1) * 128],
                                              identity=identb2[0:ncb, 0:ncb])
                        return ins
                    P.op("pe", tru, r=[("Ucb", s), "identb2"], w=[("p2", g4 % 4)])
                    P.op("act" if g4 % 2 == 0 else "dve",
                         (lambda e, g4=g4, c0=c0, ncb=ncb, pbb=pbb: e.copy(
                             out=U8[:, g4 * 4:g4 * 4 + 4, c0:c0 + ncb], in_=pbb[:, 0:512].rearrange("p (q c) -> p q c", q=4)[:, :, 0:ncb]))
                         if g4 % 2 == 0 else
                         (lambda e, g4=g4, c0=c0, ncb=ncb, pbb=pbb: e.tensor_copy(
                             out=U8[:, g4 * 4:g4 * 4 + 4, c0:c0 + ncb], in_=pbb[:, 0:512].rearrange("p (q c) -> p q c", q=4)[:, :, 0:ncb])),
                         r=[("p2", g4 % 4)], w=["U8"])
            P.flush()
        if debug == 4:
            d1 = nc.dram_tensor("dbg_tloc", [128, 32 * 128], BF16, kind="ExternalOutput").ap()
            d2 = nc.dram_tensor("dbg_bendt", [128, 2 * 32 * 2 * 64], BF16, kind="ExternalOutput").ap()
            d3 = nc.dram_tensor("dbg_dm", [128, 2 * 16 * 2 * 128], BF16, kind="ExternalOutput").ap()
            d4 = nc.dram_tensor("dbg_u8", [128, 32 * NCHe], BF16, kind="ExternalOutput").ap()
            d5 = nc.dram_tensor("dbg_mu3", [128, 128], F32, kind="ExternalOutput").ap()
            P.dmaop("sp", lambda e: e.dma_start(out=d1, in_=Tloc[:].rearrange("p a b -> p (a b)")), w=["d1"])
            P.dmaop("sp", lambda e: e.dma_start(out=d2, in_=BendT[:].rearrange("p a b c d -> p (a b c d)")), w=["d2"])
            P.dmaop("sp", lambda e: e.dma_start(out=d3, in_=Dm[:].rearrange("p a b c d -> p (a b c d)")), w=["d3"])
            P.dmaop("sp", lambda e: e.dma_start(out=d4, in_=U8[:].rearrange("p a b -> p (a b)")), w=["d4"])
            P.dmaop("sp", lambda e: e.dma_start(out=d5, in_=mu3[:].rearrange("p a b c d -> p (a b c d)")), w=["d5"])
            P.flush()
            DONE.append(1)

        def hk(d, lo, hi):
            return [("Hall", d, q) for q in range(lo, hi)]
        P.op("pool", lambda e: e.memset(Hall[0][:, :, :, 0:1], 0.0), w=hk(0, 0, 1))
        P.op("pool", lambda e: e.memset(Hall[1][:, :, :, NCHe:NCHe + 1], 0.0), w=hk(1, NCHe, NCHe + 1))
        xlo = [NCC + 1, 0]
        clo = [1, NCX]
        ei = 0
        for d in range(2):
            for pl in range(2):
                pc = PS2[4 + pl]
                for gp in range(16):
                    px = PS2[(d * 32 + pl * 16 + gp) % 4]
                    pk = ("p2", (d * 32 + pl * 16 + gp) % 4)

                    def mms(e, d=d, pl=pl, gp=gp, px=px, pc=pc):
                        ins = None
                        for gh in range(2):
                            g = 16 * gh + gp
                            e.matmul(px[64 * gh:64 * gh + 64, 0:NCX], lhsT=BendT[:, d, g, pl, :], rhs=U8[:, g, NCC:NCC + NCX],
                                     start=True, stop=True, tile_position=(0, 64 * gh))
                            ins = e.matmul(pc[64 * gh:64 * gh + 64, gp * 32:gp * 32 + NCC], lhsT=BendT[:, d, g, pl, :], rhs=U8[:, g, 0:NCC],
                                           start=True, stop=True, tile_position=(0, 64 * gh))
                        return ins
                    P.op("pe", mms, r=["BendT", "U8"], w=[pk, ("p2c", 4 + pl, gp)])
                    dst = Hall[d][:, gp, pl, xlo[d]:xlo[d] + NCX]
                    if ei % 2 == 0:
                        P.op("act", lambda e, dst=dst, px=px: e.copy(out=dst, in_=px[:, 0:NCX]), r=[pk], w=hk(d, xlo[d], xlo[d] + NCX))
                    else:
                        P.op("dve", lambda e, dst=dst, px=px: e.tensor_copy(out=dst, in_=px[:, 0:NCX]), r=[pk], w=hk(d, xlo[d], xlo[d] + NCX))
                    ei += 1
                P.op("act", lambda e, d=d, pl=pl, pc=pc: e.copy(out=Hall[d][:, :, pl, clo[d]:clo[d] + NCC],
                                                                in_=pc[:, 0:512].rearrange("p (g c) -> p g c", g=16)[:, :, 0:NCC]),
                     r=[("p2c", 4 + pl, gp) for gp in range(16)], w=hk(d, clo[d], clo[d] + NCC))
        LB = 16
        NB = NCHe // LB
        assert NB * LB == NCHe
        with ExitStack() as tc:
            Rl = [sb(tc, "Rl_%d" % d, [128, 16, 3, NB + 1]) for d in range(2)]
            TA = [sb(tc, "TA_%d" % d, [128, 16, 2, NB]) for d in range(2)]
            TB = [sb(tc, "TB_%d" % d, [128, 16, 2, NB]) for d in range(2)]
            Ea = Rl
            ET = [sb(tc, "ET_%d" % d, [128, 2, 16, 2]) for d in range(2)]

            def hview(d, i):
                st0 = (1 + i) if d == 0 else (LB - 1 - i)
                return Hall[d][:, :, :, st0:st0 + LB * (NB - 1) + 1:LB]

            def hkeys(d, i):
                st0 = (1 + i) if d == 0 else (LB - 1 - i)
                return [("Hall", d, st0 + LB * m) for m in range(NB)]

            def mub(d, k, which, n):
                return mu16[:, d, k, which].unsqueeze(3).to_broadcast([128, 16, 2, n])
            for d in range(2):
                eng = "dve"
                for i in range(LB):
                    hv = hview(d, i)
                    hk_i = hkeys(d, i)
                    if i == 0:
                        P.op(eng, lambda e, d=d, hv=hv: e.tensor_copy(out=Rl[d][:, :, 0:2, 0:NB], in_=hv), r=hk_i, w=[("Rl", d)])
                    else:
                        P.op(eng, lambda e, d=d: e.tensor_tensor(out=TA[d][:], in0=Rl[d][:, :, 0:2, 0:NB], in1=mub(d, 1, 0, NB), op=ALU.mult),
                             r=[("Rl", d), "mu16"], w=[("TA", d)])
                        P.op(eng, lambda e, d=d: e.tensor_tensor(out=TB[d][:], in0=Rl[d][:, :, 1:3, 0:NB], in1=mub(d, 1, 1, NB), op=ALU.mult),
                             r=[("Rl", d), "mu16"], w=[("TB", d)])
                        P.op(eng, lambda e, d=d: e.tensor_tensor(out=TA[d][:], in0=TA[d][:], in1=TB[d][:], op=ALU.add),
                             r=[("TA", d), ("TB", d)], w=[("TA", d)])
                        P.op(eng, lambda e, d=d, hv=hv: e.tensor_tensor(out=Rl[d][:, :, 0:2, 0:NB], in0=TA[d][:], in1=hv, op=ALU.add),
                             r=[("TA", d)] + hk_i, w=[("Rl", d)])
                        P.op("act", lambda e, d=d, hv=hv: e.copy(out=hv, in_=Rl[d][:, :, 0:2, 0:NB]), r=[("Rl", d)], w=hk_i)
                    if i < LB - 1:
                        P.op(eng, lambda e, d=d: e.tensor_copy(out=Rl[d][:, :, 2, 0:NB], in_=Rl[d][:, :, 0, 0:NB]), r=[("Rl", d)], w=[("Rl", d)])
            for d in range(2):
                eng = "dve" if d == 0 else "pool"
                P.op(eng, lambda e, d=d: e.memset(Ea[d][:], 0.0), w=[("Rl", d)])
                order = list(range(NB - 1)) if d == 0 else list(range(NB - 1, 0, -1))
                for m in order:
                    mn = m + 1 if d == 0 else m - 1
                    pend = (1 + 16 * m + 15) if d == 0 else (16 * m)
                    P.op(eng, lambda e, d=d, m=m: e.tensor_tensor(out=ET[d][:, 0], in0=Ea[d][:, :, 0:2, m], in1=mu16[:, d, 16, 0], op=ALU.mult),
                         r=[("Rl", d), "mu16"], w=[("ET", d, 0)])
                    P.op(eng, lambda e, d=d, m=m: e.tensor_tensor(out=ET[d][:, 1], in0=Ea[d][:, :, 1:3, m], in1=mu16[:, d, 16, 1], op=ALU.mult),
                         r=[("Rl", d), "mu16"], w=[("ET", d, 1)])
                    P.op(eng, lambda e, d=d: e.tensor_tensor(out=ET[d][:, 0], in0=ET[d][:, 0], in1=ET[d][:, 1], op=ALU.add),
                         r=[("ET", d, 0), ("ET", d, 1)], w=[("ET", d, 0)])
                    P.op(eng, lambda e, d=d, mn=mn, pend=pend: e.tensor_tensor(out=Ea[d][:, :, 0:2, mn], in0=ET[d][:, 0], in1=Hall[d][:, :, :, pend], op=ALU.add),
                         r=[("ET", d, 0), ("Hall", d, pend)], w=[("Rl", d)])
                    P.op(eng, lambda e, d=d, mn=mn: e.tensor_copy(out=Ea[d][:, :, 2, mn], in_=Ea[d][:, :, 0, mn]), r=[("Rl", d)], w=[("Rl", d)])
            for d in range(2):
                eng = "dve"
                for i in range(LB):
                    hv = hview(d, i)
                    hk_i = hkeys(d, i)
                    P.op(eng, lambda e, d=d, i=i: e.tensor_tensor(out=TA[d][:], in0=Ea[d][:, :, 0:2, 0:NB], in1=mub(d, i + 1, 0, NB), op=ALU.mult),
                         r=[("Rl", d), "mu16"], w=[("TA", d)])
                    P.op(eng, lambda e, d=d, i=i: e.tensor_tensor(out=TB[d][:], in0=Ea[d][:, :, 1:3, 0:NB], in1=mub(d, i + 1, 1, NB), op=ALU.mult),
                         r=[("Rl", d), "mu16"], w=[("TB", d)])
                    P.op(eng, lambda e, d=d: e.tensor_tensor(out=TA[d][:], in0=TA[d][:], in1=TB[d][:], op=ALU.add),
                         r=[("TA", d), ("TB", d)], w=[("TA", d)])
                    P.op(eng, lambda e, d=d, hv=hv: e.tensor_tensor(out=hv, in0=hv, in1=TA[d][:], op=ALU.add), r=[("TA", d)] + hk_i, w=hk_i)
            P.flush()
        NCB = (NCX + 127) // 128
        NPASS = 2 if NCB >= 2 else 1
        CBP = NCB // NPASS
        NCP = NCX // NPASS
        Yc = sb(st, "Yc", [128, CBP, 4096], BF16)
        Ysb = sb(st, "Ysb", [128, 2, NCP])
        allH = [hk(0, 0, HW), hk(1, 0, HW)]
        for pz in range(NPASS):
            c_lo = pz * NCP
            for g in range(32):
                gh, gp = g // 16, g % 16
                rows = slice(64 * gh, 64 * gh + 64)
                pr = PS2[g % 2]
                prk = ("p2", g % 2)

                def mmy(e, g=g, gp=gp, rows=rows, pr=pr, c_lo=c_lo):
                    e.matmul(pr[:, 0:NCP], lhsT=Tloc[:, g, :], rhs=U8[:, g, NCC + c_lo:NCC + c_lo + NCP], start=True, stop=False)
                    e.matmul(pr[:, 0:NCP], lhsT=Dm[rows, 0, gp, 0, :], rhs=Hall[0][rows, gp, 0, NCC + c_lo:NCC + c_lo + NCP], start=False, stop=False)
                    e.matmul(pr[:, 0:NCP], lhsT=Dm[rows, 0, gp, 1, :], rhs=Hall[0][rows, gp, 1, NCC + c_lo:NCC + c_lo + NCP], start=False, stop=False)
                    e.matmul(pr[:, 0:NCP], lhsT=Dm[rows, 1, gp, 0, :], rhs=Hall[1][rows, gp, 0, 1 + c_lo:1 + c_lo + NCP], start=False, stop=False)
                    return e.matmul(pr[:, 0:NCP], lhsT=Dm[rows, 1, gp, 1, :], rhs=Hall[1][rows, gp, 1, 1 + c_lo:1 + c_lo + NCP], start=False, stop=True)
                P.op("pe", mmy, r=[("Tloc", g), "U8", "Dm"] + allH[0] + allH[1], w=[prk])
                ys = g % 2
                if g % 2 == 0:
                    P.op("act", lambda e, ys=ys, pr=pr: e.copy(out=Ysb[:, ys, :], in_=pr[:, 0:NCP]), r=[prk], w=[("Ysb", ys)])
                else:
                    P.op("dve", lambda e, ys=ys, pr=pr: e.tensor_copy(out=Ysb[:, ys, :], in_=pr[:, 0:NCP]), r=[prk], w=[("Ysb", ys)])
                for cb in range(CBP):
                    ncb = min(128, NCP - cb * 128)
                    pt_ = PS2[2 + (g * CBP + cb) % 4]
                    ptk = ("p2", 2 + (g * CBP + cb) % 4)
                    P.op("pe", lambda e, ys=ys, cb=cb, ncb=ncb, pt_=pt_: e.transpose(out=pt_[0:ncb, 0:128], in_=Ysb[:, ys, cb * 128:cb * 128 + ncb],
                                                                                   identity=ident[:]), r=[("Ysb", ys), "ident"], w=[ptk])
                    oap = Yc[0:ncb, cb, :].rearrange("c (j g o) -> c g j o", j=8, g=32)[:, g]
                    iap = pt_[0:ncb, 0:128].rearrange("c (j o) -> c j o", j=8)
                    if (g + cb) % 2 == 0:
                        P.op("dve", lambda e, oap=oap, iap=iap: e.tensor_copy(out=oap, in_=iap), r=[ptk], w=[("Yc", cb)])
                    else:
                        P.op("act", lambda e, oap=oap, iap=iap: e.copy(out=oap, in_=iap), r=[ptk], w=[("Yc", cb)])
            for cb in range(CBP):
                ncb = min(128, NCP - cb * 128)
                r0 = c_lo + cb * 128
                P.dmaop("sp", lambda e, cb=cb, ncb=ncb, r0=r0: e.dma_start(out=y_s[r0:r0 + ncb, :], in_=Yc[0:ncb, cb, :]),
                        r=[("Yc", cb)], w=["y_s"])
        P.flush()
    if DONE:
        es.close()
        return nc
    if debug == 5:
        dbg = nc.dram_tensor("dbg_y", [NXe // 8, 4096], BF16, kind="ExternalOutput").ap()
        P.dmaop("sp", lambda e: e.dma_start(out=dbg, in_=y_s[0:NXe // 8, :]), w=["dbg"])
        P.flush()
        es.close()
        return nc

    if debug in (0, 3, 6):
      with ExitStack() as st:
        vt = sb(st, "vt", [128, NTe, 8, 80], BF16)
        kTh = sb(st, "kTh", [128, 2, NTe * 128], BF16)
        qTh = sb(st, "qTh", [128, 2, NXe], BF16)
        pT = sb(st, "pT", [128, 5, 512], BF16)
        osb = sb(st, "osb", [128, 2, 512])
        rr = sb(st, "rr", [128, 512])
        ao = sb(st, "ao", [64, 2, 512])
        ones1 = sb(st, "ones1", [128, 64])
        PSs = [ps(st, "ps_s%d" % i) for i in range(5)]
        PSo = [ps(st, "ps_o%d" % i) for i in range(2)]
        PSb = ps(st, "ps_b")
        P.op("pool", lambda e: e.memset(vt[:], 1.0), w=["vt"])
        P.op("pool", lambda e: e.memset(ones1[:], 1.0), w=["ones1"])
        for kt in range(NTe):
            P.dmaop("sp" if kt % 2 == 0 else "act",
                    lambda e, kt=kt: e.dma_start(out=vt[:, kt, :, 0:64],
                                                 in_=v_s[kt * 128:(kt + 1) * 128, :].rearrange("p (h d) -> p h d", h=8)),
                    r=["v_s"], w=["vt"])
        cnt = 0
        for h in range(H):
            hs = h % 2
            P.dmaop("sp", lambda e, h=h, hs=hs: e.dma_start(out=kTh[0:96, hs, :], in_=kT_s[h, :, 0:NTe * 128]), r=["kT_s"], w=[("kTh", hs)])
            P.dmaop("act", lambda e, h=h, hs=hs: e.dma_start(out=qTh[0:96, hs, :], in_=qT_s[h, :, 0:NXe]), r=["qT_s"], w=[("qTh", hs)])
            for g in range(NQG):
                og = (h * NQG + g) % 2

                def smm(e, kt, hs=hs, g=g):
                    return e.matmul(PSs[kt % 5][:, 0:QG], lhsT=kTh[0:96, hs, kt * 128:(kt + 1) * 128],
                                    rhs=qTh[0:96, hs, g * QG:(g + 1) * QG], start=True, stop=True)

                def pvm(e, kt, h=h, og=og):
                    return e.matmul(PSo[og][0:65, 0:QG], lhsT=vt[:, kt, h, 0:65], rhs=pT[:, kt % 5, 0:QG],
                                    start=(kt == 0), stop=(kt == NTe - 1))
                LOOK = 3
                for step in range(NTe + LOOK):
                    if step < NTe:
                        kt = step
                        P.op("pe", lambda e, kt=kt, f=smm: f(e, kt), r=[("kTh", hs), ("qTh", hs)], w=[("pss", kt % 5)])
                        P.op("act", lambda e, kt=kt: e.activation(out=pT[:, kt % 5, 0:QG], in_=PSs[kt % 5][:, 0:QG], func=AF.Exp),
                             r=[("pss", kt % 5)], w=[("pT", kt % 5)])
                    if step >= LOOK:
                        kt = step - LOOK
                        P.op("pe", lambda e, kt=kt, f=pvm: f(e, kt), r=[("pT", kt % 5), "vt"], w=[("pso", og)])
                P.op("dve", lambda e, og=og: e.tensor_copy(out=osb[0:65, og, 0:QG], in_=PSo[og][0:65, 0:QG]), r=[("pso", og)], w=[("osb", og)])
                P.op("dve", lambda e, og=og: e.reciprocal(out=rr[64:65, 0:QG], in_=osb[64:65, og, 0:QG]), r=[("osb", og)], w=["rr"])
                P.op("pe", lambda e: e.matmul(PSb[0:64, 0:QG], lhsT=ones1[64:65, 0:64], rhs=rr[64:65, 0:QG], start=True, stop=True),
                     r=["rr", "ones1"], w=["psb"])
                P.op("dve", lambda e, og=og: e.tensor_tensor(out=ao[:, og, 0:QG], in0=osb[0:64, og, 0:QG], in1=PSb[0:64, 0:QG], op=ALU.mult),
                     r=[("osb", og), "psb"], w=[("ao", og)])
                P.dmaop("sp", lambda e, h=h, g=g, og=og: e.dma_start(out=attnT_s[h, :, g * QG:(g + 1) * QG], in_=ao[:, og, 0:QG]),
                        r=[("ao", og)], w=["attnT_s"])
        P.flush()

    CAPe = 2 * NXe // NE
    SLT = min(128, CAPe)
    NRC = CAPe // SLT
    NXT = NXe // 128
    h2b_s = nc.dram_tensor("h2b_s", [NX, D], BF16, kind="Internal").ap()

    if debug in (0, 6):
      with ExitStack() as sp:
        idxT = sb(sp, "idxT", [128, NRC, 16], I32)
        gateT = sb(sp, "gateT", [128, NRC, 16])
        sp45 = ExitStack()
        affT = sb(sp45, "affT", [48, NXe // 2])
        with ExitStack() as st:
            wglu = sb(st, "wglu", [128, 4, 512], BF16)
            wsso = sb(st, "wsso", [128, 4, D], BF16)
            wmla = sb(st, "wmla", [64, 8, D], BF16)
            wout = sb(st, "wout", [128, 8, D], BF16)
            wrt = sb(st, "wrt", [128, 8, 16])
            bglu = sb(st, "bglu", [128, 512])
            identb = sb(st, "identb4", [128, 128], BF16)
            TL4 = [dict(), dict()]
            for _s in range(2):
                TL4[_s]['yt'] = sb(st, "yt_%d" % _s, [128, 512], BF16)
                TL4[_s]['yg'] = sb(st, "yg_%d" % _s, [128, 512])
                TL4[_s]['t1'] = sb(st, "t1_%d" % _s, [128, 512])
                TL4[_s]['ygb'] = sb(st, "ygb_%d" % _s, [128, 512], BF16)
                TL4[_s]['ygT'] = sb(st, "ygT_%d" % _s, [128, 4, 128], BF16)
                TL4[_s]['sg'] = sb(st, "sg_%d" % _s, [128, 512])
                TL4[_s]['zb'] = sb(st, "zb_%d" % _s, [128, 512], BF16)
                TL4[_s]['zT'] = sb(st, "zT_%d" % _s, [128, 4, 128], BF16)
                TL4[_s]['at32'] = sb(st, "at32_%d" % _s, [64, 8, 128])
                TL4[_s]['atb'] = sb(st, "atb_%d" % _s, [64, 8, 128], BF16)
                TL4[_s]['gt'] = sb(st, "gt_%d" % _s, [128, 2048])
                TL4[_s]['m1'] = sb(st, "m1_%d" % _s, [128, D])
                TL4[_s]['m2'] = sb(st, "m2_%d" % _s, [128, D])
                TL4[_s]['mb'] = sb(st, "mb_%d" % _s, [128, D], BF16)
                TL4[_s]['mT'] = sb(st, "mT_%d" % _s, [128, 8, 128], BF16)
                TL4[_s]['xt4'] = sb(st, "xt4_%d" % _s, [128, D])
                TL4[_s]['xm'] = sb(st, "xm_%d" % _s, [128, D])
                TL4[_s]['jk'] = sb(st, "jk_%d" % _s, [128, D])
                TL4[_s]['h2'] = sb(st, "h2_%d" % _s, [128, D])
                TL4[_s]['h2b'] = sb(st, "h2b_%d" % _s, [128, D], BF16)
                TL4[_s]['h2T'] = sb(st, "h2T_%d" % _s, [128, 8, 128])
                TL4[_s]['s4'] = sb(st, "s4_%d" % _s, [128, 8])
                TL4[_s]['lg'] = sb(st, "lg_%d" % _s, [128, 16])
                TL4[_s]['af'] = sb(st, "af_%d" % _s, [128, 48])
            BALL = [ps(st, "ph4_%d" % i) for i in range(8)]
            P.dmaop("pool", lambda e: e.dma_start(out=wglu[:], in_=w_glu.rearrange("(k p) n -> p k n", p=128)), w=["wglu"])
            P.dmaop("pool", lambda e: e.dma_start(out=wsso[:], in_=w_ssm_o.rearrange("(k p) n -> p k n", p=128)), w=["wsso"])
            P.dmaop("pool", lambda e: e.dma_start(out=wmla[:], in_=w_mla_o.rearrange("(h v) n -> v h n", v=64)), w=["wmla"])
            P.dmaop("pool", lambda e: e.dma_start(out=wout[:], in_=w_out.rearrange("(k p) n -> p k n", p=128)), w=["wout"])
            P.dmaop("sp", lambda e: e.dma_start(out=wrt[:], in_=w_router.rearrange("(k p) n -> p k n", p=128)), w=["wrt"])
            P.dmaop("sp", lambda e: e.dma_start(out=bglu[:], in_=b_glu.partition_broadcast(128)), w=["bglu"])
            P.op("dve", lambda e: e.tensor_copy(out=identb[:], in_=ident[:]), r=["ident"], w=["identb4"])
            ysv = y_s.rearrange("c (j f) -> (c j) f", j=8)
            for _s in range(2):
                P.op("pool", lambda e, _s=_s: e.memset(TL4[_s]['af'][:], 0.0), w=[("slot", _s, "af")])
            P.op("pool", lambda e: e.memset(affT[:], 0.0), w=["affT"])
            SHARED4 = ["wglu", "wsso", "wmla", "wout", "wrt", "bglu", "identb4", "ident", "modx", "y_s", "gates_s", "attnT_s", "out", "h2b_s", "affT"]
            ALIAS4 = {"b4": "b2", "b5": "b3", "b6": "b2", "b7": "b3"}

            def tile4(i, s):
                Pq = Keyed(P, s, SHARED4, ALIAS4)
                t0 = i * 128
                yt = TL4[s]['yt']
                yg = TL4[s]['yg']
                t1 = TL4[s]['t1']
                ygb = TL4[s]['ygb']
                ygT = TL4[s]['ygT']
                sg = TL4[s]['sg']
                zb = TL4[s]['zb']
                zT = TL4[s]['zT']
                at32 = TL4[s]['at32']
                atb = TL4[s]['atb']
                gt = TL4[s]['gt']
                m1 = TL4[s]['m1']
                m2 = TL4[s]['m2']
                mb = TL4[s]['mb']
                mT = TL4[s]['mT']
                xt4 = TL4[s]['xt4']
                xm = TL4[s]['xm']
                jk = TL4[s]['jk']
                h2 = TL4[s]['h2']
                h2b = TL4[s]['h2b']
                h2T = TL4[s]['h2T']
                s4 = TL4[s]['s4']
                lg = TL4[s]['lg']
                af = TL4[s]['af']
                bk = BALL[4 * s:4 * s + 4]
                B = [bk[0], bk[1], bk[2], bk[3], bk[2], bk[3], bk[2], bk[3]]
                B0b = B[0][:].bitcast(BF16)
                Pq.dmaop("sp", lambda e, t0=t0: e.dma_start(out=yt[:], in_=ysv[t0:t0 + 128, :]), r=["y_s"], w=["yt"])
                Pq.dmaop("act", lambda e, t0=t0: e.dma_start(out=gt[:], in_=gates_s[t0:t0 + 128, :]), r=["gates_s"], w=["gt"])
                Pq.dmaop("sp", lambda e, t0=t0: e.dma_start(out=at32[:], in_=attnT_s[:, :, t0:t0 + 128].rearrange("h v t -> v h t")),
                        r=["attnT_s"], w=["at32"])
                Pq.dmaop("act", lambda e, t0=t0: e.dma_start(out=xt4[:], in_=xc[NCTX + t0:NCTX + t0 + 128, :]), w=["xt4"])
                Pq.op("pool", lambda e: e.tensor_tensor(out=t1[:], in0=yt[:], in1=yt[:], op=ALU.mult), r=["yt"], w=["t1"])
                Pq.op("dve", lambda e: e.tensor_scalar(out=t1[:], in0=t1[:], scalar1=0.044715, scalar2=1.0, op0=ALU.mult, op1=ALU.add),
                     r=["t1"], w=["t1"])
                Pq.op("dve", lambda e: e.tensor_tensor(out=t1[:], in0=t1[:], in1=yt[:], op=ALU.mult), r=["t1", "yt"], w=["t1"])
                Pq.op("act", lambda e: e.activation(out=t1[:], in_=t1[:], func=AF.Tanh, scale=0.7978845608028654), r=["t1"], w=["t1"])
                Pq.op("dve", lambda e: e.tensor_scalar(out=t1[:], in0=t1[:], scalar1=1.0, scalar2=0.5, op0=ALU.add, op1=ALU.mult),
                     r=["t1"], w=["t1"])
                Pq.op("dve", lambda e: e.tensor_tensor(out=yg[:], in0=t1[:], in1=yt[:], op=ALU.mult), r=["t1", "yt"], w=["yg"])
                Pq.op("pool", lambda e: e.tensor_copy(out=ygb[:], in_=yg[:]), r=["yg"], w=["ygb"])

                def tr4(src, n):
                    def f(e):
                        ins = None
                        for k in range(n):
                            ins = e.transpose(out=B0b[:, k * 128:(k + 1) * 128], in_=src[:, k * 128:(k + 1) * 128], identity=identb[:])
                        return ins
                    return f
                Pq.op("pe", tr4(ygb, 4), r=["ygb", "identb4"], w=["b0"])
                Pq.op("act", lambda e: e.copy(out=ygT[:].rearrange("p a b -> p (a b)"), in_=B0b[:, 0:512]), r=["b0"], w=["ygT"])

                def mmglu(e):
                    ins = None
                    for k in range(4):
                        ins = e.matmul(B[1][:, 0:512], lhsT=ygT[:, k, :], rhs=wglu[:, k, :], start=(k == 0), stop=(k == 3))
                    return ins
                Pq.op("pe", mmglu, r=["ygT", "wglu"], w=["b1"])
                Pq.op("dve", lambda e: e.tensor_tensor(out=sg[:], in0=B[1][:, 0:512], in1=bglu[:], op=ALU.add), r=["b1", "bglu"], w=["sg"])
                Pq.op("act", lambda e: e.activation(out=sg[:], in_=sg[:], func=AF.Sigmoid), r=["sg"], w=["sg"])
                Pq.op("dve", lambda e: e.tensor_tensor(out=zb[:], in0=sg[:], in1=yg[:], op=ALU.mult), r=["sg", "yg"], w=["zb"])
                Pq.op("pe", tr4(zb, 4), r=["zb", "identb4"], w=["b0"])
                Pq.op("act", lambda e: e.copy(out=zT[:].rearrange("p a b -> p (a b)"), in_=B0b[:, 0:512]), r=["b0"], w=["zT"])

                def mmsso(e):
                    ins = None
                    for hf in range(2):
                        for k in range(4):
                            ins = e.matmul(B[2 + hf][:, 0:512], lhsT=zT[:, k, :], rhs=wsso[:, k, hf * 512:(hf + 1) * 512], start=(k == 0), stop=(k == 3))
                    return ins
                Pq.op("pe", mmsso, r=["zT", "wsso"], w=["b2", "b3"])
                for hf in range(2):
                    cs = slice(hf * 512, (hf + 1) * 512)
                    Pq.op("dve", lambda e, hf=hf, cs=cs: e.tensor_tensor(out=m1[:, cs], in0=B[2 + hf][:, 0:512], in1=gt[:, cs], op=ALU.mult),
                          r=["b%d" % (2 + hf), "gt"], w=[("m1", hf)])
                Pq.op("pool", lambda e: e.tensor_copy(out=atb[:], in_=at32[:]), r=["at32"], w=["atb"])

                def mmat(e):
                    ins = None
                    for hf in range(2):
                        for h in range(8):
                            ins = e.matmul(B[4 + hf][:, 0:512], lhsT=atb[:, h, :], rhs=wmla[:, h, hf * 512:(hf + 1) * 512], start=(h == 0), stop=(h == 7))
                    return ins
                Pq.op("pe", mmat, r=["atb", "wmla"], w=["b4", "b5"])
                for hf in range(2):
                    cs = slice(hf * 512, (hf + 1) * 512)
                    cs2 = slice(D + hf * 512, D + (hf + 1) * 512)
                    Pq.op("dve", lambda e, hf=hf, cs=cs, cs2=cs2: e.tensor_tensor(out=m2[:, cs], in0=B[4 + hf][:, 0:512], in1=gt[:, cs2], op=ALU.mult),
                          r=["b%d" % (4 + hf), "gt"], w=[("m2", hf)])
                Pq.op("pool", lambda e: e.tensor_tensor(out=mb[:], in0=m1[:], in1=m2[:], op=ALU.add),
                     r=[("m1", 0), ("m1", 1), ("m2", 0), ("m2", 1)], w=["mb"])
                Pq.op("pe", tr4(mb, 8), r=["mb", "identb4"], w=["b0"])
                Pq.op("act", lambda e: e.copy(out=mT[:].rearrange("p a b -> p (a b)"), in_=B0b[:, 0:1024]), r=["b0"], w=["mT"])

                def mmout(e):
                    ins = None
                    for hf in range(2):
                        for k in range(8):
                            ins = e.matmul(B[6 + hf][:, 0:512], lhsT=mT[:, k, :], rhs=wout[:, k, hf * 512:(hf + 1) * 512], start=(k == 0), stop=(k == 7))
                    return ins
                Pq.op("pe", mmout, r=["mT", "wout"], w=["b6", "b7"])
                for hf in range(2):
                    cs = slice(hf * 512, (hf + 1) * 512)
                    Pq.op("dve", lambda e, hf=hf, cs=cs: e.tensor_tensor(out=xm[:, cs], in0=B[6 + hf][:, 0:512], in1=modx[:, 2 * D + hf * 512:2 * D + (hf + 1) * 512],
                                                                    op=ALU.mult), r=["b%d" % (6 + hf), "modx"], w=[("xm", hf)])
                Pq.op("pool", lambda e: e.tensor_tensor(out=xm[:], in0=xm[:], in1=xt4[:], op=ALU.add), r=[("xm", 0), ("xm", 1), "xt4"], w=[("xm", 0), ("xm", 1)])
                Pq.dmaop("sp", lambda e, t0=t0: e.dma_start(out=out[t0:t0 + 128, :], in_=xm[:]), r=[("xm", 0), ("xm", 1)], w=["out"])
                Pq.op("act", lambda e: e.activation(out=jk[:], in_=xm[:], func=AF.Square, accum_out=s4[:, 0:1]), r=[("xm", 0), ("xm", 1)], w=["jk", "s4a"])
                Pq.op("dve", lambda e: e.tensor_scalar(out=s4[:, 1:2], in0=s4[:, 0:1], scalar1=1.0 / D, scalar2=EPS, op0=ALU.mult, op1=ALU.add), r=["s4a"], w=["s4b"])
                Pq.op("act", lambda e: e.activation(out=s4[:, 1:2], in_=s4[:, 1:2], func=AF.Sqrt), r=["s4b"], w=["s4b"])
                Pq.op("dve", lambda e: e.reciprocal(out=s4[:, 1:2], in_=s4[:, 1:2]), r=["s4b"], w=["s4b"])
                Pq.op("dve", lambda e: e.scalar_tensor_tensor(out=h2[:], in0=xm[:], scalar=s4[:, 1:2], in1=modx[:, 4 * D:5 * D], op0=ALU.mult, op1=ALU.mult),
                     r=[("xm", 0), ("xm", 1), "s4b", "modx"], w=["h2"])
                Pq.op("pool", lambda e: e.tensor_tensor(out=h2[:], in0=h2[:], in1=modx[:, 3 * D:4 * D], op=ALU.add), r=["h2", "modx"], w=["h2"])
                Pq.op("act", lambda e: e.copy(out=h2b[:], in_=h2[:]), r=["h2"], w=["h2b"])
                Pq.dmaop("act", lambda e, t0=t0: e.dma_start(out=h2b_s[t0:t0 + 128, :], in_=h2b[:]), r=["h2b"], w=["h2b_s"])
                def trh2(e):
                    ins = None
                    for k in range(8):
                        ins = e.transpose(out=B[2 + k // 4][:, (k % 4) * 128:(k % 4 + 1) * 128], in_=h2[:, k * 128:(k + 1) * 128], identity=ident[:])
                    return ins
                Pq.op("pe", trh2, r=["h2", "ident", ("m1", 0), ("m1", 1)], w=["b2", "b3"])
                Pq.op("act", lambda e: e.copy(out=h2T[:, 0:4, :].rearrange("p a b -> p (a b)"), in_=B[2][:, 0:512]), r=["b2"], w=[("h2T", 0)])
                Pq.op("dve", lambda e: e.tensor_copy(out=h2T[:, 4:8, :].rearrange("p a b -> p (a b)"), in_=B[3][:, 0:512]), r=["b3"], w=[("h2T", 1)])

                def mmrt(e):
                    ins = None
                    for k in range(8):
                        ins = e.matmul(B[1][:, 0:16], lhsT=h2T[:, k, :], rhs=wrt[:, k, :], start=(k == 0), stop=(k == 7))
                    return ins
                Pq.op("pe", mmrt, r=[("h2T", 0), ("h2T", 1), "wrt", "sg"], w=["b1"])
                Pq.op("dve", lambda e: e.tensor_copy(out=lg[:], in_=B[1][:, 0:16]), r=["b1"], w=["lg"])
                Pq.op("dve", lambda e: e.tensor_reduce(out=s4[:, 2:3], in_=lg[:], axis=AX.X, op=ALU.max), r=["lg"], w=["s4c"])
                Pq.op("dve", lambda e: e.tensor_scalar(out=s4[:, 3:4], in0=s4[:, 2:3], scalar1=-1.0, scalar2=None, op0=ALU.mult), r=["s4c"], w=["s4d"])
                hb_ = 1 if t0 >= NXe // 2 else 0
                afc = slice(32 * hb_, 32 * hb_ + 16)
                Pq.op("act", lambda e, afc=afc: e.activation(out=af[:, afc], in_=lg[:], func=AF.Exp, bias=s4[:, 3:4], accum_out=s4[:, 4:5]), r=["lg", "s4d"], w=["af", "s4e"])
                Pq.op("dve", lambda e: e.reciprocal(out=s4[:, 5:6], in_=s4[:, 4:5]), r=["s4e"], w=["s4f"])
                Pq.op("dve", lambda e, afc=afc: e.tensor_scalar(out=af[:, afc], in0=af[:, afc], scalar1=s4[:, 5:6], scalar2=None, op0=ALU.mult), r=["af", "s4f"], w=["af"])
                Pq.op("pe", lambda e: e.transpose(out=B[4][0:48, 0:128], in_=af[:], identity=ident[:]), r=["af", "ident", ("m2", 0), ("m2", 1)], w=["b4"])
                tcol = t0 - hb_ * (NXe // 2)
                Pq.op("act", lambda e, hb_=hb_, tcol=tcol: e.copy(out=affT[32 * hb_:32 * hb_ + 16, tcol:tcol + 128], in_=B[4][32 * hb_:32 * hb_ + 16, 0:128]),
                      r=["b4"], w=["affT"])
                return Pq.cap
            for i in range(0, NXT, 2):
                caps = [tile4(i, 0)] + ([tile4(i + 1, 1)] if i + 1 < NXT else [])
                interleave(P, caps, chunk=int(os.environ.get("ILV", "2")))
            P.flush()
        with ExitStack() as st:
            NH = NXe // 2
            wk = sb(st, "wk", [48, NH])
            vals = sb(st, "vals", [48, CAPe])
            idxu = sb(st, "idxu", [48, CAPe], U32)
            idxf = sb(st, "idxf", [48, CAPe])
            jrev = sb(st, "jrev", [128, 128])
            tA = sb(st, "tA", [128, 2, 16])
            tB = sb(st, "tB", [128, 2, 16])
            tM = sb(st, "tM", [128, 3, 16])
            B5 = [ps(st, "ph5_%d" % i) for i in range(4)]
            P.dmaop("sp", lambda e: e.dma_start(out=jrev[:], in_=jrev_d), w=["jrev"])
            P.op("dve", lambda e: e.tensor_copy(out=wk[:], in_=affT[:]), r=["affT"], w=["wk"])
            for r_ in range(CAPe // 8):
                sl = slice(r_ * 8, r_ * 8 + 8)
                P.op("dve", lambda e, sl=sl: e.max(out=vals[:, sl], in_=wk[:]), r=["wk"], w=[("vals", r_)])
                P.op("dve", lambda e, sl=sl: e.max_index(out=idxu[:, sl], in_max=vals[:, sl], in_values=wk[:]), r=["wk", ("vals", r_)], w=[("idxu", r_)])
                P.op("dve", lambda e, sl=sl: e.match_replace(out=wk[:], in_to_replace=vals[:, sl], in_values=wk[:], imm_value=-1.0),
                     r=["wk", ("vals", r_), ("idxu", r_)], w=["wk"])
            allv = [("vals", r_) for r_ in range(CAPe // 8)]
            alli = [("idxu", r_) for r_ in range(CAPe // 8)]
            P.op("dve", lambda e: e.tensor_copy(out=idxf[:], in_=idxu[:]), r=alli, w=["idxf"])
            P.op("dve", lambda e: e.tensor_scalar(out=idxf[32:48, :], in0=idxf[32:48, :], scalar1=float(NH), scalar2=None, op0=ALU.add), r=["idxf"], w=["idxf"])
            Jb = jrev[0:SLT, 128 - SLT:128]
            for rc in range(NRC):
                cs = slice(rc * SLT, (rc + 1) * SLT)
                rb = NRC - 1 - rc
                cb_ = slice(rb * SLT, (rb + 1) * SLT)
                for w_, src in ((0, vals), (1, idxf)):
                    rk = allv if w_ == 0 else ["idxf"]
                    P.op("pe", lambda e, cs=cs, src=src, w_=w_: e.transpose(out=B5[w_][0:SLT, 0:16], in_=src[0:16, cs], identity=ident[0:16, 0:16]),
                         r=rk + ["ident"], w=[("b5", w_)])
                    P.op("act", lambda e, w_=w_: e.copy(out=tA[0:SLT, w_, :], in_=B5[w_][0:SLT, 0:16]), r=[("b5", w_)], w=[("tA", w_)])
                    P.op("pe", lambda e, cb_=cb_, src=src, w_=w_: e.transpose(out=B5[2 + w_][0:SLT, 0:16], in_=src[32:48, cb_], identity=ident[32:48, 32:48]),
                         r=rk + ["ident"], w=[("b5", 2 + w_)])
                    P.op("act", lambda e, w_=w_: e.copy(out=tB[0:SLT, w_, :], in_=B5[2 + w_][0:SLT, 0:16]), r=[("b5", 2 + w_)], w=[("tB", w_)])
                    P.op("pe", lambda e, w_=w_: e.matmul(B5[2 + w_][0:SLT, 0:16], lhsT=Jb, rhs=tB[0:SLT, w_, :], start=True, stop=True),
                         r=[("tB", w_), "jrev"], w=[("b5", 2 + w_)])
                P.op("dve", lambda e: e.tensor_tensor(out=tM[0:SLT, 0, :], in0=tA[0:SLT, 0, :], in1=B5[2][0:SLT, 0:16], op=ALU.is_gt),
                     r=[("tA", 0), ("b5", 2)], w=[("tM", 0)])
                P.op("dve", lambda e, rc=rc: e.tensor_tensor(out=gateT[0:SLT, rc, :], in0=tA[0:SLT, 0, :], in1=B5[2][0:SLT, 0:16], op=ALU.max),
                     r=[("tA", 0), ("b5", 2)], w=["gateT"])
                P.op("dve", lambda e: e.tensor_tensor(out=tM[0:SLT, 1, :], in0=tA[0:SLT, 1, :], in1=B5[3][0:SLT, 0:16], op=ALU.subtract),
                     r=[("tA", 1), ("b5", 3)], w=[("tM", 1)])
                P.op("dve", lambda e: e.tensor_tensor(out=tM[0:SLT, 1, :], in0=tM[0:SLT, 1, :], in1=tM[0:SLT, 0, :], op=ALU.mult),
                     r=[("tM", 1), ("tM", 0)], w=[("tM", 1)])
                P.op("dve", lambda e: e.tensor_tensor(out=tM[0:SLT, 2, :], in0=tM[0:SLT, 1, :], in1=B5[3][0:SLT, 0:16], op=ALU.add),
                     r=[("tM", 1), ("b5", 3)], w=[("tM", 2)])
                P.op("dve", lambda e, rc=rc: e.tensor_copy(out=idxT[0:SLT, rc, :], in_=tM[0:SLT, 2, :]), r=[("tM", 2)], w=["idxT"])
            P.flush()
        sp45.close()
        NEe = NE if debug == 0 else int(os.environ.get("NEE", "16"))
        with ExitStack() as st:
            identb = sb(st, "identb6", [128, 128], BF16)
            xs = sb(st, "xs", [128, 1, NRC, D], BF16)
            xsT = sb(st, "xsT", [128, 2, 8, CAPe], BF16)
            wg = sb(st, "wg", [128, 3, 8, 512], BF16)
            wu = sb(st, "wu", [128, 3, 8, 512], BF16)
            wd = sb(st, "wd", [128, 2, 22, 512], BF16)
            sgt = sb(st, "sgt", [128, 2, CAPe])
            hidT = sb(st, "hidT", [128, 22, CAPe], BF16)
            ys = sb(st, "ys", [128, 1, NRC, D])
            B = [ps(st, "ph6_%d" % i) for i in range(8)]
            B0b = B[0][:].bitcast(BF16)
            P.op("dve", lambda e: e.tensor_copy(out=identb[:], in_=ident[:]), r=["ident"], w=["identb6"])
            wgi = 0
            wdi = 0
            def prep(ex):
                sl = ex % 2
                for rc in range(NRC):
                    P.dmaop("pool", lambda e, rc=rc, ex=ex, sl=sl: e.indirect_dma_start(
                        out=xs[0:SLT, 0, rc, :], out_offset=None, in_=h2b_s[0:NXe, :],
                        in_offset=bass.IndirectOffsetOnAxis(ap=idxT[0:SLT, rc, ex:ex + 1], axis=0)),
                        r=["idxT", "h2b_s"], w=[("xs", 0, rc)])

                    def trx(e, rc=rc, sl=sl):
                        ins = None
                        for k in range(8):
                            ins = e.transpose(out=B0b[:, k * 128:k * 128 + SLT], in_=xs[0:SLT, 0, rc, k * 128:(k + 1) * 128], identity=identb[0:SLT, 0:SLT])
                        return ins
                    P.op("pe", trx, r=[("xs", 0, rc), "identb6"], w=["b0"])
                    P.op("act", lambda e, rc=rc, sl=sl: e.copy(out=xsT[:, sl, :, rc * SLT:(rc + 1) * SLT],
                                                               in_=B0b[:, 0:1024].rearrange("p (k c) -> p k c", k=8)[:, :, 0:SLT]), r=["b0"], w=[("xsT", sl)])
            prep(0)
            pending = []
            for ex in range(NEe):
                xsl = ex % 2
                wgv = w_e_gate[ex].rearrange("(dc p) f -> p dc f", p=128)
                wuv = w_e_up[ex].rearrange("(dc p) f -> p dc f", p=128)
                wdv = w_e_down[ex].rearrange("(fc p) d -> p fc d", p=128)
                for grp in range(6):
                    ws = wgi % 3
                    wgi += 1
                    f0 = grp * 512
                    fw = min(512, FF - f0)
                    P.dmaop("pool", lambda e, ws=ws, f0=f0, fw=fw, wgv=wgv: e.dma_start(out=wg[:, ws, :, 0:fw], in_=wgv[:, :, f0:f0 + fw]), w=[("wg", ws)])
                    P.dmaop("pool", lambda e, ws=ws, f0=f0, fw=fw, wuv=wuv: e.dma_start(out=wu[:, ws, :, 0:fw], in_=wuv[:, :, f0:f0 + fw]), w=[("wu", ws)])
                    if grp == 1:
                        for f_ in pending:
                            f_()
                        pending = []
                    for q in range(fw // 128):
                        fc = grp * 4 + q
                        pg = B[1 + fc % 2]
                        pu = B[3 + fc % 2]

                        def mmgu(e, ws=ws, q=q, pg=pg, pu=pu, xsl=xsl):
                            ins = None
                            for k in range(8):
                                e.matmul(pg[:, 0:CAPe], lhsT=wg[:, ws, k, q * 128:(q + 1) * 128], rhs=xsT[:, xsl, k, :], start=(k == 0), stop=(k == 7))
                            for k in range(8):
                                ins = e.matmul(pu[:, 0:CAPe], lhsT=wu[:, ws, k, q * 128:(q + 1) * 128], rhs=xsT[:, xsl, k, :], start=(k == 0), stop=(k == 7))
                            return ins
                        P.op("pe", mmgu, r=[("wg", ws), ("wu", ws), ("xsT", xsl)], w=[("b6", 1 + fc % 2), ("b6", 3 + fc % 2)])
                        P.op("act", lambda e, fc=fc, pg=pg: e.activation(out=sgt[:, fc % 2, :], in_=pg[:, 0:CAPe], func=AF.Silu),
                             r=[("b6", 1 + fc % 2)], w=[("sgt", fc % 2)])
                        P.op("dve", lambda e, fc=fc, pu=pu: e.tensor_tensor(out=hidT[:, fc, :], in0=sgt[:, fc % 2, :], in1=pu[:, 0:CAPe], op=ALU.mult),
                             r=[("sgt", fc % 2), ("b6", 3 + fc % 2)], w=[("hidT", fc)])
                hk_ = [("hidT", fc) for fc in range(22)]
                if ex + 1 < NEe:
                    prep(ex + 1)
                for dq in range(2):
                    ws = wdi % 2
                    wdi += 1
                    P.dmaop("pool", lambda e, ws=ws, dq=dq, wdv=wdv: e.dma_start(out=wd[:, ws], in_=wdv[:, :, dq * 512:(dq + 1) * 512]), w=[("wd", ws)])
                    for rc in range(NRC):
                        py = B[5 + (dq * NRC + rc) % 2]
                        pyk = ("b6", 5 + (dq * NRC + rc) % 2)

                        def mmd(e, ws=ws, rc=rc, py=py):
                            ins = None
                            for fc in range(22):
                                ins = e.matmul(py[0:SLT, 0:512], lhsT=hidT[:, fc, rc * SLT:(rc + 1) * SLT], rhs=wd[:, ws, fc, :], start=(fc == 0), stop=(fc == 21))
                            return ins
                        P.op("pe", mmd, r=hk_ + [("wd", ws)], w=[pyk])
                        P.op("dve", lambda e, rc=rc, dq=dq, py=py, ex=ex, xsl=xsl: e.scalar_tensor_tensor(
                            out=ys[0:SLT, 0, rc, dq * 512:(dq + 1) * 512], in0=py[0:SLT, 0:512], scalar=gateT[0:SLT, rc, ex:ex + 1],
                            in1=modx[0:SLT, 5 * D + dq * 512:5 * D + (dq + 1) * 512], op0=ALU.mult, op1=ALU.mult),
                            r=[pyk, "gateT", "modx"], w=[("ys", 0, rc, dq)])
                def scat(ex=ex):
                    prevk = [("outx", (ex - 1) % 2, rc2) for rc2 in range(NRC)] if ex > 0 else ["out"]
                    for rc in range(NRC):
                        P.dmaop("pool", lambda e, rc=rc, ex=ex: e.indirect_dma_start(
                            out=out[0:NXe, :], out_offset=bass.IndirectOffsetOnAxis(ap=idxT[0:SLT, rc, ex:ex + 1], axis=0),
                            in_=ys[0:SLT, 0, rc, :], in_offset=None, compute_op=ALU.add),
                            r=[("ys", 0, rc, dq) for dq in range(2)] + ["idxT"] + prevk, w=[("outx", ex % 2, rc)])
                pending.append(scat)
            for f_ in pending:
                f_()
            P.flush()
        if debug == 6:
            DONE.append(1)
    if DONE:
        es.close()
        return nc

    if debug == 3:
        dbg = nc.dram_tensor("dbg", [H, 64, 256], F32, kind="ExternalOutput").ap()
        P.dmaop("sp", lambda e: e.dma_start(out=dbg, in_=attnT_s[:, :, 0:256]), w=["dbg"])
        P.flush()
        es.close()
        return nc

    if debug == 2:
        dbg = nc.dram_tensor("dbg", [H, QK, 512], BF16, kind="ExternalOutput").ap()
        dbg2 = nc.dram_tensor("dbg2", [512, 512], F32, kind="ExternalOutput").ap()
        dbg3 = nc.dram_tensor("dbg3", [H, QK, 256], BF16, kind="ExternalOutput").ap()
        P.dmaop("sp", lambda e: e.dma_start(out=dbg, in_=kT_s[:, :, 0:512]), w=["dbg"])
        P.dmaop("sp", lambda e: e.dma_start(out=dbg2, in_=u_s[0:512, :]), w=["dbg2"])
        P.dmaop("sp", lambda e: e.dma_start(out=dbg3, in_=qT_s[:, :, 0:256]), w=["dbg3"])
        P.flush()
        es.close()
        return nc

    if debug == 1:
        dbg = nc.dram_tensor("dbg", [128, 6 * D], F32, kind="ExternalOutput").ap()
        P.dmaop("sp", lambda e: e.dma_start(out=dbg, in_=modx[:]), r=["modx"], w=["dbg"])
        P.flush()
        es.close()
        return nc

    es.close()
    return nc


def _consts():
    ident = np.eye(128, dtype=np.float32)
    n = NX
    rows = n // 64
    row = np.repeat(np.arange(rows, dtype=np.float32), 64)
    col = np.tile(np.arange(64, dtype=np.float32), rows)
    inv = (10000.0 ** (-np.arange(8, dtype=np.float32) / 8)).astype(np.float32)
    ang = np.stack([row[:, None] * inv, col[:, None] * inv], axis=1).astype(np.float32)
    rope = np.zeros((NT, 32), np.float32)
    rope[:NCTX, :16] = 1.0
    rope[NCTX:, :16] = np.cos(ang).reshape(n, 16)
    rope[NCTX:, 16:] = np.sin(ang).reshape(n, 16)
    cm = np.zeros((128, 256), np.float32)
    for jp in range(8):
        for j in range(8):
            if jp <= j:
                cm[jp * 16:(jp + 1) * 16, j * 16:(j + 1) * 16] = 1.0
            if jp >= j:
                cm[jp * 16:(jp + 1) * 16, 128 + j * 16:128 + (j + 1) * 16] = 1.0
    return ident, rope, cm


def make_in_maps(inputs):
    ident, rope, cm = _consts()
    f = lambda a: np.ascontiguousarray(np.asarray(a, dtype=np.float32))
    maps = []
    for b in range(NCORES):
        m = {"xc": f(np.concatenate([inputs["ctx"][b], inputs["x"][b]], axis=0)),
             "cb": f(inputs["c"][b]), "c_ctx": f(inputs["c_ctx"]),
             "ident": ident, "rope": rope, "cmask": cm, "jrev": np.ascontiguousarray(ident[::-1])}
        for k in ["w_ada", "b_ada", "norm1_g", "norm2_g", "w_in", "q_a_g", "w_qb", "kv_a_g", "w_kvb", "q_norm_g",
                  "k_norm_g", "w_mla_o", "ssm_lam_re", "ssm_lam_im", "ssm_log_dt", "ssm_b_re", "ssm_b_im",
                  "ssm_c_re", "ssm_c_im", "ssm_d", "w_glu", "b_glu", "w_ssm_o", "w_out", "w_router",
                  "w_e_gate", "w_e_up", "w_e_down"]:
            m[k] = f(np.asarray(inputs[k])[0])
        maps.append(m)
    return maps


def kernel(**inputs):
    nc = build()
    maps = make_in_maps(inputs)
    res = run_bass_kernel_spmd(nc, maps, core_ids=list(range(NCORES)))
    return np.stack([np.asarray(r["out"], dtype=np.float32) for r in res.results], axis=0)
```

```python
import math
import os
from contextlib import ExitStack

import numpy as np
import concourse.bass as bass
import concourse.mybir as mybir
from concourse.bass_utils import run_bass_kernel_spmd

F32 = mybir.dt.float32
F32R = mybir.dt.float32r
BF16 = mybir.dt.bfloat16
U32 = mybir.dt.uint32
I32 = mybir.dt.int32
ALU = mybir.AluOpType
AF = mybir.ActivationFunctionType
AX = mybir.AxisListType

D = 1024
NX = 4096
NCTX = 256
NT = NX + NCTX
NTILE = NT // 128
NCH = NT // 8
NCH_C = NCTX // 8
H = 8
QK = 96
NE = 16
FF = 2816
CAP = 512
EPS = 1e-6
IN_COLS = 3232
NCORES = 8

ENG = {"pe": "tensor", "act": "scalar", "dve": "vector", "pool": "gpsimd", "sp": "sync"}


class Prog:
    def __init__(self, nc, sems, dsems):
        self.nc = nc
        self.ops = []
        self.lastw = {}
        self.readers = {}
        self.sems = sems
        self.dsems = dsems
        self.sig = {e: 0 for e in ENG}
        self.dcnt = {e: [0] * len(dsems[e]) for e in dsems}
        self.dnext = {e: 0 for e in dsems}
        self.seen = {e: {} for e in ENG}
        self.emitted = 0
        self.ccsem = None
        self.cccnt = 0

    def op(self, eng, fn, r=(), w=(), dma=False):
        i = len(self.ops)
        deps = set()
        for k in list(r) + list(w):
            if k in self.lastw:
                deps.add(self.lastw[k])
        for k in w:
            for j in self.readers.get(k, ()):
                deps.add(j)
        deps.discard(i)
        o = dict(eng=eng, fn=fn, deps=deps, dma=dma, need=False, val=None, sem=None, idx=i)
        self.ops.append(o)
        for k in w:
            self.lastw[k] = i
            self.readers[k] = []
        for k in r:
            if k not in w:
                self.readers.setdefault(k, []).append(i)
        return i

    def dmaop(self, eng, fn, r=(), w=()):
        return self.op(eng, fn, r, w, dma=True)

    def ccop(self, fn, r=(), w=()):
        return self.op("pool", fn, r, w, dma="cc")

    def flush(self, final_wait_eng="sp"):
        nc = self.nc
        ops = self.ops[self.emitted:]
        pos = {}
        cnt = {e: 0 for e in ENG}
        for o in self.ops[:self.emitted]:
            pass
        for o in ops:
            pos[o["idx"]] = cnt[o["eng"]]
            cnt[o["eng"]] += 1
        for o in ops:
            real = []
            for d in o["deps"]:
                if d < self.emitted:
                    continue
                p = self.ops[d]
                if not p["dma"] and p["eng"] == o["eng"]:
                    if o["eng"] == "pe":
                        continue
                    if o["dma"]:
                        continue
                real.append(d)
                p["need"] = True
            o["real"] = real
        for o in ops:
            if o["dma"]:
                for d in o["deps"]:
                    if d >= self.emitted:
                        p = self.ops[d]
                        if not p["dma"] and p["eng"] == o["eng"] and d not in o["real"]:
                            o["real"].append(d)
                            p["need"] = True
        for o in ops:
            if o["dma"] == "cc":
                o["prev"] = self.cccnt
                self.cccnt += 1
                o["sem"] = self.ccsem
                o["val"] = self.cccnt
            elif o["dma"]:
                e = o["eng"]
                k = self.dnext[e]
                self.dnext[e] = (k + 1) % len(self.dsems[e])
                o["prev"] = self.dcnt[e][k]
                self.dcnt[e][k] += 16
                o["sem"] = self.dsems[e][k]
                o["val"] = self.dcnt[e][k]
            elif o["need"]:
                self.sig[o["eng"]] += 1
                o["sem"] = self.sems[o["eng"]]
                o["val"] = self.sig[o["eng"]]
        byeng = {e: [o for o in ops if o["eng"] == e] for e in ENG}
        self_ = self
        seen = self.seen
        allops = self.ops
        dsems = self.dsems
        dcnt = self.dcnt

        def body(e):
            def f(engine):
                sn = seen[e]

                def wait(sem, val):
                    key = id(sem)
                    if sn.get(key, 0) >= val:
                        return
                    sn[key] = val
                    engine.wait_ge(sem, val)

                for o in byeng[e]:
                    for d in o["real"]:
                        p = allops[d]
                        wait(p["sem"], p["val"])
                    if o["dma"]:
                        if o["prev"] > 0:
                            wait(o["sem"], o["prev"])
                        ins = o["fn"](engine)
                        if o["dma"] == "cc":
                            ins.then_inc(o["sem"])
                        else:
                            ins.then_inc(o["sem"], 16)
                    else:
                        ins = o["fn"](engine)
                        if o["need"]:
                            ins.then_inc(o["sem"], 1)
                if e in dsems:
                    for k, s in enumerate(dsems[e]):
                        if dcnt[e][k] > 0:
                            wait(s, dcnt[e][k])
                if e == "pool" and self_.cccnt > 0:
                    wait(self_.ccsem, self_.cccnt)
            return f

        with nc.Block() as block:
            for e in ENG:
                getattr(block, ENG[e])(body(e))
        self.emitted = len(self.ops)
        self.lastw = {}
        self.readers = {}


class Keyed:
    def __init__(self, P, slot, shared, alias=None):
        self.P, self.slot, self.shared, self.alias = P, slot, set(shared), (alias or {})
        self.cap = []

    def _k(self, keys):
        out = []
        for k in keys:
            k = self.alias.get(k, k)
            base = k[0] if isinstance(k, tuple) else k
            out.append(k if base in self.shared else ("slot", self.slot, k))
        return out

    def op(self, eng, fn, r=(), w=(), dma=False):
        self.cap.append((eng, fn, self._k(r), self._k(w), dma))

    def dmaop(self, eng, fn, r=(), w=()):
        self.op(eng, fn, r, w, dma=True)


def interleave(P, caps, chunk=3):
    pos = [0] * len(caps)
    live = True
    while live:
        live = False
        for i, c in enumerate(caps):
            n = 0
            while pos[i] < len(c) and n < chunk:
                eng, fn, r, w, dma = c[pos[i]]
                P.op(eng, fn, r, w, dma=dma)
                pos[i] += 1
                n += 1
            if pos[i] < len(c):
                live = True


def r32(ap):
    return ap.bitcast(F32R)


def build(debug=0):
    nc = bass.Bass("TRN2", target_bir_lowering=False)
    es = ExitStack()
    DONE = []

    def din(name, shape, dt=F32):
        return nc.dram_tensor(name, list(shape), dt, kind="ExternalInput").ap()

    def dscr(name, shape, dt=F32):
        return nc.dram_tensor(name, list(shape), dt, kind="Internal").ap()

    xc = din("xc", [NT, D])
    cb = din("cb", [D])
    cctx = din("c_ctx", [D])
    w_ada = din("w_ada", [D, 6 * D])
    b_ada = din("b_ada", [6 * D])
    norm1_g = din("norm1_g", [D])
    norm2_g = din("norm2_g", [D])
    w_in = din("w_in", [D, IN_COLS])
    q_a_g = din("q_a_g", [384])
    w_qb = din("w_qb", [384, 768])
    kv_a_g = din("kv_a_g", [256])
    w_kvb = din("w_kvb", [256, 1024])
    q_norm_g = din("q_norm_g", [96])
    k_norm_g = din("k_norm_g", [96])
    w_mla_o = din("w_mla_o", [512, D])
    lam_re = din("ssm_lam_re", [2, 32, 64])
    lam_im = din("ssm_lam_im", [2, 32, 64])
    log_dt = din("ssm_log_dt", [2, 32])
    b_re = din("ssm_b_re", [2, 32, 64, 16])
    b_im = din("ssm_b_im", [2, 32, 64, 16])
    c_re = din("ssm_c_re", [2, 32, 16, 64])
    c_im = din("ssm_c_im", [2, 32, 16, 64])
    ssm_d = din("ssm_d", [512])
    w_glu = din("w_glu", [512, 512])
    b_glu = din("b_glu", [512])
    w_ssm_o = din("w_ssm_o", [512, D])
    w_out = din("w_out", [D, D])
    w_router = din("w_router", [D, NE])
    if debug in (0, 6):
        w_e_gate = din("w_e_gate", [NE // 2, D, FF])
        w_e_up = din("w_e_up", [NE // 2, D, FF])
        w_e_down = din("w_e_down", [NE // 2, FF, D])
    sel_d = din("sel", [NE, NE // 2])
    ident_d = din("ident", [128, 128])
    rope_d = din("rope", [NT, 32])
    jrev_d = din("jrev", [128, 128])
    cmask_d = din("cmask", [128, 256])
    small = debug in (2, 3, 4, 5, 6)
    NTe = 4 if small else NTILE
    NXe = (NTe - 2) * 128
    NXO = NXe // 2
    NTO = NXO // 128
    out = nc.dram_tensor("out", [NXO, D], F32, kind="ExternalOutput").ap()

    u_s = dscr("u_s", [NT, 512])
    gates_s = dscr("gates_s", [NXO, 2048])
    qT_s = dscr("qT_s", [H, QK, NXO], BF16)
    kT_s = dscr("kT_s", [H, QK, NT], BF16)
    v_s = dscr("v_s", [NT, 512], BF16)
    attnT_s = dscr("attnT_s", [H, 64, NXO])
    y_s = dscr("y_s", [NXO // 8, 8 * 512], BF16)
    xm_s = dscr("xm_s", [NXO, D])
    NCHK = 2 if NXO >= 2048 else 1
    RCH = NXO // NCHK
    h2b_own_c = [nc.dram_tensor("h2b_own%d" % c, [RCH, D], BF16) for c in range(NCHK)]
    h2b_ag_c = [nc.dram_tensor("h2b_ag%d" % c, [2 * RCH, D], BF16) for c in range(NCHK)]
    h2b_all_t = nc.dram_tensor("h2b_all", [2 * NXO, D], BF16)
    aff_own_t = nc.dram_tensor("aff_own", [NE, NXO], F32)
    aff_all_t = nc.dram_tensor("aff_all", [2 * NE, NXO], F32)
    acc_t = nc.dram_tensor("acc", [2 * NXO, D], F32)
    rs_out_t = nc.dram_tensor("rs_out", [NXO, D], F32)
    h2b_all, aff_own, aff_all, acc, rs_out = (t.ap() for t in (h2b_all_t, aff_own_t, aff_all_t, acc_t, rs_out_t))
    PAIRS = [[0, 1], [2, 3], [4, 5], [6, 7]]
    h2_s = dscr("h2_s", [NX, D])

    sems = {e: es.enter_context(nc.semaphore("s_" + e)) for e in ENG}
    dsems = {e: [es.enter_context(nc.semaphore("d_%s%d" % (e, k))) for k in range(8)]
             for e in ("sp", "act", "pool")}
    P = Prog(nc, sems, dsems)
    P.ccsem = es.enter_context(nc.semaphore("cc_sem"))

    def sb(stack, name, shape, dt=F32):
        return stack.enter_context(nc.sbuf_tensor("t_" + name, list(shape), dt))

    def ps(stack, name, shape=(128, 512), dt=F32):
        return stack.enter_context(nc.psum_tensor("p_" + name, list(shape), dt))

    ident = sb(es, "ident", [128, 128])
    modx = sb(es, "modx", [128, 6 * D])
    es1 = ExitStack()
    modc = sb(es1, "modc", [128, 2 * D])
    P.dmaop("sp", lambda e: e.dma_start(out=ident[:], in_=ident_d), w=["ident"])

    with ExitStack() as st:
        cT = sb(st, "cT", [128, 2, 8])
        sc = sb(st, "sc", [128, 2, 8])
        lbc = sb(st, "lbc", [128, 2, 8, 128], BF16)
        wa = sb(st, "wa", [128, 2, 8, 512], BF16)
        bb = sb(st, "bb", [128, 6 * D])
        g1b = sb(st, "g1b", [128, D])
        g2b = sb(st, "g2b", [128, D])
        pm = [ps(st, "pm%d" % i) for i in range(4)]
        P.dmaop("sp", lambda e: e.dma_start(out=cT[:, 0, :], in_=cb.rearrange("(dc p) -> p dc", p=128),
                                            allow_slow_non_contiguous=True), w=["cT0"])
        P.dmaop("sp", lambda e: e.dma_start(out=cT[:, 1, :], in_=cctx.rearrange("(dc p) -> p dc", p=128),
                                            allow_slow_non_contiguous=True), w=["cT1"])
        P.dmaop("act", lambda e: e.dma_start(out=bb[:], in_=b_ada.partition_broadcast(128)), w=["bb"])
        P.dmaop("act", lambda e: e.dma_start(out=g1b[:], in_=norm1_g.partition_broadcast(128)), w=["g1b"])
        P.dmaop("act", lambda e: e.dma_start(out=g2b[:], in_=norm2_g.partition_broadcast(128)), w=["g2b"])
        P.op("act", lambda e: e.activation(out=sc[:], in_=cT[:], func=AF.Silu), r=["cT0", "cT1"], w=["sc"])
        P.op("dve", lambda e: e.tensor_copy(out=lbc[:], in_=sc[:].unsqueeze(3).to_broadcast([128, 2, 8, 128])),
             r=["sc"], w=["lbc"])
        wv = w_ada.rearrange("(dc p) n -> p dc n", p=128)
        for ct in range(12):
            s = ct % 2
            P.dmaop("pool",
                    lambda e, ct=ct, s=s: e.dma_start(out=wa[:, s], in_=wv[:, :, ct * 512:(ct + 1) * 512]),
                    w=[("wa", s)])
            for which in range(2 if ct < 4 else 1):
                pt = pm[(ct * 2 + which) % 4]
                pk = ("pm", (ct * 2 + which) % 4)

                def mm(e, pt=pt, s=s, which=which):
                    ins = None
                    for dc in range(8):
                        ins = e.matmul(pt[:], lhsT=lbc[:, which, dc, :], rhs=wa[:, s, dc, :],
                                       start=(dc == 0), stop=(dc == 7))
                    return ins
                P.op("pe", mm, r=["lbc", ("wa", s)], w=[pk])
                dst = modx if which == 0 else modc
                P.op("dve", lambda e, pt=pt, dst=dst, ct=ct: e.tensor_tensor(
                    out=dst[:, ct * 512:(ct + 1) * 512], in0=pt[:], in1=bb[:, ct * 512:(ct + 1) * 512], op=ALU.add),
                    r=[pk, "bb"], w=["modx" if which == 0 else "modc"])
        P.op("dve", lambda e: e.scalar_tensor_tensor(out=modx[:, D:2 * D], in0=modx[:, D:2 * D], scalar=1.0, in1=g1b[:],
                                                     op0=ALU.add, op1=ALU.mult), r=["modx", "g1b"], w=["modx"])
        P.op("dve", lambda e: e.scalar_tensor_tensor(out=modx[:, 4 * D:5 * D], in0=modx[:, 4 * D:5 * D], scalar=1.0, in1=g2b[:],
                                                     op0=ALU.add, op1=ALU.mult), r=["modx", "g2b"], w=["modx"])
        P.op("dve", lambda e: e.scalar_tensor_tensor(out=modc[:, D:2 * D], in0=modc[:, D:2 * D], scalar=1.0, in1=g1b[:],
                                                     op0=ALU.add, op1=ALU.mult), r=["modc", "g1b"], w=["modc"])
        P.flush()


    with ExitStack() as st:
      if debug != 1:
          TL1 = [dict(), dict()]
          w_in_sb = sb(st, "w_in_sb", [128, 8, IN_COLS], BF16)
          w_qb_sb = sb(st, "w_qb_sb", [128, 3, 768], BF16)
          w_kvb_sb = sb(st, "w_kvb_sb", [128, 2, 1024], BF16)
          qag = sb(st, "qag", [128, 384])
          kvag = sb(st, "kvag", [128, 256])
          qng = sb(st, "qng", [128, 96])
          kng = sb(st, "kng", [128, 96])
          identb = sb(st, "identb", [128, 128], BF16)
          xt = sb(st, "xt", [128, 2, D])
          TL1[0]['junk'] = sb(st, "junk_0", [128, D]); TL1[1]['junk'] = sb(st, "junk_1", [128, D])
          TL1[0]['hh'] = sb(st, "hh_0", [128, D]); TL1[1]['hh'] = sb(st, "hh_1", [128, D])
          TL1[0]['hb'] = sb(st, "hb_0", [128, D], BF16); TL1[1]['hb'] = sb(st, "hb_1", [128, D], BF16)
          TL1[0]['hT'] = sb(st, "hT_0", [128, 8, 128], BF16); TL1[1]['hT'] = sb(st, "hT_1", [128, 8, 128], BF16)
          proj = sb(st, "proj", [128, 2, IN_COLS])
          st8 = sb(st, "st8", [128, 2, 64])
          TL1[0]['qn'] = sb(st, "qn_0", [128, 384], BF16); TL1[1]['qn'] = sb(st, "qn_1", [128, 384], BF16)
          TL1[0]['qnT'] = sb(st, "qnT_0", [128, 3, 128], BF16); TL1[1]['qnT'] = sb(st, "qnT_1", [128, 3, 128], BF16)
          TL1[0]['kvn'] = sb(st, "kvn_0", [128, 256], BF16); TL1[1]['kvn'] = sb(st, "kvn_1", [128, 256], BF16)
          TL1[0]['kvnT'] = sb(st, "kvnT_0", [128, 2, 128], BF16); TL1[1]['kvnT'] = sb(st, "kvnT_1", [128, 2, 128], BF16)
          TL1[0]['qsq'] = sb(st, "qsq_0", [128, 768]); TL1[1]['qsq'] = sb(st, "qsq_1", [128, 768])
          TL1[0]['qf'] = sb(st, "qf_0", [128, 8, 96]); TL1[1]['qf'] = sb(st, "qf_1", [128, 8, 96])
          TL1[0]['qb'] = sb(st, "qb_0", [128, 8, 96], BF16); TL1[1]['qb'] = sb(st, "qb_1", [128, 8, 96], BF16)
          TL1[0]['kf'] = sb(st, "kf_0", [128, 8, 96]); TL1[1]['kf'] = sb(st, "kf_1", [128, 8, 96])
          TL1[0]['kb'] = sb(st, "kb_0", [128, 8, 96], BF16); TL1[1]['kb'] = sb(st, "kb_1", [128, 8, 96], BF16)
          vb = sb(st, "vb", [128, 2, 512], BF16)
          TL1[0]['kvs'] = sb(st, "kvs_0", [128, 1024]); TL1[1]['kvs'] = sb(st, "kvs_1", [128, 1024])
          rp = sb(st, "rp", [128, 2, 32])
          TL1[0]['rt'] = sb(st, "rt_0", [128, 6, 128]); TL1[1]['rt'] = sb(st, "rt_1", [128, 6, 128])
          TL1[0]['krg'] = sb(st, "krg_0", [128, 32]); TL1[1]['krg'] = sb(st, "krg_1", [128, 32])
          TL1[0]['krr'] = sb(st, "krr_0", [128, 32]); TL1[1]['krr'] = sb(st, "krr_1", [128, 32])
          TL1[0]['qTt'] = sb(st, "qTt_0", [128, 8, 128], BF16); TL1[1]['qTt'] = sb(st, "qTt_1", [128, 8, 128], BF16)
          TL1[0]['kTt'] = sb(st, "kTt_0", [128, 8, 128], BF16); TL1[1]['kTt'] = sb(st, "kTt_1", [128, 8, 128], BF16)
          PSALL = [ps(st, "ph1_%d" % i) for i in range(8)]

          P.dmaop("pool", lambda e: e.dma_start(out=w_in_sb[:], in_=w_in.rearrange("(dc p) n -> p dc n", p=128)), w=["w_in_sb"])
          P.dmaop("pool", lambda e: e.dma_start(out=w_qb_sb[:], in_=w_qb.rearrange("(dc p) n -> p dc n", p=128)), w=["w_qb_sb"])
          P.dmaop("pool", lambda e: e.dma_start(out=w_kvb_sb[:], in_=w_kvb.rearrange("(dc p) n -> p dc n", p=128)), w=["w_kvb_sb"])
          P.dmaop("act", lambda e: e.dma_start(out=qag[:], in_=q_a_g.partition_broadcast(128)), w=["qag"])
          P.dmaop("act", lambda e: e.dma_start(out=kvag[:], in_=kv_a_g.partition_broadcast(128)), w=["kvag"])
          P.dmaop("act", lambda e: e.dma_start(out=qng[:], in_=q_norm_g.partition_broadcast(128)), w=["qng"])
          P.dmaop("act", lambda e: e.dma_start(out=kng[:], in_=k_norm_g.partition_broadcast(128)), w=["kng"])
          P.op("dve", lambda e: e.tensor_scalar(out=qng[:], in0=qng[:], scalar1=float(QK ** -0.5), scalar2=None, op0=ALU.mult),
               r=["qng"], w=["qng"])
          P.op("dve", lambda e: e.tensor_copy(out=identb[:], in_=ident[:]), r=["ident"], w=["identb"])

          def rstd(Pq, src_key, src_ap, n, dst_ap, dst_key):
              Pq.op("dve", lambda e: e.tensor_scalar(out=dst_ap, in0=src_ap, scalar1=1.0 / n, scalar2=EPS, op0=ALU.mult, op1=ALU.add),
                   r=[src_key], w=[dst_key])
              Pq.op("act", lambda e: e.activation(out=dst_ap, in_=dst_ap, func=AF.Sqrt), r=[dst_key], w=[dst_key])
              Pq.op("dve", lambda e: e.reciprocal(out=dst_ap, in_=dst_ap), r=[dst_key], w=[dst_key])

          def rope(Pq, rt, src, dst, tab, nh, tag, eng="pool"):
              sv = src.rearrange("p h (a t) -> p h a t", a=2)
              dv = dst.rearrange("p h (a t) -> p h a t", a=2)
              cosb = tab[:, 0:16].rearrange("p (a t) -> p a t", a=2).unsqueeze(1).to_broadcast([128, nh, 2, 8])
              sinb = tab[:, 16:32].rearrange("p (a t) -> p a t", a=2).unsqueeze(1).to_broadcast([128, nh, 2, 8])
              v0 = sv[:, :, :, 0:8]
              v1 = sv[:, :, :, 8:16]
              n = nh * 16
              T = [rt[:, k, 0:n].rearrange("p (h a t) -> p h a t", h=nh, a=2) for k in range(4)]
              rk = ("rt", tag)
              Pq.op(eng, lambda e: e.tensor_tensor(out=T[0], in0=v0, in1=cosb, op=ALU.mult), r=[tag + "_src", tag + "_tab"], w=[rk + (0,)])
              Pq.op(eng, lambda e: e.tensor_tensor(out=T[1], in0=v1, in1=sinb, op=ALU.mult), r=[tag + "_src", tag + "_tab"], w=[rk + (1,)])
              Pq.op(eng, lambda e: e.tensor_tensor(out=T[2], in0=v1, in1=cosb, op=ALU.mult), r=[tag + "_src", tag + "_tab"], w=[rk + (2,)])
              Pq.op(eng, lambda e: e.tensor_tensor(out=T[3], in0=v0, in1=sinb, op=ALU.mult), r=[tag + "_src", tag + "_tab"], w=[rk + (3,)])
              Pq.op(eng, lambda e: e.tensor_tensor(out=dv[:, :, :, 0:8], in0=T[0], in1=T[1], op=ALU.subtract),
                   r=[rk + (0,), rk + (1,)], w=[tag + "_dst"])
              Pq.op(eng, lambda e: e.tensor_tensor(out=dv[:, :, :, 8:16], in0=T[2], in1=T[3], op=ALU.add),
                   r=[rk + (2,), rk + (3,)], w=[tag + "_dst"])

          ntile1 = NTe
          import os
          STG = int(os.environ.get('PH1_STAGE', '9'))
          SHARED1 = ["w_in_sb", "w_qb_sb", "w_kvb_sb", "qag", "kvag", "qng", "kng", "identb", "ident", "modx", "modc", "u_s", "gates_s", "qT_s", "kT_s", "v_s"]
          ALIAS1 = {("ps", 1): ("ps", 0), "ps3": "ps2", ("ps", 6): ("ps", 4), ("ps", 7): ("ps", 5)}

          def tile1(i, s):
              Pq = Keyed(P, s, SHARED1, ALIAS1)
              junk = TL1[s]['junk']
              hh = TL1[s]['hh']
              hb = TL1[s]['hb']
              hT = TL1[s]['hT']
              qn = TL1[s]['qn']
              qnT = TL1[s]['qnT']
              kvn = TL1[s]['kvn']
              kvnT = TL1[s]['kvnT']
              qsq = TL1[s]['qsq']
              qf = TL1[s]['qf']
              qb = TL1[s]['qb']
              kf = TL1[s]['kf']
              kb = TL1[s]['kb']
              kvs = TL1[s]['kvs']
              rt = TL1[s]['rt']
              krg = TL1[s]['krg']
              krr = TL1[s]['krr']
              qTt = TL1[s]['qTt']
              kTt = TL1[s]['kTt']
              bk = PSALL[4 * s:4 * s + 4]
              PS = [bk[0], bk[0], bk[1], bk[1], bk[2], bk[3], bk[2], bk[3]]
              PSb2 = PS[2][:].bitcast(BF16)
              PSb3 = PS[3][:].bitcast(BF16)
              isx = i >= 2
              own = 2 <= i < 2 + NTO
              t0 = i * 128
              xq = t0 - NCTX
              G = (modx if isx else modc)[:, D:2 * D]
              SH = (modx if isx else modc)[:, 0:D]
              mk = "modx" if isx else "modc"
              Pq.dmaop("sp", lambda e, s=s, t0=t0: e.dma_start(out=xt[:, s, :], in_=xc[t0:t0 + 128, :]), w=[("xt", s)])
              Pq.dmaop("sp", lambda e, s=s, t0=t0: e.dma_start(out=rp[:, s, :], in_=rope_d[t0:t0 + 128, :]), w=[("rp", s)])
              Pq.op("act", lambda e, s=s: e.activation(out=junk[:], in_=xt[:, s, :], func=AF.Square, accum_out=st8[:, s, 0:1]),
                   r=[("xt", s)], w=["junk", ("st", s, 0)])
              rstd(Pq, ("st", s, 0), st8[:, s, 0:1], D, st8[:, s, 1:2], ("st", s, 1))
              Pq.op("dve", lambda e, s=s, G=G: e.scalar_tensor_tensor(out=hh[:], in0=xt[:, s, :], scalar=st8[:, s, 1:2], in1=G,
                                                                  op0=ALU.mult, op1=ALU.mult),
                   r=[("xt", s), ("st", s, 1), mk], w=["hh"])
              Pq.op("pool", lambda e, SH=SH: e.tensor_tensor(out=hb[:], in0=hh[:], in1=SH, op=ALU.add), r=["hh", mk], w=["hb"])

              def tr_h(e):
                  ins = None
                  for dc in range(8):
                      ins = e.transpose(out=PSb2[:, dc * 128:(dc + 1) * 128], in_=hb[:, dc * 128:(dc + 1) * 128], identity=identb[:])
                  return ins
              Pq.op("pe", tr_h, r=["hb", "identb"], w=["ps2"])
              Pq.op("act", lambda e: e.copy(out=hT[:].rearrange("p a b -> p (a b)"), in_=PSb2[:, 0:1024]), r=["ps2"], w=["hT"])
              for ctile in range(7):
                  c0 = ctile * 512
                  n = min(512, IN_COLS - c0)
                  pk = ctile % 2

                  def mmin(e, c0=c0, n=n, pk=pk):
                      ins = None
                      for dc in range(8):
                          ins = e.matmul(PS[pk][:, 0:n], lhsT=hT[:, dc, :], rhs=w_in_sb[:, dc, c0:c0 + n], start=(dc == 0), stop=(dc == 7))
                      return ins
                  Pq.op("pe", mmin, r=["hT", "w_in_sb"], w=[("ps", pk)])
                  if ctile % 2 == 0:
                      Pq.op("dve", lambda e, c0=c0, n=n, pk=pk, s=s: e.tensor_copy(out=proj[:, s, c0:c0 + n], in_=PS[pk][:, 0:n]),
                           r=[("ps", pk)], w=[("proj", s, ctile)])
                  else:
                      Pq.op("act", lambda e, c0=c0, n=n, pk=pk, s=s: e.copy(out=proj[:, s, c0:c0 + n], in_=PS[pk][:, 0:n]),
                           r=[("ps", pk)], w=[("proj", s, ctile)])
              pj = [("proj", s, c) for c in range(7)]
              Pq.dmaop("sp", lambda e, s=s, t0=t0: e.dma_start(out=u_s[t0:t0 + 128, :], in_=proj[:, s, 0:512]), r=[pj[0]], w=["u_s"])
              if own:
                  Pq.op("act", lambda e, s=s: e.activation(out=proj[:, s, 1184:3232], in_=proj[:, s, 1184:3232], func=AF.Sigmoid),
                        r=pj[2:], w=pj[2:])
                  Pq.dmaop("sp", lambda e, xq=xq, s=s: e.dma_start(out=gates_s[xq:xq + 128, :], in_=proj[:, s, 1184:3232]), r=pj[2:], w=["gates_s"])
                  Pq.op("act", lambda e, s=s: e.activation(out=junk[:, 0:384], in_=proj[:, s, 512:896], func=AF.Square,
                                                          accum_out=st8[:, s, 2:3]), r=[pj[1]], w=["junk", ("st", s, 2)])
                  rstd(Pq, ("st", s, 2), st8[:, s, 2:3], 384, st8[:, s, 3:4], ("st", s, 3))
                  Pq.op("dve", lambda e, s=s: e.scalar_tensor_tensor(out=qn[:], in0=proj[:, s, 512:896], scalar=st8[:, s, 3:4], in1=qag[:],
                                                                    op0=ALU.mult, op1=ALU.mult), r=[pj[1], ("st", s, 3), "qag"], w=["qn"])

                  def tr_q(e):
                      ins = None
                      for k in range(3):
                          ins = e.transpose(out=PSb2[:, k * 128:(k + 1) * 128], in_=qn[:, k * 128:(k + 1) * 128], identity=identb[:])
                      return ins
                  Pq.op("pe", tr_q, r=["qn", "identb"], w=["ps2"])
                  Pq.op("dve", lambda e: e.tensor_copy(out=qnT[:].rearrange("p a b -> p (a b)"), in_=PSb2[:, 0:384]), r=["ps2"], w=["qnT"])

                  def mmq(e):
                      ins = None
                      for (pi, c0, n) in ((4, 0, 512), (5, 512, 256)):
                          for k in range(3):
                              ins = e.matmul(PS[pi][:, 0:n], lhsT=qnT[:, k, :], rhs=w_qb_sb[:, k, c0:c0 + n], start=(k == 0), stop=(k == 2))
                      return ins
                  Pq.op("pe", mmq, r=["qnT", "w_qb_sb"], w=[("ps", 4), ("ps", 5)])
                  qfl = qf[:].rearrange("p h d -> p (h d)")
                  Pq.op("act", lambda e: e.copy(out=qfl[:, 0:512], in_=PS[4][:, 0:512]), r=[("ps", 4)], w=["qf"])
                  Pq.op("act", lambda e: e.copy(out=qfl[:, 512:768], in_=PS[5][:, 0:256]), r=[("ps", 5)], w=["qf"])
                  Pq.op("pool", lambda e: e.tensor_tensor(out=qsq[:], in0=qfl, in1=qfl, op=ALU.mult), r=["qf"], w=["qsq"])
                  Pq.op("dve", lambda e, s=s: e.tensor_reduce(out=st8[:, s, 8:16], in_=qsq[:].rearrange("p (h d) -> p h d", h=8),
                                                             axis=AX.X, op=ALU.add), r=["qsq"], w=[("st", s, 8)])
                  rstd(Pq, ("st", s, 8), st8[:, s, 8:16], QK, st8[:, s, 16:24], ("st", s, 16))
                  Pq.op("dve", lambda e, s=s: e.tensor_tensor(out=qf[:], in0=qf[:], in1=st8[:, s, 16:24].unsqueeze(2).to_broadcast([128, 8, 96]),
                                                             op=ALU.mult), r=["qf", ("st", s, 16)], w=["qf"])
                  Pq.op("dve", lambda e: e.tensor_tensor(out=qf[:], in0=qf[:], in1=qng[:].unsqueeze(1).to_broadcast([128, 8, 96]),
                                                        op=ALU.mult), r=["qf", "qng"], w=["qf", "q_src"])
                  Pq.op("act", lambda e: e.copy(out=qb[:, :, 0:64], in_=qf[:, :, 0:64]), r=["qf"], w=["qb"])
                  Pq.op("pool", lambda e, s=s: e.tensor_copy(out=rt[:, 5, 0:32], in_=rp[:, s, :]), r=[("rp", s)], w=["q_tab"])
                  rope(Pq, rt, qf[:, :, 64:96], qb[:, :, 64:96], rt[:, 5, 0:32], 8, "q")

                  def tr_qh(e):
                      ins = None
                      for h in range(8):
                          ins = e.transpose(out=PSb3[0:96, h * 128:(h + 1) * 128], in_=qb[:, h, :], identity=identb[:])
                      return ins
                  Pq.op("pe", tr_qh, r=["qb", "q_dst", "identb"], w=["ps3"])
                  Pq.op("act", lambda e: e.copy(out=qTt[0:96].rearrange("p a b -> p (a b)"), in_=PSb3[0:96, 0:1024]), r=["ps3"], w=["qTt"])
                  Pq.dmaop("act", lambda e, xq=xq: e.dma_start(out=qT_s[:, :, xq:xq + 128].rearrange("h d t -> d h t"), in_=qTt[0:96]),
                          r=["qTt"], w=["qT_s"])
              Pq.op("act", lambda e, s=s: e.activation(out=junk[:, 0:256], in_=proj[:, s, 896:1152], func=AF.Square,
                                                      accum_out=st8[:, s, 4:5]), r=[pj[1], pj[2]], w=["junk", ("st", s, 4)])
              rstd(Pq, ("st", s, 4), st8[:, s, 4:5], 256, st8[:, s, 5:6], ("st", s, 5))
              Pq.op("dve", lambda e, s=s: e.scalar_tensor_tensor(out=kvn[:], in0=proj[:, s, 896:1152], scalar=st8[:, s, 5:6], in1=kvag[:],
                                                                op0=ALU.mult, op1=ALU.mult), r=[pj[1], pj[2], ("st", s, 5), "kvag"], w=["kvn"])

              def tr_kv(e):
                  ins = None
                  for k in range(2):
                      ins = e.transpose(out=PSb2[:, k * 128:(k + 1) * 128], in_=kvn[:, k * 128:(k + 1) * 128], identity=identb[:])
                  return ins
              Pq.op("pe", tr_kv, r=["kvn", "identb"], w=["ps2"])
              Pq.op("dve", lambda e: e.tensor_copy(out=kvnT[:].rearrange("p a b -> p (a b)"), in_=PSb2[:, 0:256]), r=["ps2"], w=["kvnT"])

              def mmkv(e):
                  ins = None
                  for (pi, c0) in ((6, 0), (7, 512)):
                      for k in range(2):
                          ins = e.matmul(PS[pi][:, 0:512], lhsT=kvnT[:, k, :], rhs=w_kvb_sb[:, k, c0:c0 + 512], start=(k == 0), stop=(k == 1))
                  return ins
              Pq.op("pe", mmkv, r=["kvnT", "w_kvb_sb"], w=[("ps", 6), ("ps", 7)])
              for half in range(2):
                  (Pq.op("act", lambda e, half=half: e.copy(out=kvs[:, half * 512:(half + 1) * 512], in_=PS[6 + half][:, 0:512]),
                        r=[("ps", 6 + half)], w=[("kvs", half)]) if half == 0 else
                   Pq.op("dve", lambda e, half=half: e.tensor_copy(out=kvs[:, half * 512:(half + 1) * 512], in_=PS[6 + half][:, 0:512]),
                        r=[("ps", 6 + half)], w=[("kvs", half)]))
              kvs3 = kvs[:].rearrange("p (h d) -> p h d", h=8)
              kvk = [("kvs", 0), ("kvs", 1)]
              Pq.op("pool", lambda e, s=s: e.tensor_copy(out=vb[:, s].rearrange("p (h d) -> p h d", h=8), in_=kvs3[:, :, 64:128]),
                   r=kvk, w=[("vb", s)])
              Pq.dmaop("sp", lambda e, s=s, t0=t0: e.dma_start(out=v_s[t0:t0 + 128, :], in_=vb[:, s]),
                      r=[("vb", s)], w=["v_s"])
              Pq.op("act", lambda e: e.copy(out=kf[:, :, 0:64], in_=kvs3[:, :, 0:64]), r=kvk, w=[("kf", 0), ("kf", 1)])
              kfk = [("kf", 0), ("kf", 1)]
              Pq.op("pool", lambda e: e.tensor_tensor(out=qsq[:, 0:512].rearrange("p (h d) -> p h d", h=8), in0=kf[:, :, 0:64], in1=kf[:, :, 0:64],
                                                     op=ALU.mult), r=kfk, w=["qsq"])
              Pq.op("dve", lambda e, s=s: e.tensor_reduce(out=st8[:, s, 24:32], in_=qsq[:, 0:512].rearrange("p (h d) -> p h d", h=8),
                                                         axis=AX.X, op=ALU.add), r=["qsq"], w=[("st", s, 24)])
              Pq.op("act", lambda e, s=s: e.activation(out=junk[:, 0:32], in_=proj[:, s, 1152:1184], func=AF.Square,
                                                      accum_out=st8[:, s, 6:7]), r=[pj[2]], w=["junk", ("st", s, 6)])
              Pq.op("dve", lambda e, s=s: e.tensor_scalar(out=st8[:, s, 24:32], in0=st8[:, s, 24:32], scalar1=st8[:, s, 6:7], scalar2=None,
                                                         op0=ALU.add), r=[("st", s, 24), ("st", s, 6)], w=[("st", s, 24)])
              rstd(Pq, ("st", s, 24), st8[:, s, 24:32], QK, st8[:, s, 32:40], ("st", s, 32))
              Pq.op("dve", lambda e, s=s: e.tensor_tensor(out=kf[:, :, 0:64], in0=kf[:, :, 0:64],
                                                         in1=st8[:, s, 32:40].unsqueeze(2).to_broadcast([128, 8, 64]), op=ALU.mult),
                   r=kfk + [("st", s, 32)], w=kfk)
              Pq.op("dve", lambda e: e.tensor_tensor(out=kb[:, :, 0:64], in0=kf[:, :, 0:64],
                                                    in1=kng[:, 0:64].unsqueeze(1).to_broadcast([128, 8, 64]), op=ALU.mult),
                   r=kfk + ["kng"], w=["kb"])
              Pq.op("pool", lambda e, s=s: e.tensor_tensor(out=krg[:], in0=proj[:, s, 1152:1184], in1=kng[:, 64:96], op=ALU.mult),
                   r=[pj[2], "kng"], w=["krg", "k_src"])
              Pq.op("pool", lambda e, s=s: e.tensor_copy(out=rt[:, 4, 0:32], in_=rp[:, s, :]), r=[("rp", s)], w=["k_tab"])
              rope(Pq, rt, krg[:].unsqueeze(1), krr[:].unsqueeze(1), rt[:, 4, 0:32], 1, "k")
              Pq.op("dve", lambda e, s=s: e.tensor_tensor(out=kb[:, :, 64:96], in0=krr[:].unsqueeze(1).to_broadcast([128, 8, 32]),
                                                         in1=st8[:, s, 32:40].unsqueeze(2).to_broadcast([128, 8, 32]), op=ALU.mult),
                   r=["k_dst", ("st", s, 32)], w=["kb"])

              def tr_kh(e):
                  ins = None
                  for h in range(8):
                      ins = e.transpose(out=PSb3[0:96, h * 128:(h + 1) * 128], in_=kb[:, h, :], identity=identb[:])
                  return ins
              Pq.op("pe", tr_kh, r=["kb", "identb"], w=["ps3"])
              Pq.op("dve", lambda e: e.tensor_copy(out=kTt[0:96].rearrange("p a b -> p (a b)"), in_=PSb3[0:96, 0:1024]), r=["ps3"], w=["kTt"])
              Pq.dmaop("act", lambda e, t0=t0: e.dma_start(out=kT_s[:, :, t0:t0 + 128].rearrange("h d t -> d h t"), in_=kTt[0:96]),
                      r=["kTt"], w=["kT_s"])
              return Pq.cap
          for i in range(0, ntile1, 2):
              caps = [tile1(i, 0)] + ([tile1(i + 1, 1)] if i + 1 < ntile1 else [])
              interleave(P, caps, chunk=int(os.environ.get("ILV", "2")))
          P.flush()

    QG = min(512, NXO)
    NQG = NXO // QG

    es1.close()
    NCX = NXe // 8
    NCC = NCTX // 8
    NCHe = NCX + NCC
    HW = NCHe + 2
    if debug in (0, 4, 5, 6):
      with ExitStack() as st:
        BendT = sb(st, "BendT", [128, 2, 32, 2, 64], BF16)
        Dm = sb(st, "Dm", [128, 2, 16, 2, 128], BF16)
        Tloc = sb(st, "Tloc", [128, 32, 128], BF16)
        mu3 = sb(st, "mu3", [128, 2, 2, 16, 2])
        mu16 = sb(st, "mu16", [128, 2, 17, 2, 16, 2])
        PS2 = [ps(st, "ph2_%d" % i) for i in range(8)]
        with ExitStack() as tt:
            lre = sb(tt, "lre", [128, 32]); lim = sb(tt, "lim", [128, 32]); ldt = sb(tt, "ldt", [128, 32])
            bre = sb(tt, "bre", [128, 32, 16]); bim = sb(tt, "bim", [128, 32, 16])
            cre = sb(tt, "cre", [128, 32, 16]); cim = sb(tt, "cim", [128, 32, 16])
            dsk = sb(tt, "dsk", [128, 32])
            cmk = sb(tt, "cmk", [128, 256])
            tm = sb(tt, "tm", [128, 12, 32])
            ti = sb(tt, "ti", [128, 32], I32)
            pwr = sb(tt, "pwr", [128, 9, 32]); pwi = sb(tt, "pwi", [128, 9, 32])
            nwr = sb(tt, "nwr", [128, 9, 32]); nwi = sb(tt, "nwi", [128, 9, 32])
            Bbr = sb(tt, "Bbr", [128, 32, 16]); Bbi = sb(tt, "Bbi", [128, 32, 16])
            MX = [sb(tt, "MX%d" % i, [128, 16, 8, 16]) for i in range(4)]
            MT = [sb(tt, "MT%d" % i, [128, 16, 8, 16]) for i in range(4)]
            TL = sb(tt, "TL", [128, 2, 128])
            for gh in range(2):
                rows = slice(64 * gh, 64 * gh + 64)
                gs = slice(16 * gh, 16 * gh + 16)
                for d in range(2):
                    for (dst, src, nm) in ((lre, lam_re, "lre"), (lim, lam_im, "lim")):
                        P.dmaop("sp", lambda e, dst=dst, src=src, rows=rows, gs=gs, d=d: e.dma_start(
                            out=dst[rows, 16 * d:16 * d + 16], in_=src[d, gs, :].rearrange("g p -> p g"),
                            allow_slow_non_contiguous=True), w=[nm])
                    P.dmaop("sp", lambda e, rows=rows, gs=gs, d=d: e.dma_start(
                        out=ldt[rows, 16 * d:16 * d + 16], in_=log_dt[d, gs].partition_broadcast(64)), w=["ldt"])
                for (dst, src, nm) in ((bre, b_re, "bre"), (bim, b_im, "bim")):
                    for d in range(2):
                        P.dmaop("act", lambda e, dst=dst, src=src, rows=rows, gs=gs, d=d: e.dma_start(
                            out=dst[rows, 16 * d:16 * d + 16, :], in_=src[d, gs, :, :].rearrange("g p h -> p g h")), w=[nm])
                for (dst, src, nm) in ((cre, c_re, "cre"), (cim, c_im, "cim")):
                    for d in range(2):
                        P.dmaop("sp" if d == 0 else "act", lambda e, dst=dst, src=src, rows=rows, gs=gs, d=d: e.dma_start(
                            out=dst[rows, 16 * d:16 * d + 16, :], in_=src[d, gs, :, :].rearrange("g o p -> p g o"),
                            allow_slow_non_contiguous=True), w=[nm])
            for j in range(8):
                P.dmaop("sp", lambda e, j=j: e.dma_start(out=dsk[16 * j:16 * j + 16, :], in_=ssm_d.rearrange("(g h) -> h g", h=16),
                                                        allow_slow_non_contiguous=True), w=["dsk"])
            P.dmaop("sp", lambda e: e.dma_start(out=cmk[:], in_=cmask_d), w=["cmk"])

            K = [0]

            def T_(i):
                return tm[:, i, :]

            def dv(fn, r, w, eng="dve"):
                P.op(eng, fn, r=r, w=w)

            def tt2(out, a, b, op, r, w, eng="dve"):
                P.op(eng, lambda e: e.tensor_tensor(out=out, in0=a, in1=b, op=op), r=r, w=w)

            def ts(out, a, s1, op0, s2=None, op1=None, r=(), w=(), eng="dve"):
                if op1 is None:
                    P.op(eng, lambda e: e.tensor_scalar(out=out, in0=a, scalar1=s1, scalar2=None, op0=op0), r=r, w=w)
                else:
                    P.op(eng, lambda e: e.tensor_scalar(out=out, in0=a, scalar1=s1, scalar2=s2, op0=op0, op1=op1), r=r, w=w)
            PI = math.pi
            P.op("act", lambda e: e.activation(out=T_(0), in_=ldt[:], func=AF.Exp), r=["ldt"], w=["t0"])
            tt2(T_(1), lre[:], T_(0), ALU.mult, ["lre", "t0"], ["t1"])
            tt2(T_(2), lim[:], T_(0), ALU.mult, ["lim", "t0"], ["t2"])
            P.op("act", lambda e: e.activation(out=T_(3), in_=T_(1), func=AF.Exp), r=["t1"], w=["t3"])
            ts(T_(4), T_(2), 1.0 / (2 * PI), ALU.mult, r=["t2"], w=["t4"])
            P.op("dve", lambda e: e.tensor_copy(out=ti[:], in_=T_(4)), r=["t4"], w=["ti"])
            P.op("dve", lambda e: e.tensor_copy(out=T_(4), in_=ti[:]), r=["ti"], w=["t4"])
            P.op("dve", lambda e: e.scalar_tensor_tensor(out=T_(5), in0=T_(4), scalar=-2 * PI, in1=T_(2), op0=ALU.mult, op1=ALU.add),
                 r=["t4", "t2"], w=["t5"])
            for (src_i, dst_i) in ((5, 5),):
                ts(T_(6), T_(5), PI, ALU.is_gt, -2 * PI, ALU.mult, r=["t5"], w=["t6"])
                tt2(T_(5), T_(5), T_(6), ALU.add, ["t5", "t6"], ["t5"])
                ts(T_(6), T_(5), -PI, ALU.is_lt, 2 * PI, ALU.mult, r=["t5"], w=["t6"])
                tt2(T_(5), T_(5), T_(6), ALU.add, ["t5", "t6"], ["t5"])
            ts(T_(7), T_(5), PI / 2, ALU.add, r=["t5"], w=["t7"])
            ts(T_(6), T_(7), PI, ALU.is_gt, -2 * PI, ALU.mult, r=["t7"], w=["t6"])
            tt2(T_(7), T_(7), T_(6), ALU.add, ["t7", "t6"], ["t7"])
            P.op("act", lambda e: e.activation(out=T_(8), in_=T_(5), func=AF.Sin), r=["t5"], w=["t8"])
            P.op("act", lambda e: e.activation(out=T_(9), in_=T_(7), func=AF.Sin), r=["t7"], w=["t9"])
            P.op("pool", lambda e: e.memset(pwr[:, 0, :], 1.0), w=[("pw", 0)])
            P.op("pool", lambda e: e.memset(pwi[:, 0, :], 0.0), w=[("pw", 0)])
            tt2(pwr[:, 1, :], T_(3), T_(9), ALU.mult, ["t3", "t9"], [("pw", 1)])
            tt2(pwi[:, 1, :], T_(3), T_(8), ALU.mult, ["t3", "t8"], [("pw", 1)])
            for k in range(2, 9):
                a_r, a_i = pwr[:, k - 1, :], pwi[:, k - 1, :]
                tt2(T_(10), a_r, pwr[:, 1, :], ALU.mult, [("pw", k - 1), ("pw", 1)], ["t10"])
                tt2(T_(11), a_i, pwi[:, 1, :], ALU.mult, [("pw", k - 1), ("pw", 1)], ["t11"])
                tt2(pwr[:, k, :], T_(10), T_(11), ALU.subtract, ["t10", "t11"], [("pw", k)])
                tt2(T_(10), a_r, pwi[:, 1, :], ALU.mult, [("pw", k - 1), ("pw", 1)], ["t10"])
                tt2(T_(11), a_i, pwr[:, 1, :], ALU.mult, [("pw", k - 1), ("pw", 1)], ["t11"])
                tt2(pwi[:, k, :], T_(10), T_(11), ALU.add, ["t10", "t11"], [("pw", k)])
            pwk = [("pw", k) for k in range(9)]
            tt2(nwr[:], pwr[:], pwr[:], ALU.mult, pwk, ["nwr"])
            tt2(nwi[:], pwi[:], pwi[:], ALU.mult, pwk, ["nwi"])
            tt2(nwr[:], nwr[:], nwi[:], ALU.add, ["nwr", "nwi"], ["nwr"])
            P.op("dve", lambda e: e.reciprocal(out=nwr[:], in_=nwr[:]), r=["nwr"], w=["nwr"])
            P.op("dve", lambda e: e.scalar_tensor_tensor(out=nwi[:], in0=pwi[:], scalar=-1.0, in1=nwr[:], op0=ALU.mult, op1=ALU.mult),
                 r=pwk + ["nwr"], w=["nwi"])
            tt2(nwr[:], pwr[:], nwr[:], ALU.mult, pwk + ["nwr", "nwi"], ["nwr"])
            for d in range(2):
                dsl = slice(16 * d, 16 * d + 16)
                for pl in range(2):
                    P.op("dve", lambda e, d=d, pl=pl, dsl=dsl: e.tensor_copy(out=mu3[:, d, 0, :, pl], in_=pwr[:, 8, dsl]), r=pwk, w=["mu3"])
                P.op("dve", lambda e, d=d, dsl=dsl: e.tensor_scalar(out=mu3[:, d, 1, :, 0], in0=pwi[:, 8, dsl], scalar1=-1.0, scalar2=None, op0=ALU.mult),
                     r=pwk, w=["mu3"])
                P.op("dve", lambda e, d=d, dsl=dsl: e.tensor_copy(out=mu3[:, d, 1, :, 1], in_=pwi[:, 8, dsl]), r=pwk, w=["mu3"])
            q16r = sb(tt, "q16r", [128, 17, 32]); q16i = sb(tt, "q16i", [128, 17, 32])
            P.op("pool", lambda e: e.memset(q16r[:, 0, :], 1.0), w=[("q16", 0)])
            P.op("pool", lambda e: e.memset(q16i[:, 0, :], 0.0), w=[("q16", 0)])
            P.op("pool", lambda e: e.tensor_copy(out=q16r[:, 1, :], in_=pwr[:, 8, :]), r=pwk, w=[("q16", 1)])
            P.op("pool", lambda e: e.tensor_copy(out=q16i[:, 1, :], in_=pwi[:, 8, :]), r=pwk, w=[("q16", 1)])
            for k in range(2, 17):
                a_r, a_i = q16r[:, k - 1, :], q16i[:, k - 1, :]
                tt2(T_(10), a_r, q16r[:, 1, :], ALU.mult, [("q16", k - 1), ("q16", 1)], ["t10"])
                tt2(T_(11), a_i, q16i[:, 1, :], ALU.mult, [("q16", k - 1), ("q16", 1)], ["t11"])
                tt2(q16r[:, k, :], T_(10), T_(11), ALU.subtract, ["t10", "t11"], [("q16", k)])
                tt2(T_(10), a_r, q16i[:, 1, :], ALU.mult, [("q16", k - 1), ("q16", 1)], ["t10"])
                tt2(T_(11), a_i, q16r[:, 1, :], ALU.mult, [("q16", k - 1), ("q16", 1)], ["t11"])
                tt2(q16i[:, k, :], T_(10), T_(11), ALU.add, ["t10", "t11"], [("q16", k)])
            q16k = [("q16", k) for k in range(17)]
            for d in range(2):
                dsl = slice(16 * d, 16 * d + 16)
                for pl in range(2):
                    P.op("dve", lambda e, d=d, pl=pl, dsl=dsl: e.tensor_copy(out=mu16[:, d, :, 0, :, pl], in_=q16r[:, :, dsl]), r=q16k, w=["mu16"])
                P.op("dve", lambda e, d=d, dsl=dsl: e.tensor_scalar(out=mu16[:, d, :, 1, :, 0], in0=q16i[:, :, dsl], scalar1=-1.0, scalar2=None, op0=ALU.mult),
                     r=q16k, w=["mu16"])
                P.op("dve", lambda e, d=d, dsl=dsl: e.tensor_copy(out=mu16[:, d, :, 1, :, 1], in_=q16i[:, :, dsl]), r=q16k, w=["mu16"])
            tt2(T_(0), lre[:], lre[:], ALU.mult, ["lre"], ["t0"])
            tt2(T_(1), lim[:], lim[:], ALU.mult, ["lim"], ["t1"])
            tt2(T_(0), T_(0), T_(1), ALU.add, ["t0", "t1"], ["t0"])
            P.op("dve", lambda e: e.reciprocal(out=T_(0), in_=T_(0)), r=["t0"], w=["t0"])
            ts(T_(1), pwr[:, 1, :], -1.0, ALU.add, r=[("pw", 1)], w=["t1"])
            tt2(T_(2), T_(1), lre[:], ALU.mult, ["t1", "lre"], ["t2"])
            tt2(T_(3), pwi[:, 1, :], lim[:], ALU.mult, [("pw", 1), "lim"], ["t3"])
            tt2(T_(2), T_(2), T_(3), ALU.add, ["t2", "t3"], ["t2"])
            tt2(T_(2), T_(2), T_(0), ALU.mult, ["t2", "t0"], ["t2"])
            tt2(T_(3), pwi[:, 1, :], lre[:], ALU.mult, [("pw", 1), "lre"], ["t3"])
            tt2(T_(4), T_(1), lim[:], ALU.mult, ["t1", "lim"], ["t4"])
            tt2(T_(3), T_(3), T_(4), ALU.subtract, ["t3", "t4"], ["t3"])
            tt2(T_(3), T_(3), T_(0), ALU.mult, ["t3", "t0"], ["t3"])
            cfr = T_(2).unsqueeze(2).to_broadcast([128, 32, 16])
            cfi = T_(3).unsqueeze(2).to_broadcast([128, 32, 16])
            tmpA = MT[0][:].rearrange("p a b c -> p (a b c)")[:, 0:512].rearrange("p (g h) -> p g h", h=16)
            tt2(Bbr[:], bre[:], cfr, ALU.mult, ["bre", "t2"], ["Bbr"])
            tt2(tmpA, bim[:], cfi, ALU.mult, ["bim", "t3"], ["MT0"])
            tt2(Bbr[:], Bbr[:], tmpA, ALU.subtract, ["Bbr", "MT0"], ["Bbr"])
            tt2(Bbi[:], bim[:], cfr, ALU.mult, ["bim", "t2"], ["Bbi"])
            tt2(tmpA, bre[:], cfi, ALU.mult, ["bre", "t3"], ["MT0"])
            tt2(Bbi[:], Bbi[:], tmpA, ALU.add, ["Bbi", "MT0"], ["Bbi"])

            def cprod(out_re, out_im, pw_re, pw_im, koff, kstep, vr, vi, d, keys_in, key_out, neg_im=False, eng="dve"):
                dsl = slice(16 * d, 16 * d + 16)
                if kstep == 1:
                    ksl = slice(koff, koff + 8)
                    pr = pw_re[:, ksl, dsl].rearrange("p j g -> p g j").unsqueeze(3).to_broadcast([128, 16, 8, 16])
                    pi = pw_im[:, ksl, dsl].rearrange("p j g -> p g j").unsqueeze(3).to_broadcast([128, 16, 8, 16])
                else:
                    pr = None
                vrb = vr[:, dsl, :].unsqueeze(2).to_broadcast([128, 16, 8, 16])
                vib = vi[:, dsl, :].unsqueeze(2).to_broadcast([128, 16, 8, 16])
                t1 = MT[2][:]
                t2 = MT[3][:]
                tt2(t1, vrb, pr, ALU.mult, keys_in, ["MT2"], eng)
                tt2(t2, vib, pi, ALU.mult, keys_in, ["MT3"], eng)
                tt2(out_re, t1, t2, ALU.subtract, ["MT2", "MT3"], [key_out[0]], eng)
                tt2(t1, vrb, pi, ALU.mult, keys_in, ["MT2"], eng)
                tt2(t2, vib, pr, ALU.mult, keys_in, ["MT3"], eng)
                tt2(out_im, t1, t2, (ALU.add), ["MT2", "MT3"], [key_out[1]], eng)
                if neg_im:
                    ts(out_im, out_im, -1.0, ALU.mult, r=[key_out[1]], w=[key_out[1]], eng=eng)

            dpr = sb(tt, "dpr", [128, 9, 32]); dpi = sb(tt, "dpi", [128, 9, 32])
            for k in range(9):
                P.op("pool", lambda e, k=k: e.tensor_copy(out=dpr[:, k, :], in_=pwr[:, 8 - k, :]), r=pwk, w=["dpr"])
                P.op("pool", lambda e, k=k: e.tensor_copy(out=dpi[:, k, :], in_=pwi[:, 8 - k, :]), r=pwk, w=["dpi"])
            tk = pwk + ["nwr", "nwi", "dpr", "dpi", "Bbr", "Bbi", "cre", "cim"]
            pTL = PS2[0]
            for d in range(2):
                if d == 0:
                    cprod(MX[0][:], MX[1][:], nwr, nwi, 0, 1, Bbr, Bbi, d, tk, ("MX0", "MX1"))
                    cprod(MX[2][:], MX[3][:], pwr, pwi, 0, 1, cre, cim, d, tk, ("MX2", "MX3"), neg_im=True)
                    cprod(MT[0][:], MT[1][:], dpr, dpi, 1, 1, Bbr, Bbi, d, tk, ("MT0", "MT1"))
                else:
                    cprod(MX[0][:], MX[1][:], pwr, pwi, 0, 1, Bbr, Bbi, d, tk, ("MX0", "MX1"))
                    cprod(MX[2][:], MX[3][:], nwr, nwi, 0, 1, cre, cim, d, tk, ("MX2", "MX3"), neg_im=True)
                    P.op("pool", lambda e: e.tensor_copy(out=MT[0][:], in_=MX[0][:]), r=["MX0"], w=["MT0"])
                    P.op("pool", lambda e: e.tensor_copy(out=MT[1][:], in_=MX[1][:]), r=["MX1"], w=["MT1"])
                for pl in range(2):
                    for g in range(32):
                        gh, gp = g // 16, g % 16
                        pb = PS2[2 + (g % 4)]

                        def trb(e, pl=pl, gh=gh, gp=gp, pb=pb):
                            return e.transpose(out=pb[:, 0:64], in_=MT[pl][64 * gh:64 * gh + 64, gp].rearrange("p j h -> p (j h)"),
                                               identity=ident[64 * gh:64 * gh + 64, 64 * gh:64 * gh + 64])
                        P.op("pe", trb, r=["MT0" if pl == 0 else "MT1", "ident"], w=[("p2", 2 + g % 4)])
                        P.op("act" if g % 2 == 0 else "dve",
                             (lambda e, pb=pb, d=d, g=g, pl=pl: e.copy(out=BendT[:, d, g, pl, :], in_=pb[:, 0:64])) if g % 2 == 0 else
                             (lambda e, pb=pb, d=d, g=g, pl=pl: e.tensor_copy(out=BendT[:, d, g, pl, :], in_=pb[:, 0:64])),
                             r=[("p2", 2 + g % 4)], w=["BendT"])
                if d == 0:
                    cprod(MT[0][:], MT[1][:], pwr, pwi, 1, 1, cre, cim, d, tk + ["BendT"], ("MT0", "MT1"), neg_im=True)
                else:
                    cprod(MT[0][:], MT[1][:], dpr, dpi, 0, 1, cre, cim, d, tk + ["BendT"], ("MT0", "MT1"), neg_im=True)
                P.op("act", lambda e, d=d: e.copy(out=Dm[:, d, :, 0, :], in_=MT[0][:].rearrange("p g j o -> p g (j o)")), r=["MT0"], w=["Dm"])
                P.op("act", lambda e, d=d: e.copy(out=Dm[:, d, :, 1, :], in_=MT[1][:].rearrange("p g j o -> p g (j o)")), r=["MT1"], w=["Dm"])
                for g in range(32):
                    gh, gp = g // 16, g % 16
                    rows = slice(64 * gh, 64 * gh + 64)
                    pq = PS2[6 + (g % 2)]

                    def mtl(e, rows=rows, gp=gp, pq=pq):
                        e.matmul(pq[:, 0:128], lhsT=MX[0][rows, gp].rearrange("p j h -> p (j h)"), rhs=MX[2][rows, gp].rearrange("p j h -> p (j h)"),
                                 start=True, stop=False)
                        return e.matmul(pq[:, 0:128], lhsT=MX[1][rows, gp].rearrange("p j h -> p (j h)"),
                                        rhs=MX[3][rows, gp].rearrange("p j h -> p (j h)"), start=False, stop=True)
                    P.op("pe", mtl, r=["MX0", "MX1", "MX2", "MX3"], w=[("p2", 6 + g % 2)])
                    if d == 0:
                        P.op("dve", lambda e, g=g, pq=pq: e.tensor_tensor(out=Tloc[:, g, :], in0=pq[:, 0:128], in1=cmk[:, 0:128], op=ALU.mult),
                             r=[("p2", 6 + g % 2), "cmk"], w=[("Tloc", g)])
                    else:
                        P.op("dve", lambda e, g=g, pq=pq: e.tensor_tensor(out=TL[:, g % 2, :], in0=pq[:, 0:128], in1=cmk[:, 128:256], op=ALU.mult),
                             r=[("p2", 6 + g % 2), "cmk"], w=[("TL", g % 2)])
                        P.op("pool", lambda e, g=g: e.tensor_tensor(out=Tloc[:, g, :], in0=Tloc[:, g, :], in1=TL[:, g % 2, :], op=ALU.add),
                             r=[("Tloc", g), ("TL", g % 2)], w=[("Tloc", g)])
                        P.op("pool", lambda e, g=g: e.scalar_tensor_tensor(out=Tloc[:, g, :], in0=ident[:], scalar=dsk[:, g:g + 1], in1=Tloc[:, g, :],
                                                                          op0=ALU.mult, op1=ALU.add) if False else
                             e.tensor_scalar(out=TL[:, g % 2, :], in0=ident[:], scalar1=dsk[:, g:g + 1], scalar2=None, op0=ALU.mult),
                             r=["ident", "dsk", ("Tloc", g)], w=[("TL", g % 2)])
                        P.op("pool", lambda e, g=g: e.tensor_tensor(out=Tloc[:, g, :], in0=Tloc[:, g, :], in1=TL[:, g % 2, :], op=ALU.add),
                             r=[("Tloc", g), ("TL", g % 2)], w=[("Tloc", g)])
            P.flush()
        U8 = sb(st, "U8", [128, 32, NCHe], BF16)
        Hall = [sb(st, "Hall%d" % d, [128, 16, 2, HW], BF16) for d in range(2)]
        with ExitStack() as tu:
            Uc = sb(tu, "Uc", [128, 1, 4096])
            Ucb = sb(tu, "Ucb", [128, 1, 4096], BF16)
            identb2 = sb(tu, "identb2", [128, 128], BF16)
            P.op("dve", lambda e: e.tensor_copy(out=identb2[:], in_=ident[:]), r=["ident"], w=["identb2"])
            u8v = u_s.rearrange("(c j) f -> c (j f)", j=8)
            nblk = (NCHe + 127) // 128
            for cb in range(nblk):
                c0 = cb * 128
                ncb = min(128, NCHe - c0)
                s = 0
                P.dmaop("sp", lambda e, s=s, c0=c0, ncb=ncb: e.dma_start(out=Uc[0:ncb, s, :], in_=u8v[c0:c0 + ncb, :]), r=["u_s"], w=[("Uc", s)])
                P.op("dve", lambda e, s=s, ncb=ncb: e.tensor_copy(
                    out=Ucb[0:ncb, s, :].rearrange("c (g j h) -> c g j h", g=32, j=8),
                    in_=Uc[0:ncb, s, :].rearrange("c (j g h) -> c g j h", j=8, g=32)), r=[("Uc", s)], w=[("Ucb", s)])
                for g4 in range(8):
                    pb = PS2[g4 % 4]
                    pbb = pb[:].bitcast(BF16)

                    def tru(e, g4=g4, s=s, ncb=ncb, pbb=pbb):
                        ins = None
                        for q in range(4):
                            g = g4 * 4 + q
                            ins = e.transpose(out=pbb[:, q * 128:q * 128 + ncb], in_=Ucb[0:ncb, s, g * 128:(g + 1) * 128],
                                              identity=identb2[0:ncb, 0:ncb])
                        return ins
                    P.op("pe", tru, r=[("Ucb", s), "identb2"], w=[("p2", g4 % 4)])
                    P.op("act" if g4 % 2 == 0 else "dve",
                         (lambda e, g4=g4, c0=c0, ncb=ncb, pbb=pbb: e.copy(
                             out=U8[:, g4 * 4:g4 * 4 + 4, c0:c0 + ncb], in_=pbb[:, 0:512].rearrange("p (q c) -> p q c", q=4)[:, :, 0:ncb]))
                         if g4 % 2 == 0 else
                         (lambda e, g4=g4, c0=c0, ncb=ncb, pbb=pbb: e.tensor_copy(
                             out=U8[:, g4 * 4:g4 * 4 + 4, c0:c0 + ncb], in_=pbb[:, 0:512].rearrange("p (q c) -> p q c", q=4)[:, :, 0:ncb])),
                         r=[("p2", g4 % 4)], w=["U8"])
            P.flush()
        if debug == 4:
            d1 = nc.dram_tensor("dbg_tloc", [128, 32 * 128], BF16, kind="ExternalOutput").ap()
            d2 = nc.dram_tensor("dbg_bendt", [128, 2 * 32 * 2 * 64], BF16, kind="ExternalOutput").ap()
            d3 = nc.dram_tensor("dbg_dm", [128, 2 * 16 * 2 * 128], BF16, kind="ExternalOutput").ap()
            d4 = nc.dram_tensor("dbg_u8", [128, 32 * NCHe], BF16, kind="ExternalOutput").ap()
            d5 = nc.dram_tensor("dbg_mu3", [128, 128], F32, kind="ExternalOutput").ap()
            P.dmaop("sp", lambda e: e.dma_start(out=d1, in_=Tloc[:].rearrange("p a b -> p (a b)")), w=["d1"])
            P.dmaop("sp", lambda e: e.dma_start(out=d2, in_=BendT[:].rearrange("p a b c d -> p (a b c d)")), w=["d2"])
            P.dmaop("sp", lambda e: e.dma_start(out=d3, in_=Dm[:].rearrange("p a b c d -> p (a b c d)")), w=["d3"])
            P.dmaop("sp", lambda e: e.dma_start(out=d4, in_=U8[:].rearrange("p a b -> p (a b)")), w=["d4"])
            P.dmaop("sp", lambda e: e.dma_start(out=d5, in_=mu3[:].rearrange("p a b c d -> p (a b c d)")), w=["d5"])
            P.flush()
            DONE.append(1)

        def hk(d, lo, hi):
            return [("Hall", d, q) for q in range(lo, hi)]
        P.op("pool", lambda e: e.memset(Hall[0][:, :, :, 0:1], 0.0), w=hk(0, 0, 1))
        P.op("pool", lambda e: e.memset(Hall[1][:, :, :, NCHe:NCHe + 1], 0.0), w=hk(1, NCHe, NCHe + 1))
        xlo = [NCC + 1, 0]
        clo = [1, NCX]
        ei = 0
        for d in range(2):
            for pl in range(2):
                pc = PS2[4 + pl]
                for gp in range(16):
                    px = PS2[(d * 32 + pl * 16 + gp) % 4]
                    pk = ("p2", (d * 32 + pl * 16 + gp) % 4)

                    def mms(e, d=d, pl=pl, gp=gp, px=px, pc=pc):
                        ins = None
                        for gh in range(2):
                            g = 16 * gh + gp
                            e.matmul(px[64 * gh:64 * gh + 64, 0:NCX], lhsT=BendT[:, d, g, pl, :], rhs=U8[:, g, NCC:NCC + NCX],
                                     start=True, stop=True, tile_position=(0, 64 * gh))
                            ins = e.matmul(pc[64 * gh:64 * gh + 64, gp * 32:gp * 32 + NCC], lhsT=BendT[:, d, g, pl, :], rhs=U8[:, g, 0:NCC],
                                           start=True, stop=True, tile_position=(0, 64 * gh))
                        return ins
                    P.op("pe", mms, r=["BendT", "U8"], w=[pk, ("p2c", 4 + pl, gp)])
                    dst = Hall[d][:, gp, pl, xlo[d]:xlo[d] + NCX]
                    if ei % 2 == 0:
                        P.op("act", lambda e, dst=dst, px=px: e.copy(out=dst, in_=px[:, 0:NCX]), r=[pk], w=hk(d, xlo[d], xlo[d] + NCX))
                    else:
                        P.op("dve", lambda e, dst=dst, px=px: e.tensor_copy(out=dst, in_=px[:, 0:NCX]), r=[pk], w=hk(d, xlo[d], xlo[d] + NCX))
                    ei += 1
                P.op("act", lambda e, d=d, pl=pl, pc=pc: e.copy(out=Hall[d][:, :, pl, clo[d]:clo[d] + NCC],
                                                                in_=pc[:, 0:512].rearrange("p (g c) -> p g c", g=16)[:, :, 0:NCC]),
                     r=[("p2c", 4 + pl, gp) for gp in range(16)], w=hk(d, clo[d], clo[d] + NCC))
        LB = 16
        NB = NCHe // LB
        assert NB * LB == NCHe
        with ExitStack() as tc:
            Rl = [sb(tc, "Rl_%d" % d, [128, 16, 3, NB + 1]) for d in range(2)]
            TA = [sb(tc, "TA_%d" % d, [128, 16, 2, NB]) for d in range(2)]
            TB = [sb(tc, "TB_%d" % d, [128, 16, 2, NB]) for d in range(2)]
            Ea = Rl
            ET = [sb(tc, "ET_%d" % d, [128, 2, 16, 2]) for d in range(2)]

            def hview(d, i):
                st0 = (1 + i) if d == 0 else (LB - 1 - i)
                return Hall[d][:, :, :, st0:st0 + LB * (NB - 1) + 1:LB]

            def hkeys(d, i):
                st0 = (1 + i) if d == 0 else (LB - 1 - i)
                return [("Hall", d, st0 + LB * m) for m in range(NB)]

            def mub(d, k, which, n):
                return mu16[:, d, k, which].unsqueeze(3).to_broadcast([128, 16, 2, n])
            for d in range(2):
                eng = "dve"
                for i in range(LB):
                    hv = hview(d, i)
                    hk_i = hkeys(d, i)
                    if i == 0:
                        P.op(eng, lambda e, d=d, hv=hv: e.tensor_copy(out=Rl[d][:, :, 0:2, 0:NB], in_=hv), r=hk_i, w=[("Rl", d)])
                    else:
                        P.op(eng, lambda e, d=d: e.tensor_tensor(out=TA[d][:], in0=Rl[d][:, :, 0:2, 0:NB], in1=mub(d, 1, 0, NB), op=ALU.mult),
                             r=[("Rl", d), "mu16"], w=[("TA", d)])
                        P.op(eng, lambda e, d=d: e.tensor_tensor(out=TB[d][:], in0=Rl[d][:, :, 1:3, 0:NB], in1=mub(d, 1, 1, NB), op=ALU.mult),
                             r=[("Rl", d), "mu16"], w=[("TB", d)])
                        P.op(eng, lambda e, d=d: e.tensor_tensor(out=TA[d][:], in0=TA[d][:], in1=TB[d][:], op=ALU.add),
                             r=[("TA", d), ("TB", d)], w=[("TA", d)])
                        P.op(eng, lambda e, d=d, hv=hv: e.tensor_tensor(out=Rl[d][:, :, 0:2, 0:NB], in0=TA[d][:], in1=hv, op=ALU.add),
                             r=[("TA", d)] + hk_i, w=[("Rl", d)])
                        P.op("act", lambda e, d=d, hv=hv: e.copy(out=hv, in_=Rl[d][:, :, 0:2, 0:NB]), r=[("Rl", d)], w=hk_i)
                    if i < LB - 1:
                        P.op(eng, lambda e, d=d: e.tensor_copy(out=Rl[d][:, :, 2, 0:NB], in_=Rl[d][:, :, 0, 0:NB]), r=[("Rl", d)], w=[("Rl", d)])
            for d in range(2):
                eng = "dve" if d == 0 else "pool"
                P.op(eng, lambda e, d=d: e.memset(Ea[d][:], 0.0), w=[("Rl", d)])
                order = list(range(NB - 1)) if d == 0 else list(range(NB - 1, 0, -1))
                for m in order:
                    mn = m + 1 if d == 0 else m - 1
                    pend = (1 + 16 * m + 15) if d == 0 else (16 * m)
                    P.op(eng, lambda e, d=d, m=m: e.tensor_tensor(out=ET[d][:, 0], in0=Ea[d][:, :, 0:2, m], in1=mu16[:, d, 16, 0], op=ALU.mult),
                         r=[("Rl", d), "mu16"], w=[("ET", d, 0)])
                    P.op(eng, lambda e, d=d, m=m: e.tensor_tensor(out=ET[d][:, 1], in0=Ea[d][:, :, 1:3, m], in1=mu16[:, d, 16, 1], op=ALU.mult),
                         r=[("Rl", d), "mu16"], w=[("ET", d, 1)])
                    P.op(eng, lambda e, d=d: e.tensor_tensor(out=ET[d][:, 0], in0=ET[d][:, 0], in1=ET[d][:, 1], op=ALU.add),
                         r=[("ET", d, 0), ("ET", d, 1)], w=[("ET", d, 0)])
                    P.op(eng, lambda e, d=d, mn=mn, pend=pend: e.tensor_tensor(out=Ea[d][:, :, 0:2, mn], in0=ET[d][:, 0], in1=Hall[d][:, :, :, pend], op=ALU.add),
                         r=[("ET", d, 0), ("Hall", d, pend)], w=[("Rl", d)])
                    P.op(eng, lambda e, d=d, mn=mn: e.tensor_copy(out=Ea[d][:, :, 2, mn], in_=Ea[d][:, :, 0, mn]), r=[("Rl", d)], w=[("Rl", d)])
            for d in range(2):
                eng = "dve"
                for i in range(LB):
                    hv = hview(d, i)
                    hk_i = hkeys(d, i)
                    P.op(eng, lambda e, d=d, i=i: e.tensor_tensor(out=TA[d][:], in0=Ea[d][:, :, 0:2, 0:NB], in1=mub(d, i + 1, 0, NB), op=ALU.mult),
                         r=[("Rl", d), "mu16"], w=[("TA", d)])
                    P.op(eng, lambda e, d=d, i=i: e.tensor_tensor(out=TB[d][:], in0=Ea[d][:, :, 1:3, 0:NB], in1=mub(d, i + 1, 1, NB), op=ALU.mult),
                         r=[("Rl", d), "mu16"], w=[("TB", d)])
                    P.op(eng, lambda e, d=d: e.tensor_tensor(out=TA[d][:], in0=TA[d][:], in1=TB[d][:], op=ALU.add),
                         r=[("TA", d), ("TB", d)], w=[("TA", d)])
                    P.op(eng, lambda e, d=d, hv=hv: e.tensor_tensor(out=hv, in0=hv, in1=TA[d][:], op=ALU.add), r=[("TA", d)] + hk_i, w=hk_i)
            P.flush()
        NCXo = NXO // 8
        NCB = (NCXo + 127) // 128
        NPASS = 1
        CBP = NCB // NPASS
        NCP = NCXo // NPASS
        Yc = sb(st, "Yc", [128, CBP, 4096], BF16)
        Ysb = sb(st, "Ysb", [128, 2, NCP])
        allH = [hk(0, 0, HW), hk(1, 0, HW)]
        for pz in range(NPASS):
            c_lo = pz * NCP
            for g in range(32):
                gh, gp = g // 16, g % 16
                rows = slice(64 * gh, 64 * gh + 64)
                pr = PS2[g % 2]
                prk = ("p2", g % 2)

                def mmy(e, g=g, gp=gp, rows=rows, pr=pr, c_lo=c_lo):
                    e.matmul(pr[:, 0:NCP], lhsT=Tloc[:, g, :], rhs=U8[:, g, NCC + c_lo:NCC + c_lo + NCP], start=True, stop=False)
                    e.matmul(pr[:, 0:NCP], lhsT=Dm[rows, 0, gp, 0, :], rhs=Hall[0][rows, gp, 0, NCC + c_lo:NCC + c_lo + NCP], start=False, stop=False)
                    e.matmul(pr[:, 0:NCP], lhsT=Dm[rows, 0, gp, 1, :], rhs=Hall[0][rows, gp, 1, NCC + c_lo:NCC + c_lo + NCP], start=False, stop=False)
                    e.matmul(pr[:, 0:NCP], lhsT=Dm[rows, 1, gp, 0, :], rhs=Hall[1][rows, gp, 0, 1 + c_lo:1 + c_lo + NCP], start=False, stop=False)
                    return e.matmul(pr[:, 0:NCP], lhsT=Dm[rows, 1, gp, 1, :], rhs=Hall[1][rows, gp, 1, 1 + c_lo:1 + c_lo + NCP], start=False, stop=True)
                P.op("pe", mmy, r=[("Tloc", g), "U8", "Dm"] + allH[0] + allH[1], w=[prk])
                ys = g % 2
                if g % 2 == 0:
                    P.op("act", lambda e, ys=ys, pr=pr: e.copy(out=Ysb[:, ys, :], in_=pr[:, 0:NCP]), r=[prk], w=[("Ysb", ys)])
                else:
                    P.op("dve", lambda e, ys=ys, pr=pr: e.tensor_copy(out=Ysb[:, ys, :], in_=pr[:, 0:NCP]), r=[prk], w=[("Ysb", ys)])
                for cb in range(CBP):
                    ncb = min(128, NCP - cb * 128)
                    pt_ = PS2[2 + (g * CBP + cb) % 4]
                    ptk = ("p2", 2 + (g * CBP + cb) % 4)
                    P.op("pe", lambda e, ys=ys, cb=cb, ncb=ncb, pt_=pt_: e.transpose(out=pt_[0:ncb, 0:128], in_=Ysb[:, ys, cb * 128:cb * 128 + ncb],
                                                                                   identity=ident[:]), r=[("Ysb", ys), "ident"], w=[ptk])
                    oap = Yc[0:ncb, cb, :].rearrange("c (j g o) -> c g j o", j=8, g=32)[:, g]
                    iap = pt_[0:ncb, 0:128].rearrange("c (j o) -> c j o", j=8)
                    if (g + cb) % 2 == 0:
                        P.op("dve", lambda e, oap=oap, iap=iap: e.tensor_copy(out=oap, in_=iap), r=[ptk], w=[("Yc", cb)])
                    else:
                        P.op("act", lambda e, oap=oap, iap=iap: e.copy(out=oap, in_=iap), r=[ptk], w=[("Yc", cb)])
            for cb in range(CBP):
                ncb = min(128, NCP - cb * 128)
                r0 = c_lo + cb * 128
                P.dmaop("sp", lambda e, cb=cb, ncb=ncb, r0=r0: e.dma_start(out=y_s[r0:r0 + ncb, :], in_=Yc[0:ncb, cb, :]),
                        r=[("Yc", cb)], w=["y_s"])
        P.flush()
    if DONE:
        es.close()
        return nc
    if debug == 5:
        dbg = nc.dram_tensor("dbg_y", [NXe // 8, 4096], BF16, kind="ExternalOutput").ap()
        P.dmaop("sp", lambda e: e.dma_start(out=dbg, in_=y_s[0:NXe // 8, :]), w=["dbg"])
        P.flush()
        es.close()
        return nc

    if debug in (0, 3, 6):
      with ExitStack() as st:
        vt = sb(st, "vt", [128, NTe, 8, 80], BF16)
        kTh = sb(st, "kTh", [128, 2, NTe * 128], BF16)
        qTh = sb(st, "qTh", [128, 2, NXO], BF16)
        pT = sb(st, "pT", [128, 5, 512], BF16)
        osb = sb(st, "osb", [128, 2, 512])
        rr = sb(st, "rr", [128, 512])
        ao = sb(st, "ao", [64, 2, 512])
        ones1 = sb(st, "ones1", [128, 64])
        PSs = [ps(st, "ps_s%d" % i) for i in range(5)]
        PSo = [ps(st, "ps_o%d" % i) for i in range(2)]
        PSb = ps(st, "ps_b")
        P.op("pool", lambda e: e.memset(vt[:], 1.0), w=["vt"])
        P.op("pool", lambda e: e.memset(ones1[:], 1.0), w=["ones1"])
        for kt in range(NTe):
            P.dmaop("sp" if kt % 2 == 0 else "act",
                    lambda e, kt=kt: e.dma_start(out=vt[:, kt, :, 0:64],
                                                 in_=v_s[kt * 128:(kt + 1) * 128, :].rearrange("p (h d) -> p h d", h=8)),
                    r=["v_s"], w=["vt"])
        cnt = 0
        for h in range(H):
            hs = h % 2
            P.dmaop("sp", lambda e, h=h, hs=hs: e.dma_start(out=kTh[0:96, hs, :], in_=kT_s[h, :, 0:NTe * 128]), r=["kT_s"], w=[("kTh", hs)])
            P.dmaop("act", lambda e, h=h, hs=hs: e.dma_start(out=qTh[0:96, hs, :], in_=qT_s[h, :, 0:NXO]), r=["qT_s"], w=[("qTh", hs)])
            for g in range(NQG):
                og = (h * NQG + g) % 2

                def smm(e, kt, hs=hs, g=g):
                    return e.matmul(PSs[kt % 5][:, 0:QG], lhsT=kTh[0:96, hs, kt * 128:(kt + 1) * 128],
                                    rhs=qTh[0:96, hs, g * QG:(g + 1) * QG], start=True, stop=True)

                def pvm(e, kt, h=h, og=og):
                    return e.matmul(PSo[og][0:65, 0:QG], lhsT=vt[:, kt, h, 0:65], rhs=pT[:, kt % 5, 0:QG],
                                    start=(kt == 0), stop=(kt == NTe - 1))
                LOOK = 3
                for step in range(NTe + LOOK):
                    if step < NTe:
                        kt = step
                        P.op("pe", lambda e, kt=kt, f=smm: f(e, kt), r=[("kTh", hs), ("qTh", hs)], w=[("pss", kt % 5)])
                        P.op("act", lambda e, kt=kt: e.activation(out=pT[:, kt % 5, 0:QG], in_=PSs[kt % 5][:, 0:QG], func=AF.Exp),
                             r=[("pss", kt % 5)], w=[("pT", kt % 5)])
                    if step >= LOOK:
                        kt = step - LOOK
                        P.op("pe", lambda e, kt=kt, f=pvm: f(e, kt), r=[("pT", kt % 5), "vt"], w=[("pso", og)])
                P.op("dve", lambda e, og=og: e.tensor_copy(out=osb[0:65, og, 0:QG], in_=PSo[og][0:65, 0:QG]), r=[("pso", og)], w=[("osb", og)])
                P.op("dve", lambda e, og=og: e.reciprocal(out=rr[64:65, 0:QG], in_=osb[64:65, og, 0:QG]), r=[("osb", og)], w=["rr"])
                P.op("pe", lambda e: e.matmul(PSb[0:64, 0:QG], lhsT=ones1[64:65, 0:64], rhs=rr[64:65, 0:QG], start=True, stop=True),
                     r=["rr", "ones1"], w=["psb"])
                P.op("dve", lambda e, og=og: e.tensor_tensor(out=ao[:, og, 0:QG], in0=osb[0:64, og, 0:QG], in1=PSb[0:64, 0:QG], op=ALU.mult),
                     r=[("osb", og), "psb"], w=[("ao", og)])
                P.dmaop("sp", lambda e, h=h, g=g, og=og: e.dma_start(out=attnT_s[h, :, g * QG:(g + 1) * QG], in_=ao[:, og, 0:QG]),
                        r=[("ao", og)], w=["attnT_s"])
        P.flush()

    CAPe = 2 * NXe // NE
    SLT = min(128, CAPe)
    NRC = CAPe // SLT
    NXT = NXO // 128

    if debug in (0, 6):
      with ExitStack() as sp:
        idxT = sb(sp, "idxT", [128, NRC, 16], I32)
        gateT = sb(sp, "gateT", [128, NRC, 16])
        sp45 = ExitStack()
        affT = sb(sp45, "affT", [48, NXO])
        affo = sb(sp45, "affo", [16, NXO])
        with ExitStack() as st:
            wglu = sb(st, "wglu", [128, 4, 512], BF16)
            wsso = sb(st, "wsso", [128, 4, D], BF16)
            wmla = sb(st, "wmla", [64, 8, D], BF16)
            wout = sb(st, "wout", [128, 8, D], BF16)
            wrt = sb(st, "wrt", [128, 8, 16])
            bglu = sb(st, "bglu", [128, 512])
            identb = sb(st, "identb4", [128, 128], BF16)
            TL4 = [dict(), dict()]
            for _s in range(2):
                TL4[_s]['yt'] = sb(st, "yt_%d" % _s, [128, 512], BF16)
                TL4[_s]['yg'] = sb(st, "yg_%d" % _s, [128, 512])
                TL4[_s]['t1'] = sb(st, "t1_%d" % _s, [128, 512])
                TL4[_s]['ygb'] = sb(st, "ygb_%d" % _s, [128, 512], BF16)
                TL4[_s]['ygT'] = sb(st, "ygT_%d" % _s, [128, 4, 128], BF16)
                TL4[_s]['sg'] = sb(st, "sg_%d" % _s, [128, 512])
                TL4[_s]['zb'] = sb(st, "zb_%d" % _s, [128, 512], BF16)
                TL4[_s]['zT'] = sb(st, "zT_%d" % _s, [128, 4, 128], BF16)
                TL4[_s]['at32'] = sb(st, "at32_%d" % _s, [64, 8, 128])
                TL4[_s]['atb'] = sb(st, "atb_%d" % _s, [64, 8, 128], BF16)
                TL4[_s]['gt'] = sb(st, "gt_%d" % _s, [128, 2048])
                TL4[_s]['m1'] = sb(st, "m1_%d" % _s, [128, D])
                TL4[_s]['m2'] = sb(st, "m2_%d" % _s, [128, D])
                TL4[_s]['mb'] = sb(st, "mb_%d" % _s, [128, D], BF16)
                TL4[_s]['mT'] = sb(st, "mT_%d" % _s, [128, 8, 128], BF16)
                TL4[_s]['xt4'] = sb(st, "xt4_%d" % _s, [128, D])
                TL4[_s]['xm'] = sb(st, "xm_%d" % _s, [128, D])
                TL4[_s]['jk'] = sb(st, "jk_%d" % _s, [128, D])
                TL4[_s]['h2'] = sb(st, "h2_%d" % _s, [128, D])
                TL4[_s]['h2b'] = sb(st, "h2b_%d" % _s, [128, D], BF16)
                TL4[_s]['h2T'] = sb(st, "h2T_%d" % _s, [128, 8, 128])
                TL4[_s]['s4'] = sb(st, "s4_%d" % _s, [128, 8])
                TL4[_s]['lg'] = sb(st, "lg_%d" % _s, [128, 16])
                TL4[_s]['af'] = sb(st, "af_%d" % _s, [128, 48])
            BALL = [ps(st, "ph4_%d" % i) for i in range(8)]
            P.dmaop("pool", lambda e: e.dma_start(out=wglu[:], in_=w_glu.rearrange("(k p) n -> p k n", p=128)), w=["wglu"])
            P.dmaop("pool", lambda e: e.dma_start(out=wsso[:], in_=w_ssm_o.rearrange("(k p) n -> p k n", p=128)), w=["wsso"])
            P.dmaop("pool", lambda e: e.dma_start(out=wmla[:], in_=w_mla_o.rearrange("(h v) n -> v h n", v=64)), w=["wmla"])
            P.dmaop("pool", lambda e: e.dma_start(out=wout[:], in_=w_out.rearrange("(k p) n -> p k n", p=128)), w=["wout"])
            P.dmaop("sp", lambda e: e.dma_start(out=wrt[:], in_=w_router.rearrange("(k p) n -> p k n", p=128)), w=["wrt"])
            P.dmaop("sp", lambda e: e.dma_start(out=bglu[:], in_=b_glu.partition_broadcast(128)), w=["bglu"])
            P.op("dve", lambda e: e.tensor_copy(out=identb[:], in_=ident[:]), r=["ident"], w=["identb4"])
            ysv = y_s.rearrange("c (j f) -> (c j) f", j=8)
            for _s in range(2):
                P.op("pool", lambda e, _s=_s: e.memset(TL4[_s]['af'][:], 0.0), w=[("slot", _s, "af")])
            P.op("pool", lambda e: e.memset(affT[:], 0.0), w=["affT"])
            SHARED4 = ["wglu", "wsso", "wmla", "wout", "wrt", "bglu", "identb4", "ident", "modx", "y_s", "gates_s", "attnT_s", "xm_s", "h2b_own", "affo"]
            ALIAS4 = {"b4": "b2", "b5": "b3", "b6": "b2", "b7": "b3"}

            def tile4(i, s):
                Pq = Keyed(P, s, SHARED4, ALIAS4)
                t0 = i * 128
                yt = TL4[s]['yt']
                yg = TL4[s]['yg']
                t1 = TL4[s]['t1']
                ygb = TL4[s]['ygb']
                ygT = TL4[s]['ygT']
                sg = TL4[s]['sg']
                zb = TL4[s]['zb']
                zT = TL4[s]['zT']
                at32 = TL4[s]['at32']
                atb = TL4[s]['atb']
                gt = TL4[s]['gt']
                m1 = TL4[s]['m1']
                m2 = TL4[s]['m2']
                mb = TL4[s]['mb']
                mT = TL4[s]['mT']
                xt4 = TL4[s]['xt4']
                xm = TL4[s]['xm']
                jk = TL4[s]['jk']
                h2 = TL4[s]['h2']
                h2b = TL4[s]['h2b']
                h2T = TL4[s]['h2T']
                s4 = TL4[s]['s4']
                lg = TL4[s]['lg']
                af = TL4[s]['af']
                bk = BALL[4 * s:4 * s + 4]
                B = [bk[0], bk[1], bk[2], bk[3], bk[2], bk[3], bk[2], bk[3]]
                B0b = B[0][:].bitcast(BF16)
                Pq.dmaop("sp", lambda e, t0=t0: e.dma_start(out=yt[:], in_=ysv[t0:t0 + 128, :]), r=["y_s"], w=["yt"])
                Pq.dmaop("act", lambda e, t0=t0: e.dma_start(out=gt[:], in_=gates_s[t0:t0 + 128, :]), r=["gates_s"], w=["gt"])
                Pq.dmaop("sp", lambda e, t0=t0: e.dma_start(out=at32[:], in_=attnT_s[:, :, t0:t0 + 128].rearrange("h v t -> v h t")),
                        r=["attnT_s"], w=["at32"])
                Pq.dmaop("act", lambda e, t0=t0: e.dma_start(out=xt4[:], in_=xc[NCTX + t0:NCTX + t0 + 128, :]), w=["xt4"])
                Pq.op("pool", lambda e: e.tensor_tensor(out=t1[:], in0=yt[:], in1=yt[:], op=ALU.mult), r=["yt"], w=["t1"])
                Pq.op("dve", lambda e: e.tensor_scalar(out=t1[:], in0=t1[:], scalar1=0.044715, scalar2=1.0, op0=ALU.mult, op1=ALU.add),
                     r=["t1"], w=["t1"])
                Pq.op("dve", lambda e: e.tensor_tensor(out=t1[:], in0=t1[:], in1=yt[:], op=ALU.mult), r=["t1", "yt"], w=["t1"])
                Pq.op("act", lambda e: e.activation(out=t1[:], in_=t1[:], func=AF.Tanh, scale=0.7978845608028654), r=["t1"], w=["t1"])
                Pq.op("dve", lambda e: e.tensor_scalar(out=t1[:], in0=t1[:], scalar1=1.0, scalar2=0.5, op0=ALU.add, op1=ALU.mult),
                     r=["t1"], w=["t1"])
                Pq.op("dve", lambda e: e.tensor_tensor(out=yg[:], in0=t1[:], in1=yt[:], op=ALU.mult), r=["t1", "yt"], w=["yg"])
                Pq.op("pool", lambda e: e.tensor_copy(out=ygb[:], in_=yg[:]), r=["yg"], w=["ygb"])

                def tr4(src, n):
                    def f(e):
                        ins = None
                        for k in range(n):
                            ins = e.transpose(out=B0b[:, k * 128:(k + 1) * 128], in_=src[:, k * 128:(k + 1) * 128], identity=identb[:])
                        return ins
                    return f
                Pq.op("pe", tr4(ygb, 4), r=["ygb", "identb4"], w=["b0"])
                Pq.op("act", lambda e: e.copy(out=ygT[:].rearrange("p a b -> p (a b)"), in_=B0b[:, 0:512]), r=["b0"], w=["ygT"])

                def mmglu(e):
                    ins = None
                    for k in range(4):
                        ins = e.matmul(B[1][:, 0:512], lhsT=ygT[:, k, :], rhs=wglu[:, k, :], start=(k == 0), stop=(k == 3))
                    return ins
                Pq.op("pe", mmglu, r=["ygT", "wglu"], w=["b1"])
                Pq.op("dve", lambda e: e.tensor_tensor(out=sg[:], in0=B[1][:, 0:512], in1=bglu[:], op=ALU.add), r=["b1", "bglu"], w=["sg"])
                Pq.op("act", lambda e: e.activation(out=sg[:], in_=sg[:], func=AF.Sigmoid), r=["sg"], w=["sg"])
                Pq.op("dve", lambda e: e.tensor_tensor(out=zb[:], in0=sg[:], in1=yg[:], op=ALU.mult), r=["sg", "yg"], w=["zb"])
                Pq.op("pe", tr4(zb, 4), r=["zb", "identb4"], w=["b0"])
                Pq.op("act", lambda e: e.copy(out=zT[:].rearrange("p a b -> p (a b)"), in_=B0b[:, 0:512]), r=["b0"], w=["zT"])

                def mmsso(e):
                    ins = None
                    for hf in range(2):
                        for k in range(4):
                            ins = e.matmul(B[2 + hf][:, 0:512], lhsT=zT[:, k, :], rhs=wsso[:, k, hf * 512:(hf + 1) * 512], start=(k == 0), stop=(k == 3))
                    return ins
                Pq.op("pe", mmsso, r=["zT", "wsso"], w=["b2", "b3"])
                for hf in range(2):
                    cs = slice(hf * 512, (hf + 1) * 512)
                    Pq.op("dve", lambda e, hf=hf, cs=cs: e.tensor_tensor(out=m1[:, cs], in0=B[2 + hf][:, 0:512], in1=gt[:, cs], op=ALU.mult),
                          r=["b%d" % (2 + hf), "gt"], w=[("m1", hf)])
                Pq.op("pool", lambda e: e.tensor_copy(out=atb[:], in_=at32[:]), r=["at32"], w=["atb"])

                def mmat(e):
                    ins = None
                    for hf in range(2):
                        for h in range(8):
                            ins = e.matmul(B[4 + hf][:, 0:512], lhsT=atb[:, h, :], rhs=wmla[:, h, hf * 512:(hf + 1) * 512], start=(h == 0), stop=(h == 7))
                    return ins
                Pq.op("pe", mmat, r=["atb", "wmla"], w=["b4", "b5"])
                for hf in range(2):
                    cs = slice(hf * 512, (hf + 1) * 512)
                    cs2 = slice(D + hf * 512, D + (hf + 1) * 512)
                    Pq.op("dve", lambda e, hf=hf, cs=cs, cs2=cs2: e.tensor_tensor(out=m2[:, cs], in0=B[4 + hf][:, 0:512], in1=gt[:, cs2], op=ALU.mult),
                          r=["b%d" % (4 + hf), "gt"], w=[("m2", hf)])
                Pq.op("pool", lambda e: e.tensor_tensor(out=mb[:], in0=m1[:], in1=m2[:], op=ALU.add),
                     r=[("m1", 0), ("m1", 1), ("m2", 0), ("m2", 1)], w=["mb"])
                Pq.op("pe", tr4(mb, 8), r=["mb", "identb4"], w=["b0"])
                Pq.op("act", lambda e: e.copy(out=mT[:].rearrange("p a b -> p (a b)"), in_=B0b[:, 0:1024]), r=["b0"], w=["mT"])

                def mmout(e):
                    ins = None
                    for hf in range(2):
                        for k in range(8):
                            ins = e.matmul(B[6 + hf][:, 0:512], lhsT=mT[:, k, :], rhs=wout[:, k, hf * 512:(hf + 1) * 512], start=(k == 0), stop=(k == 7))
                    return ins
                Pq.op("pe", mmout, r=["mT", "wout"], w=["b6", "b7"])
                for hf in range(2):
                    cs = slice(hf * 512, (hf + 1) * 512)
                    Pq.op("dve", lambda e, hf=hf, cs=cs: e.tensor_tensor(out=xm[:, cs], in0=B[6 + hf][:, 0:512], in1=modx[:, 2 * D + hf * 512:2 * D + (hf + 1) * 512],
                                                                    op=ALU.mult), r=["b%d" % (6 + hf), "modx"], w=[("xm", hf)])
                Pq.op("pool", lambda e: e.tensor_tensor(out=xm[:], in0=xm[:], in1=xt4[:], op=ALU.add), r=[("xm", 0), ("xm", 1), "xt4"], w=[("xm", 0), ("xm", 1)])
                Pq.dmaop("sp", lambda e, t0=t0: e.dma_start(out=xm_s[t0:t0 + 128, :], in_=xm[:]), r=[("xm", 0), ("xm", 1)], w=["xm_s"])
                Pq.op("act", lambda e: e.activation(out=jk[:], in_=xm[:], func=AF.Square, accum_out=s4[:, 0:1]), r=[("xm", 0), ("xm", 1)], w=["jk", "s4a"])
                Pq.op("dve", lambda e: e.tensor_scalar(out=s4[:, 1:2], in0=s4[:, 0:1], scalar1=1.0 / D, scalar2=EPS, op0=ALU.mult, op1=ALU.add), r=["s4a"], w=["s4b"])
                Pq.op("act", lambda e: e.activation(out=s4[:, 1:2], in_=s4[:, 1:2], func=AF.Sqrt), r=["s4b"], w=["s4b"])
                Pq.op("dve", lambda e: e.reciprocal(out=s4[:, 1:2], in_=s4[:, 1:2]), r=["s4b"], w=["s4b"])
                Pq.op("dve", lambda e: e.scalar_tensor_tensor(out=h2[:], in0=xm[:], scalar=s4[:, 1:2], in1=modx[:, 4 * D:5 * D], op0=ALU.mult, op1=ALU.mult),
                     r=[("xm", 0), ("xm", 1), "s4b", "modx"], w=["h2"])
                Pq.op("pool", lambda e: e.tensor_tensor(out=h2[:], in0=h2[:], in1=modx[:, 3 * D:4 * D], op=ALU.add), r=["h2", "modx"], w=["h2"])
                Pq.op("act", lambda e: e.copy(out=h2b[:], in_=h2[:]), r=["h2"], w=["h2b"])
                Pq.dmaop("act", lambda e, t0=t0: e.dma_start(out=h2b_own_c[t0 // RCH].ap()[t0 % RCH:t0 % RCH + 128, :], in_=h2b[:]), r=["h2b"], w=["h2b_own"])
                def trh2(e):
                    ins = None
                    for k in range(8):
                        ins = e.transpose(out=B[2 + k // 4][:, (k % 4) * 128:(k % 4 + 1) * 128], in_=h2[:, k * 128:(k + 1) * 128], identity=ident[:])
                    return ins
                Pq.op("pe", trh2, r=["h2", "ident", ("m1", 0), ("m1", 1)], w=["b2", "b3"])
                Pq.op("act", lambda e: e.copy(out=h2T[:, 0:4, :].rearrange("p a b -> p (a b)"), in_=B[2][:, 0:512]), r=["b2"], w=[("h2T", 0)])
                Pq.op("dve", lambda e: e.tensor_copy(out=h2T[:, 4:8, :].rearrange("p a b -> p (a b)"), in_=B[3][:, 0:512]), r=["b3"], w=[("h2T", 1)])

                def mmrt(e):
                    ins = None
                    for k in range(8):
                        ins = e.matmul(B[1][:, 0:16], lhsT=h2T[:, k, :], rhs=wrt[:, k, :], start=(k == 0), stop=(k == 7))
                    return ins
                Pq.op("pe", mmrt, r=[("h2T", 0), ("h2T", 1), "wrt", "sg"], w=["b1"])
                Pq.op("dve", lambda e: e.tensor_copy(out=lg[:], in_=B[1][:, 0:16]), r=["b1"], w=["lg"])
                Pq.op("dve", lambda e: e.tensor_reduce(out=s4[:, 2:3], in_=lg[:], axis=AX.X, op=ALU.max), r=["lg"], w=["s4c"])
                Pq.op("dve", lambda e: e.tensor_scalar(out=s4[:, 3:4], in0=s4[:, 2:3], scalar1=-1.0, scalar2=None, op0=ALU.mult), r=["s4c"], w=["s4d"])
                hb_ = 0
                afc = slice(0, 16)
                Pq.op("act", lambda e, afc=afc: e.activation(out=af[:, afc], in_=lg[:], func=AF.Exp, bias=s4[:, 3:4], accum_out=s4[:, 4:5]), r=["lg", "s4d"], w=["af", "s4e"])
                Pq.op("dve", lambda e: e.reciprocal(out=s4[:, 5:6], in_=s4[:, 4:5]), r=["s4e"], w=["s4f"])
                Pq.op("dve", lambda e, afc=afc: e.tensor_scalar(out=af[:, afc], in0=af[:, afc], scalar1=s4[:, 5:6], scalar2=None, op0=ALU.mult), r=["af", "s4f"], w=["af"])
                Pq.op("pe", lambda e: e.transpose(out=B[4][0:48, 0:128], in_=af[:], identity=ident[:]), r=["af", "ident", ("m2", 0), ("m2", 1)], w=["b4"])
                Pq.op("act", lambda e, t0=t0: e.copy(out=affo[:, t0:t0 + 128], in_=B[4][0:16, 0:128]), r=["b4"], w=["affo"])
                return Pq.cap
            for i in range(0, NXT, 2):
                caps = [tile4(i, 0)] + ([tile4(i + 1, 1)] if i + 1 < NXT else [])
                interleave(P, caps, chunk=int(os.environ.get("ILV", "2")))
            P.flush()
        with ExitStack() as st:
            NH = NXO
            wk = sb(st, "wk", [48, NH])
            vals = sb(st, "vals", [48, CAPe])
            idxu = sb(st, "idxu", [48, CAPe], U32)
            idxf = sb(st, "idxf", [48, CAPe])
            jrev = sb(st, "jrev", [128, 128])
            tA = sb(st, "tA", [128, 2, 16])
            tB = sb(st, "tB", [128, 2, 16])
            tM = sb(st, "tM", [128, 3, 16])
            B5 = [ps(st, "ph5_%d" % i) for i in range(4)]
            P.dmaop("sp", lambda e: e.dma_start(out=jrev[:], in_=jrev_d), w=["jrev"])
            a01 = sb(st, "a01", [16, 2, NXO])
            selt = sb(st, "selt", [16, 8])
            P.dmaop("sp", lambda e: e.dma_start(out=selt[:], in_=sel_d), w=["selt"])
            P.dmaop("sp", lambda e: e.dma_start(out=aff_own, in_=affo[:]), r=["affo"], w=["aff_own"])
            P.ccop(lambda e: e.collective_compute("AllGather", ALU.bypass, replica_groups=PAIRS, ins=[aff_own_t.ap().opt()], outs=[aff_all_t.ap().opt()]),
                   r=["aff_own"], w=["aff_all"])
            for c in range(NCHK):
                P.ccop(lambda e, c=c: e.collective_compute("AllGather", ALU.bypass, replica_groups=PAIRS, ins=[h2b_own_c[c].ap().opt()], outs=[h2b_ag_c[c].ap().opt()]),
                       r=["h2b_own"], w=[("h2b_ag", c)])
                for rk_ in range(2):
                    P.dmaop("act", lambda e, c=c, rk_=rk_: e.dma_start(out=h2b_all[rk_ * NXO + c * RCH:rk_ * NXO + (c + 1) * RCH, :],
                                                                      in_=h2b_ag_c[c].ap()[rk_ * RCH:(rk_ + 1) * RCH, :]), r=[("h2b_ag", c)], w=["h2b_all"])
            for rk_ in range(2):
                P.dmaop("sp", lambda e, rk_=rk_: e.dma_start(out=a01[:, rk_, :], in_=aff_all[16 * rk_:16 * rk_ + 16, :]), r=["aff_all"], w=[("a01", rk_)])
            for rk_ in range(2):
                for c0 in range(0, NXO, 512):
                    n = min(512, NXO - c0)
                    pb_ = B5[rk_]
                    P.op("pe", lambda e, rk_=rk_, c0=c0, n=n, pb_=pb_: e.matmul(pb_[32 * rk_:32 * rk_ + 8, 0:n], lhsT=selt[:, :], rhs=a01[:, rk_, c0:c0 + n],
                                                                             start=True, stop=True, tile_position=(0, 32 * rk_)),
                         r=["selt", ("a01", rk_)], w=[("b5", rk_)])
                    P.op("act", lambda e, rk_=rk_, c0=c0, n=n, pb_=pb_: e.copy(out=affT[32 * rk_:32 * rk_ + 8, c0:c0 + n], in_=pb_[32 * rk_:32 * rk_ + 8, 0:n]),
                         r=[("b5", rk_)], w=["affT"])
            P.op("dve", lambda e: e.tensor_copy(out=wk[:], in_=affT[:]), r=["affT"], w=["wk"])
            for r_ in range(CAPe // 8):
                sl = slice(r_ * 8, r_ * 8 + 8)
                P.op("dve", lambda e, sl=sl: e.max(out=vals[:, sl], in_=wk[:]), r=["wk"], w=[("vals", r_)])
                P.op("dve", lambda e, sl=sl: e.max_index(out=idxu[:, sl], in_max=vals[:, sl], in_values=wk[:]), r=["wk", ("vals", r_)], w=[("idxu", r_)])
                P.op("dve", lambda e, sl=sl: e.match_replace(out=wk[:], in_to_replace=vals[:, sl], in_values=wk[:], imm_value=-1.0),
                     r=["wk", ("vals", r_), ("idxu", r_)], w=["wk"])
            allv = [("vals", r_) for r_ in range(CAPe // 8)]
            alli = [("idxu", r_) for r_ in range(CAPe // 8)]
            P.op("dve", lambda e: e.tensor_copy(out=idxf[:], in_=idxu[:]), r=alli, w=["idxf"])
            P.op("dve", lambda e: e.tensor_scalar(out=idxf[32:48, :], in0=idxf[32:48, :], scalar1=float(NH), scalar2=None, op0=ALU.add), r=["idxf"], w=["idxf"])
            Jb = jrev[0:SLT, 128 - SLT:128]
            for rc in range(NRC):
                cs = slice(rc * SLT, (rc + 1) * SLT)
                rb = NRC - 1 - rc
                cb_ = slice(rb * SLT, (rb + 1) * SLT)
                for w_, src in ((0, vals), (1, idxf)):
                    rk = allv if w_ == 0 else ["idxf"]
                    P.op("pe", lambda e, cs=cs, src=src, w_=w_: e.transpose(out=B5[w_][0:SLT, 0:16], in_=src[0:16, cs], identity=ident[0:16, 0:16]),
                         r=rk + ["ident"], w=[("b5", w_)])
                    P.op("act", lambda e, w_=w_: e.copy(out=tA[0:SLT, w_, :], in_=B5[w_][0:SLT, 0:16]), r=[("b5", w_)], w=[("tA", w_)])
                    P.op("pe", lambda e, cb_=cb_, src=src, w_=w_: e.transpose(out=B5[2 + w_][0:SLT, 0:16], in_=src[32:48, cb_], identity=ident[32:48, 32:48]),
                         r=rk + ["ident"], w=[("b5", 2 + w_)])
                    P.op("act", lambda e, w_=w_: e.copy(out=tB[0:SLT, w_, :], in_=B5[2 + w_][0:SLT, 0:16]), r=[("b5", 2 + w_)], w=[("tB", w_)])
                    P.op("pe", lambda e, w_=w_: e.matmul(B5[2 + w_][0:SLT, 0:16], lhsT=Jb, rhs=tB[0:SLT, w_, :], start=True, stop=True),
                         r=[("tB", w_), "jrev"], w=[("b5", 2 + w_)])
                P.op("dve", lambda e: e.tensor_tensor(out=tM[0:SLT, 0, :], in0=tA[0:SLT, 0, :], in1=B5[2][0:SLT, 0:16], op=ALU.is_gt),
                     r=[("tA", 0), ("b5", 2)], w=[("tM", 0)])
                P.op("dve", lambda e, rc=rc: e.tensor_tensor(out=gateT[0:SLT, rc, :], in0=tA[0:SLT, 0, :], in1=B5[2][0:SLT, 0:16], op=ALU.max),
                     r=[("tA", 0), ("b5", 2)], w=["gateT"])
                P.op("dve", lambda e: e.tensor_tensor(out=tM[0:SLT, 1, :], in0=tA[0:SLT, 1, :], in1=B5[3][0:SLT, 0:16], op=ALU.subtract),
                     r=[("tA", 1), ("b5", 3)], w=[("tM", 1)])
                P.op("dve", lambda e: e.tensor_tensor(out=tM[0:SLT, 1, :], in0=tM[0:SLT, 1, :], in1=tM[0:SLT, 0, :], op=ALU.mult),
                     r=[("tM", 1), ("tM", 0)], w=[("tM", 1)])
                P.op("dve", lambda e: e.tensor_tensor(out=tM[0:SLT, 2, :], in0=tM[0:SLT, 1, :], in1=B5[3][0:SLT, 0:16], op=ALU.add),
                     r=[("tM", 1), ("b5", 3)], w=[("tM", 2)])
                P.op("dve", lambda e, rc=rc: e.tensor_copy(out=idxT[0:SLT, rc, :], in_=tM[0:SLT, 2, :]), r=[("tM", 2)], w=["idxT"])
            P.flush()
        sp45.close()
        NEe = NE // 2 if debug == 0 else int(os.environ.get("NEE", "8"))
        with ExitStack() as st:
            identb = sb(st, "identb6", [128, 128], BF16)
            xs = sb(st, "xs", [128, 1, NRC, D], BF16)
            xsT = sb(st, "xsT", [128, 2, 8, CAPe], BF16)
            wg = sb(st, "wg", [128, 3, 8, 512], BF16)
            wu = sb(st, "wu", [128, 3, 8, 512], BF16)
            wd = sb(st, "wd", [128, 2, 22, 512], BF16)
            sgt = sb(st, "sgt", [128, 2, CAPe])
            hidT = sb(st, "hidT", [128, 22, CAPe], BF16)
            ys = sb(st, "ys", [128, 1, NRC, D])
            B = [ps(st, "ph6_%d" % i) for i in range(8)]
            B0b = B[0][:].bitcast(BF16)
            P.op("dve", lambda e: e.tensor_copy(out=identb[:], in_=ident[:]), r=["ident"], w=["identb6"])
            zk = [("ys", 0, 0, dq) for dq in range(2)]
            P.op("pool", lambda e: e.memset(ys[:, 0, 0, :], 0.0), w=zk)
            for r0 in range(0, 2 * NXO, 128):
                P.dmaop("sp" if (r0 // 128) % 2 == 0 else "act",
                        lambda e, r0=r0: e.dma_start(out=acc[r0:r0 + 128, :], in_=ys[:, 0, 0, :]), r=zk, w=["acc0"])
            wgi = 0
            wdi = 0
            def prep(ex):
                sl = ex % 2
                for rc in range(NRC):
                    P.dmaop("pool", lambda e, rc=rc, ex=ex, sl=sl: e.indirect_dma_start(
                        out=xs[0:SLT, 0, rc, :], out_offset=None, in_=h2b_all[0:2 * NXO, :],
                        in_offset=bass.IndirectOffsetOnAxis(ap=idxT[0:SLT, rc, ex:ex + 1], axis=0)),
                        r=["idxT", "h2b_all"], w=[("xs", 0, rc)])

                    def trx(e, rc=rc, sl=sl):
                        ins = None
                        for k in range(8):
                            ins = e.transpose(out=B0b[:, k * 128:k * 128 + SLT], in_=xs[0:SLT, 0, rc, k * 128:(k + 1) * 128], identity=identb[0:SLT, 0:SLT])
                        return ins
                    P.op("pe", trx, r=[("xs", 0, rc), "identb6"], w=["b0"])
                    P.op("act", lambda e, rc=rc, sl=sl: e.copy(out=xsT[:, sl, :, rc * SLT:(rc + 1) * SLT],
                                                               in_=B0b[:, 0:1024].rearrange("p (k c) -> p k c", k=8)[:, :, 0:SLT]), r=["b0"], w=[("xsT", sl)])
            prep(0)
            pending = []
            for ex in range(NEe):
                xsl = ex % 2
                wgv = w_e_gate[ex].rearrange("(dc p) f -> p dc f", p=128)
                wuv = w_e_up[ex].rearrange("(dc p) f -> p dc f", p=128)
                wdv = w_e_down[ex].rearrange("(fc p) d -> p fc d", p=128)
                for grp in range(6):
                    ws = wgi % 3
                    wgi += 1
                    f0 = grp * 512
                    fw = min(512, FF - f0)
                    P.dmaop("pool", lambda e, ws=ws, f0=f0, fw=fw, wgv=wgv: e.dma_start(out=wg[:, ws, :, 0:fw], in_=wgv[:, :, f0:f0 + fw]), w=[("wg", ws)])
                    P.dmaop("pool", lambda e, ws=ws, f0=f0, fw=fw, wuv=wuv: e.dma_start(out=wu[:, ws, :, 0:fw], in_=wuv[:, :, f0:f0 + fw]), w=[("wu", ws)])
                    if grp == 1:
                        for f_ in pending:
                            f_()
                        pending = []
                    for q in range(fw // 128):
                        fc = grp * 4 + q
                        pg = B[1 + fc % 2]
                        pu = B[3 + fc % 2]

                        def mmgu(e, ws=ws, q=q, pg=pg, pu=pu, xsl=xsl):
                            ins = None
                            for k in range(8):
                                e.matmul(pg[:, 0:CAPe], lhsT=wg[:, ws, k, q * 128:(q + 1) * 128], rhs=xsT[:, xsl, k, :], start=(k == 0), stop=(k == 7))
                            for k in range(8):
                                ins = e.matmul(pu[:, 0:CAPe], lhsT=wu[:, ws, k, q * 128:(q + 1) * 128], rhs=xsT[:, xsl, k, :], start=(k == 0), stop=(k == 7))
                            return ins
                        P.op("pe", mmgu, r=[("wg", ws), ("wu", ws), ("xsT", xsl)], w=[("b6", 1 + fc % 2), ("b6", 3 + fc % 2)])
                        P.op("act", lambda e, fc=fc, pg=pg: e.activation(out=sgt[:, fc % 2, :], in_=pg[:, 0:CAPe], func=AF.Silu),
                             r=[("b6", 1 + fc % 2)], w=[("sgt", fc % 2)])
                        P.op("dve", lambda e, fc=fc, pu=pu: e.tensor_tensor(out=hidT[:, fc, :], in0=sgt[:, fc % 2, :], in1=pu[:, 0:CAPe], op=ALU.mult),
                             r=[("sgt", fc % 2), ("b6", 3 + fc % 2)], w=[("hidT", fc)])
                hk_ = [("hidT", fc) for fc in range(22)]
                if ex + 1 < NEe:
                    prep(ex + 1)
                for dq in range(2):
                    ws = wdi % 2
                    wdi += 1
                    P.dmaop("pool", lambda e, ws=ws, dq=dq, wdv=wdv: e.dma_start(out=wd[:, ws], in_=wdv[:, :, dq * 512:(dq + 1) * 512]), w=[("wd", ws)])
                    for rc in range(NRC):
                        py = B[5 + (dq * NRC + rc) % 2]
                        pyk = ("b6", 5 + (dq * NRC + rc) % 2)

                        def mmd(e, ws=ws, rc=rc, py=py):
                            ins = None
                            for fc in range(22):
                                ins = e.matmul(py[0:SLT, 0:512], lhsT=hidT[:, fc, rc * SLT:(rc + 1) * SLT], rhs=wd[:, ws, fc, :], start=(fc == 0), stop=(fc == 21))
                            return ins
                        P.op("pe", mmd, r=hk_ + [("wd", ws)], w=[pyk])
                        P.op("dve", lambda e, rc=rc, dq=dq, py=py, ex=ex, xsl=xsl: e.scalar_tensor_tensor(
                            out=ys[0:SLT, 0, rc, dq * 512:(dq + 1) * 512], in0=py[0:SLT, 0:512], scalar=gateT[0:SLT, rc, ex:ex + 1],
                            in1=modx[0:SLT, 5 * D + dq * 512:5 * D + (dq + 1) * 512], op0=ALU.mult, op1=ALU.mult),
                            r=[pyk, "gateT", "modx"], w=[("ys", 0, rc, dq)])
                def scat(ex=ex):
                    prevk = [("outx", (ex - 1) % 2, rc2) for rc2 in range(NRC)] if ex > 0 else ["acc0"]
                    for rc in range(NRC):
                        P.dmaop("pool", lambda e, rc=rc, ex=ex: e.indirect_dma_start(
                            out=acc[0:2 * NXO, :], out_offset=bass.IndirectOffsetOnAxis(ap=idxT[0:SLT, rc, ex:ex + 1], axis=0),
                            in_=ys[0:SLT, 0, rc, :], in_offset=None, compute_op=ALU.add),
                            r=[("ys", 0, rc, dq) for dq in range(2)] + ["idxT"] + prevk, w=[("outx", ex % 2, rc)])
                pending.append(scat)
            for f_ in pending:
                f_()
            P.flush()
        with ExitStack() as st7:
            fa = sb(st7, "fa", [128, 4 * D])
            P.ccop(lambda e: e.collective_compute("ReduceScatter", ALU.add, replica_groups=PAIRS, ins=[acc_t.ap().opt()], outs=[rs_out_t.ap().opt()]),
                   w=["rs_out"])
            for i in range(NXT):
                t0 = i * 128
                k = i % 2
                P.dmaop("sp", lambda e, t0=t0, k=k: e.dma_start(out=fa[:, k * 2 * D:k * 2 * D + D], in_=xm_s[t0:t0 + 128, :]), w=[("fa", k, 0)])
                P.dmaop("act", lambda e, t0=t0, k=k: e.dma_start(out=fa[:, k * 2 * D + D:(k + 1) * 2 * D], in_=rs_out[t0:t0 + 128, :]), r=["rs_out"], w=[("fa", k, 1)])
                P.op("dve", lambda e, k=k: e.tensor_tensor(out=fa[:, k * 2 * D:k * 2 * D + D], in0=fa[:, k * 2 * D:k * 2 * D + D],
                                                          in1=fa[:, k * 2 * D + D:(k + 1) * 2 * D], op=ALU.add), r=[("fa", k, 0), ("fa", k, 1)], w=[("fa", k, 0)])
                P.dmaop("sp", lambda e, t0=t0, k=k: e.dma_start(out=out[t0:t0 + 128, :], in_=fa[:, k * 2 * D:k * 2 * D + D]), r=[("fa", k, 0)], w=["out"])
            P.flush()
        if debug == 6:
            DONE.append(1)
    if DONE:
        es.close()
        return nc

    if debug == 3:
        dbg = nc.dram_tensor("dbg", [H, 64, 256], F32, kind="ExternalOutput").ap()
        P.dmaop("sp", lambda e: e.dma_start(out=dbg, in_=attnT_s[:, :, 0:256]), w=["dbg"])
        P.flush()
        es.close()
        return nc

    if debug == 2:
        dbg = nc.dram_tensor("dbg", [H, QK, 512], BF16, kind="ExternalOutput").ap()
        dbg2 = nc.dram_tensor("dbg2", [512, 512], F32, kind="ExternalOutput").ap()
        dbg3 = nc.dram_tensor("dbg3", [H, QK, 256], BF16, kind="ExternalOutput").ap()
        P.dmaop("sp", lambda e: e.dma_start(out=dbg, in_=kT_s[:, :, 0:512]), w=["dbg"])
        P.dmaop("sp", lambda e: e.dma_start(out=dbg2, in_=u_s[0:512, :]), w=["dbg2"])
        P.dmaop("sp", lambda e: e.dma_start(out=dbg3, in_=qT_s[:, :, 0:256]), w=["dbg3"])
        P.flush()
        es.close()
        return nc

    if debug == 1:
        dbg = nc.dram_tensor("dbg", [128, 6 * D], F32, kind="ExternalOutput").ap()
        P.dmaop("sp", lambda e: e.dma_start(out=dbg, in_=modx[:]), r=["modx"], w=["dbg"])
        P.flush()
        es.close()
        return nc

    es.close()
    return nc


def _consts():
    ident = np.eye(128, dtype=np.float32)
    n = NX
    rows = n // 64
    row = np.repeat(np.arange(rows, dtype=np.float32), 64)
    col = np.tile(np.arange(64, dtype=np.float32), rows)
    inv = (10000.0 ** (-np.arange(8, dtype=np.float32) / 8)).astype(np.float32)
    ang = np.stack([row[:, None] * inv, col[:, None] * inv], axis=1).astype(np.float32)
    rope = np.zeros((NT, 32), np.float32)
    rope[:NCTX, :16] = 1.0
    rope[NCTX:, :16] = np.cos(ang).reshape(n, 16)
    rope[NCTX:, 16:] = np.sin(ang).reshape(n, 16)
    cm = np.zeros((128, 256), np.float32)
    for jp in range(8):
        for j in range(8):
            if jp <= j:
                cm[jp * 16:(jp + 1) * 16, j * 16:(j + 1) * 16] = 1.0
            if jp >= j:
                cm[jp * 16:(jp + 1) * 16, 128 + j * 16:128 + (j + 1) * 16] = 1.0
    return ident, rope, cm


def make_in_maps(inputs, nx=NX):
    ident, rope, cm = _consts()
    f = lambda a: np.ascontiguousarray(np.asarray(a, dtype=np.float32))
    maps = []
    dirk = ("ssm_lam_re", "ssm_lam_im", "ssm_log_dt", "ssm_b_re", "ssm_b_im", "ssm_c_re", "ssm_c_im")
    for b in range(NCORES // 2):
        for r in range(2):
            x_b = np.asarray(inputs["x"][b])[:nx]
            ctx_b = np.asarray(inputs["ctx"][b])
            rope_x = rope[NCTX:NCTX + nx]
            if r == 1:
                x_b, ctx_b, rope_x = x_b[::-1], ctx_b[::-1], rope_x[::-1]
            xc = np.zeros((NT, D), np.float32)
            xc[:NCTX] = ctx_b
            xc[NCTX:NCTX + nx] = x_b
            rope_r = rope.copy()
            rope_r[NCTX:NCTX + nx] = rope_x
            sel = np.zeros((NE, NE // 2), np.float32)
            sel[np.arange(NE // 2) + (NE // 2) * r, np.arange(NE // 2)] = 1.0
            m = {"xc": xc, "cb": f(inputs["c"][b]), "c_ctx": f(inputs["c_ctx"]), "ident": ident, "rope": rope_r, "cmask": cm,
                 "jrev": np.ascontiguousarray(ident[::-1]), "sel": sel}
            for k in ["w_ada", "b_ada", "norm1_g", "norm2_g", "w_in", "q_a_g", "w_qb", "kv_a_g", "w_kvb", "q_norm_g",
                      "k_norm_g", "w_mla_o", "ssm_d", "w_glu", "b_glu", "w_ssm_o", "w_out", "w_router"]:
                m[k] = f(np.asarray(inputs[k])[0])
            for k in dirk:
                a_ = np.asarray(inputs[k])[0]
                m[k] = f(a_[::-1] if r == 1 else a_)
            e0 = (NE // 2) * r
            for k in ("w_e_gate", "w_e_up", "w_e_down"):
                m[k] = f(np.asarray(inputs[k])[0][e0:e0 + NE // 2])
            maps.append(m)
    return maps


def assemble(results, nx=NX):
    h = nx // 2
    out = np.zeros((NCORES // 2, nx, D), np.float32)
    for b in range(NCORES // 2):
        out[b, :h] = np.asarray(results[2 * b]["out"], dtype=np.float32)[:h]
        out[b, h:] = np.asarray(results[2 * b + 1]["out"], dtype=np.float32)[:h][::-1]
    return out


def kernel(**inputs):
    nc = build()
    maps = make_in_maps(inputs)
    res = run_bass_kernel_spmd(nc, maps, core_ids=list(range(NCORES)))
    return assemble(res.results)
```

```python
import math
import os
from contextlib import ExitStack

import numpy as np
import concourse.bass as bass
import concourse.mybir as mybir
from concourse.bass_utils import run_bass_kernel_spmd

F32 = mybir.dt.float32
F32R = mybir.dt.float32r
BF16 = mybir.dt.bfloat16
U32 = mybir.dt.uint32
I32 = mybir.dt.int32
ALU = mybir.AluOpType
AF = mybir.ActivationFunctionType
AX = mybir.AxisListType

D = 1024
NX = 4096
NCTX = 256
NT = NX + NCTX
NTILE = NT // 128
NCH = NT // 8
NCH_C = NCTX // 8
H = 8
QK = 96
NE = 16
FF = 2816
CAP = 512
EPS = 1e-6
IN_COLS = 3232
NCORES = 8

ENG = {"pe": "tensor", "act": "scalar", "dve": "vector", "pool": "gpsimd", "sp": "sync"}


class Prog:
    def __init__(self, nc, sems, dsems):
        self.nc = nc
        self.ops = []
        self.lastw = {}
        self.readers = {}
        self.sems = sems
        self.dsems = dsems
        self.sig = {e: 0 for e in ENG}
        self.dcnt = {e: [0] * len(dsems[e]) for e in dsems}
        self.dnext = {e: 0 for e in dsems}
        self.seen = {e: {} for e in ENG}
        self.emitted = 0
        self.ccsem = None
        self.cccnt = 0

    def op(self, eng, fn, r=(), w=(), dma=False):
        i = len(self.ops)
        deps = set()
        for k in list(r) + list(w):
            if k in self.lastw:
                deps.add(self.lastw[k])
        for k in w:
            for j in self.readers.get(k, ()):
                deps.add(j)
        deps.discard(i)
        o = dict(eng=eng, fn=fn, deps=deps, dma=dma, need=False, val=None, sem=None, idx=i)
        self.ops.append(o)
        for k in w:
            self.lastw[k] = i
            self.readers[k] = []
        for k in r:
            if k not in w:
                self.readers.setdefault(k, []).append(i)
        return i

    def dmaop(self, eng, fn, r=(), w=()):
        return self.op(eng, fn, r, w, dma=True)

    def ccop(self, fn, r=(), w=()):
        return self.op("pool", fn, r, w, dma="cc")

    def flush(self, final_wait_eng="sp"):
        nc = self.nc
        ops = self.ops[self.emitted:]
        pos = {}
        cnt = {e: 0 for e in ENG}
        for o in self.ops[:self.emitted]:
            pass
        for o in ops:
            pos[o["idx"]] = cnt[o["eng"]]
            cnt[o["eng"]] += 1
        for o in ops:
            real = []
            for d in o["deps"]:
                if d < self.emitted:
                    continue
                p = self.ops[d]
                if not p["dma"] and p["eng"] == o["eng"]:
                    if o["eng"] == "pe":
                        continue
                    if o["dma"]:
                        continue
                real.append(d)
                p["need"] = True
            o["real"] = real
        for o in ops:
            if o["dma"]:
                for d in o["deps"]:
                    if d >= self.emitted:
                        p = self.ops[d]
                        if not p["dma"] and p["eng"] == o["eng"] and d not in o["real"]:
                            o["real"].append(d)
                            p["need"] = True
        for o in ops:
            if o["dma"] == "cc":
                o["prev"] = self.cccnt
                self.cccnt += 1
                o["sem"] = self.ccsem
                o["val"] = self.cccnt
            elif o["dma"]:
                e = o["eng"]
                k = self.dnext[e]
                self.dnext[e] = (k + 1) % len(self.dsems[e])
                o["prev"] = self.dcnt[e][k]
                self.dcnt[e][k] += 16
                o["sem"] = self.dsems[e][k]
                o["val"] = self.dcnt[e][k]
            elif o["need"]:
                self.sig[o["eng"]] += 1
                o["sem"] = self.sems[o["eng"]]
                o["val"] = self.sig[o["eng"]]
        byeng = {e: [o for o in ops if o["eng"] == e] for e in ENG}
        self_ = self
        seen = self.seen
        allops = self.ops
        dsems = self.dsems
        dcnt = self.dcnt

        def body(e):
            def f(engine):
                sn = seen[e]

                def wait(sem, val):
                    key = id(sem)
                    if sn.get(key, 0) >= val:
                        return
                    sn[key] = val
                    engine.wait_ge(sem, val)

                for o in byeng[e]:
                    for d in o["real"]:
                        p = allops[d]
                        wait(p["sem"], p["val"])
                    if o["dma"]:
                        if o["prev"] > 0:
                            wait(o["sem"], o["prev"])
                        ins = o["fn"](engine)
                        if o["dma"] == "cc":
                            ins.then_inc(o["sem"])
                        else:
                            ins.then_inc(o["sem"], 16)
                    else:
                        ins = o["fn"](engine)
                        if o["need"]:
                            ins.then_inc(o["sem"], 1)
                if e in dsems:
                    for k, s in enumerate(dsems[e]):
                        if dcnt[e][k] > 0:
                            wait(s, dcnt[e][k])
                if e == "pool" and self_.cccnt > 0:
                    wait(self_.ccsem, self_.cccnt)
            return f

        with nc.Block() as block:
            for e in ENG:
                getattr(block, ENG[e])(body(e))
        self.emitted = len(self.ops)
        self.lastw = {}
        self.readers = {}


class Keyed:
    def __init__(self, P, slot, shared, alias=None):
        self.P, self.slot, self.shared, self.alias = P, slot, set(shared), (alias or {})
        self.cap = []

    def _k(self, keys):
        out = []
        for k in keys:
            k = self.alias.get(k, k)
            base = k[0] if isinstance(k, tuple) else k
            out.append(k if base in self.shared else ("slot", self.slot, k))
        return out

    def op(self, eng, fn, r=(), w=(), dma=False):
        self.cap.append((eng, fn, self._k(r), self._k(w), dma))

    def dmaop(self, eng, fn, r=(), w=()):
        self.op(eng, fn, r, w, dma=True)


def interleave(P, caps, chunk=3):
    pos = [0] * len(caps)
    live = True
    while live:
        live = False
        for i, c in enumerate(caps):
            n = 0
            while pos[i] < len(c) and n < chunk:
                eng, fn, r, w, dma = c[pos[i]]
                P.op(eng, fn, r, w, dma=dma)
                pos[i] += 1
                n += 1
            if pos[i] < len(c):
                live = True


def r32(ap):
    return ap.bitcast(F32R)


def build(debug=0):
    nc = bass.Bass("TRN2", target_bir_lowering=False)
    es = ExitStack()
    DONE = []

    def din(name, shape, dt=F32):
        return nc.dram_tensor(name, list(shape), dt, kind="ExternalInput").ap()

    def dscr(name, shape, dt=F32):
        return nc.dram_tensor(name, list(shape), dt, kind="Internal").ap()

    xc = din("xc", [NT, D])
    cb = din("cb", [D])
    cctx = din("c_ctx", [D])
    w_ada = din("w_ada", [D, 6 * D])
    b_ada = din("b_ada", [6 * D])
    norm1_g = din("norm1_g", [D])
    norm2_g = din("norm2_g", [D])
    w_in = din("w_in", [D, IN_COLS])
    q_a_g = din("q_a_g", [384])
    w_qb = din("w_qb", [384, 768])
    kv_a_g = din("kv_a_g", [256])
    w_kvb = din("w_kvb", [256, 1024])
    q_norm_g = din("q_norm_g", [96])
    k_norm_g = din("k_norm_g", [96])
    w_mla_o = din("w_mla_o", [512, D])
    lam_re = din("ssm_lam_re", [2, 32, 64])
    lam_im = din("ssm_lam_im", [2, 32, 64])
    log_dt = din("ssm_log_dt", [2, 32])
    b_re = din("ssm_b_re", [2, 32, 64, 16])
    b_im = din("ssm_b_im", [2, 32, 64, 16])
    c_re = din("ssm_c_re", [2, 32, 16, 64])
    c_im = din("ssm_c_im", [2, 32, 16, 64])
    ssm_d = din("ssm_d", [512])
    w_glu = din("w_glu", [512, 512])
    b_glu = din("b_glu", [512])
    w_ssm_o = din("w_ssm_o", [512, D])
    w_out = din("w_out", [D, D])
    w_router = din("w_router", [D, NE])
    if debug in (0, 6):
        w_e_gate = din("w_e_gate", [NE // 2, D, FF])
        w_e_up = din("w_e_up", [NE // 2, D, FF])
        w_e_down = din("w_e_down", [NE // 2, FF, D])
    sel_d = din("sel", [NE, NE // 2])
    ident_d = din("ident", [128, 128])
    rope_d = din("rope", [NT, 32])
    jrev_d = din("jrev", [128, 128])
    cmask_d = din("cmask", [128, 256])
    small = debug in (2, 3, 4, 5, 6)
    NTe = 4 if small else NTILE
    NXe = (NTe - 2) * 128
    NXO = NXe // 2
    NTO = NXO // 128
    out = nc.dram_tensor("out", [NXO, D], F32, kind="ExternalOutput").ap()

    u_s = dscr("u_s", [NT, 512])
    gates_s = dscr("gates_s", [NXO, 2048])
    qT_s = dscr("qT_s", [H, QK, NXO], BF16)
    kT_s = dscr("kT_s", [H, QK, NT], BF16)
    v_s = dscr("v_s", [NT, 512], BF16)
    attnT_s = dscr("attnT_s", [H, 64, NXO])
    y_s = dscr("y_s", [NXO // 8, 8 * 512], BF16)
    xm_s = dscr("xm_s", [NXO, D])
    NCHK = 2 if NXO >= 2048 else 1
    RCH = NXO // NCHK
    h2b_own_c = [nc.dram_tensor("h2b_own%d" % c, [RCH, D], BF16) for c in range(NCHK)]
    h2b_ag_c = [nc.dram_tensor("h2b_ag%d" % c, [2 * RCH, D], BF16) for c in range(NCHK)]
    h2b_all_t = nc.dram_tensor("h2b_all", [2 * NXO, D], BF16)
    aff_own_t = nc.dram_tensor("aff_own", [NE, NXO], F32)
    aff_all_t = nc.dram_tensor("aff_all", [2 * NE, NXO], F32)
    acc_t = nc.dram_tensor("acc", [2 * NXO, D], F32)
    rs_out_t = nc.dram_tensor("rs_out", [NXO, D], F32)
    h2b_all, aff_own, aff_all, acc, rs_out = (t.ap() for t in (h2b_all_t, aff_own_t, aff_all_t, acc_t, rs_out_t))
    PAIRS = [[0, 1], [2, 3], [4, 5], [6, 7]]
    h2_s = dscr("h2_s", [NX, D])

    sems = {e: es.enter_context(nc.semaphore("s_" + e)) for e in ENG}
    dsems = {e: [es.enter_context(nc.semaphore("d_%s%d" % (e, k))) for k in range(8)]
             for e in ("sp", "act", "pool")}
    P = Prog(nc, sems, dsems)
    P.ccsem = es.enter_context(nc.semaphore("cc_sem"))

    def sb(stack, name, shape, dt=F32):
        return stack.enter_context(nc.sbuf_tensor("t_" + name, list(shape), dt))

    def ps(stack, name, shape=(128, 512), dt=F32):
        return stack.enter_context(nc.psum_tensor("p_" + name, list(shape), dt))

    ident = sb(es, "ident", [128, 128])
    modx = sb(es, "modx", [128, 6 * D])
    es1 = ExitStack()
    modc = sb(es1, "modc", [128, 2 * D])
    P.dmaop("sp", lambda e: e.dma_start(out=ident[:], in_=ident_d), w=["ident"])

    with ExitStack() as st:
        cT = sb(st, "cT", [128, 2, 8])
        sc = sb(st, "sc", [128, 2, 8])
        lbc = sb(st, "lbc", [128, 2, 8, 128], BF16)
        wa = sb(st, "wa", [128, 2, 8, 512], BF16)
        bb = sb(st, "bb", [128, 6 * D])
        g1b = sb(st, "g1b", [128, D])
        g2b = sb(st, "g2b", [128, D])
        pm = [ps(st, "pm%d" % i) for i in range(4)]
        P.dmaop("sp", lambda e: e.dma_start(out=cT[:, 0, :], in_=cb.rearrange("(dc p) -> p dc", p=128),
                                            allow_slow_non_contiguous=True), w=["cT0"])
        P.dmaop("sp", lambda e: e.dma_start(out=cT[:, 1, :], in_=cctx.rearrange("(dc p) -> p dc", p=128),
                                            allow_slow_non_contiguous=True), w=["cT1"])
        P.dmaop("act", lambda e: e.dma_start(out=bb[:], in_=b_ada.partition_broadcast(128)), w=["bb"])
        P.dmaop("act", lambda e: e.dma_start(out=g1b[:], in_=norm1_g.partition_broadcast(128)), w=["g1b"])
        P.dmaop("act", lambda e: e.dma_start(out=g2b[:], in_=norm2_g.partition_broadcast(128)), w=["g2b"])
        P.op("act", lambda e: e.activation(out=sc[:], in_=cT[:], func=AF.Silu), r=["cT0", "cT1"], w=["sc"])
        P.op("dve", lambda e: e.tensor_copy(out=lbc[:], in_=sc[:].unsqueeze(3).to_broadcast([128, 2, 8, 128])),
             r=["sc"], w=["lbc"])
        wv = w_ada.rearrange("(dc p) n -> p dc n", p=128)
        for ct in range(12):
            s = ct % 2
            P.dmaop("pool",
                    lambda e, ct=ct, s=s: e.dma_start(out=wa[:, s], in_=wv[:, :, ct * 512:(ct + 1) * 512]),
                    w=[("wa", s)])
            for which in range(2 if ct < 4 else 1):
                pt = pm[(ct * 2 + which) % 4]
                pk = ("pm", (ct * 2 + which) % 4)

                def mm(e, pt=pt, s=s, which=which):
                    ins = None
                    for dc in range(8):
                        ins = e.matmul(pt[:], lhsT=lbc[:, which, dc, :], rhs=wa[:, s, dc, :],
                                       start=(dc == 0), stop=(dc == 7))
                    return ins
                P.op("pe", mm, r=["lbc", ("wa", s)], w=[pk])
                dst = modx if which == 0 else modc
                P.op("dve", lambda e, pt=pt, dst=dst, ct=ct: e.tensor_tensor(
                    out=dst[:, ct * 512:(ct + 1) * 512], in0=pt[:], in1=bb[:, ct * 512:(ct + 1) * 512], op=ALU.add),
                    r=[pk, "bb"], w=["modx" if which == 0 else "modc"])
        P.op("dve", lambda e: e.scalar_tensor_tensor(out=modx[:, D:2 * D], in0=modx[:, D:2 * D], scalar=1.0, in1=g1b[:],
                                                     op0=ALU.add, op1=ALU.mult), r=["modx", "g1b"], w=["modx"])
        P.op("dve", lambda e: e.scalar_tensor_tensor(out=modx[:, 4 * D:5 * D], in0=modx[:, 4 * D:5 * D], scalar=1.0, in1=g2b[:],
                                                     op0=ALU.add, op1=ALU.mult), r=["modx", "g2b"], w=["modx"])
        P.op("dve", lambda e: e.scalar_tensor_tensor(out=modc[:, D:2 * D], in0=modc[:, D:2 * D], scalar=1.0, in1=g1b[:],
                                                     op0=ALU.add, op1=ALU.mult), r=["modc", "g1b"], w=["modc"])
        P.flush()


    with ExitStack() as st:
      if debug != 1:
          TL1 = [dict(), dict()]
          w_in_sb = sb(st, "w_in_sb", [128, 8, IN_COLS], BF16)
          w_qb_sb = sb(st, "w_qb_sb", [128, 3, 768], BF16)
          w_kvb_sb = sb(st, "w_kvb_sb", [128, 2, 1024], BF16)
          qag = sb(st, "qag", [128, 384])
          kvag = sb(st, "kvag", [128, 256])
          qng = sb(st, "qng", [128, 96])
          kng = sb(st, "kng", [128, 96])
          identb = sb(st, "identb", [128, 128], BF16)
          xt = sb(st, "xt", [128, 2, D])
          TL1[0]['junk'] = sb(st, "junk_0", [128, D]); TL1[1]['junk'] = sb(st, "junk_1", [128, D])
          TL1[0]['hh'] = sb(st, "hh_0", [128, D]); TL1[1]['hh'] = sb(st, "hh_1", [128, D])
          TL1[0]['hb'] = sb(st, "hb_0", [128, D], BF16); TL1[1]['hb'] = sb(st, "hb_1", [128, D], BF16)
          TL1[0]['hT'] = sb(st, "hT_0", [128, 8, 128], BF16); TL1[1]['hT'] = sb(st, "hT_1", [128, 8, 128], BF16)
          proj = sb(st, "proj", [128, 2, IN_COLS])
          st8 = sb(st, "st8", [128, 2, 64])
          TL1[0]['qn'] = sb(st, "qn_0", [128, 384], BF16); TL1[1]['qn'] = sb(st, "qn_1", [128, 384], BF16)
          TL1[0]['qnT'] = sb(st, "qnT_0", [128, 3, 128], BF16); TL1[1]['qnT'] = sb(st, "qnT_1", [128, 3, 128], BF16)
          TL1[0]['kvn'] = sb(st, "kvn_0", [128, 256], BF16); TL1[1]['kvn'] = sb(st, "kvn_1", [128, 256], BF16)
          TL1[0]['kvnT'] = sb(st, "kvnT_0", [128, 2, 128], BF16); TL1[1]['kvnT'] = sb(st, "kvnT_1", [128, 2, 128], BF16)
          TL1[0]['qsq'] = sb(st, "qsq_0", [128, 768]); TL1[1]['qsq'] = sb(st, "qsq_1", [128, 768])
          TL1[0]['qf'] = sb(st, "qf_0", [128, 8, 96]); TL1[1]['qf'] = sb(st, "qf_1", [128, 8, 96])
          TL1[0]['qb'] = sb(st, "qb_0", [128, 8, 96], BF16); TL1[1]['qb'] = sb(st, "qb_1", [128, 8, 96], BF16)
          TL1[0]['kf'] = sb(st, "kf_0", [128, 8, 96]); TL1[1]['kf'] = sb(st, "kf_1", [128, 8, 96])
          TL1[0]['kb'] = sb(st, "kb_0", [128, 8, 96], BF16); TL1[1]['kb'] = sb(st, "kb_1", [128, 8, 96], BF16)
          vb = sb(st, "vb", [128, 2, 512], BF16)
          TL1[0]['kvs'] = sb(st, "kvs_0", [128, 1024]); TL1[1]['kvs'] = sb(st, "kvs_1", [128, 1024])
          rp = sb(st, "rp", [128, 2, 32])
          TL1[0]['rt'] = sb(st, "rt_0", [128, 6, 128]); TL1[1]['rt'] = sb(st, "rt_1", [128, 6, 128])
          TL1[0]['krg'] = sb(st, "krg_0", [128, 32]); TL1[1]['krg'] = sb(st, "krg_1", [128, 32])
          TL1[0]['krr'] = sb(st, "krr_0", [128, 32]); TL1[1]['krr'] = sb(st, "krr_1", [128, 32])
          TL1[0]['qTt'] = sb(st, "qTt_0", [128, 8, 128], BF16); TL1[1]['qTt'] = sb(st, "qTt_1", [128, 8, 128], BF16)
          TL1[0]['kTt'] = sb(st, "kTt_0", [128, 8, 128], BF16); TL1[1]['kTt'] = sb(st, "kTt_1", [128, 8, 128], BF16)
          PSALL = [ps(st, "ph1_%d" % i) for i in range(8)]

          P.dmaop("pool", lambda e: e.dma_start(out=w_in_sb[:], in_=w_in.rearrange("(dc p) n -> p dc n", p=128)), w=["w_in_sb"])
          P.dmaop("pool", lambda e: e.dma_start(out=w_qb_sb[:], in_=w_qb.rearrange("(dc p) n -> p dc n", p=128)), w=["w_qb_sb"])
          P.dmaop("pool", lambda e: e.dma_start(out=w_kvb_sb[:], in_=w_kvb.rearrange("(dc p) n -> p dc n", p=128)), w=["w_kvb_sb"])
          P.dmaop("act", lambda e: e.dma_start(out=qag[:], in_=q_a_g.partition_broadcast(128)), w=["qag"])
          P.dmaop("act", lambda e: e.dma_start(out=kvag[:], in_=kv_a_g.partition_broadcast(128)), w=["kvag"])
          P.dmaop("act", lambda e: e.dma_start(out=qng[:], in_=q_norm_g.partition_broadcast(128)), w=["qng"])
          P.dmaop("act", lambda e: e.dma_start(out=kng[:], in_=k_norm_g.partition_broadcast(128)), w=["kng"])
          P.op("dve", lambda e: e.tensor_scalar(out=qng[:], in0=qng[:], scalar1=float(QK ** -0.5), scalar2=None, op0=ALU.mult),
               r=["qng"], w=["qng"])
          P.op("dve", lambda e: e.tensor_copy(out=identb[:], in_=ident[:]), r=["ident"], w=["identb"])

          def rstd(Pq, src_key, src_ap, n, dst_ap, dst_key):
              Pq.op("dve", lambda e: e.tensor_scalar(out=dst_ap, in0=src_ap, scalar1=1.0 / n, scalar2=EPS, op0=ALU.mult, op1=ALU.add),
                   r=[src_key], w=[dst_key])
              Pq.op("act", lambda e: e.activation(out=dst_ap, in_=dst_ap, func=AF.Sqrt), r=[dst_key], w=[dst_key])
              Pq.op("dve", lambda e: e.reciprocal(out=dst_ap, in_=dst_ap), r=[dst_key], w=[dst_key])

          def rope(Pq, rt, src, dst, tab, nh, tag, eng="pool"):
              sv = src.rearrange("p h (a t) -> p h a t", a=2)
              dv = dst.rearrange("p h (a t) -> p h a t", a=2)
              cosb = tab[:, 0:16].rearrange("p (a t) -> p a t", a=2).unsqueeze(1).to_broadcast([128, nh, 2, 8])
              sinb = tab[:, 16:32].rearrange("p (a t) -> p a t", a=2).unsqueeze(1).to_broadcast([128, nh, 2, 8])
              v0 = sv[:, :, :, 0:8]
              v1 = sv[:, :, :, 8:16]
              n = nh * 16
              T = [rt[:, k, 0:n].rearrange("p (h a t) -> p h a t", h=nh, a=2) for k in range(4)]
              rk = ("rt", tag)
              Pq.op(eng, lambda e: e.tensor_tensor(out=T[0], in0=v0, in1=cosb, op=ALU.mult), r=[tag + "_src", tag + "_tab"], w=[rk + (0,)])
              Pq.op(eng, lambda e: e.tensor_tensor(out=T[1], in0=v1, in1=sinb, op=ALU.mult), r=[tag + "_src", tag + "_tab"], w=[rk + (1,)])
              Pq.op(eng, lambda e: e.tensor_tensor(out=T[2], in0=v1, in1=cosb, op=ALU.mult), r=[tag + "_src", tag + "_tab"], w=[rk + (2,)])
              Pq.op(eng, lambda e: e.tensor_tensor(out=T[3], in0=v0, in1=sinb, op=ALU.mult), r=[tag + "_src", tag + "_tab"], w=[rk + (3,)])
              Pq.op(eng, lambda e: e.tensor_tensor(out=dv[:, :, :, 0:8], in0=T[0], in1=T[1], op=ALU.subtract),
                   r=[rk + (0,), rk + (1,)], w=[tag + "_dst"])
              Pq.op(eng, lambda e: e.tensor_tensor(out=dv[:, :, :, 8:16], in0=T[2], in1=T[3], op=ALU.add),
                   r=[rk + (2,), rk + (3,)], w=[tag + "_dst"])

          ntile1 = NTe
          import os
          STG = int(os.environ.get('PH1_STAGE', '9'))
          SHARED1 = ["w_in_sb", "w_qb_sb", "w_kvb_sb", "qag", "kvag", "qng", "kng", "identb", "ident", "modx", "modc", "u_s", "gates_s", "qT_s", "kT_s", "v_s"]
          ALIAS1 = {("ps", 1): ("ps", 0), "ps3": "ps2", ("ps", 6): ("ps", 4), ("ps", 7): ("ps", 5)}

          def tile1(i, s):
              Pq = Keyed(P, s, SHARED1, ALIAS1)
              junk = TL1[s]['junk']
              hh = TL1[s]['hh']
              hb = TL1[s]['hb']
              hT = TL1[s]['hT']
              qn = TL1[s]['qn']
              qnT = TL1[s]['qnT']
              kvn = TL1[s]['kvn']
              kvnT = TL1[s]['kvnT']
              qsq = TL1[s]['qsq']
              qf = TL1[s]['qf']
              qb = TL1[s]['qb']
              kf = TL1[s]['kf']
              kb = TL1[s]['kb']
              kvs = TL1[s]['kvs']
              rt = TL1[s]['rt']
              krg = TL1[s]['krg']
              krr = TL1[s]['krr']
              qTt = TL1[s]['qTt']
              kTt = TL1[s]['kTt']
              bk = PSALL[4 * s:4 * s + 4]
              PS = [bk[0], bk[0], bk[1], bk[1], bk[2], bk[3], bk[2], bk[3]]
              PSb2 = PS[2][:].bitcast(BF16)
              PSb3 = PS[3][:].bitcast(BF16)
              isx = i >= 2
              own = 2 <= i < 2 + NTO
              t0 = i * 128
              xq = t0 - NCTX
              G = (modx if isx else modc)[:, D:2 * D]
              SH = (modx if isx else modc)[:, 0:D]
              mk = "modx" if isx else "modc"
              Pq.dmaop("sp", lambda e, s=s, t0=t0: e.dma_start(out=xt[:, s, :], in_=xc[t0:t0 + 128, :]), w=[("xt", s)])
              Pq.dmaop("sp", lambda e, s=s, t0=t0: e.dma_start(out=rp[:, s, :], in_=rope_d[t0:t0 + 128, :]), w=[("rp", s)])
              Pq.op("act", lambda e, s=s: e.activation(out=junk[:], in_=xt[:, s, :], func=AF.Square, accum_out=st8[:, s, 0:1]),
                   r=[("xt", s)], w=["junk", ("st", s, 0)])
              rstd(Pq, ("st", s, 0), st8[:, s, 0:1], D, st8[:, s, 1:2], ("st", s, 1))
              Pq.op("dve", lambda e, s=s, G=G: e.scalar_tensor_tensor(out=hh[:], in0=xt[:, s, :], scalar=st8[:, s, 1:2], in1=G,
                                                                  op0=ALU.mult, op1=ALU.mult),
                   r=[("xt", s), ("st", s, 1), mk], w=["hh"])
              Pq.op("pool", lambda e, SH=SH: e.tensor_tensor(out=hb[:], in0=hh[:], in1=SH, op=ALU.add), r=["hh", mk], w=["hb"])

              def tr_h(e):
                  ins = None
                  for dc in range(8):
                      ins = e.transpose(out=PSb2[:, dc * 128:(dc + 1) * 128], in_=hb[:, dc * 128:(dc + 1) * 128], identity=identb[:])
                  return ins
              Pq.op("pe", tr_h, r=["hb", "identb"], w=["ps2"])
              Pq.op("act", lambda e: e.copy(out=hT[:].rearrange("p a b -> p (a b)"), in_=PSb2[:, 0:1024]), r=["ps2"], w=["hT"])
              segs = ([(c, c * 512, min(512, IN_COLS - c * 512), [c]) for c in range(7)] if own
                      else [(0, 0, 512, [0]), (1, 896, 288, [1, 2])])
              for (ctile, c0, n, wkeys) in segs:
                  pk = ctile % 2

                  def mmin(e, c0=c0, n=n, pk=pk):
                      ins = None
                      for dc in range(8):
                          ins = e.matmul(PS[pk][:, 0:n], lhsT=hT[:, dc, :], rhs=w_in_sb[:, dc, c0:c0 + n], start=(dc == 0), stop=(dc == 7))
                      return ins
                  Pq.op("pe", mmin, r=["hT", "w_in_sb"], w=[("ps", pk)])
                  if ctile % 2 == 0:
                      Pq.op("dve", lambda e, c0=c0, n=n, pk=pk, s=s: e.tensor_copy(out=proj[:, s, c0:c0 + n], in_=PS[pk][:, 0:n]),
                           r=[("ps", pk)], w=[("proj", s, c_) for c_ in wkeys])
                  else:
                      Pq.op("act", lambda e, c0=c0, n=n, pk=pk, s=s: e.copy(out=proj[:, s, c0:c0 + n], in_=PS[pk][:, 0:n]),
                           r=[("ps", pk)], w=[("proj", s, c_) for c_ in wkeys])
              pj = [("proj", s, c) for c in range(7)]
              Pq.dmaop("sp", lambda e, s=s, t0=t0: e.dma_start(out=u_s[t0:t0 + 128, :], in_=proj[:, s, 0:512]), r=[pj[0]], w=["u_s"])
              if own:
                  Pq.op("act", lambda e, s=s: e.activation(out=proj[:, s, 1184:3232], in_=proj[:, s, 1184:3232], func=AF.Sigmoid),
                        r=pj[2:], w=pj[2:])
                  Pq.dmaop("sp", lambda e, xq=xq, s=s: e.dma_start(out=gates_s[xq:xq + 128, :], in_=proj[:, s, 1184:3232]), r=pj[2:], w=["gates_s"])
                  Pq.op("act", lambda e, s=s: e.activation(out=junk[:, 0:384], in_=proj[:, s, 512:896], func=AF.Square,
                                                          accum_out=st8[:, s, 2:3]), r=[pj[1]], w=["junk", ("st", s, 2)])
                  rstd(Pq, ("st", s, 2), st8[:, s, 2:3], 384, st8[:, s, 3:4], ("st", s, 3))
                  Pq.op("dve", lambda e, s=s: e.scalar_tensor_tensor(out=qn[:], in0=proj[:, s, 512:896], scalar=st8[:, s, 3:4], in1=qag[:],
                                                                    op0=ALU.mult, op1=ALU.mult), r=[pj[1], ("st", s, 3), "qag"], w=["qn"])

                  def tr_q(e):
                      ins = None
                      for k in range(3):
                          ins = e.transpose(out=PSb2[:, k * 128:(k + 1) * 128], in_=qn[:, k * 128:(k + 1) * 128], identity=identb[:])
                      return ins
                  Pq.op("pe", tr_q, r=["qn", "identb"], w=["ps2"])
                  Pq.op("dve", lambda e: e.tensor_copy(out=qnT[:].rearrange("p a b -> p (a b)"), in_=PSb2[:, 0:384]), r=["ps2"], w=["qnT"])

                  def mmq(e):
                      ins = None
                      for (pi, c0, n) in ((4, 0, 512), (5, 512, 256)):
                          for k in range(3):
                              ins = e.matmul(PS[pi][:, 0:n], lhsT=qnT[:, k, :], rhs=w_qb_sb[:, k, c0:c0 + n], start=(k == 0), stop=(k == 2))
                      return ins
                  Pq.op("pe", mmq, r=["qnT", "w_qb_sb"], w=[("ps", 4), ("ps", 5)])
                  qfl = qf[:].rearrange("p h d -> p (h d)")
                  Pq.op("act", lambda e: e.copy(out=qfl[:, 0:512], in_=PS[4][:, 0:512]), r=[("ps", 4)], w=["qf"])
                  Pq.op("act", lambda e: e.copy(out=qfl[:, 512:768], in_=PS[5][:, 0:256]), r=[("ps", 5)], w=["qf"])
                  Pq.op("pool", lambda e: e.tensor_tensor(out=qsq[:], in0=qfl, in1=qfl, op=ALU.mult), r=["qf"], w=["qsq"])
                  Pq.op("dve", lambda e, s=s: e.tensor_reduce(out=st8[:, s, 8:16], in_=qsq[:].rearrange("p (h d) -> p h d", h=8),
                                                             axis=AX.X, op=ALU.add), r=["qsq"], w=[("st", s, 8)])
                  rstd(Pq, ("st", s, 8), st8[:, s, 8:16], QK, st8[:, s, 16:24], ("st", s, 16))
                  Pq.op("dve", lambda e, s=s: e.tensor_tensor(out=qf[:], in0=qf[:], in1=st8[:, s, 16:24].unsqueeze(2).to_broadcast([128, 8, 96]),
                                                             op=ALU.mult), r=["qf", ("st", s, 16)], w=["qf"])
                  Pq.op("dve", lambda e: e.tensor_tensor(out=qf[:], in0=qf[:], in1=qng[:].unsqueeze(1).to_broadcast([128, 8, 96]),
                                                        op=ALU.mult), r=["qf", "qng"], w=["qf", "q_src"])
                  Pq.op("act", lambda e: e.copy(out=qb[:, :, 0:64], in_=qf[:, :, 0:64]), r=["qf"], w=["qb"])
                  Pq.op("pool", lambda e, s=s: e.tensor_copy(out=rt[:, 5, 0:32], in_=rp[:, s, :]), r=[("rp", s)], w=["q_tab"])
                  rope(Pq, rt, qf[:, :, 64:96], qb[:, :, 64:96], rt[:, 5, 0:32], 8, "q")

                  def tr_qh(e):
                      ins = None
                      for h in range(8):
                          ins = e.transpose(out=PSb3[0:96, h * 128:(h + 1) * 128], in_=qb[:, h, :], identity=identb[:])
                      return ins
                  Pq.op("pe", tr_qh, r=["qb", "q_dst", "identb"], w=["ps3"])
                  Pq.op("act", lambda e: e.copy(out=qTt[0:96].rearrange("p a b -> p (a b)"), in_=PSb3[0:96, 0:1024]), r=["ps3"], w=["qTt"])
                  Pq.dmaop("act", lambda e, xq=xq: e.dma_start(out=qT_s[:, :, xq:xq + 128].rearrange("h d t -> d h t"), in_=qTt[0:96]),
                          r=["qTt"], w=["qT_s"])
              Pq.op("act", lambda e, s=s: e.activation(out=junk[:, 0:256], in_=proj[:, s, 896:1152], func=AF.Square,
                                                      accum_out=st8[:, s, 4:5]), r=[pj[1], pj[2]], w=["junk", ("st", s, 4)])
              rstd(Pq, ("st", s, 4), st8[:, s, 4:5], 256, st8[:, s, 5:6], ("st", s, 5))
              Pq.op("dve", lambda e, s=s: e.scalar_tensor_tensor(out=kvn[:], in0=proj[:, s, 896:1152], scalar=st8[:, s, 5:6], in1=kvag[:],
                                                                op0=ALU.mult, op1=ALU.mult), r=[pj[1], pj[2], ("st", s, 5), "kvag"], w=["kvn"])

              def tr_kv(e):
                  ins = None
                  for k in range(2):
                      ins = e.transpose(out=PSb2[:, k * 128:(k + 1) * 128], in_=kvn[:, k * 128:(k + 1) * 128], identity=identb[:])
                  return ins
              Pq.op("pe", tr_kv, r=["kvn", "identb"], w=["ps2"])
              Pq.op("dve", lambda e: e.tensor_copy(out=kvnT[:].rearrange("p a b -> p (a b)"), in_=PSb2[:, 0:256]), r=["ps2"], w=["kvnT"])

              def mmkv(e):
                  ins = None
                  for (pi, c0) in ((6, 0), (7, 512)):
                      for k in range(2):
                          ins = e.matmul(PS[pi][:, 0:512], lhsT=kvnT[:, k, :], rhs=w_kvb_sb[:, k, c0:c0 + 512], start=(k == 0), stop=(k == 1))
                  return ins
              Pq.op("pe", mmkv, r=["kvnT", "w_kvb_sb"], w=[("ps", 6), ("ps", 7)])
              for half in range(2):
                  (Pq.op("act", lambda e, half=half: e.copy(out=kvs[:, half * 512:(half + 1) * 512], in_=PS[6 + half][:, 0:512]),
                        r=[("ps", 6 + half)], w=[("kvs", half)]) if half == 0 else
                   Pq.op("dve", lambda e, half=half: e.tensor_copy(out=kvs[:, half * 512:(half + 1) * 512], in_=PS[6 + half][:, 0:512]),
                        r=[("ps", 6 + half)], w=[("kvs", half)]))
              kvs3 = kvs[:].rearrange("p (h d) -> p h d", h=8)
              kvk = [("kvs", 0), ("kvs", 1)]
              Pq.op("pool", lambda e, s=s: e.tensor_copy(out=vb[:, s].rearrange("p (h d) -> p h d", h=8), in_=kvs3[:, :, 64:128]),
                   r=kvk, w=[("vb", s)])
              Pq.dmaop("sp", lambda e, s=s, t0=t0: e.dma_start(out=v_s[t0:t0 + 128, :], in_=vb[:, s]),
                      r=[("vb", s)], w=["v_s"])
              Pq.op("act", lambda e: e.copy(out=kf[:, :, 0:64], in_=kvs3[:, :, 0:64]), r=kvk, w=[("kf", 0), ("kf", 1)])
              kfk = [("kf", 0), ("kf", 1)]
              Pq.op("pool", lambda e: e.tensor_tensor(out=qsq[:, 0:512].rearrange("p (h d) -> p h d", h=8), in0=kf[:, :, 0:64], in1=kf[:, :, 0:64],
                                                     op=ALU.mult), r=kfk, w=["qsq"])
              Pq.op("dve", lambda e, s=s: e.tensor_reduce(out=st8[:, s, 24:32], in_=qsq[:, 0:512].rearrange("p (h d) -> p h d", h=8),
                                                         axis=AX.X, op=ALU.add), r=["qsq"], w=[("st", s, 24)])
              Pq.op("act", lambda e, s=s: e.activation(out=junk[:, 0:32], in_=proj[:, s, 1152:1184], func=AF.Square,
                                                      accum_out=st8[:, s, 6:7]), r=[pj[2]], w=["junk", ("st", s, 6)])
              Pq.op("dve", lambda e, s=s: e.tensor_scalar(out=st8[:, s, 24:32], in0=st8[:, s, 24:32], scalar1=st8[:, s, 6:7], scalar2=None,
                                                         op0=ALU.add), r=[("st", s, 24), ("st", s, 6)], w=[("st", s, 24)])
              rstd(Pq, ("st", s, 24), st8[:, s, 24:32], QK, st8[:, s, 32:40], ("st", s, 32))
              Pq.op("dve", lambda e, s=s: e.tensor_tensor(out=kf[:, :, 0:64], in0=kf[:, :, 0:64],
                                                         in1=st8[:, s, 32:40].unsqueeze(2).to_broadcast([128, 8, 64]), op=ALU.mult),
                   r=kfk + [("st", s, 32)], w=kfk)
              Pq.op("dve", lambda e: e.tensor_tensor(out=kb[:, :, 0:64], in0=kf[:, :, 0:64],
                                                    in1=kng[:, 0:64].unsqueeze(1).to_broadcast([128, 8, 64]), op=ALU.mult),
                   r=kfk + ["kng"], w=["kb"])
              Pq.op("pool", lambda e, s=s: e.tensor_tensor(out=krg[:], in0=proj[:, s, 1152:1184], in1=kng[:, 64:96], op=ALU.mult),
                   r=[pj[2], "kng"], w=["krg", "k_src"])
              Pq.op("pool", lambda e, s=s: e.tensor_copy(out=rt[:, 4, 0:32], in_=rp[:, s, :]), r=[("rp", s)], w=["k_tab"])
              rope(Pq, rt, krg[:].unsqueeze(1), krr[:].unsqueeze(1), rt[:, 4, 0:32], 1, "k")
              Pq.op("dve", lambda e, s=s: e.tensor_tensor(out=kb[:, :, 64:96], in0=krr[:].unsqueeze(1).to_broadcast([128, 8, 32]),
                                                         in1=st8[:, s, 32:40].unsqueeze(2).to_broadcast([128, 8, 32]), op=ALU.mult),
                   r=["k_dst", ("st", s, 32)], w=["kb"])

              def tr_kh(e):
                  ins = None
                  for h in range(8):
                      ins = e.transpose(out=PSb3[0:96, h * 128:(h + 1) * 128], in_=kb[:, h, :], identity=identb[:])
                  return ins
              Pq.op("pe", tr_kh, r=["kb", "identb"], w=["ps3"])
              Pq.op("dve", lambda e: e.tensor_copy(out=kTt[0:96].rearrange("p a b -> p (a b)"), in_=PSb3[0:96, 0:1024]), r=["ps3"], w=["kTt"])
              Pq.dmaop("act", lambda e, t0=t0: e.dma_start(out=kT_s[:, :, t0:t0 + 128].rearrange("h d t -> d h t"), in_=kTt[0:96]),
                      r=["kTt"], w=["kT_s"])
              return Pq.cap
          for i in range(0, ntile1, 2):
              caps = [tile1(i, 0)] + ([tile1(i + 1, 1)] if i + 1 < ntile1 else [])
              interleave(P, caps, chunk=int(os.environ.get("ILV", "2")))
          P.flush()

    QG = min(512, NXO)
    NQG = NXO // QG

    es1.close()
    NCX = NXe // 8
    NCC = NCTX // 8
    NCHe = NCX + NCC
    HW = NCHe + 2
    if debug in (0, 4, 5, 6):
      with ExitStack() as st:
        BendT = sb(st, "BendT", [128, 2, 32, 2, 64], BF16)
        Dm = sb(st, "Dm", [128, 2, 16, 2, 128], BF16)
        Tloc = sb(st, "Tloc", [128, 32, 128], BF16)
        mu3 = sb(st, "mu3", [128, 2, 2, 16, 2])
        mu16 = sb(st, "mu16", [128, 2, 17, 2, 16, 2])
        PS2 = [ps(st, "ph2_%d" % i) for i in range(8)]
        with ExitStack() as tt:
            lre = sb(tt, "lre", [128, 32]); lim = sb(tt, "lim", [128, 32]); ldt = sb(tt, "ldt", [128, 32])
            bre = sb(tt, "bre", [128, 32, 16]); bim = sb(tt, "bim", [128, 32, 16])
            cre = sb(tt, "cre", [128, 32, 16]); cim = sb(tt, "cim", [128, 32, 16])
            dsk = sb(tt, "dsk", [128, 32])
            cmk = sb(tt, "cmk", [128, 256])
            tm = sb(tt, "tm", [128, 12, 32])
            ti = sb(tt, "ti", [128, 32], I32)
            pwr = sb(tt, "pwr", [128, 9, 32]); pwi = sb(tt, "pwi", [128, 9, 32])
            nwr = sb(tt, "nwr", [128, 9, 32]); nwi = sb(tt, "nwi", [128, 9, 32])
            Bbr = sb(tt, "Bbr", [128, 32, 16]); Bbi = sb(tt, "Bbi", [128, 32, 16])
            MX = [sb(tt, "MX%d" % i, [128, 16, 8, 16]) for i in range(4)]
            MT = [sb(tt, "MT%d" % i, [128, 16, 8, 16]) for i in range(4)]
            TL = sb(tt, "TL", [128, 2, 128])
            for gh in range(2):
                rows = slice(64 * gh, 64 * gh + 64)
                gs = slice(16 * gh, 16 * gh + 16)
                for d in range(2):
                    for (dst, src, nm) in ((lre, lam_re, "lre"), (lim, lam_im, "lim")):
                        P.dmaop("sp", lambda e, dst=dst, src=src, rows=rows, gs=gs, d=d: e.dma_start(
                            out=dst[rows, 16 * d:16 * d + 16], in_=src[d, gs, :].rearrange("g p -> p g"),
                            allow_slow_non_contiguous=True), w=[nm])
                    P.dmaop("sp", lambda e, rows=rows, gs=gs, d=d: e.dma_start(
                        out=ldt[rows, 16 * d:16 * d + 16], in_=log_dt[d, gs].partition_broadcast(64)), w=["ldt"])
                for (dst, src, nm) in ((bre, b_re, "bre"), (bim, b_im, "bim")):
                    for d in range(2):
                        P.dmaop("act", lambda e, dst=dst, src=src, rows=rows, gs=gs, d=d: e.dma_start(
                            out=dst[rows, 16 * d:16 * d + 16, :], in_=src[d, gs, :, :].rearrange("g p h -> p g h")), w=[nm])
                for (dst, src, nm) in ((cre, c_re, "cre"), (cim, c_im, "cim")):
                    for d in range(2):
                        P.dmaop("sp" if d == 0 else "act", lambda e, dst=dst, src=src, rows=rows, gs=gs, d=d: e.dma_start(
                            out=dst[rows, 16 * d:16 * d + 16, :], in_=src[d, gs, :, :].rearrange("g o p -> p g o"),
                            allow_slow_non_contiguous=True), w=[nm])
            for j in range(8):
                P.dmaop("sp", lambda e, j=j: e.dma_start(out=dsk[16 * j:16 * j + 16, :], in_=ssm_d.rearrange("(g h) -> h g", h=16),
                                                        allow_slow_non_contiguous=True), w=["dsk"])
            P.dmaop("sp", lambda e: e.dma_start(out=cmk[:], in_=cmask_d), w=["cmk"])

            K = [0]

            def T_(i):
                return tm[:, i, :]

            def dv(fn, r, w, eng="dve"):
                P.op(eng, fn, r=r, w=w)

            def tt2(out, a, b, op, r, w, eng="dve"):
                P.op(eng, lambda e: e.tensor_tensor(out=out, in0=a, in1=b, op=op), r=r, w=w)

            def ts(out, a, s1, op0, s2=None, op1=None, r=(), w=(), eng="dve"):
                if op1 is None:
                    P.op(eng, lambda e: e.tensor_scalar(out=out, in0=a, scalar1=s1, scalar2=None, op0=op0), r=r, w=w)
                else:
                    P.op(eng, lambda e: e.tensor_scalar(out=out, in0=a, scalar1=s1, scalar2=s2, op0=op0, op1=op1), r=r, w=w)
            PI = math.pi
            P.op("act", lambda e: e.activation(out=T_(0), in_=ldt[:], func=AF.Exp), r=["ldt"], w=["t0"])
            tt2(T_(1), lre[:], T_(0), ALU.mult, ["lre", "t0"], ["t1"])
            tt2(T_(2), lim[:], T_(0), ALU.mult, ["lim", "t0"], ["t2"])
            P.op("act", lambda e: e.activation(out=T_(3), in_=T_(1), func=AF.Exp), r=["t1"], w=["t3"])
            ts(T_(4), T_(2), 1.0 / (2 * PI), ALU.mult, r=["t2"], w=["t4"])
            P.op("dve", lambda e: e.tensor_copy(out=ti[:], in_=T_(4)), r=["t4"], w=["ti"])
            P.op("dve", lambda e: e.tensor_copy(out=T_(4), in_=ti[:]), r=["ti"], w=["t4"])
            P.op("dve", lambda e: e.scalar_tensor_tensor(out=T_(5), in0=T_(4), scalar=-2 * PI, in1=T_(2), op0=ALU.mult, op1=ALU.add),
                 r=["t4", "t2"], w=["t5"])
            for (src_i, dst_i) in ((5, 5),):
                ts(T_(6), T_(5), PI, ALU.is_gt, -2 * PI, ALU.mult, r=["t5"], w=["t6"])
                tt2(T_(5), T_(5), T_(6), ALU.add, ["t5", "t6"], ["t5"])
                ts(T_(6), T_(5), -PI, ALU.is_lt, 2 * PI, ALU.mult, r=["t5"], w=["t6"])
                tt2(T_(5), T_(5), T_(6), ALU.add, ["t5", "t6"], ["t5"])
            ts(T_(7), T_(5), PI / 2, ALU.add, r=["t5"], w=["t7"])
            ts(T_(6), T_(7), PI, ALU.is_gt, -2 * PI, ALU.mult, r=["t7"], w=["t6"])
            tt2(T_(7), T_(7), T_(6), ALU.add, ["t7", "t6"], ["t7"])
            P.op("act", lambda e: e.activation(out=T_(8), in_=T_(5), func=AF.Sin), r=["t5"], w=["t8"])
            P.op("act", lambda e: e.activation(out=T_(9), in_=T_(7), func=AF.Sin), r=["t7"], w=["t9"])
            P.op("pool", lambda e: e.memset(pwr[:, 0, :], 1.0), w=[("pw", 0)])
            P.op("pool", lambda e: e.memset(pwi[:, 0, :], 0.0), w=[("pw", 0)])
            tt2(pwr[:, 1, :], T_(3), T_(9), ALU.mult, ["t3", "t9"], [("pw", 1)])
            tt2(pwi[:, 1, :], T_(3), T_(8), ALU.mult, ["t3", "t8"], [("pw", 1)])
            for k in range(2, 9):
                a_r, a_i = pwr[:, k - 1, :], pwi[:, k - 1, :]
                tt2(T_(10), a_r, pwr[:, 1, :], ALU.mult, [("pw", k - 1), ("pw", 1)], ["t10"])
                tt2(T_(11), a_i, pwi[:, 1, :], ALU.mult, [("pw", k - 1), ("pw", 1)], ["t11"])
                tt2(pwr[:, k, :], T_(10), T_(11), ALU.subtract, ["t10", "t11"], [("pw", k)])
                tt2(T_(10), a_r, pwi[:, 1, :], ALU.mult, [("pw", k - 1), ("pw", 1)], ["t10"])
                tt2(T_(11), a_i, pwr[:, 1, :], ALU.mult, [("pw", k - 1), ("pw", 1)], ["t11"])
                tt2(pwi[:, k, :], T_(10), T_(11), ALU.add, ["t10", "t11"], [("pw", k)])
            pwk = [("pw", k) for k in range(9)]
            tt2(nwr[:], pwr[:], pwr[:], ALU.mult, pwk, ["nwr"])
            tt2(nwi[:], pwi[:], pwi[:], ALU.mult, pwk, ["nwi"])
            tt2(nwr[:], nwr[:], nwi[:], ALU.add, ["nwr", "nwi"], ["nwr"])
            P.op("dve", lambda e: e.reciprocal(out=nwr[:], in_=nwr[:]), r=["nwr"], w=["nwr"])
            P.op("dve", lambda e: e.scalar_tensor_tensor(out=nwi[:], in0=pwi[:], scalar=-1.0, in1=nwr[:], op0=ALU.mult, op1=ALU.mult),
                 r=pwk + ["nwr"], w=["nwi"])
            tt2(nwr[:], pwr[:], nwr[:], ALU.mult, pwk + ["nwr", "nwi"], ["nwr"])
            for d in range(2):
                dsl = slice(16 * d, 16 * d + 16)
                for pl in range(2):
                    P.op("dve", lambda e, d=d, pl=pl, dsl=dsl: e.tensor_copy(out=mu3[:, d, 0, :, pl], in_=pwr[:, 8, dsl]), r=pwk, w=["mu3"])
                P.op("dve", lambda e, d=d, dsl=dsl: e.tensor_scalar(out=mu3[:, d, 1, :, 0], in0=pwi[:, 8, dsl], scalar1=-1.0, scalar2=None, op0=ALU.mult),
                     r=pwk, w=["mu3"])
                P.op("dve", lambda e, d=d, dsl=dsl: e.tensor_copy(out=mu3[:, d, 1, :, 1], in_=pwi[:, 8, dsl]), r=pwk, w=["mu3"])
            q16r = sb(tt, "q16r", [128, 17, 32]); q16i = sb(tt, "q16i", [128, 17, 32])
            P.op("pool", lambda e: e.memset(q16r[:, 0, :], 1.0), w=[("q16", 0)])
            P.op("pool", lambda e: e.memset(q16i[:, 0, :], 0.0), w=[("q16", 0)])
            P.op("pool", lambda e: e.tensor_copy(out=q16r[:, 1, :], in_=pwr[:, 8, :]), r=pwk, w=[("q16", 1)])
            P.op("pool", lambda e: e.tensor_copy(out=q16i[:, 1, :], in_=pwi[:, 8, :]), r=pwk, w=[("q16", 1)])
            for k in range(2, 17):
                a_r, a_i = q16r[:, k - 1, :], q16i[:, k - 1, :]
                tt2(T_(10), a_r, q16r[:, 1, :], ALU.mult, [("q16", k - 1), ("q16", 1)], ["t10"])
                tt2(T_(11), a_i, q16i[:, 1, :], ALU.mult, [("q16", k - 1), ("q16", 1)], ["t11"])
                tt2(q16r[:, k, :], T_(10), T_(11), ALU.subtract, ["t10", "t11"], [("q16", k)])
                tt2(T_(10), a_r, q16i[:, 1, :], ALU.mult, [("q16", k - 1), ("q16", 1)], ["t10"])
                tt2(T_(11), a_i, q16r[:, 1, :], ALU.mult, [("q16", k - 1), ("q16", 1)], ["t11"])
                tt2(q16i[:, k, :], T_(10), T_(11), ALU.add, ["t10", "t11"], [("q16", k)])
            q16k = [("q16", k) for k in range(17)]
            for d in range(2):
                dsl = slice(16 * d, 16 * d + 16)
                for pl in range(2):
                    P.op("dve", lambda e, d=d, pl=pl, dsl=dsl: e.tensor_copy(out=mu16[:, d, :, 0, :, pl], in_=q16r[:, :, dsl]), r=q16k, w=["mu16"])
                P.op("dve", lambda e, d=d, dsl=dsl: e.tensor_scalar(out=mu16[:, d, :, 1, :, 0], in0=q16i[:, :, dsl], scalar1=-1.0, scalar2=None, op0=ALU.mult),
                     r=q16k, w=["mu16"])
                P.op("dve", lambda e, d=d, dsl=dsl: e.tensor_copy(out=mu16[:, d, :, 1, :, 1], in_=q16i[:, :, dsl]), r=q16k, w=["mu16"])
            tt2(T_(0), lre[:], lre[:], ALU.mult, ["lre"], ["t0"])
            tt2(T_(1), lim[:], lim[:], ALU.mult, ["lim"], ["t1"])
            tt2(T_(0), T_(0), T_(1), ALU.add, ["t0", "t1"], ["t0"])
            P.op("dve", lambda e: e.reciprocal(out=T_(0), in_=T_(0)), r=["t0"], w=["t0"])
            ts(T_(1), pwr[:, 1, :], -1.0, ALU.add, r=[("pw", 1)], w=["t1"])
            tt2(T_(2), T_(1), lre[:], ALU.mult, ["t1", "lre"], ["t2"])
            tt2(T_(3), pwi[:, 1, :], lim[:], ALU.mult, [("pw", 1), "lim"], ["t3"])
            tt2(T_(2), T_(2), T_(3), ALU.add, ["t2", "t3"], ["t2"])
            tt2(T_(2), T_(2), T_(0), ALU.mult, ["t2", "t0"], ["t2"])
            tt2(T_(3), pwi[:, 1, :], lre[:], ALU.mult, [("pw", 1), "lre"], ["t3"])
            tt2(T_(4), T_(1), lim[:], ALU.mult, ["t1", "lim"], ["t4"])
            tt2(T_(3), T_(3), T_(4), ALU.subtract, ["t3", "t4"], ["t3"])
            tt2(T_(3), T_(3), T_(0), ALU.mult, ["t3", "t0"], ["t3"])
            cfr = T_(2).unsqueeze(2).to_broadcast([128, 32, 16])
            cfi = T_(3).unsqueeze(2).to_broadcast([128, 32, 16])
            tmpA = MT[0][:].rearrange("p a b c -> p (a b c)")[:, 0:512].rearrange("p (g h) -> p g h", h=16)
            tt2(Bbr[:], bre[:], cfr, ALU.mult, ["bre", "t2"], ["Bbr"])
            tt2(tmpA, bim[:], cfi, ALU.mult, ["bim", "t3"], ["MT0"])
            tt2(Bbr[:], Bbr[:], tmpA, ALU.subtract, ["Bbr", "MT0"], ["Bbr"])
            tt2(Bbi[:], bim[:], cfr, ALU.mult, ["bim", "t2"], ["Bbi"])
            tt2(tmpA, bre[:], cfi, ALU.mult, ["bre", "t3"], ["MT0"])
            tt2(Bbi[:], Bbi[:], tmpA, ALU.add, ["Bbi", "MT0"], ["Bbi"])

            def cprod(out_re, out_im, pw_re, pw_im, koff, kstep, vr, vi, d, keys_in, key_out, neg_im=False, eng="dve"):
                dsl = slice(16 * d, 16 * d + 16)
                if kstep == 1:
                    ksl = slice(koff, koff + 8)
                    pr = pw_re[:, ksl, dsl].rearrange("p j g -> p g j").unsqueeze(3).to_broadcast([128, 16, 8, 16])
                    pi = pw_im[:, ksl, dsl].rearrange("p j g -> p g j").unsqueeze(3).to_broadcast([128, 16, 8, 16])
                else:
                    pr = None
                vrb = vr[:, dsl, :].unsqueeze(2).to_broadcast([128, 16, 8, 16])
                vib = vi[:, dsl, :].unsqueeze(2).to_broadcast([128, 16, 8, 16])
                t1 = MT[2][:]
                t2 = MT[3][:]
                tt2(t1, vrb, pr, ALU.mult, keys_in, ["MT2"], eng)
                tt2(t2, vib, pi, ALU.mult, keys_in, ["MT3"], eng)
                tt2(out_re, t1, t2, ALU.subtract, ["MT2", "MT3"], [key_out[0]], eng)
                tt2(t1, vrb, pi, ALU.mult, keys_in, ["MT2"], eng)
                tt2(t2, vib, pr, ALU.mult, keys_in, ["MT3"], eng)
                tt2(out_im, t1, t2, (ALU.add), ["MT2", "MT3"], [key_out[1]], eng)
                if neg_im:
                    ts(out_im, out_im, -1.0, ALU.mult, r=[key_out[1]], w=[key_out[1]], eng=eng)

            dpr = sb(tt, "dpr", [128, 9, 32]); dpi = sb(tt, "dpi", [128, 9, 32])
            for k in range(9):
                P.op("pool", lambda e, k=k: e.tensor_copy(out=dpr[:, k, :], in_=pwr[:, 8 - k, :]), r=pwk, w=["dpr"])
                P.op("pool", lambda e, k=k: e.tensor_copy(out=dpi[:, k, :], in_=pwi[:, 8 - k, :]), r=pwk, w=["dpi"])
            tk = pwk + ["nwr", "nwi", "dpr", "dpi", "Bbr", "Bbi", "cre", "cim"]
            pTL = PS2[0]
            for d in range(2):
                if d == 0:
                    cprod(MX[0][:], MX[1][:], nwr, nwi, 0, 1, Bbr, Bbi, d, tk, ("MX0", "MX1"))
                    cprod(MX[2][:], MX[3][:], pwr, pwi, 0, 1, cre, cim, d, tk, ("MX2", "MX3"), neg_im=True)
                    cprod(MT[0][:], MT[1][:], dpr, dpi, 1, 1, Bbr, Bbi, d, tk, ("MT0", "MT1"))
                else:
                    cprod(MX[0][:], MX[1][:], pwr, pwi, 0, 1, Bbr, Bbi, d, tk, ("MX0", "MX1"))
                    cprod(MX[2][:], MX[3][:], nwr, nwi, 0, 1, cre, cim, d, tk, ("MX2", "MX3"), neg_im=True)
                    P.op("pool", lambda e: e.tensor_copy(out=MT[0][:], in_=MX[0][:]), r=["MX0"], w=["MT0"])
                    P.op("pool", lambda e: e.tensor_copy(out=MT[1][:], in_=MX[1][:]), r=["MX1"], w=["MT1"])
                for pl in range(2):
                    for g in range(32):
                        gh, gp = g // 16, g % 16
                        pb = PS2[2 + (g % 4)]

                        def trb(e, pl=pl, gh=gh, gp=gp, pb=pb):
                            return e.transpose(out=pb[:, 0:64], in_=MT[pl][64 * gh:64 * gh + 64, gp].rearrange("p j h -> p (j h)"),
                                               identity=ident[64 * gh:64 * gh + 64, 64 * gh:64 * gh + 64])
                        P.op("pe", trb, r=["MT0" if pl == 0 else "MT1", "ident"], w=[("p2", 2 + g % 4)])
                        P.op("act" if g % 2 == 0 else "dve",
                             (lambda e, pb=pb, d=d, g=g, pl=pl: e.copy(out=BendT[:, d, g, pl, :], in_=pb[:, 0:64])) if g % 2 == 0 else
                             (lambda e, pb=pb, d=d, g=g, pl=pl: e.tensor_copy(out=BendT[:, d, g, pl, :], in_=pb[:, 0:64])),
                             r=[("p2", 2 + g % 4)], w=["BendT"])
                if d == 0:
                    cprod(MT[0][:], MT[1][:], pwr, pwi, 1, 1, cre, cim, d, tk + ["BendT"], ("MT0", "MT1"), neg_im=True)
                else:
                    cprod(MT[0][:], MT[1][:], dpr, dpi, 0, 1, cre, cim, d, tk + ["BendT"], ("MT0", "MT1"), neg_im=True)
                P.op("act", lambda e, d=d: e.copy(out=Dm[:, d, :, 0, :], in_=MT[0][:].rearrange("p g j o -> p g (j o)")), r=["MT0"], w=["Dm"])
                P.op("act", lambda e, d=d: e.copy(out=Dm[:, d, :, 1, :], in_=MT[1][:].rearrange("p g j o -> p g (j o)")), r=["MT1"], w=["Dm"])
                for g in range(32):
                    gh, gp = g // 16, g % 16
                    rows = slice(64 * gh, 64 * gh + 64)
                    pq = PS2[6 + (g % 2)]

                    def mtl(e, rows=rows, gp=gp, pq=pq):
                        e.matmul(pq[:, 0:128], lhsT=MX[0][rows, gp].rearrange("p j h -> p (j h)"), rhs=MX[2][rows, gp].rearrange("p j h -> p (j h)"),
                                 start=True, stop=False)
                        return e.matmul(pq[:, 0:128], lhsT=MX[1][rows, gp].rearrange("p j h -> p (j h)"),
                                        rhs=MX[3][rows, gp].rearrange("p j h -> p (j h)"), start=False, stop=True)
                    P.op("pe", mtl, r=["MX0", "MX1", "MX2", "MX3"], w=[("p2", 6 + g % 2)])
                    if d == 0:
                        P.op("dve", lambda e, g=g, pq=pq: e.tensor_tensor(out=Tloc[:, g, :], in0=pq[:, 0:128], in1=cmk[:, 0:128], op=ALU.mult),
                             r=[("p2", 6 + g % 2), "cmk"], w=[("Tloc", g)])
                    else:
                        P.op("dve", lambda e, g=g, pq=pq: e.tensor_tensor(out=TL[:, g % 2, :], in0=pq[:, 0:128], in1=cmk[:, 128:256], op=ALU.mult),
                             r=[("p2", 6 + g % 2), "cmk"], w=[("TL", g % 2)])
                        P.op("pool", lambda e, g=g: e.tensor_tensor(out=Tloc[:, g, :], in0=Tloc[:, g, :], in1=TL[:, g % 2, :], op=ALU.add),
                             r=[("Tloc", g), ("TL", g % 2)], w=[("Tloc", g)])
                        P.op("pool", lambda e, g=g: e.scalar_tensor_tensor(out=Tloc[:, g, :], in0=ident[:], scalar=dsk[:, g:g + 1], in1=Tloc[:, g, :],
                                                                          op0=ALU.mult, op1=ALU.add) if False else
                             e.tensor_scalar(out=TL[:, g % 2, :], in0=ident[:], scalar1=dsk[:, g:g + 1], scalar2=None, op0=ALU.mult),
                             r=["ident", "dsk", ("Tloc", g)], w=[("TL", g % 2)])
                        P.op("pool", lambda e, g=g: e.tensor_tensor(out=Tloc[:, g, :], in0=Tloc[:, g, :], in1=TL[:, g % 2, :], op=ALU.add),
                             r=[("Tloc", g), ("TL", g % 2)], w=[("Tloc", g)])
            P.flush()
        U8 = sb(st, "U8", [128, 32, NCHe], BF16)
        Hall = [sb(st, "Hall%d" % d, [128, 16, 2, HW], BF16) for d in range(2)]
        with ExitStack() as tu:
            Uc = sb(tu, "Uc", [128, 1, 4096])
            Ucb = sb(tu, "Ucb", [128, 1, 4096], BF16)
            identb2 = sb(tu, "identb2", [128, 128], BF16)
            P.op("dve", lambda e: e.tensor_copy(out=identb2[:], in_=ident[:]), r=["ident"], w=["identb2"])
            u8v = u_s.rearrange("(c j) f -> c (j f)", j=8)
            nblk = (NCHe + 127) // 128
            for cb in range(nblk):
                c0 = cb * 128
                ncb = min(128, NCHe - c0)
                s = 0
                P.dmaop("sp", lambda e, s=s, c0=c0, ncb=ncb: e.dma_start(out=Uc[0:ncb, s, :], in_=u8v[c0:c0 + ncb, :]), r=["u_s"], w=[("Uc", s)])
                P.op("dve", lambda e, s=s, ncb=ncb: e.tensor_copy(
                    out=Ucb[0:ncb, s, :].rearrange("c (g j h) -> c g j h", g=32, j=8),
                    in_=Uc[0:ncb, s, :].rearrange("c (j g h) -> c g j h", j=8, g=32)), r=[("Uc", s)], w=[("Ucb", s)])
                for g4 in range(8):
                    pb = PS2[g4 % 4]
                    pbb = pb[:].bitcast(BF16)

                    def tru(e, g4=g4, s=s, ncb=ncb, pbb=pbb):
                        ins = None
                        for q in range(4):
                            g = g4 * 4 + q
                            ins = e.transpose(out=pbb[:, q * 128:q * 128 + ncb], in_=Ucb[0:ncb, s, g * 128:(g + 1) * 128],
                                              identity=identb2[0:ncb, 0:ncb])
                        return ins
                    P.op("pe", tru, r=[("Ucb", s), "identb2"], w=[("p2", g4 % 4)])
                    P.op("act" if g4 % 2 == 0 else "dve",
                         (lambda e, g4=g4, c0=c0, ncb=ncb, pbb=pbb: e.copy(
                             out=U8[:, g4 * 4:g4 * 4 + 4, c0:c0 + ncb], in_=pbb[:, 0:512].rearrange("p (q c) -> p q c", q=4)[:, :, 0:ncb]))
                         if g4 % 2 == 0 else
                         (lambda e, g4=g4, c0=c0, ncb=ncb, pbb=pbb: e.tensor_copy(
                             out=U8[:, g4 * 4:g4 * 4 + 4, c0:c0 + ncb], in_=pbb[:, 0:512].rearrange("p (q c) -> p q c", q=4)[:, :, 0:ncb])),
                         r=[("p2", g4 % 4)], w=["U8"])
            P.flush()
        if debug == 4:
            d1 = nc.dram_tensor("dbg_tloc", [128, 32 * 128], BF16, kind="ExternalOutput").ap()
            d2 = nc.dram_tensor("dbg_bendt", [128, 2 * 32 * 2 * 64], BF16, kind="ExternalOutput").ap()
            d3 = nc.dram_tensor("dbg_dm", [128, 2 * 16 * 2 * 128], BF16, kind="ExternalOutput").ap()
            d4 = nc.dram_tensor("dbg_u8", [128, 32 * NCHe], BF16, kind="ExternalOutput").ap()
            d5 = nc.dram_tensor("dbg_mu3", [128, 128], F32, kind="ExternalOutput").ap()
            P.dmaop("sp", lambda e: e.dma_start(out=d1, in_=Tloc[:].rearrange("p a b -> p (a b)")), w=["d1"])
            P.dmaop("sp", lambda e: e.dma_start(out=d2, in_=BendT[:].rearrange("p a b c d -> p (a b c d)")), w=["d2"])
            P.dmaop("sp", lambda e: e.dma_start(out=d3, in_=Dm[:].rearrange("p a b c d -> p (a b c d)")), w=["d3"])
            P.dmaop("sp", lambda e: e.dma_start(out=d4, in_=U8[:].rearrange("p a b -> p (a b)")), w=["d4"])
            P.dmaop("sp", lambda e: e.dma_start(out=d5, in_=mu3[:].rearrange("p a b c d -> p (a b c d)")), w=["d5"])
            P.flush()
            DONE.append(1)

        def hk(d, lo, hi):
            return [("Hall", d, q) for q in range(lo, hi)]
        P.op("pool", lambda e: e.memset(Hall[0][:, :, :, 0:1], 0.0), w=hk(0, 0, 1))
        P.op("pool", lambda e: e.memset(Hall[1][:, :, :, NCHe:NCHe + 1], 0.0), w=hk(1, NCHe, NCHe + 1))
        xlo = [NCC + 1, 0]
        clo = [1, NCX]
        ei = 0
        for d in range(2):
            for pl in range(2):
                pc = PS2[4 + pl]
                for gp in range(16):
                    px = PS2[(d * 32 + pl * 16 + gp) % 4]
                    pk = ("p2", (d * 32 + pl * 16 + gp) % 4)

                    def mms(e, d=d, pl=pl, gp=gp, px=px, pc=pc):
                        ins = None
                        for gh in range(2):
                            g = 16 * gh + gp
                            e.matmul(px[64 * gh:64 * gh + 64, 0:NCX], lhsT=BendT[:, d, g, pl, :], rhs=U8[:, g, NCC:NCC + NCX],
                                     start=True, stop=True, tile_position=(0, 64 * gh))
                            ins = e.matmul(pc[64 * gh:64 * gh + 64, gp * 32:gp * 32 + NCC], lhsT=BendT[:, d, g, pl, :], rhs=U8[:, g, 0:NCC],
                                           start=True, stop=True, tile_position=(0, 64 * gh))
                        return ins
                    P.op("pe", mms, r=["BendT", "U8"], w=[pk, ("p2c", 4 + pl, gp)])
                    dst = Hall[d][:, gp, pl, xlo[d]:xlo[d] + NCX]
                    if ei % 2 == 0:
                        P.op("act", lambda e, dst=dst, px=px: e.copy(out=dst, in_=px[:, 0:NCX]), r=[pk], w=hk(d, xlo[d], xlo[d] + NCX))
                    else:
                        P.op("dve", lambda e, dst=dst, px=px: e.tensor_copy(out=dst, in_=px[:, 0:NCX]), r=[pk], w=hk(d, xlo[d], xlo[d] + NCX))
                    ei += 1
                P.op("act", lambda e, d=d, pl=pl, pc=pc: e.copy(out=Hall[d][:, :, pl, clo[d]:clo[d] + NCC],
                                                                in_=pc[:, 0:512].rearrange("p (g c) -> p g c", g=16)[:, :, 0:NCC]),
                     r=[("p2c", 4 + pl, gp) for gp in range(16)], w=hk(d, clo[d], clo[d] + NCC))
        LB = 16
        NB = NCHe // LB
        assert NB * LB == NCHe
        with ExitStack() as tc:
            Rl = [sb(tc, "Rl_%d" % d, [128, 16, 3, NB + 1]) for d in range(2)]
            TA = [sb(tc, "TA_%d" % d, [128, 16, 2, NB]) for d in range(2)]
            TB = [sb(tc, "TB_%d" % d, [128, 16, 2, NB]) for d in range(2)]
            Ea = Rl
            ET = [sb(tc, "ET_%d" % d, [128, 2, 16, 2]) for d in range(2)]

            def hview(d, i):
                st0 = (1 + i) if d == 0 else (LB - 1 - i)
                return Hall[d][:, :, :, st0:st0 + LB * (NB - 1) + 1:LB]

            def hkeys(d, i):
                st0 = (1 + i) if d == 0 else (LB - 1 - i)
                return [("Hall", d, st0 + LB * m) for m in range(NB)]

            def mub(d, k, which, n):
                return mu16[:, d, k, which].unsqueeze(3).to_broadcast([128, 16, 2, n])
            for d in range(2):
                eng = "dve"
                for i in range(LB):
                    hv = hview(d, i)
                    hk_i = hkeys(d, i)
                    if i == 0:
                        P.op(eng, lambda e, d=d, hv=hv: e.tensor_copy(out=Rl[d][:, :, 0:2, 0:NB], in_=hv), r=hk_i, w=[("Rl", d)])
                    else:
                        P.op(eng, lambda e, d=d: e.tensor_tensor(out=TA[d][:], in0=Rl[d][:, :, 0:2, 0:NB], in1=mub(d, 1, 0, NB), op=ALU.mult),
                             r=[("Rl", d), "mu16"], w=[("TA", d)])
                        P.op(eng, lambda e, d=d: e.tensor_tensor(out=TB[d][:], in0=Rl[d][:, :, 1:3, 0:NB], in1=mub(d, 1, 1, NB), op=ALU.mult),
                             r=[("Rl", d), "mu16"], w=[("TB", d)])
                        P.op(eng, lambda e, d=d: e.tensor_tensor(out=TA[d][:], in0=TA[d][:], in1=TB[d][:], op=ALU.add),
                             r=[("TA", d), ("TB", d)], w=[("TA", d)])
                        P.op(eng, lambda e, d=d, hv=hv: e.tensor_tensor(out=Rl[d][:, :, 0:2, 0:NB], in0=TA[d][:], in1=hv, op=ALU.add),
                             r=[("TA", d)] + hk_i, w=[("Rl", d)])
                        P.op("act", lambda e, d=d, hv=hv: e.copy(out=hv, in_=Rl[d][:, :, 0:2, 0:NB]), r=[("Rl", d)], w=hk_i)
                    if i < LB - 1:
                        P.op(eng, lambda e, d=d: e.tensor_copy(out=Rl[d][:, :, 2, 0:NB], in_=Rl[d][:, :, 0, 0:NB]), r=[("Rl", d)], w=[("Rl", d)])
            for d in range(2):
                eng = "dve" if d == 0 else "pool"
                P.op(eng, lambda e, d=d: e.memset(Ea[d][:], 0.0), w=[("Rl", d)])
                order = list(range(NB - 1)) if d == 0 else list(range(NB - 1, 0, -1))
                for m in order:
                    mn = m + 1 if d == 0 else m - 1
                    pend = (1 + 16 * m + 15) if d == 0 else (16 * m)
                    P.op(eng, lambda e, d=d, m=m: e.tensor_tensor(out=ET[d][:, 0], in0=Ea[d][:, :, 0:2, m], in1=mu16[:, d, 16, 0], op=ALU.mult),
                         r=[("Rl", d), "mu16"], w=[("ET", d, 0)])
                    P.op(eng, lambda e, d=d, m=m: e.tensor_tensor(out=ET[d][:, 1], in0=Ea[d][:, :, 1:3, m], in1=mu16[:, d, 16, 1], op=ALU.mult),
                         r=[("Rl", d), "mu16"], w=[("ET", d, 1)])
                    P.op(eng, lambda e, d=d: e.tensor_tensor(out=ET[d][:, 0], in0=ET[d][:, 0], in1=ET[d][:, 1], op=ALU.add),
                         r=[("ET", d, 0), ("ET", d, 1)], w=[("ET", d, 0)])
                    P.op(eng, lambda e, d=d, mn=mn, pend=pend: e.tensor_tensor(out=Ea[d][:, :, 0:2, mn], in0=ET[d][:, 0], in1=Hall[d][:, :, :, pend], op=ALU.add),
                         r=[("ET", d, 0), ("Hall", d, pend)], w=[("Rl", d)])
                    P.op(eng, lambda e, d=d, mn=mn: e.tensor_copy(out=Ea[d][:, :, 2, mn], in_=Ea[d][:, :, 0, mn]), r=[("Rl", d)], w=[("Rl", d)])
            for d in range(2):
                eng = "dve"
                for i in range(LB):
                    hv = hview(d, i)
                    hk_i = hkeys(d, i)
                    P.op(eng, lambda e, d=d, i=i: e.tensor_tensor(out=TA[d][:], in0=Ea[d][:, :, 0:2, 0:NB], in1=mub(d, i + 1, 0, NB), op=ALU.mult),
                         r=[("Rl", d), "mu16"], w=[("TA", d)])
                    P.op(eng, lambda e, d=d, i=i: e.tensor_tensor(out=TB[d][:], in0=Ea[d][:, :, 1:3, 0:NB], in1=mub(d, i + 1, 1, NB), op=ALU.mult),
                         r=[("Rl", d), "mu16"], w=[("TB", d)])
                    P.op(eng, lambda e, d=d: e.tensor_tensor(out=TA[d][:], in0=TA[d][:], in1=TB[d][:], op=ALU.add),
                         r=[("TA", d), ("TB", d)], w=[("TA", d)])
                    P.op(eng, lambda e, d=d, hv=hv: e.tensor_tensor(out=hv, in0=hv, in1=TA[d][:], op=ALU.add), r=[("TA", d)] + hk_i, w=hk_i)
            P.flush()
        NCXo = NXO // 8
        NCB = (NCXo + 127) // 128
        NPASS = 1
        CBP = NCB // NPASS
        NCP = NCXo // NPASS
        Yc = sb(st, "Yc", [128, CBP, 4096], BF16)
        Ysb = sb(st, "Ysb", [128, 2, NCP])
        allH = [hk(0, 0, HW), hk(1, 0, HW)]
        for pz in range(NPASS):
            c_lo = pz * NCP
            for g in range(32):
                gh, gp = g // 16, g % 16
                rows = slice(64 * gh, 64 * gh + 64)
                pr = PS2[g % 2]
                prk = ("p2", g % 2)

                def mmy(e, g=g, gp=gp, rows=rows, pr=pr, c_lo=c_lo):
                    e.matmul(pr[:, 0:NCP], lhsT=Tloc[:, g, :], rhs=U8[:, g, NCC + c_lo:NCC + c_lo + NCP], start=True, stop=False)
                    e.matmul(pr[:, 0:NCP], lhsT=Dm[rows, 0, gp, 0, :], rhs=Hall[0][rows, gp, 0, NCC + c_lo:NCC + c_lo + NCP], start=False, stop=False)
                    e.matmul(pr[:, 0:NCP], lhsT=Dm[rows, 0, gp, 1, :], rhs=Hall[0][rows, gp, 1, NCC + c_lo:NCC + c_lo + NCP], start=False, stop=False)
                    e.matmul(pr[:, 0:NCP], lhsT=Dm[rows, 1, gp, 0, :], rhs=Hall[1][rows, gp, 0, 1 + c_lo:1 + c_lo + NCP], start=False, stop=False)
                    return e.matmul(pr[:, 0:NCP], lhsT=Dm[rows, 1, gp, 1, :], rhs=Hall[1][rows, gp, 1, 1 + c_lo:1 + c_lo + NCP], start=False, stop=True)
                P.op("pe", mmy, r=[("Tloc", g), "U8", "Dm"] + allH[0] + allH[1], w=[prk])
                ys = g % 2
                if g % 2 == 0:
                    P.op("act", lambda e, ys=ys, pr=pr: e.copy(out=Ysb[:, ys, :], in_=pr[:, 0:NCP]), r=[prk], w=[("Ysb", ys)])
                else:
                    P.op("dve", lambda e, ys=ys, pr=pr: e.tensor_copy(out=Ysb[:, ys, :], in_=pr[:, 0:NCP]), r=[prk], w=[("Ysb", ys)])
                for cb in range(CBP):
                    ncb = min(128, NCP - cb * 128)
                    pt_ = PS2[2 + (g * CBP + cb) % 4]
                    ptk = ("p2", 2 + (g * CBP + cb) % 4)
                    P.op("pe", lambda e, ys=ys, cb=cb, ncb=ncb, pt_=pt_: e.transpose(out=pt_[0:ncb, 0:128], in_=Ysb[:, ys, cb * 128:cb * 128 + ncb],
                                                                                   identity=ident[:]), r=[("Ysb", ys), "ident"], w=[ptk])
                    oap = Yc[0:ncb, cb, :].rearrange("c (j g o) -> c g j o", j=8, g=32)[:, g]
                    iap = pt_[0:ncb, 0:128].rearrange("c (j o) -> c j o", j=8)
                    if (g + cb) % 2 == 0:
                        P.op("dve", lambda e, oap=oap, iap=iap: e.tensor_copy(out=oap, in_=iap), r=[ptk], w=[("Yc", cb)])
                    else:
                        P.op("act", lambda e, oap=oap, iap=iap: e.copy(out=oap, in_=iap), r=[ptk], w=[("Yc", cb)])
            for cb in range(CBP):
                ncb = min(128, NCP - cb * 128)
                r0 = c_lo + cb * 128
                P.dmaop("sp", lambda e, cb=cb, ncb=ncb, r0=r0: e.dma_start(out=y_s[r0:r0 + ncb, :], in_=Yc[0:ncb, cb, :]),
                        r=[("Yc", cb)], w=["y_s"])
        P.flush()
    if DONE:
        es.close()
        return nc
    if debug == 5:
        dbg = nc.dram_tensor("dbg_y", [NXe // 8, 4096], BF16, kind="ExternalOutput").ap()
        P.dmaop("sp", lambda e: e.dma_start(out=dbg, in_=y_s[0:NXe // 8, :]), w=["dbg"])
        P.flush()
        es.close()
        return nc

    if debug in (0, 3, 6):
      with ExitStack() as st:
        vt = sb(st, "vt", [128, NTe, 8, 80], BF16)
        kTh = sb(st, "kTh", [128, 2, NTe * 128], BF16)
        qTh = sb(st, "qTh", [128, 2, NXO], BF16)
        pT = sb(st, "pT", [128, 5, 512], BF16)
        osb = sb(st, "osb", [128, 2, 512])
        rr = sb(st, "rr", [128, 512])
        ao = sb(st, "ao", [64, 2, 512])
        ones1 = sb(st, "ones1", [128, 64])
        PSs = [ps(st, "ps_s%d" % i) for i in range(5)]
        PSo = [ps(st, "ps_o%d" % i) for i in range(2)]
        PSb = ps(st, "ps_b")
        P.op("pool", lambda e: e.memset(vt[:], 1.0), w=["vt"])
        P.op("pool", lambda e: e.memset(ones1[:], 1.0), w=["ones1"])
        for kt in range(NTe):
            P.dmaop("sp" if kt % 2 == 0 else "act",
                    lambda e, kt=kt: e.dma_start(out=vt[:, kt, :, 0:64],
                                                 in_=v_s[kt * 128:(kt + 1) * 128, :].rearrange("p (h d) -> p h d", h=8)),
                    r=["v_s"], w=["vt"])
        cnt = 0
        for h in range(H):
            hs = h % 2
            P.dmaop("sp", lambda e, h=h, hs=hs: e.dma_start(out=kTh[0:96, hs, :], in_=kT_s[h, :, 0:NTe * 128]), r=["kT_s"], w=[("kTh", hs)])
            P.dmaop("act", lambda e, h=h, hs=hs: e.dma_start(out=qTh[0:96, hs, :], in_=qT_s[h, :, 0:NXO]), r=["qT_s"], w=[("qTh", hs)])
            for g in range(NQG):
                og = (h * NQG + g) % 2

                def smm(e, kt, hs=hs, g=g):
                    return e.matmul(PSs[kt % 5][:, 0:QG], lhsT=kTh[0:96, hs, kt * 128:(kt + 1) * 128],
                                    rhs=qTh[0:96, hs, g * QG:(g + 1) * QG], start=True, stop=True)

                def pvm(e, kt, h=h, og=og):
                    return e.matmul(PSo[og][0:65, 0:QG], lhsT=vt[:, kt, h, 0:65], rhs=pT[:, kt % 5, 0:QG],
                                    start=(kt == 0), stop=(kt == NTe - 1))
                LOOK = 3
                for step in range(NTe + LOOK):
                    if step < NTe:
                        kt = step
                        P.op("pe", lambda e, kt=kt, f=smm: f(e, kt), r=[("kTh", hs), ("qTh", hs)], w=[("pss", kt % 5)])
                        P.op("act", lambda e, kt=kt: e.activation(out=pT[:, kt % 5, 0:QG], in_=PSs[kt % 5][:, 0:QG], func=AF.Exp),
                             r=[("pss", kt % 5)], w=[("pT", kt % 5)])
                    if step >= LOOK:
                        kt = step - LOOK
                        P.op("pe", lambda e, kt=kt, f=pvm: f(e, kt), r=[("pT", kt % 5), "vt"], w=[("pso", og)])
                P.op("dve", lambda e, og=og: e.tensor_copy(out=osb[0:65, og, 0:QG], in_=PSo[og][0:65, 0:QG]), r=[("pso", og)], w=[("osb", og)])
                P.op("dve", lambda e, og=og: e.reciprocal(out=rr[64:65, 0:QG], in_=osb[64:65, og, 0:QG]), r=[("osb", og)], w=["rr"])
                P.op("pe", lambda e: e.matmul(PSb[0:64, 0:QG], lhsT=ones1[64:65, 0:64], rhs=rr[64:65, 0:QG], start=True, stop=True),
                     r=["rr", "ones1"], w=["psb"])
                P.op("dve", lambda e, og=og: e.tensor_tensor(out=ao[:, og, 0:QG], in0=osb[0:64, og, 0:QG], in1=PSb[0:64, 0:QG], op=ALU.mult),
                     r=[("osb", og), "psb"], w=[("ao", og)])
                P.dmaop("sp", lambda e, h=h, g=g, og=og: e.dma_start(out=attnT_s[h, :, g * QG:(g + 1) * QG], in_=ao[:, og, 0:QG]),
                        r=[("ao", og)], w=["attnT_s"])
        P.flush()

    CAPe = 2 * NXe // NE
    SLT = min(128, CAPe)
    NRC = CAPe // SLT
    NXT = NXO // 128

    if debug in (0, 6):
      with ExitStack() as sp:
        idxT = sb(sp, "idxT", [128, NRC, 16], I32)
        gateT = sb(sp, "gateT", [128, NRC, 16])
        sp45 = ExitStack()
        affT = sb(sp45, "affT", [48, NXO])
        affo = sb(sp45, "affo", [16, NXO])
        with ExitStack() as st:
            wglu = sb(st, "wglu", [128, 4, 512], BF16)
            wsso = sb(st, "wsso", [128, 4, D], BF16)
            wmla = sb(st, "wmla", [64, 8, D], BF16)
            wout = sb(st, "wout", [128, 8, D], BF16)
            wrt = sb(st, "wrt", [128, 8, 16])
            bglu = sb(st, "bglu", [128, 512])
            identb = sb(st, "identb4", [128, 128], BF16)
            TL4 = [dict(), dict()]
            for _s in range(2):
                TL4[_s]['yt'] = sb(st, "yt_%d" % _s, [128, 512], BF16)
                TL4[_s]['yg'] = sb(st, "yg_%d" % _s, [128, 512])
                TL4[_s]['t1'] = sb(st, "t1_%d" % _s, [128, 512])
                TL4[_s]['ygb'] = sb(st, "ygb_%d" % _s, [128, 512], BF16)
                TL4[_s]['ygT'] = sb(st, "ygT_%d" % _s, [128, 4, 128], BF16)
                TL4[_s]['sg'] = sb(st, "sg_%d" % _s, [128, 512])
                TL4[_s]['zb'] = sb(st, "zb_%d" % _s, [128, 512], BF16)
                TL4[_s]['zT'] = sb(st, "zT_%d" % _s, [128, 4, 128], BF16)
                TL4[_s]['at32'] = sb(st, "at32_%d" % _s, [64, 8, 128])
                TL4[_s]['atb'] = sb(st, "atb_%d" % _s, [64, 8, 128], BF16)
                TL4[_s]['gt'] = sb(st, "gt_%d" % _s, [128, 2048])
                TL4[_s]['m1'] = sb(st, "m1_%d" % _s, [128, D])
                TL4[_s]['m2'] = sb(st, "m2_%d" % _s, [128, D])
                TL4[_s]['mb'] = sb(st, "mb_%d" % _s, [128, D], BF16)
                TL4[_s]['mT'] = sb(st, "mT_%d" % _s, [128, 8, 128], BF16)
                TL4[_s]['xt4'] = sb(st, "xt4_%d" % _s, [128, D])
                TL4[_s]['xm'] = sb(st, "xm_%d" % _s, [128, D])
                TL4[_s]['jk'] = sb(st, "jk_%d" % _s, [128, D])
                TL4[_s]['h2'] = sb(st, "h2_%d" % _s, [128, D])
                TL4[_s]['h2b'] = sb(st, "h2b_%d" % _s, [128, D], BF16)
                TL4[_s]['h2T'] = sb(st, "h2T_%d" % _s, [128, 8, 128])
                TL4[_s]['s4'] = sb(st, "s4_%d" % _s, [128, 8])
                TL4[_s]['lg'] = sb(st, "lg_%d" % _s, [128, 16])
                TL4[_s]['af'] = sb(st, "af_%d" % _s, [128, 48])
            BALL = [ps(st, "ph4_%d" % i) for i in range(8)]
            P.dmaop("pool", lambda e: e.dma_start(out=wglu[:], in_=w_glu.rearrange("(k p) n -> p k n", p=128)), w=["wglu"])
            P.dmaop("pool", lambda e: e.dma_start(out=wsso[:], in_=w_ssm_o.rearrange("(k p) n -> p k n", p=128)), w=["wsso"])
            P.dmaop("pool", lambda e: e.dma_start(out=wmla[:], in_=w_mla_o.rearrange("(h v) n -> v h n", v=64)), w=["wmla"])
            P.dmaop("pool", lambda e: e.dma_start(out=wout[:], in_=w_out.rearrange("(k p) n -> p k n", p=128)), w=["wout"])
            P.dmaop("sp", lambda e: e.dma_start(out=wrt[:], in_=w_router.rearrange("(k p) n -> p k n", p=128)), w=["wrt"])
            P.dmaop("sp", lambda e: e.dma_start(out=bglu[:], in_=b_glu.partition_broadcast(128)), w=["bglu"])
            P.op("dve", lambda e: e.tensor_copy(out=identb[:], in_=ident[:]), r=["ident"], w=["identb4"])
            ysv = y_s.rearrange("c (j f) -> (c j) f", j=8)
            for _s in range(2):
                P.op("pool", lambda e, _s=_s: e.memset(TL4[_s]['af'][:], 0.0), w=[("slot", _s, "af")])
            P.op("pool", lambda e: e.memset(TL4[0]['jk'][:], 0.0), w=[("slot", 0, "jk")])
            for r0 in range(0, 2 * NXO, 128):
                P.dmaop("sp" if (r0 // 128) % 2 == 0 else "act",
                        lambda e, r0=r0: e.dma_start(out=acc[r0:r0 + 128, :], in_=TL4[0]['jk'][:]), r=[("slot", 0, "jk")], w=["acc0"])
            P.op("pool", lambda e: e.memset(affT[:], 0.0), w=["affT"])
            SHARED4 = ["wglu", "wsso", "wmla", "wout", "wrt", "bglu", "identb4", "ident", "modx", "y_s", "gates_s", "attnT_s", "xm_s", "h2b_own", "affo"]
            ALIAS4 = {"b4": "b2", "b5": "b3", "b6": "b2", "b7": "b3"}

            def tile4(i, s):
                Pq = Keyed(P, s, SHARED4, ALIAS4)
                t0 = i * 128
                yt = TL4[s]['yt']
                yg = TL4[s]['yg']
                t1 = TL4[s]['t1']
                ygb = TL4[s]['ygb']
                ygT = TL4[s]['ygT']
                sg = TL4[s]['sg']
                zb = TL4[s]['zb']
                zT = TL4[s]['zT']
                at32 = TL4[s]['at32']
                atb = TL4[s]['atb']
                gt = TL4[s]['gt']
                m1 = TL4[s]['m1']
                m2 = TL4[s]['m2']
                mb = TL4[s]['mb']
                mT = TL4[s]['mT']
                xt4 = TL4[s]['xt4']
                xm = TL4[s]['xm']
                jk = TL4[s]['jk']
                h2 = TL4[s]['h2']
                h2b = TL4[s]['h2b']
                h2T = TL4[s]['h2T']
                s4 = TL4[s]['s4']
                lg = TL4[s]['lg']
                af = TL4[s]['af']
                bk = BALL[4 * s:4 * s + 4]
                B = [bk[0], bk[1], bk[2], bk[3], bk[2], bk[3], bk[2], bk[3]]
                B0b = B[0][:].bitcast(BF16)
                Pq.dmaop("sp", lambda e, t0=t0: e.dma_start(out=yt[:], in_=ysv[t0:t0 + 128, :]), r=["y_s"], w=["yt"])
                Pq.dmaop("act", lambda e, t0=t0: e.dma_start(out=gt[:], in_=gates_s[t0:t0 + 128, :]), r=["gates_s"], w=["gt"])
                Pq.dmaop("sp", lambda e, t0=t0: e.dma_start(out=at32[:], in_=attnT_s[:, :, t0:t0 + 128].rearrange("h v t -> v h t")),
                        r=["attnT_s"], w=["at32"])
                Pq.dmaop("act", lambda e, t0=t0: e.dma_start(out=xt4[:], in_=xc[NCTX + t0:NCTX + t0 + 128, :]), w=["xt4"])
                Pq.op("pool", lambda e: e.tensor_tensor(out=t1[:], in0=yt[:], in1=yt[:], op=ALU.mult), r=["yt"], w=["t1"])
                Pq.op("dve", lambda e: e.tensor_scalar(out=t1[:], in0=t1[:], scalar1=0.044715, scalar2=1.0, op0=ALU.mult, op1=ALU.add),
                     r=["t1"], w=["t1"])
                Pq.op("dve", lambda e: e.tensor_tensor(out=t1[:], in0=t1[:], in1=yt[:], op=ALU.mult), r=["t1", "yt"], w=["t1"])
                Pq.op("act", lambda e: e.activation(out=t1[:], in_=t1[:], func=AF.Tanh, scale=0.7978845608028654), r=["t1"], w=["t1"])
                Pq.op("dve", lambda e: e.tensor_scalar(out=t1[:], in0=t1[:], scalar1=1.0, scalar2=0.5, op0=ALU.add, op1=ALU.mult),
                     r=["t1"], w=["t1"])
                Pq.op("dve", lambda e: e.tensor_tensor(out=yg[:], in0=t1[:], in1=yt[:], op=ALU.mult), r=["t1", "yt"], w=["yg"])
                Pq.op("pool", lambda e: e.tensor_copy(out=ygb[:], in_=yg[:]), r=["yg"], w=["ygb"])

                def tr4(src, n):
                    def f(e):
                        ins = None
                        for k in range(n):
                            ins = e.transpose(out=B0b[:, k * 128:(k + 1) * 128], in_=src[:, k * 128:(k + 1) * 128], identity=identb[:])
                        return ins
                    return f
                Pq.op("pe", tr4(ygb, 4), r=["ygb", "identb4"], w=["b0"])
                Pq.op("act", lambda e: e.copy(out=ygT[:].rearrange("p a b -> p (a b)"), in_=B0b[:, 0:512]), r=["b0"], w=["ygT"])

                def mmglu(e):
                    ins = None
                    for k in range(4):
                        ins = e.matmul(B[1][:, 0:512], lhsT=ygT[:, k, :], rhs=wglu[:, k, :], start=(k == 0), stop=(k == 3))
                    return ins
                Pq.op("pe", mmglu, r=["ygT", "wglu"], w=["b1"])
                Pq.op("dve", lambda e: e.tensor_tensor(out=sg[:], in0=B[1][:, 0:512], in1=bglu[:], op=ALU.add), r=["b1", "bglu"], w=["sg"])
                Pq.op("act", lambda e: e.activation(out=sg[:], in_=sg[:], func=AF.Sigmoid), r=["sg"], w=["sg"])
                Pq.op("dve", lambda e: e.tensor_tensor(out=zb[:], in0=sg[:], in1=yg[:], op=ALU.mult), r=["sg", "yg"], w=["zb"])
                Pq.op("pe", tr4(zb, 4), r=["zb", "identb4"], w=["b0"])
                Pq.op("act", lambda e: e.copy(out=zT[:].rearrange("p a b -> p (a b)"), in_=B0b[:, 0:512]), r=["b0"], w=["zT"])

                def mmsso(e):
                    ins = None
                    for hf in range(2):
                        for k in range(4):
                            ins = e.matmul(B[2 + hf][:, 0:512], lhsT=zT[:, k, :], rhs=wsso[:, k, hf * 512:(hf + 1) * 512], start=(k == 0), stop=(k == 3))
                    return ins
                Pq.op("pe", mmsso, r=["zT", "wsso"], w=["b2", "b3"])
                for hf in range(2):
                    cs = slice(hf * 512, (hf + 1) * 512)
                    Pq.op("dve", lambda e, hf=hf, cs=cs: e.tensor_tensor(out=m1[:, cs], in0=B[2 + hf][:, 0:512], in1=gt[:, cs], op=ALU.mult),
                          r=["b%d" % (2 + hf), "gt"], w=[("m1", hf)])
                Pq.op("pool", lambda e: e.tensor_copy(out=atb[:], in_=at32[:]), r=["at32"], w=["atb"])

                def mmat(e):
                    ins = None
                    for hf in range(2):
                        for h in range(8):
                            ins = e.matmul(B[4 + hf][:, 0:512], lhsT=atb[:, h, :], rhs=wmla[:, h, hf * 512:(hf + 1) * 512], start=(h == 0), stop=(h == 7))
                    return ins
                Pq.op("pe", mmat, r=["atb", "wmla"], w=["b4", "b5"])
                for hf in range(2):
                    cs = slice(hf * 512, (hf + 1) * 512)
                    cs2 = slice(D + hf * 512, D + (hf + 1) * 512)
                    Pq.op("dve", lambda e, hf=hf, cs=cs, cs2=cs2: e.tensor_tensor(out=m2[:, cs], in0=B[4 + hf][:, 0:512], in1=gt[:, cs2], op=ALU.mult),
                          r=["b%d" % (4 + hf), "gt"], w=[("m2", hf)])
                Pq.op("pool", lambda e: e.tensor_tensor(out=mb[:], in0=m1[:], in1=m2[:], op=ALU.add),
                     r=[("m1", 0), ("m1", 1), ("m2", 0), ("m2", 1)], w=["mb"])
                Pq.op("pe", tr4(mb, 8), r=["mb", "identb4"], w=["b0"])
                Pq.op("act", lambda e: e.copy(out=mT[:].rearrange("p a b -> p (a b)"), in_=B0b[:, 0:1024]), r=["b0"], w=["mT"])

                def mmout(e):
                    ins = None
                    for hf in range(2):
                        for k in range(8):
                            ins = e.matmul(B[6 + hf][:, 0:512], lhsT=mT[:, k, :], rhs=wout[:, k, hf * 512:(hf + 1) * 512], start=(k == 0), stop=(k == 7))
                    return ins
                Pq.op("pe", mmout, r=["mT", "wout"], w=["b6", "b7"])
                for hf in range(2):
                    cs = slice(hf * 512, (hf + 1) * 512)
                    Pq.op("dve", lambda e, hf=hf, cs=cs: e.tensor_tensor(out=xm[:, cs], in0=B[6 + hf][:, 0:512], in1=modx[:, 2 * D + hf * 512:2 * D + (hf + 1) * 512],
                                                                    op=ALU.mult), r=["b%d" % (6 + hf), "modx"], w=[("xm", hf)])
                Pq.op("pool", lambda e: e.tensor_tensor(out=xm[:], in0=xm[:], in1=xt4[:], op=ALU.add), r=[("xm", 0), ("xm", 1), "xt4"], w=[("xm", 0), ("xm", 1)])
                Pq.dmaop("sp", lambda e, t0=t0: e.dma_start(out=xm_s[t0:t0 + 128, :], in_=xm[:]), r=[("xm", 0), ("xm", 1)], w=["xm_s"])
                Pq.op("act", lambda e: e.activation(out=jk[:], in_=xm[:], func=AF.Square, accum_out=s4[:, 0:1]), r=[("xm", 0), ("xm", 1)], w=["jk", "s4a"])
                Pq.op("dve", lambda e: e.tensor_scalar(out=s4[:, 1:2], in0=s4[:, 0:1], scalar1=1.0 / D, scalar2=EPS, op0=ALU.mult, op1=ALU.add), r=["s4a"], w=["s4b"])
                Pq.op("act", lambda e: e.activation(out=s4[:, 1:2], in_=s4[:, 1:2], func=AF.Sqrt), r=["s4b"], w=["s4b"])
                Pq.op("dve", lambda e: e.reciprocal(out=s4[:, 1:2], in_=s4[:, 1:2]), r=["s4b"], w=["s4b"])
                Pq.op("dve", lambda e: e.scalar_tensor_tensor(out=h2[:], in0=xm[:], scalar=s4[:, 1:2], in1=modx[:, 4 * D:5 * D], op0=ALU.mult, op1=ALU.mult),
                     r=[("xm", 0), ("xm", 1), "s4b", "modx"], w=["h2"])
                Pq.op("pool", lambda e: e.tensor_tensor(out=h2[:], in0=h2[:], in1=modx[:, 3 * D:4 * D], op=ALU.add), r=["h2", "modx"], w=["h2"])
                Pq.op("act", lambda e: e.copy(out=h2b[:], in_=h2[:]), r=["h2"], w=["h2b"])
                Pq.dmaop("act", lambda e, t0=t0: e.dma_start(out=h2b_own_c[t0 // RCH].ap()[t0 % RCH:t0 % RCH + 128, :], in_=h2b[:]), r=["h2b"], w=["h2b_own"])
                def trh2(e):
                    ins = None
                    for k in range(8):
                        ins = e.transpose(out=B[2 + k // 4][:, (k % 4) * 128:(k % 4 + 1) * 128], in_=h2[:, k * 128:(k + 1) * 128], identity=ident[:])
                    return ins
                Pq.op("pe", trh2, r=["h2", "ident", ("m1", 0), ("m1", 1)], w=["b2", "b3"])
                Pq.op("act", lambda e: e.copy(out=h2T[:, 0:4, :].rearrange("p a b -> p (a b)"), in_=B[2][:, 0:512]), r=["b2"], w=[("h2T", 0)])
                Pq.op("dve", lambda e: e.tensor_copy(out=h2T[:, 4:8, :].rearrange("p a b -> p (a b)"), in_=B[3][:, 0:512]), r=["b3"], w=[("h2T", 1)])

                def mmrt(e):
                    ins = None
                    for k in range(8):
                        ins = e.matmul(B[1][:, 0:16], lhsT=h2T[:, k, :], rhs=wrt[:, k, :], start=(k == 0), stop=(k == 7))
                    return ins
                Pq.op("pe", mmrt, r=[("h2T", 0), ("h2T", 1), "wrt", "sg"], w=["b1"])
                Pq.op("dve", lambda e: e.tensor_copy(out=lg[:], in_=B[1][:, 0:16]), r=["b1"], w=["lg"])
                Pq.op("dve", lambda e: e.tensor_reduce(out=s4[:, 2:3], in_=lg[:], axis=AX.X, op=ALU.max), r=["lg"], w=["s4c"])
                Pq.op("dve", lambda e: e.tensor_scalar(out=s4[:, 3:4], in0=s4[:, 2:3], scalar1=-1.0, scalar2=None, op0=ALU.mult), r=["s4c"], w=["s4d"])
                hb_ = 0
                afc = slice(0, 16)
                Pq.op("act", lambda e, afc=afc: e.activation(out=af[:, afc], in_=lg[:], func=AF.Exp, bias=s4[:, 3:4], accum_out=s4[:, 4:5]), r=["lg", "s4d"], w=["af", "s4e"])
                Pq.op("dve", lambda e: e.reciprocal(out=s4[:, 5:6], in_=s4[:, 4:5]), r=["s4e"], w=["s4f"])
                Pq.op("dve", lambda e, afc=afc: e.tensor_scalar(out=af[:, afc], in0=af[:, afc], scalar1=s4[:, 5:6], scalar2=None, op0=ALU.mult), r=["af", "s4f"], w=["af"])
                Pq.op("pe", lambda e: e.transpose(out=B[4][0:48, 0:128], in_=af[:], identity=ident[:]), r=["af", "ident", ("m2", 0), ("m2", 1)], w=["b4"])
                Pq.op("act", lambda e, t0=t0: e.copy(out=affo[:, t0:t0 + 128], in_=B[4][0:16, 0:128]), r=["b4"], w=["affo"])
                return Pq.cap
            for i in range(0, NXT, 2):
                caps = [tile4(i, 0)] + ([tile4(i + 1, 1)] if i + 1 < NXT else [])
                interleave(P, caps, chunk=int(os.environ.get("ILV", "2")))
            P.flush()
        with ExitStack() as st:
            NH = NXO
            wk = sb(st, "wk", [48, NH])
            vals = sb(st, "vals", [48, CAPe])
            idxu = sb(st, "idxu", [48, CAPe], U32)
            idxf = sb(st, "idxf", [48, CAPe])
            jrev = sb(st, "jrev", [128, 128])
            tA = sb(st, "tA", [128, 2, 16])
            tB = sb(st, "tB", [128, 2, 16])
            tM = sb(st, "tM", [128, 3, 16])
            B5 = [ps(st, "ph5_%d" % i) for i in range(4)]
            P.dmaop("sp", lambda e: e.dma_start(out=jrev[:], in_=jrev_d), w=["jrev"])
            a01 = sb(st, "a01", [16, 2, NXO])
            selt = sb(st, "selt", [16, 8])
            P.dmaop("sp", lambda e: e.dma_start(out=selt[:], in_=sel_d), w=["selt"])
            P.dmaop("sp", lambda e: e.dma_start(out=aff_own, in_=affo[:]), r=["affo"], w=["aff_own"])
            P.ccop(lambda e: e.collective_compute("AllGather", ALU.bypass, replica_groups=PAIRS, ins=[aff_own_t.ap().opt()], outs=[aff_all_t.ap().opt()]),
                   r=["aff_own"], w=["aff_all"])
            for c in range(NCHK):
                P.ccop(lambda e, c=c: e.collective_compute("AllGather", ALU.bypass, replica_groups=PAIRS, ins=[h2b_own_c[c].ap().opt()], outs=[h2b_ag_c[c].ap().opt()]),
                       r=["h2b_own"], w=[("h2b_ag", c)])
                for rk_ in range(2):
                    P.dmaop("act", lambda e, c=c, rk_=rk_: e.dma_start(out=h2b_all[rk_ * NXO + c * RCH:rk_ * NXO + (c + 1) * RCH, :],
                                                                      in_=h2b_ag_c[c].ap()[rk_ * RCH:(rk_ + 1) * RCH, :]), r=[("h2b_ag", c)], w=["h2b_all"])
            for rk_ in range(2):
                P.dmaop("sp", lambda e, rk_=rk_: e.dma_start(out=a01[:, rk_, :], in_=aff_all[16 * rk_:16 * rk_ + 16, :]), r=["aff_all"], w=[("a01", rk_)])
            for rk_ in range(2):
                for c0 in range(0, NXO, 512):
                    n = min(512, NXO - c0)
                    pb_ = B5[rk_]
                    P.op("pe", lambda e, rk_=rk_, c0=c0, n=n, pb_=pb_: e.matmul(pb_[32 * rk_:32 * rk_ + 8, 0:n], lhsT=selt[:, :], rhs=a01[:, rk_, c0:c0 + n],
                                                                             start=True, stop=True, tile_position=(0, 32 * rk_)),
                         r=["selt", ("a01", rk_)], w=[("b5", rk_)])
                    P.op("act", lambda e, rk_=rk_, c0=c0, n=n, pb_=pb_: e.copy(out=affT[32 * rk_:32 * rk_ + 8, c0:c0 + n], in_=pb_[32 * rk_:32 * rk_ + 8, 0:n]),
                         r=[("b5", rk_)], w=["affT"])
            P.op("dve", lambda e: e.tensor_copy(out=wk[:], in_=affT[:]), r=["affT"], w=["wk"])
            for r_ in range(CAPe // 8):
                sl = slice(r_ * 8, r_ * 8 + 8)
                P.op("dve", lambda e, sl=sl: e.max(out=vals[:, sl], in_=wk[:]), r=["wk"], w=[("vals", r_)])
                P.op("dve", lambda e, sl=sl: e.max_index(out=idxu[:, sl], in_max=vals[:, sl], in_values=wk[:]), r=["wk", ("vals", r_)], w=[("idxu", r_)])
                P.op("dve", lambda e, sl=sl: e.match_replace(out=wk[:], in_to_replace=vals[:, sl], in_values=wk[:], imm_value=-1.0),
                     r=["wk", ("vals", r_), ("idxu", r_)], w=["wk"])
            allv = [("vals", r_) for r_ in range(CAPe // 8)]
            alli = [("idxu", r_) for r_ in range(CAPe // 8)]
            P.op("dve", lambda e: e.tensor_copy(out=idxf[:], in_=idxu[:]), r=alli, w=["idxf"])
            P.op("dve", lambda e: e.tensor_scalar(out=idxf[32:48, :], in0=idxf[32:48, :], scalar1=float(NH), scalar2=None, op0=ALU.add), r=["idxf"], w=["idxf"])
            Jb = jrev[0:SLT, 128 - SLT:128]
            for rc in range(NRC):
                cs = slice(rc * SLT, (rc + 1) * SLT)
                rb = NRC - 1 - rc
                cb_ = slice(rb * SLT, (rb + 1) * SLT)
                for w_, src in ((0, vals), (1, idxf)):
                    rk = allv if w_ == 0 else ["idxf"]
                    P.op("pe", lambda e, cs=cs, src=src, w_=w_: e.transpose(out=B5[w_][0:SLT, 0:16], in_=src[0:16, cs], identity=ident[0:16, 0:16]),
                         r=rk + ["ident"], w=[("b5", w_)])
                    P.op("act", lambda e, w_=w_: e.copy(out=tA[0:SLT, w_, :], in_=B5[w_][0:SLT, 0:16]), r=[("b5", w_)], w=[("tA", w_)])
                    P.op("pe", lambda e, cb_=cb_, src=src, w_=w_: e.transpose(out=B5[2 + w_][0:SLT, 0:16], in_=src[32:48, cb_], identity=ident[32:48, 32:48]),
                         r=rk + ["ident"], w=[("b5", 2 + w_)])
                    P.op("act", lambda e, w_=w_: e.copy(out=tB[0:SLT, w_, :], in_=B5[2 + w_][0:SLT, 0:16]), r=[("b5", 2 + w_)], w=[("tB", w_)])
                    P.op("pe", lambda e, w_=w_: e.matmul(B5[2 + w_][0:SLT, 0:16], lhsT=Jb, rhs=tB[0:SLT, w_, :], start=True, stop=True),
                         r=[("tB", w_), "jrev"], w=[("b5", 2 + w_)])
                P.op("dve", lambda e: e.tensor_tensor(out=tM[0:SLT, 0, :], in0=tA[0:SLT, 0, :], in1=B5[2][0:SLT, 0:16], op=ALU.is_gt),
                     r=[("tA", 0), ("b5", 2)], w=[("tM", 0)])
                P.op("dve", lambda e, rc=rc: e.tensor_tensor(out=gateT[0:SLT, rc, :], in0=tA[0:SLT, 0, :], in1=B5[2][0:SLT, 0:16], op=ALU.max),
                     r=[("tA", 0), ("b5", 2)], w=["gateT"])
                P.op("dve", lambda e: e.tensor_tensor(out=tM[0:SLT, 1, :], in0=tA[0:SLT, 1, :], in1=B5[3][0:SLT, 0:16], op=ALU.subtract),
                     r=[("tA", 1), ("b5", 3)], w=[("tM", 1)])
                P.op("dve", lambda e: e.tensor_tensor(out=tM[0:SLT, 1, :], in0=tM[0:SLT, 1, :], in1=tM[0:SLT, 0, :], op=ALU.mult),
                     r=[("tM", 1), ("tM", 0)], w=[("tM", 1)])
                P.op("dve", lambda e: e.tensor_tensor(out=tM[0:SLT, 2, :], in0=tM[0:SLT, 1, :], in1=B5[3][0:SLT, 0:16], op=ALU.add),
                     r=[("tM", 1), ("b5", 3)], w=[("tM", 2)])
                P.op("dve", lambda e, rc=rc: e.tensor_copy(out=idxT[0:SLT, rc, :], in_=tM[0:SLT, 2, :]), r=[("tM", 2)], w=["idxT"])
            P.flush()
        sp45.close()
        NEe = NE // 2 if debug == 0 else int(os.environ.get("NEE", "8"))
        with ExitStack() as st:
            identb = sb(st, "identb6", [128, 128], BF16)
            xs = sb(st, "xs", [128, 1, NRC, D], BF16)
            xsT = sb(st, "xsT", [128, 2, 8, CAPe], BF16)
            wg = sb(st, "wg", [128, 3, 8, 512], BF16)
            wu = sb(st, "wu", [128, 3, 8, 512], BF16)
            wd = sb(st, "wd", [128, 2, 22, 512], BF16)
            sgt = sb(st, "sgt", [128, 2, CAPe])
            hidT = sb(st, "hidT", [128, 22, CAPe], BF16)
            ys = sb(st, "ys", [128, 1, NRC, D])
            B = [ps(st, "ph6_%d" % i) for i in range(8)]
            B0b = B[0][:].bitcast(BF16)
            P.op("dve", lambda e: e.tensor_copy(out=identb[:], in_=ident[:]), r=["ident"], w=["identb6"])
            wgi = 0
            wdi = 0
            def prep(ex):
                sl = ex % 2
                for rc in range(NRC):
                    P.dmaop("pool", lambda e, rc=rc, ex=ex, sl=sl: e.indirect_dma_start(
                        out=xs[0:SLT, 0, rc, :], out_offset=None, in_=h2b_all[0:2 * NXO, :],
                        in_offset=bass.IndirectOffsetOnAxis(ap=idxT[0:SLT, rc, ex:ex + 1], axis=0)),
                        r=["idxT", "h2b_all"], w=[("xs", 0, rc)])

                    def trx(e, rc=rc, sl=sl):
                        ins = None
                        for k in range(8):
                            ins = e.transpose(out=B0b[:, k * 128:k * 128 + SLT], in_=xs[0:SLT, 0, rc, k * 128:(k + 1) * 128], identity=identb[0:SLT, 0:SLT])
                        return ins
                    P.op("pe", trx, r=[("xs", 0, rc), "identb6"], w=["b0"])
                    P.op("act", lambda e, rc=rc, sl=sl: e.copy(out=xsT[:, sl, :, rc * SLT:(rc + 1) * SLT],
                                                               in_=B0b[:, 0:1024].rearrange("p (k c) -> p k c", k=8)[:, :, 0:SLT]), r=["b0"], w=[("xsT", sl)])
            prep(0)
            pending = []
            for ex in range(NEe):
                xsl = ex % 2
                wgv = w_e_gate[ex].rearrange("(dc p) f -> p dc f", p=128)
                wuv = w_e_up[ex].rearrange("(dc p) f -> p dc f", p=128)
                wdv = w_e_down[ex].rearrange("(fc p) d -> p fc d", p=128)
                for grp in range(6):
                    ws = wgi % 3
                    wgi += 1
                    f0 = grp * 512
                    fw = min(512, FF - f0)
                    P.dmaop("pool", lambda e, ws=ws, f0=f0, fw=fw, wgv=wgv: e.dma_start(out=wg[:, ws, :, 0:fw], in_=wgv[:, :, f0:f0 + fw]), w=[("wg", ws)])
                    P.dmaop("pool", lambda e, ws=ws, f0=f0, fw=fw, wuv=wuv: e.dma_start(out=wu[:, ws, :, 0:fw], in_=wuv[:, :, f0:f0 + fw]), w=[("wu", ws)])
                    if grp == 1:
                        for f_ in pending:
                            f_()
                        pending = []
                    for q in range(fw // 128):
                        fc = grp * 4 + q
                        pg = B[1 + fc % 2]
                        pu = B[3 + fc % 2]

                        def mmgu(e, ws=ws, q=q, pg=pg, pu=pu, xsl=xsl):
                            ins = None
                            for k in range(8):
                                e.matmul(pg[:, 0:CAPe], lhsT=wg[:, ws, k, q * 128:(q + 1) * 128], rhs=xsT[:, xsl, k, :], start=(k == 0), stop=(k == 7))
                            for k in range(8):
                                ins = e.matmul(pu[:, 0:CAPe], lhsT=wu[:, ws, k, q * 128:(q + 1) * 128], rhs=xsT[:, xsl, k, :], start=(k == 0), stop=(k == 7))
                            return ins
                        P.op("pe", mmgu, r=[("wg", ws), ("wu", ws), ("xsT", xsl)], w=[("b6", 1 + fc % 2), ("b6", 3 + fc % 2)])
                        P.op("act", lambda e, fc=fc, pg=pg: e.activation(out=sgt[:, fc % 2, :], in_=pg[:, 0:CAPe], func=AF.Silu),
                             r=[("b6", 1 + fc % 2)], w=[("sgt", fc % 2)])
                        P.op("dve", lambda e, fc=fc, pu=pu: e.tensor_tensor(out=hidT[:, fc, :], in0=sgt[:, fc % 2, :], in1=pu[:, 0:CAPe], op=ALU.mult),
                             r=[("sgt", fc % 2), ("b6", 3 + fc % 2)], w=[("hidT", fc)])
                hk_ = [("hidT", fc) for fc in range(22)]
                if ex + 1 < NEe:
                    prep(ex + 1)
                for dq in range(2):
                    ws = wdi % 2
                    wdi += 1
                    P.dmaop("pool", lambda e, ws=ws, dq=dq, wdv=wdv: e.dma_start(out=wd[:, ws], in_=wdv[:, :, dq * 512:(dq + 1) * 512]), w=[("wd", ws)])
                    for rc in range(NRC):
                        py = B[5 + (dq * NRC + rc) % 2]
                        pyk = ("b6", 5 + (dq * NRC + rc) % 2)

                        def mmd(e, ws=ws, rc=rc, py=py):
                            ins = None
                            for fc in range(22):
                                ins = e.matmul(py[0:SLT, 0:512], lhsT=hidT[:, fc, rc * SLT:(rc + 1) * SLT], rhs=wd[:, ws, fc, :], start=(fc == 0), stop=(fc == 21))
                            return ins
                        P.op("pe", mmd, r=hk_ + [("wd", ws)], w=[pyk])
                        P.op("dve", lambda e, rc=rc, dq=dq, py=py, ex=ex, xsl=xsl: e.scalar_tensor_tensor(
                            out=ys[0:SLT, 0, rc, dq * 512:(dq + 1) * 512], in0=py[0:SLT, 0:512], scalar=gateT[0:SLT, rc, ex:ex + 1],
                            in1=modx[0:SLT, 5 * D + dq * 512:5 * D + (dq + 1) * 512], op0=ALU.mult, op1=ALU.mult),
                            r=[pyk, "gateT", "modx"], w=[("ys", 0, rc, dq)])
                def scat(ex=ex):
                    prevk = [("outx", (ex - 1) % 2, rc2) for rc2 in range(NRC)] if ex > 0 else ["acc0"]
                    for rc in range(NRC):
                        P.dmaop("pool", lambda e, rc=rc, ex=ex: e.indirect_dma_start(
                            out=acc[0:2 * NXO, :], out_offset=bass.IndirectOffsetOnAxis(ap=idxT[0:SLT, rc, ex:ex + 1], axis=0),
                            in_=ys[0:SLT, 0, rc, :], in_offset=None, compute_op=ALU.add),
                            r=[("ys", 0, rc, dq) for dq in range(2)] + ["idxT"] + prevk, w=[("outx", ex % 2, rc)])
                pending.append(scat)
            for f_ in pending:
                f_()
            P.flush()
        with ExitStack() as st7:
            fa = sb(st7, "fa", [128, 4 * D])
            P.ccop(lambda e: e.collective_compute("ReduceScatter", ALU.add, replica_groups=PAIRS, ins=[acc_t.ap().opt()], outs=[rs_out_t.ap().opt()]),
                   w=["rs_out"])
            for i in range(NXT):
                t0 = i * 128
                k = i % 2
                P.dmaop("sp", lambda e, t0=t0, k=k: e.dma_start(out=fa[:, k * 2 * D:k * 2 * D + D], in_=xm_s[t0:t0 + 128, :]), w=[("fa", k, 0)])
                P.dmaop("act", lambda e, t0=t0, k=k: e.dma_start(out=fa[:, k * 2 * D + D:(k + 1) * 2 * D], in_=rs_out[t0:t0 + 128, :]), r=["rs_out"], w=[("fa", k, 1)])
                P.op("dve", lambda e, k=k: e.tensor_tensor(out=fa[:, k * 2 * D:k * 2 * D + D], in0=fa[:, k * 2 * D:k * 2 * D + D],
                                                          in1=fa[:, k * 2 * D + D:(k + 1) * 2 * D], op=ALU.add), r=[("fa", k, 0), ("fa", k, 1)], w=[("fa", k, 0)])
                P.dmaop("sp", lambda e, t0=t0, k=k: e.dma_start(out=out[t0:t0 + 128, :], in_=fa[:, k * 2 * D:k * 2 * D + D]), r=[("fa", k, 0)], w=["out"])
            P.flush()
        if debug == 6:
            DONE.append(1)
    if DONE:
        es.close()
        return nc

    if debug == 3:
        dbg = nc.dram_tensor("dbg", [H, 64, 256], F32, kind="ExternalOutput").ap()
        P.dmaop("sp", lambda e: e.dma_start(out=dbg, in_=attnT_s[:, :, 0:256]), w=["dbg"])
        P.flush()
        es.close()
        return nc

    if debug == 2:
        dbg = nc.dram_tensor("dbg", [H, QK, 512], BF16, kind="ExternalOutput").ap()
        dbg2 = nc.dram_tensor("dbg2", [512, 512], F32, kind="ExternalOutput").ap()
        dbg3 = nc.dram_tensor("dbg3", [H, QK, 256], BF16, kind="ExternalOutput").ap()
        P.dmaop("sp", lambda e: e.dma_start(out=dbg, in_=kT_s[:, :, 0:512]), w=["dbg"])
        P.dmaop("sp", lambda e: e.dma_start(out=dbg2, in_=u_s[0:512, :]), w=["dbg2"])
        P.dmaop("sp", lambda e: e.dma_start(out=dbg3, in_=qT_s[:, :, 0:256]), w=["dbg3"])
        P.flush()
        es.close()
        return nc

    if debug == 1:
        dbg = nc.dram_tensor("dbg", [128, 6 * D], F32, kind="ExternalOutput").ap()
        P.dmaop("sp", lambda e: e.dma_start(out=dbg, in_=modx[:]), r=["modx"], w=["dbg"])
        P.flush()
        es.close()
        return nc

    es.close()
    return nc


def _consts():
    ident = np.eye(128, dtype=np.float32)
    n = NX
    rows = n // 64
    row = np.repeat(np.arange(rows, dtype=np.float32), 64)
    col = np.tile(np.arange(64, dtype=np.float32), rows)
    inv = (10000.0 ** (-np.arange(8, dtype=np.float32) / 8)).astype(np.float32)
    ang = np.stack([row[:, None] * inv, col[:, None] * inv], axis=1).astype(np.float32)
    rope = np.zeros((NT, 32), np.float32)
    rope[:NCTX, :16] = 1.0
    rope[NCTX:, :16] = np.cos(ang).reshape(n, 16)
    rope[NCTX:, 16:] = np.sin(ang).reshape(n, 16)
    cm = np.zeros((128, 256), np.float32)
    for jp in range(8):
        for j in range(8):
            if jp <= j:
                cm[jp * 16:(jp + 1) * 16, j * 16:(j + 1) * 16] = 1.0
            if jp >= j:
                cm[jp * 16:(jp + 1) * 16, 128 + j * 16:128 + (j + 1) * 16] = 1.0
    return ident, rope, cm


def make_in_maps(inputs, nx=NX):
    ident, rope, cm = _consts()
    f = lambda a: np.ascontiguousarray(np.asarray(a, dtype=np.float32))
    maps = []
    dirk = ("ssm_lam_re", "ssm_lam_im", "ssm_log_dt", "ssm_b_re", "ssm_b_im", "ssm_c_re", "ssm_c_im")
    for b in range(NCORES // 2):
        for r in range(2):
            x_b = np.asarray(inputs["x"][b])[:nx]
            ctx_b = np.asarray(inputs["ctx"][b])
            rope_x = rope[NCTX:NCTX + nx]
            if r == 1:
                x_b, ctx_b, rope_x = x_b[::-1], ctx_b[::-1], rope_x[::-1]
            xc = np.zeros((NT, D), np.float32)
            xc[:NCTX] = ctx_b
            xc[NCTX:NCTX + nx] = x_b
            rope_r = rope.copy()
            rope_r[NCTX:NCTX + nx] = rope_x
            sel = np.zeros((NE, NE // 2), np.float32)
            sel[np.arange(NE // 2) + (NE // 2) * r, np.arange(NE // 2)] = 1.0
            m = {"xc": xc, "cb": f(inputs["c"][b]), "c_ctx": f(inputs["c_ctx"]), "ident": ident, "rope": rope_r, "cmask": cm,
                 "jrev": np.ascontiguousarray(ident[::-1]), "sel": sel}
            for k in ["w_ada", "b_ada", "norm1_g", "norm2_g", "w_in", "q_a_g", "w_qb", "kv_a_g", "w_kvb", "q_norm_g",
                      "k_norm_g", "w_mla_o", "ssm_d", "w_glu", "b_glu", "w_ssm_o", "w_out", "w_router"]:
                m[k] = f(np.asarray(inputs[k])[0])
            for k in dirk:
                a_ = np.asarray(inputs[k])[0]
                m[k] = f(a_[::-1] if r == 1 else a_)
            e0 = (NE // 2) * r
            for k in ("w_e_gate", "w_e_up", "w_e_down"):
                m[k] = f(np.asarray(inputs[k])[0][e0:e0 + NE // 2])
            maps.append(m)
    return maps


def assemble(results, nx=NX):
    h = nx // 2
    out = np.zeros((NCORES // 2, nx, D), np.float32)
    for b in range(NCORES // 2):
        out[b, :h] = np.asarray(results[2 * b]["out"], dtype=np.float32)[:h]
        out[b, h:] = np.asarray(results[2 * b + 1]["out"], dtype=np.float32)[:h][::-1]
    return out


def kernel(**inputs):
    nc = build()
    maps = make_in_maps(inputs)
    res = run_bass_kernel_spmd(nc, maps, core_ids=list(range(NCORES)))
    return assemble(res.results)
```

```python
import math
import os
from contextlib import ExitStack

import numpy as np
import concourse.bass as bass
import concourse.mybir as mybir
from concourse.bass_utils import run_bass_kernel_spmd

F32 = mybir.dt.float32
F32R = mybir.dt.float32r
BF16 = mybir.dt.bfloat16
U32 = mybir.dt.uint32
I32 = mybir.dt.int32
ALU = mybir.AluOpType
AF = mybir.ActivationFunctionType
AX = mybir.AxisListType

D = 1024
NX = 4096
NCTX = 256
NT = NX + NCTX
NTILE = NT // 128
NCH = NT // 8
NCH_C = NCTX // 8
H = 8
QK = 96
NE = 16
FF = 2816
CAP = 512
EPS = 1e-6
IN_COLS = 3232
NCORES = 8

ENG = {"pe": "tensor", "act": "scalar", "dve": "vector", "pool": "gpsimd", "sp": "sync"}


class Prog:
    def __init__(self, nc, sems, dsems):
        self.nc = nc
        self.ops = []
        self.lastw = {}
        self.readers = {}
        self.sems = sems
        self.dsems = dsems
        self.sig = {e: 0 for e in ENG}
        self.dcnt = {e: [0] * len(dsems[e]) for e in dsems}
        self.dnext = {e: 0 for e in dsems}
        self.seen = {e: {} for e in ENG}
        self.emitted = 0
        self.ccsem = None
        self.cccnt = 0

    def op(self, eng, fn, r=(), w=(), dma=False):
        i = len(self.ops)
        deps = set()
        for k in list(r) + list(w):
            if k in self.lastw:
                deps.add(self.lastw[k])
        for k in w:
            for j in self.readers.get(k, ()):
                deps.add(j)
        deps.discard(i)
        o = dict(eng=eng, fn=fn, deps=deps, dma=dma, need=False, val=None, sem=None, idx=i)
        self.ops.append(o)
        for k in w:
            self.lastw[k] = i
            self.readers[k] = []
        for k in r:
            if k not in w:
                self.readers.setdefault(k, []).append(i)
        return i

    def dmaop(self, eng, fn, r=(), w=()):
        return self.op(eng, fn, r, w, dma=True)

    def ccop(self, fn, r=(), w=()):
        return self.op("pool", fn, r, w, dma="cc")

    def flush(self, final_wait_eng="sp"):
        nc = self.nc
        ops = self.ops[self.emitted:]
        pos = {}
        cnt = {e: 0 for e in ENG}
        for o in self.ops[:self.emitted]:
            pass
        for o in ops:
            pos[o["idx"]] = cnt[o["eng"]]
            cnt[o["eng"]] += 1
        for o in ops:
            real = []
            for d in o["deps"]:
                if d < self.emitted:
                    continue
                p = self.ops[d]
                if not p["dma"] and p["eng"] == o["eng"]:
                    if o["eng"] == "pe":
                        continue
                    if o["dma"]:
                        continue
                real.append(d)
                p["need"] = True
            o["real"] = real
        for o in ops:
            if o["dma"]:
                for d in o["deps"]:
                    if d >= self.emitted:
                        p = self.ops[d]
                        if not p["dma"] and p["eng"] == o["eng"] and d not in o["real"]:
                            o["real"].append(d)
                            p["need"] = True
        for o in ops:
            if o["dma"] == "cc":
                o["prev"] = self.cccnt
                self.cccnt += 1
                o["sem"] = self.ccsem
                o["val"] = self.cccnt
            elif o["dma"]:
                e = o["eng"]
                k = self.dnext[e]
                self.dnext[e] = (k + 1) % len(self.dsems[e])
                o["prev"] = self.dcnt[e][k]
                self.dcnt[e][k] += 16
                o["sem"] = self.dsems[e][k]
                o["val"] = self.dcnt[e][k]
            elif o["need"]:
                self.sig[o["eng"]] += 1
                o["sem"] = self.sems[o["eng"]]
                o["val"] = self.sig[o["eng"]]
        byeng = {e: [o for o in ops if o["eng"] == e] for e in ENG}
        self_ = self
        seen = self.seen
        allops = self.ops
        dsems = self.dsems
        dcnt = self.dcnt

        def body(e):
            def f(engine):
                sn = seen[e]

                def wait(sem, val):
                    key = id(sem)
                    if sn.get(key, 0) >= val:
                        return
                    sn[key] = val
                    engine.wait_ge(sem, val)

                for o in byeng[e]:
                    for d in o["real"]:
                        p = allops[d]
                        wait(p["sem"], p["val"])
                    if o["dma"]:
                        if o["prev"] > 0:
                            wait(o["sem"], o["prev"])
                        ins = o["fn"](engine)
                        if o["dma"] == "cc":
                            ins.then_inc(o["sem"])
                        else:
                            ins.then_inc(o["sem"], 16)
                    else:
                        ins = o["fn"](engine)
                        if o["need"]:
                            ins.then_inc(o["sem"], 1)
                if e in dsems:
                    for k, s in enumerate(dsems[e]):
                        if dcnt[e][k] > 0:
                            wait(s, dcnt[e][k])
                if e == "pool" and self_.cccnt > 0:
                    wait(self_.ccsem, self_.cccnt)
            return f

        with nc.Block() as block:
            for e in ENG:
                getattr(block, ENG[e])(body(e))
        self.emitted = len(self.ops)
        self.lastw = {}
        self.readers = {}


class Keyed:
    def __init__(self, P, slot, shared, alias=None):
        self.P, self.slot, self.shared, self.alias = P, slot, set(shared), (alias or {})
        self.cap = []

    def _k(self, keys):
        out = []
        for k in keys:
            k = self.alias.get(k, k)
            base = k[0] if isinstance(k, tuple) else k
            out.append(k if base in self.shared else ("slot", self.slot, k))
        return out

    def op(self, eng, fn, r=(), w=(), dma=False):
        self.cap.append((eng, fn, self._k(r), self._k(w), dma))

    def dmaop(self, eng, fn, r=(), w=()):
        self.op(eng, fn, r, w, dma=True)


def interleave(P, caps, chunk=3):
    pos = [0] * len(caps)
    live = True
    while live:
        live = False
        for i, c in enumerate(caps):
            n = 0
            while pos[i] < len(c) and n < chunk:
                eng, fn, r, w, dma = c[pos[i]]
                P.op(eng, fn, r, w, dma=dma)
                pos[i] += 1
                n += 1
            if pos[i] < len(c):
                live = True


def r32(ap):
    return ap.bitcast(F32R)


def build(debug=0):
    nc = bass.Bass("TRN2", target_bir_lowering=False)
    es = ExitStack()
    DONE = []

    def din(name, shape, dt=F32):
        return nc.dram_tensor(name, list(shape), dt, kind="ExternalInput").ap()

    def dscr(name, shape, dt=F32):
        return nc.dram_tensor(name, list(shape), dt, kind="Internal").ap()

    xc = din("xc", [NT, D])
    cb = din("cb", [D])
    cctx = din("c_ctx", [D])
    w_ada = din("w_ada", [D, 6 * D])
    b_ada = din("b_ada", [6 * D])
    norm1_g = din("norm1_g", [D])
    norm2_g = din("norm2_g", [D])
    w_in = din("w_in", [D, IN_COLS])
    q_a_g = din("q_a_g", [384])
    w_qb = din("w_qb", [384, 768])
    kv_a_g = din("kv_a_g", [256])
    w_kvb = din("w_kvb", [256, 1024])
    q_norm_g = din("q_norm_g", [96])
    k_norm_g = din("k_norm_g", [96])
    w_mla_o = din("w_mla_o", [512, D])
    lam_re = din("ssm_lam_re", [2, 32, 64])
    lam_im = din("ssm_lam_im", [2, 32, 64])
    log_dt = din("ssm_log_dt", [2, 32])
    b_re = din("ssm_b_re", [2, 32, 64, 16])
    b_im = din("ssm_b_im", [2, 32, 64, 16])
    c_re = din("ssm_c_re", [2, 32, 16, 64])
    c_im = din("ssm_c_im", [2, 32, 16, 64])
    ssm_d = din("ssm_d", [512])
    w_glu = din("w_glu", [512, 512])
    b_glu = din("b_glu", [512])
    w_ssm_o = din("w_ssm_o", [512, D])
    w_out = din("w_out", [D, D])
    w_router = din("w_router", [D, NE])
    if debug in (0, 6):
        w_e_gate = din("w_e_gate", [NE // 2, D, FF])
        w_e_up = din("w_e_up", [NE // 2, D, FF])
        w_e_down = din("w_e_down", [NE // 2, FF, D])
    sel_d = din("sel", [NE, NE // 2])
    ident_d = din("ident", [128, 128])
    rope_d = din("rope", [NT, 32])
    jrev_d = din("jrev", [128, 128])
    cmask_d = din("cmask", [128, 256])
    small = debug in (2, 3, 4, 5, 6)
    NTe = 4 if small else NTILE
    NXe = (NTe - 2) * 128
    NXO = NXe // 2
    NTO = NXO // 128
    out = nc.dram_tensor("out", [NXO, D], F32, kind="ExternalOutput").ap()

    u_s = dscr("u_s", [NT, 512])
    gates_s = dscr("gates_s", [NXO, 2048])
    qT_s = dscr("qT_s", [H, QK, NXO], BF16)
    kT_s = dscr("kT_s", [H, QK, NT], BF16)
    v_s = dscr("v_s", [NT, 512], BF16)
    attnT_s = dscr("attnT_s", [H, 64, NXO])
    y_s = dscr("y_s", [NXO // 8, 8 * 512], BF16)
    xm_s = dscr("xm_s", [NXO, D])
    NCHK = 2 if NXO >= 2048 else 1
    RCH = NXO // NCHK
    h2b_own_c = [nc.dram_tensor("h2b_own%d" % c, [RCH, D], BF16) for c in range(NCHK)]
    h2b_ag_c = [nc.dram_tensor("h2b_ag%d" % c, [2 * RCH, D], BF16) for c in range(NCHK)]
    h2b_all_t = nc.dram_tensor("h2b_all", [2 * NXO, D], BF16)
    aff_own_t = nc.dram_tensor("aff_own", [NE, NXO], F32)
    aff_all_t = nc.dram_tensor("aff_all", [2 * NE, NXO], F32)
    acc_t = nc.dram_tensor("acc", [2 * NXO, D], F32)
    rs_out_t = nc.dram_tensor("rs_out", [NXO, D], F32)
    h2b_all, aff_own, aff_all, acc, rs_out = (t.ap() for t in (h2b_all_t, aff_own_t, aff_all_t, acc_t, rs_out_t))
    PAIRS = [[0, 1], [2, 3], [4, 5], [6, 7]]
    h2_s = dscr("h2_s", [NX, D])

    sems = {e: es.enter_context(nc.semaphore("s_" + e)) for e in ENG}
    dsems = {e: [es.enter_context(nc.semaphore("d_%s%d" % (e, k))) for k in range(8)]
             for e in ("sp", "act", "pool")}
    P = Prog(nc, sems, dsems)
    P.ccsem = es.enter_context(nc.semaphore("cc_sem"))

    def sb(stack, name, shape, dt=F32):
        return stack.enter_context(nc.sbuf_tensor("t_" + name, list(shape), dt))

    def ps(stack, name, shape=(128, 512), dt=F32):
        return stack.enter_context(nc.psum_tensor("p_" + name, list(shape), dt))

    ident = sb(es, "ident", [128, 128])
    modx = sb(es, "modx", [128, 6 * D])
    es1 = ExitStack()
    modc = sb(es1, "modc", [128, 2 * D])
    P.dmaop("sp", lambda e: e.dma_start(out=ident[:], in_=ident_d), w=["ident"])

    with ExitStack() as st:
        cT = sb(st, "cT", [128, 2, 8])
        sc = sb(st, "sc", [128, 2, 8])
        lbc = sb(st, "lbc", [128, 2, 8, 128], BF16)
        wa = sb(st, "wa", [128, 2, 8, 512], BF16)
        bb = sb(st, "bb", [128, 6 * D])
        g1b = sb(st, "g1b", [128, D])
        g2b = sb(st, "g2b", [128, D])
        pm = [ps(st, "pm%d" % i) for i in range(4)]
        P.dmaop("sp", lambda e: e.dma_start(out=cT[:, 0, :], in_=cb.rearrange("(dc p) -> p dc", p=128),
                                            allow_slow_non_contiguous=True), w=["cT0"])
        P.dmaop("sp", lambda e: e.dma_start(out=cT[:, 1, :], in_=cctx.rearrange("(dc p) -> p dc", p=128),
                                            allow_slow_non_contiguous=True), w=["cT1"])
        P.dmaop("act", lambda e: e.dma_start(out=bb[:], in_=b_ada.partition_broadcast(128)), w=["bb"])
        P.dmaop("act", lambda e: e.dma_start(out=g1b[:], in_=norm1_g.partition_broadcast(128)), w=["g1b"])
        P.dmaop("act", lambda e: e.dma_start(out=g2b[:], in_=norm2_g.partition_broadcast(128)), w=["g2b"])
        P.op("act", lambda e: e.activation(out=sc[:], in_=cT[:], func=AF.Silu), r=["cT0", "cT1"], w=["sc"])
        P.op("dve", lambda e: e.tensor_copy(out=lbc[:], in_=sc[:].unsqueeze(3).to_broadcast([128, 2, 8, 128])),
             r=["sc"], w=["lbc"])
        wv = w_ada.rearrange("(dc p) n -> p dc n", p=128)
        for ct in range(12):
            s = ct % 2
            P.dmaop("pool",
                    lambda e, ct=ct, s=s: e.dma_start(out=wa[:, s], in_=wv[:, :, ct * 512:(ct + 1) * 512]),
                    w=[("wa", s)])
            for which in range(2 if ct < 4 else 1):
                pt = pm[(ct * 2 + which) % 4]
                pk = ("pm", (ct * 2 + which) % 4)

                def mm(e, pt=pt, s=s, which=which):
                    ins = None
                    for dc in range(8):
                        ins = e.matmul(pt[:], lhsT=lbc[:, which, dc, :], rhs=wa[:, s, dc, :],
                                       start=(dc == 0), stop=(dc == 7))
                    return ins
                P.op("pe", mm, r=["lbc", ("wa", s)], w=[pk])
                dst = modx if which == 0 else modc
                P.op("dve", lambda e, pt=pt, dst=dst, ct=ct: e.tensor_tensor(
                    out=dst[:, ct * 512:(ct + 1) * 512], in0=pt[:], in1=bb[:, ct * 512:(ct + 1) * 512], op=ALU.add),
                    r=[pk, "bb"], w=["modx" if which == 0 else "modc"])
        P.op("dve", lambda e: e.scalar_tensor_tensor(out=modx[:, D:2 * D], in0=modx[:, D:2 * D], scalar=1.0, in1=g1b[:],
                                                     op0=ALU.add, op1=ALU.mult), r=["modx", "g1b"], w=["modx"])
        P.op("dve", lambda e: e.scalar_tensor_tensor(out=modx[:, 4 * D:5 * D], in0=modx[:, 4 * D:5 * D], scalar=1.0, in1=g2b[:],
                                                     op0=ALU.add, op1=ALU.mult), r=["modx", "g2b"], w=["modx"])
        P.op("dve", lambda e: e.scalar_tensor_tensor(out=modc[:, D:2 * D], in0=modc[:, D:2 * D], scalar=1.0, in1=g1b[:],
                                                     op0=ALU.add, op1=ALU.mult), r=["modc", "g1b"], w=["modc"])
        P.flush()


    with ExitStack() as st:
      if debug != 1:
          TL1 = [dict(), dict()]
          w_in_sb = sb(st, "w_in_sb", [128, 8, IN_COLS], BF16)
          w_qb_sb = sb(st, "w_qb_sb", [128, 3, 768], BF16)
          w_kvb_sb = sb(st, "w_kvb_sb", [128, 2, 1024], BF16)
          qag = sb(st, "qag", [128, 384])
          kvag = sb(st, "kvag", [128, 256])
          qng = sb(st, "qng", [128, 96])
          kng = sb(st, "kng", [128, 96])
          identb = sb(st, "identb", [128, 128], BF16)
          xt = sb(st, "xt", [128, 2, D])
          TL1[0]['junk'] = sb(st, "junk_0", [128, D]); TL1[1]['junk'] = sb(st, "junk_1", [128, D])
          TL1[0]['hh'] = sb(st, "hh_0", [128, D]); TL1[1]['hh'] = sb(st, "hh_1", [128, D])
          TL1[0]['hb'] = sb(st, "hb_0", [128, D], BF16); TL1[1]['hb'] = sb(st, "hb_1", [128, D], BF16)
          TL1[0]['hT'] = sb(st, "hT_0", [128, 8, 128], BF16); TL1[1]['hT'] = sb(st, "hT_1", [128, 8, 128], BF16)
          proj = sb(st, "proj", [128, 2, IN_COLS])
          st8 = sb(st, "st8", [128, 2, 64])
          TL1[0]['qn'] = sb(st, "qn_0", [128, 384], BF16); TL1[1]['qn'] = sb(st, "qn_1", [128, 384], BF16)
          TL1[0]['qnT'] = sb(st, "qnT_0", [128, 3, 128], BF16); TL1[1]['qnT'] = sb(st, "qnT_1", [128, 3, 128], BF16)
          TL1[0]['kvn'] = sb(st, "kvn_0", [128, 256], BF16); TL1[1]['kvn'] = sb(st, "kvn_1", [128, 256], BF16)
          TL1[0]['kvnT'] = sb(st, "kvnT_0", [128, 2, 128], BF16); TL1[1]['kvnT'] = sb(st, "kvnT_1", [128, 2, 128], BF16)
          TL1[0]['qsq'] = sb(st, "qsq_0", [128, 768]); TL1[1]['qsq'] = sb(st, "qsq_1", [128, 768])
          TL1[0]['qf'] = sb(st, "qf_0", [128, 8, 96]); TL1[1]['qf'] = sb(st, "qf_1", [128, 8, 96])
          TL1[0]['qb'] = sb(st, "qb_0", [128, 8, 96], BF16); TL1[1]['qb'] = sb(st, "qb_1", [128, 8, 96], BF16)
          TL1[0]['kf'] = sb(st, "kf_0", [128, 8, 96]); TL1[1]['kf'] = sb(st, "kf_1", [128, 8, 96])
          TL1[0]['kb'] = sb(st, "kb_0", [128, 8, 96], BF16); TL1[1]['kb'] = sb(st, "kb_1", [128, 8, 96], BF16)
          vb = sb(st, "vb", [128, 2, 512], BF16)
          TL1[0]['kvs'] = sb(st, "kvs_0", [128, 1024]); TL1[1]['kvs'] = sb(st, "kvs_1", [128, 1024])
          rp = sb(st, "rp", [128, 2, 32])
          TL1[0]['rt'] = sb(st, "rt_0", [128, 6, 128]); TL1[1]['rt'] = sb(st, "rt_1", [128, 6, 128])
          TL1[0]['krg'] = sb(st, "krg_0", [128, 32]); TL1[1]['krg'] = sb(st, "krg_1", [128, 32])
          TL1[0]['krr'] = sb(st, "krr_0", [128, 32]); TL1[1]['krr'] = sb(st, "krr_1", [128, 32])
          TL1[0]['qTt'] = sb(st, "qTt_0", [128, 8, 128], BF16); TL1[1]['qTt'] = sb(st, "qTt_1", [128, 8, 128], BF16)
          TL1[0]['kTt'] = sb(st, "kTt_0", [128, 8, 128], BF16); TL1[1]['kTt'] = sb(st, "kTt_1", [128, 8, 128], BF16)
          PSALL = [ps(st, "ph1_%d" % i) for i in range(8)]

          P.dmaop("pool", lambda e: e.dma_start(out=w_in_sb[:], in_=w_in.rearrange("(dc p) n -> p dc n", p=128)), w=["w_in_sb"])
          P.dmaop("pool", lambda e: e.dma_start(out=w_qb_sb[:], in_=w_qb.rearrange("(dc p) n -> p dc n", p=128)), w=["w_qb_sb"])
          P.dmaop("pool", lambda e: e.dma_start(out=w_kvb_sb[:], in_=w_kvb.rearrange("(dc p) n -> p dc n", p=128)), w=["w_kvb_sb"])
          P.dmaop("act", lambda e: e.dma_start(out=qag[:], in_=q_a_g.partition_broadcast(128)), w=["qag"])
          P.dmaop("act", lambda e: e.dma_start(out=kvag[:], in_=kv_a_g.partition_broadcast(128)), w=["kvag"])
          P.dmaop("act", lambda e: e.dma_start(out=qng[:], in_=q_norm_g.partition_broadcast(128)), w=["qng"])
          P.dmaop("act", lambda e: e.dma_start(out=kng[:], in_=k_norm_g.partition_broadcast(128)), w=["kng"])
          P.op("dve", lambda e: e.tensor_scalar(out=qng[:], in0=qng[:], scalar1=float(QK ** -0.5), scalar2=None, op0=ALU.mult),
               r=["qng"], w=["qng"])
          P.op("dve", lambda e: e.tensor_copy(out=identb[:], in_=ident[:]), r=["ident"], w=["identb"])

          def rstd(Pq, src_key, src_ap, n, dst_ap, dst_key):
              Pq.op("dve", lambda e: e.tensor_scalar(out=dst_ap, in0=src_ap, scalar1=1.0 / n, scalar2=EPS, op0=ALU.mult, op1=ALU.add),
                   r=[src_key], w=[dst_key])
              Pq.op("act", lambda e: e.activation(out=dst_ap, in_=dst_ap, func=AF.Sqrt), r=[dst_key], w=[dst_key])
              Pq.op("dve", lambda e: e.reciprocal(out=dst_ap, in_=dst_ap), r=[dst_key], w=[dst_key])

          def rope(Pq, rt, src, dst, tab, nh, tag, eng="pool"):
              sv = src.rearrange("p h (a t) -> p h a t", a=2)
              dv = dst.rearrange("p h (a t) -> p h a t", a=2)
              cosb = tab[:, 0:16].rearrange("p (a t) -> p a t", a=2).unsqueeze(1).to_broadcast([128, nh, 2, 8])
              sinb = tab[:, 16:32].rearrange("p (a t) -> p a t", a=2).unsqueeze(1).to_broadcast([128, nh, 2, 8])
              v0 = sv[:, :, :, 0:8]
              v1 = sv[:, :, :, 8:16]
              n = nh * 16
              T = [rt[:, k, 0:n].rearrange("p (h a t) -> p h a t", h=nh, a=2) for k in range(4)]
              rk = ("rt", tag)
              Pq.op(eng, lambda e: e.tensor_tensor(out=T[0], in0=v0, in1=cosb, op=ALU.mult), r=[tag + "_src", tag + "_tab"], w=[rk + (0,)])
              Pq.op(eng, lambda e: e.tensor_tensor(out=T[1], in0=v1, in1=sinb, op=ALU.mult), r=[tag + "_src", tag + "_tab"], w=[rk + (1,)])
              Pq.op(eng, lambda e: e.tensor_tensor(out=T[2], in0=v1, in1=cosb, op=ALU.mult), r=[tag + "_src", tag + "_tab"], w=[rk + (2,)])
              Pq.op(eng, lambda e: e.tensor_tensor(out=T[3], in0=v0, in1=sinb, op=ALU.mult), r=[tag + "_src", tag + "_tab"], w=[rk + (3,)])
              Pq.op(eng, lambda e: e.tensor_tensor(out=dv[:, :, :, 0:8], in0=T[0], in1=T[1], op=ALU.subtract),
                   r=[rk + (0,), rk + (1,)], w=[tag + "_dst"])
              Pq.op(eng, lambda e: e.tensor_tensor(out=dv[:, :, :, 8:16], in0=T[2], in1=T[3], op=ALU.add),
                   r=[rk + (2,), rk + (3,)], w=[tag + "_dst"])

          ntile1 = NTe
          import os
          STG = int(os.environ.get('PH1_STAGE', '9'))
          SHARED1 = ["w_in_sb", "w_qb_sb", "w_kvb_sb", "qag", "kvag", "qng", "kng", "identb", "ident", "modx", "modc", "u_s", "gates_s", "qT_s", "kT_s", "v_s"]
          ALIAS1 = {("ps", 1): ("ps", 0), "ps3": "ps2", ("ps", 6): ("ps", 4), ("ps", 7): ("ps", 5)}

          def tile1(i, s):
              Pq = Keyed(P, s, SHARED1, ALIAS1)
              junk = TL1[s]['junk']
              hh = TL1[s]['hh']
              hb = TL1[s]['hb']
              hT = TL1[s]['hT']
              qn = TL1[s]['qn']
              qnT = TL1[s]['qnT']
              kvn = TL1[s]['kvn']
              kvnT = TL1[s]['kvnT']
              qsq = TL1[s]['qsq']
              qf = TL1[s]['qf']
              qb = TL1[s]['qb']
              kf = TL1[s]['kf']
              kb = TL1[s]['kb']
              kvs = TL1[s]['kvs']
              rt = TL1[s]['rt']
              krg = TL1[s]['krg']
              krr = TL1[s]['krr']
              qTt = TL1[s]['qTt']
              kTt = TL1[s]['kTt']
              bk = PSALL[4 * s:4 * s + 4]
              PS = [bk[0], bk[0], bk[1], bk[1], bk[2], bk[3], bk[2], bk[3]]
              PSb2 = PS[2][:].bitcast(BF16)
              PSb3 = PS[3][:].bitcast(BF16)
              isx = i >= 2
              own = 2 <= i < 2 + NTO
              t0 = i * 128
              xq = t0 - NCTX
              G = (modx if isx else modc)[:, D:2 * D]
              SH = (modx if isx else modc)[:, 0:D]
              mk = "modx" if isx else "modc"
              Pq.dmaop("sp", lambda e, s=s, t0=t0: e.dma_start(out=xt[:, s, :], in_=xc[t0:t0 + 128, :]), w=[("xt", s)])
              Pq.dmaop("sp", lambda e, s=s, t0=t0: e.dma_start(out=rp[:, s, :], in_=rope_d[t0:t0 + 128, :]), w=[("rp", s)])
              Pq.op("act", lambda e, s=s: e.activation(out=junk[:], in_=xt[:, s, :], func=AF.Square, accum_out=st8[:, s, 0:1]),
                   r=[("xt", s)], w=["junk", ("st", s, 0)])
              rstd(Pq, ("st", s, 0), st8[:, s, 0:1], D, st8[:, s, 1:2], ("st", s, 1))
              Pq.op("dve", lambda e, s=s, G=G: e.scalar_tensor_tensor(out=hh[:], in0=xt[:, s, :], scalar=st8[:, s, 1:2], in1=G,
                                                                  op0=ALU.mult, op1=ALU.mult),
                   r=[("xt", s), ("st", s, 1), mk], w=["hh"])
              Pq.op("pool", lambda e, SH=SH: e.tensor_tensor(out=hb[:], in0=hh[:], in1=SH, op=ALU.add), r=["hh", mk], w=["hb"])

              def tr_h(e):
                  ins = None
                  for dc in range(8):
                      ins = e.transpose(out=PSb2[:, dc * 128:(dc + 1) * 128], in_=hb[:, dc * 128:(dc + 1) * 128], identity=identb[:])
                  return ins
              Pq.op("pe", tr_h, r=["hb", "identb"], w=["ps2"])
              Pq.op("act", lambda e: e.copy(out=hT[:].rearrange("p a b -> p (a b)"), in_=PSb2[:, 0:1024]), r=["ps2"], w=["hT"])
              segs = ([(c, c * 512, min(512, IN_COLS - c * 512), [c]) for c in range(7)] if own
                      else [(0, 0, 512, [0]), (1, 896, 288, [1, 2])])
              for (ctile, c0, n, wkeys) in segs:
                  pk = ctile % 2

                  def mmin(e, c0=c0, n=n, pk=pk):
                      ins = None
                      for dc in range(8):
                          ins = e.matmul(PS[pk][:, 0:n], lhsT=hT[:, dc, :], rhs=w_in_sb[:, dc, c0:c0 + n], start=(dc == 0), stop=(dc == 7))
                      return ins
                  Pq.op("pe", mmin, r=["hT", "w_in_sb"], w=[("ps", pk)])
                  if ctile % 2 == 0:
                      Pq.op("dve", lambda e, c0=c0, n=n, pk=pk, s=s: e.tensor_copy(out=proj[:, s, c0:c0 + n], in_=PS[pk][:, 0:n]),
                           r=[("ps", pk)], w=[("proj", s, c_) for c_ in wkeys])
                  else:
                      Pq.op("act", lambda e, c0=c0, n=n, pk=pk, s=s: e.copy(out=proj[:, s, c0:c0 + n], in_=PS[pk][:, 0:n]),
                           r=[("ps", pk)], w=[("proj", s, c_) for c_ in wkeys])
              pj = [("proj", s, c) for c in range(7)]
              Pq.dmaop("sp", lambda e, s=s, t0=t0: e.dma_start(out=u_s[t0:t0 + 128, :], in_=proj[:, s, 0:512]), r=[pj[0]], w=["u_s"])
              if own:
                  Pq.op("act", lambda e, s=s: e.activation(out=proj[:, s, 1184:3232], in_=proj[:, s, 1184:3232], func=AF.Sigmoid),
                        r=pj[2:], w=pj[2:])
                  Pq.dmaop("sp", lambda e, xq=xq, s=s: e.dma_start(out=gates_s[xq:xq + 128, :], in_=proj[:, s, 1184:3232]), r=pj[2:], w=["gates_s"])
                  Pq.op("act", lambda e, s=s: e.activation(out=junk[:, 0:384], in_=proj[:, s, 512:896], func=AF.Square,
                                                          accum_out=st8[:, s, 2:3]), r=[pj[1]], w=["junk", ("st", s, 2)])
                  rstd(Pq, ("st", s, 2), st8[:, s, 2:3], 384, st8[:, s, 3:4], ("st", s, 3))
                  Pq.op("dve", lambda e, s=s: e.scalar_tensor_tensor(out=qn[:], in0=proj[:, s, 512:896], scalar=st8[:, s, 3:4], in1=qag[:],
                                                                    op0=ALU.mult, op1=ALU.mult), r=[pj[1], ("st", s, 3), "qag"], w=["qn"])

                  def tr_q(e):
                      ins = None
                      for k in range(3):
                          ins = e.transpose(out=PSb2[:, k * 128:(k + 1) * 128], in_=qn[:, k * 128:(k + 1) * 128], identity=identb[:])
                      return ins
                  Pq.op("pe", tr_q, r=["qn", "identb"], w=["ps2"])
                  Pq.op("dve", lambda e: e.tensor_copy(out=qnT[:].rearrange("p a b -> p (a b)"), in_=PSb2[:, 0:384]), r=["ps2"], w=["qnT"])

                  def mmq(e):
                      ins = None
                      for (pi, c0, n) in ((4, 0, 512), (5, 512, 256)):
                          for k in range(3):
                              ins = e.matmul(PS[pi][:, 0:n], lhsT=qnT[:, k, :], rhs=w_qb_sb[:, k, c0:c0 + n], start=(k == 0), stop=(k == 2))
                      return ins
                  Pq.op("pe", mmq, r=["qnT", "w_qb_sb"], w=[("ps", 4), ("ps", 5)])
                  qfl = qf[:].rearrange("p h d -> p (h d)")
                  Pq.op("act", lambda e: e.copy(out=qfl[:, 0:512], in_=PS[4][:, 0:512]), r=[("ps", 4)], w=["qf"])
                  Pq.op("act", lambda e: e.copy(out=qfl[:, 512:768], in_=PS[5][:, 0:256]), r=[("ps", 5)], w=["qf"])
                  Pq.op("pool", lambda e: e.tensor_tensor(out=qsq[:], in0=qfl, in1=qfl, op=ALU.mult), r=["qf"], w=["qsq"])
                  Pq.op("dve", lambda e, s=s: e.tensor_reduce(out=st8[:, s, 8:16], in_=qsq[:].rearrange("p (h d) -> p h d", h=8),
                                                             axis=AX.X, op=ALU.add), r=["qsq"], w=[("st", s, 8)])
                  rstd(Pq, ("st", s, 8), st8[:, s, 8:16], QK, st8[:, s, 16:24], ("st", s, 16))
                  Pq.op("dve", lambda e, s=s: e.tensor_tensor(out=qf[:], in0=qf[:], in1=st8[:, s, 16:24].unsqueeze(2).to_broadcast([128, 8, 96]),
                                                             op=ALU.mult), r=["qf", ("st", s, 16)], w=["qf"])
                  Pq.op("dve", lambda e: e.tensor_tensor(out=qf[:], in0=qf[:], in1=qng[:].unsqueeze(1).to_broadcast([128, 8, 96]),
                                                        op=ALU.mult), r=["qf", "qng"], w=["qf", "q_src"])
                  Pq.op("act", lambda e: e.copy(out=qb[:, :, 0:64], in_=qf[:, :, 0:64]), r=["qf"], w=["qb"])
                  Pq.op("pool", lambda e, s=s: e.tensor_copy(out=rt[:, 5, 0:32], in_=rp[:, s, :]), r=[("rp", s)], w=["q_tab"])
                  rope(Pq, rt, qf[:, :, 64:96], qb[:, :, 64:96], rt[:, 5, 0:32], 8, "q")

                  def tr_qh(e):
                      ins = None
                      for h in range(8):
                          ins = e.transpose(out=PSb3[0:96, h * 128:(h + 1) * 128], in_=qb[:, h, :], identity=identb[:])
                      return ins
                  Pq.op("pe", tr_qh, r=["qb", "q_dst", "identb"], w=["ps3"])
                  Pq.op("act", lambda e: e.copy(out=qTt[0:96].rearrange("p a b -> p (a b)"), in_=PSb3[0:96, 0:1024]), r=["ps3"], w=["qTt"])
                  Pq.dmaop("act", lambda e, xq=xq: e.dma_start(out=qT_s[:, :, xq:xq + 128].rearrange("h d t -> d h t"), in_=qTt[0:96]),
                          r=["qTt"], w=["qT_s"])
              Pq.op("act", lambda e, s=s: e.activation(out=junk[:, 0:256], in_=proj[:, s, 896:1152], func=AF.Square,
                                                      accum_out=st8[:, s, 4:5]), r=[pj[1], pj[2]], w=["junk", ("st", s, 4)])
              rstd(Pq, ("st", s, 4), st8[:, s, 4:5], 256, st8[:, s, 5:6], ("st", s, 5))
              Pq.op("dve", lambda e, s=s: e.scalar_tensor_tensor(out=kvn[:], in0=proj[:, s, 896:1152], scalar=st8[:, s, 5:6], in1=kvag[:],
                                                                op0=ALU.mult, op1=ALU.mult), r=[pj[1], pj[2], ("st", s, 5), "kvag"], w=["kvn"])

              def tr_kv(e):
                  ins = None
                  for k in range(2):
                      ins = e.transpose(out=PSb2[:, k * 128:(k + 1) * 128], in_=kvn[:, k * 128:(k + 1) * 128], identity=identb[:])
                  return ins
              Pq.op("pe", tr_kv, r=["kvn", "identb"], w=["ps2"])
              Pq.op("dve", lambda e: e.tensor_copy(out=kvnT[:].rearrange("p a b -> p (a b)"), in_=PSb2[:, 0:256]), r=["ps2"], w=["kvnT"])

              def mmkv(e):
                  ins = None
                  for (pi, c0) in ((6, 0), (7, 512)):
                      for k in range(2):
                          ins = e.matmul(PS[pi][:, 0:512], lhsT=kvnT[:, k, :], rhs=w_kvb_sb[:, k, c0:c0 + 512], start=(k == 0), stop=(k == 1))
                  return ins
              Pq.op("pe", mmkv, r=["kvnT", "w_kvb_sb"], w=[("ps", 6), ("ps", 7)])
              for half in range(2):
                  (Pq.op("act", lambda e, half=half: e.copy(out=kvs[:, half * 512:(half + 1) * 512], in_=PS[6 + half][:, 0:512]),
                        r=[("ps", 6 + half)], w=[("kvs", half)]) if half == 0 else
                   Pq.op("dve", lambda e, half=half: e.tensor_copy(out=kvs[:, half * 512:(half + 1) * 512], in_=PS[6 + half][:, 0:512]),
                        r=[("ps", 6 + half)], w=[("kvs", half)]))
              kvs3 = kvs[:].rearrange("p (h d) -> p h d", h=8)
              kvk = [("kvs", 0), ("kvs", 1)]
              Pq.op("pool", lambda e, s=s: e.tensor_copy(out=vb[:, s].rearrange("p (h d) -> p h d", h=8), in_=kvs3[:, :, 64:128]),
                   r=kvk, w=[("vb", s)])
              Pq.dmaop("sp", lambda e, s=s, t0=t0: e.dma_start(out=v_s[t0:t0 + 128, :], in_=vb[:, s]),
                      r=[("vb", s)], w=["v_s"])
              Pq.op("act", lambda e: e.copy(out=kf[:, :, 0:64], in_=kvs3[:, :, 0:64]), r=kvk, w=[("kf", 0), ("kf", 1)])
              kfk = [("kf", 0), ("kf", 1)]
              Pq.op("pool", lambda e: e.tensor_tensor(out=qsq[:, 0:512].rearrange("p (h d) -> p h d", h=8), in0=kf[:, :, 0:64], in1=kf[:, :, 0:64],
                                                     op=ALU.mult), r=kfk, w=["qsq"])
              Pq.op("dve", lambda e, s=s: e.tensor_reduce(out=st8[:, s, 24:32], in_=qsq[:, 0:512].rearrange("p (h d) -> p h d", h=8),
                                                         axis=AX.X, op=ALU.add), r=["qsq"], w=[("st", s, 24)])
              Pq.op("act", lambda e, s=s: e.activation(out=junk[:, 0:32], in_=proj[:, s, 1152:1184], func=AF.Square,
                                                      accum_out=st8[:, s, 6:7]), r=[pj[2]], w=["junk", ("st", s, 6)])
              Pq.op("dve", lambda e, s=s: e.tensor_scalar(out=st8[:, s, 24:32], in0=st8[:, s, 24:32], scalar1=st8[:, s, 6:7], scalar2=None,
                                                         op0=ALU.add), r=[("st", s, 24), ("st", s, 6)], w=[("st", s, 24)])
              rstd(Pq, ("st", s, 24), st8[:, s, 24:32], QK, st8[:, s, 32:40], ("st", s, 32))
              Pq.op("dve", lambda e, s=s: e.tensor_tensor(out=kf[:, :, 0:64], in0=kf[:, :, 0:64],
                                                         in1=st8[:, s, 32:40].unsqueeze(2).to_broadcast([128, 8, 64]), op=ALU.mult),
                   r=kfk + [("st", s, 32)], w=kfk)
              Pq.op("dve", lambda e: e.tensor_tensor(out=kb[:, :, 0:64], in0=kf[:, :, 0:64],
                                                    in1=kng[:, 0:64].unsqueeze(1).to_broadcast([128, 8, 64]), op=ALU.mult),
                   r=kfk + ["kng"], w=["kb"])
              Pq.op("pool", lambda e, s=s: e.tensor_tensor(out=krg[:], in0=proj[:, s, 1152:1184], in1=kng[:, 64:96], op=ALU.mult),
                   r=[pj[2], "kng"], w=["krg", "k_src"])
              Pq.op("pool", lambda e, s=s: e.tensor_copy(out=rt[:, 4, 0:32], in_=rp[:, s, :]), r=[("rp", s)], w=["k_tab"])
              rope(Pq, rt, krg[:].unsqueeze(1), krr[:].unsqueeze(1), rt[:, 4, 0:32], 1, "k")
              Pq.op("dve", lambda e, s=s: e.tensor_tensor(out=kb[:, :, 64:96], in0=krr[:].unsqueeze(1).to_broadcast([128, 8, 32]),
                                                         in1=st8[:, s, 32:40].unsqueeze(2).to_broadcast([128, 8, 32]), op=ALU.mult),
                   r=["k_dst", ("st", s, 32)], w=["kb"])

              def tr_kh(e):
                  ins = None
                  for h in range(8):
                      ins = e.transpose(out=PSb3[0:96, h * 128:(h + 1) * 128], in_=kb[:, h, :], identity=identb[:])
                  return ins
              Pq.op("pe", tr_kh, r=["kb", "identb"], w=["ps3"])
              Pq.op("dve", lambda e: e.tensor_copy(out=kTt[0:96].rearrange("p a b -> p (a b)"), in_=PSb3[0:96, 0:1024]), r=["ps3"], w=["kTt"])
              Pq.dmaop("act", lambda e, t0=t0: e.dma_start(out=kT_s[:, :, t0:t0 + 128].rearrange("h d t -> d h t"), in_=kTt[0:96]),
                      r=["kTt"], w=["kT_s"])
              return Pq.cap
          for i in range(0, ntile1, 2):
              caps = [tile1(i, 0)] + ([tile1(i + 1, 1)] if i + 1 < ntile1 else [])
              interleave(P, caps, chunk=int(os.environ.get("ILV", "2")))
          P.flush()

    QG = min(512, NXO)
    NQG = NXO // QG

    es1.close()
    NCX = NXe // 8
    NCC = NCTX // 8
    NCHe = NCX + NCC
    HW = NCHe + 2
    if debug in (0, 4, 5, 6):
      with ExitStack() as st:
        BendT = sb(st, "BendT", [128, 2, 32, 2, 64], BF16)
        Dm = sb(st, "Dm", [128, 2, 16, 2, 128], BF16)
        Tloc = sb(st, "Tloc", [128, 32, 128], BF16)
        mu3 = sb(st, "mu3", [128, 2, 2, 16, 2])
        mu16 = sb(st, "mu16", [128, 2, 17, 2, 16, 2])
        PS2 = [ps(st, "ph2_%d" % i) for i in range(8)]
        with ExitStack() as tt:
            lre = sb(tt, "lre", [128, 32]); lim = sb(tt, "lim", [128, 32]); ldt = sb(tt, "ldt", [128, 32])
            bre = sb(tt, "bre", [128, 32, 16]); bim = sb(tt, "bim", [128, 32, 16])
            cre = sb(tt, "cre", [128, 32, 16]); cim = sb(tt, "cim", [128, 32, 16])
            dsk = sb(tt, "dsk", [128, 32])
            cmk = sb(tt, "cmk", [128, 256])
            tm = sb(tt, "tm", [128, 12, 32])
            ti = sb(tt, "ti", [128, 32], I32)
            pwr = sb(tt, "pwr", [128, 9, 32]); pwi = sb(tt, "pwi", [128, 9, 32])
            nwr = sb(tt, "nwr", [128, 9, 32]); nwi = sb(tt, "nwi", [128, 9, 32])
            Bbr = sb(tt, "Bbr", [128, 32, 16]); Bbi = sb(tt, "Bbi", [128, 32, 16])
            MX = [sb(tt, "MX%d" % i, [128, 16, 8, 16]) for i in range(4)]
            MT = [sb(tt, "MT%d" % i, [128, 16, 8, 16]) for i in range(4)]
            TL = sb(tt, "TL", [128, 2, 128])
            for gh in range(2):
                rows = slice(64 * gh, 64 * gh + 64)
                gs = slice(16 * gh, 16 * gh + 16)
                for d in range(2):
                    for (dst, src, nm) in ((lre, lam_re, "lre"), (lim, lam_im, "lim")):
                        P.dmaop("sp", lambda e, dst=dst, src=src, rows=rows, gs=gs, d=d: e.dma_start(
                            out=dst[rows, 16 * d:16 * d + 16], in_=src[d, gs, :].rearrange("g p -> p g"),
                            allow_slow_non_contiguous=True), w=[nm])
                    P.dmaop("sp", lambda e, rows=rows, gs=gs, d=d: e.dma_start(
                        out=ldt[rows, 16 * d:16 * d + 16], in_=log_dt[d, gs].partition_broadcast(64)), w=["ldt"])
                for (dst, src, nm) in ((bre, b_re, "bre"), (bim, b_im, "bim")):
                    for d in range(2):
                        P.dmaop("act", lambda e, dst=dst, src=src, rows=rows, gs=gs, d=d: e.dma_start(
                            out=dst[rows, 16 * d:16 * d + 16, :], in_=src[d, gs, :, :].rearrange("g p h -> p g h")), w=[nm])
                for (dst, src, nm) in ((cre, c_re, "cre"), (cim, c_im, "cim")):
                    for d in range(2):
                        P.dmaop("sp" if d == 0 else "act", lambda e, dst=dst, src=src, rows=rows, gs=gs, d=d: e.dma_start(
                            out=dst[rows, 16 * d:16 * d + 16, :], in_=src[d, gs, :, :].rearrange("g o p -> p g o"),
                            allow_slow_non_contiguous=True), w=[nm])
            for j in range(8):
                P.dmaop("sp", lambda e, j=j: e.dma_start(out=dsk[16 * j:16 * j + 16, :], in_=ssm_d.rearrange("(g h) -> h g", h=16),
                                                        allow_slow_non_contiguous=True), w=["dsk"])
            P.dmaop("sp", lambda e: e.dma_start(out=cmk[:], in_=cmask_d), w=["cmk"])

            K = [0]

            def T_(i):
                return tm[:, i, :]

            def dv(fn, r, w, eng="dve"):
                P.op(eng, fn, r=r, w=w)

            def tt2(out, a, b, op, r, w, eng="dve"):
                P.op(eng, lambda e: e.tensor_tensor(out=out, in0=a, in1=b, op=op), r=r, w=w)

            def ts(out, a, s1, op0, s2=None, op1=None, r=(), w=(), eng="dve"):
                if op1 is None:
                    P.op(eng, lambda e: e.tensor_scalar(out=out, in0=a, scalar1=s1, scalar2=None, op0=op0), r=r, w=w)
                else:
                    P.op(eng, lambda e: e.tensor_scalar(out=out, in0=a, scalar1=s1, scalar2=s2, op0=op0, op1=op1), r=r, w=w)
            PI = math.pi
            P.op("act", lambda e: e.activation(out=T_(0), in_=ldt[:], func=AF.Exp), r=["ldt"], w=["t0"])
            tt2(T_(1), lre[:], T_(0), ALU.mult, ["lre", "t0"], ["t1"])
            tt2(T_(2), lim[:], T_(0), ALU.mult, ["lim", "t0"], ["t2"])
            P.op("act", lambda e: e.activation(out=T_(3), in_=T_(1), func=AF.Exp), r=["t1"], w=["t3"])
            ts(T_(4), T_(2), 1.0 / (2 * PI), ALU.mult, r=["t2"], w=["t4"])
            P.op("dve", lambda e: e.tensor_copy(out=ti[:], in_=T_(4)), r=["t4"], w=["ti"])
            P.op("dve", lambda e: e.tensor_copy(out=T_(4), in_=ti[:]), r=["ti"], w=["t4"])
            P.op("dve", lambda e: e.scalar_tensor_tensor(out=T_(5), in0=T_(4), scalar=-2 * PI, in1=T_(2), op0=ALU.mult, op1=ALU.add),
                 r=["t4", "t2"], w=["t5"])
            for (src_i, dst_i) in ((5, 5),):
                ts(T_(6), T_(5), PI, ALU.is_gt, -2 * PI, ALU.mult, r=["t5"], w=["t6"])
                tt2(T_(5), T_(5), T_(6), ALU.add, ["t5", "t6"], ["t5"])
                ts(T_(6), T_(5), -PI, ALU.is_lt, 2 * PI, ALU.mult, r=["t5"], w=["t6"])
                tt2(T_(5), T_(5), T_(6), ALU.add, ["t5", "t6"], ["t5"])
            ts(T_(7), T_(5), PI / 2, ALU.add, r=["t5"], w=["t7"])
            ts(T_(6), T_(7), PI, ALU.is_gt, -2 * PI, ALU.mult, r=["t7"], w=["t6"])
            tt2(T_(7), T_(7), T_(6), ALU.add, ["t7", "t6"], ["t7"])
            P.op("act", lambda e: e.activation(out=T_(8), in_=T_(5), func=AF.Sin), r=["t5"], w=["t8"])
            P.op("act", lambda e: e.activation(out=T_(9), in_=T_(7), func=AF.Sin), r=["t7"], w=["t9"])
            P.op("pool", lambda e: e.memset(pwr[:, 0, :], 1.0), w=[("pw", 0)])
            P.op("pool", lambda e: e.memset(pwi[:, 0, :], 0.0), w=[("pw", 0)])
            tt2(pwr[:, 1, :], T_(3), T_(9), ALU.mult, ["t3", "t9"], [("pw", 1)])
            tt2(pwi[:, 1, :], T_(3), T_(8), ALU.mult, ["t3", "t8"], [("pw", 1)])
            for k in range(2, 9):
                a_r, a_i = pwr[:, k - 1, :], pwi[:, k - 1, :]
                tt2(T_(10), a_r, pwr[:, 1, :], ALU.mult, [("pw", k - 1), ("pw", 1)], ["t10"])
                tt2(T_(11), a_i, pwi[:, 1, :], ALU.mult, [("pw", k - 1), ("pw", 1)], ["t11"])
                tt2(pwr[:, k, :], T_(10), T_(11), ALU.subtract, ["t10", "t11"], [("pw", k)])
                tt2(T_(10), a_r, pwi[:, 1, :], ALU.mult, [("pw", k - 1), ("pw", 1)], ["t10"])
                tt2(T_(11), a_i, pwr[:, 1, :], ALU.mult, [("pw", k - 1), ("pw", 1)], ["t11"])
                tt2(pwi[:, k, :], T_(10), T_(11), ALU.add, ["t10", "t11"], [("pw", k)])
            pwk = [("pw", k) for k in range(9)]
            tt2(nwr[:], pwr[:], pwr[:], ALU.mult, pwk, ["nwr"])
            tt2(nwi[:], pwi[:], pwi[:], ALU.mult, pwk, ["nwi"])
            tt2(nwr[:], nwr[:], nwi[:], ALU.add, ["nwr", "nwi"], ["nwr"])
            P.op("dve", lambda e: e.reciprocal(out=nwr[:], in_=nwr[:]), r=["nwr"], w=["nwr"])
            P.op("dve", lambda e: e.scalar_tensor_tensor(out=nwi[:], in0=pwi[:], scalar=-1.0, in1=nwr[:], op0=ALU.mult, op1=ALU.mult),
                 r=pwk + ["nwr"], w=["nwi"])
            tt2(nwr[:], pwr[:], nwr[:], ALU.mult, pwk + ["nwr", "nwi"], ["nwr"])
            for d in range(2):
                dsl = slice(16 * d, 16 * d + 16)
                for pl in range(2):
                    P.op("dve", lambda e, d=d, pl=pl, dsl=dsl: e.tensor_copy(out=mu3[:, d, 0, :, pl], in_=pwr[:, 8, dsl]), r=pwk, w=["mu3"])
                P.op("dve", lambda e, d=d, dsl=dsl: e.tensor_scalar(out=mu3[:, d, 1, :, 0], in0=pwi[:, 8, dsl], scalar1=-1.0, scalar2=None, op0=ALU.mult),
                     r=pwk, w=["mu3"])
                P.op("dve", lambda e, d=d, dsl=dsl: e.tensor_copy(out=mu3[:, d, 1, :, 1], in_=pwi[:, 8, dsl]), r=pwk, w=["mu3"])
            q16r = sb(tt, "q16r", [128, 17, 32]); q16i = sb(tt, "q16i", [128, 17, 32])
            P.op("pool", lambda e: e.memset(q16r[:, 0, :], 1.0), w=[("q16", 0)])
            P.op("pool", lambda e: e.memset(q16i[:, 0, :], 0.0), w=[("q16", 0)])
            P.op("pool", lambda e: e.tensor_copy(out=q16r[:, 1, :], in_=pwr[:, 8, :]), r=pwk, w=[("q16", 1)])
            P.op("pool", lambda e: e.tensor_copy(out=q16i[:, 1, :], in_=pwi[:, 8, :]), r=pwk, w=[("q16", 1)])
            for k in range(2, 17):
                a_r, a_i = q16r[:, k - 1, :], q16i[:, k - 1, :]
                tt2(T_(10), a_r, q16r[:, 1, :], ALU.mult, [("q16", k - 1), ("q16", 1)], ["t10"])
                tt2(T_(11), a_i, q16i[:, 1, :], ALU.mult, [("q16", k - 1), ("q16", 1)], ["t11"])
                tt2(q16r[:, k, :], T_(10), T_(11), ALU.subtract, ["t10", "t11"], [("q16", k)])
                tt2(T_(10), a_r, q16i[:, 1, :], ALU.mult, [("q16", k - 1), ("q16", 1)], ["t10"])
                tt2(T_(11), a_i, q16r[:, 1, :], ALU.mult, [("q16", k - 1), ("q16", 1)], ["t11"])
                tt2(q16i[:, k, :], T_(10), T_(11), ALU.add, ["t10", "t11"], [("q16", k)])
            q16k = [("q16", k) for k in range(17)]
            for d in range(2):
                dsl = slice(16 * d, 16 * d + 16)
                for pl in range(2):
                    P.op("dve", lambda e, d=d, pl=pl, dsl=dsl: e.tensor_copy(out=mu16[:, d, :, 0, :, pl], in_=q16r[:, :, dsl]), r=q16k, w=["mu16"])
                P.op("dve", lambda e, d=d, dsl=dsl: e.tensor_scalar(out=mu16[:, d, :, 1, :, 0], in0=q16i[:, :, dsl], scalar1=-1.0, scalar2=None, op0=ALU.mult),
                     r=q16k, w=["mu16"])
                P.op("dve", lambda e, d=d, dsl=dsl: e.tensor_copy(out=mu16[:, d, :, 1, :, 1], in_=q16i[:, :, dsl]), r=q16k, w=["mu16"])
            tt2(T_(0), lre[:], lre[:], ALU.mult, ["lre"], ["t0"])
            tt2(T_(1), lim[:], lim[:], ALU.mult, ["lim"], ["t1"])
            tt2(T_(0), T_(0), T_(1), ALU.add, ["t0", "t1"], ["t0"])
            P.op("dve", lambda e: e.reciprocal(out=T_(0), in_=T_(0)), r=["t0"], w=["t0"])
            ts(T_(1), pwr[:, 1, :], -1.0, ALU.add, r=[("pw", 1)], w=["t1"])
            tt2(T_(2), T_(1), lre[:], ALU.mult, ["t1", "lre"], ["t2"])
            tt2(T_(3), pwi[:, 1, :], lim[:], ALU.mult, [("pw", 1), "lim"], ["t3"])
            tt2(T_(2), T_(2), T_(3), ALU.add, ["t2", "t3"], ["t2"])
            tt2(T_(2), T_(2), T_(0), ALU.mult, ["t2", "t0"], ["t2"])
            tt2(T_(3), pwi[:, 1, :], lre[:], ALU.mult, [("pw", 1), "lre"], ["t3"])
            tt2(T_(4), T_(1), lim[:], ALU.mult, ["t1", "lim"], ["t4"])
            tt2(T_(3), T_(3), T_(4), ALU.subtract, ["t3", "t4"], ["t3"])
            tt2(T_(3), T_(3), T_(0), ALU.mult, ["t3", "t0"], ["t3"])
            cfr = T_(2).unsqueeze(2).to_broadcast([128, 32, 16])
            cfi = T_(3).unsqueeze(2).to_broadcast([128, 32, 16])
            tmpA = MT[0][:].rearrange("p a b c -> p (a b c)")[:, 0:512].rearrange("p (g h) -> p g h", h=16)
            tt2(Bbr[:], bre[:], cfr, ALU.mult, ["bre", "t2"], ["Bbr"])
            tt2(tmpA, bim[:], cfi, ALU.mult, ["bim", "t3"], ["MT0"])
            tt2(Bbr[:], Bbr[:], tmpA, ALU.subtract, ["Bbr", "MT0"], ["Bbr"])
            tt2(Bbi[:], bim[:], cfr, ALU.mult, ["bim", "t2"], ["Bbi"])
            tt2(tmpA, bre[:], cfi, ALU.mult, ["bre", "t3"], ["MT0"])
            tt2(Bbi[:], Bbi[:], tmpA, ALU.add, ["Bbi", "MT0"], ["Bbi"])

            def cprod(out_re, out_im, pw_re, pw_im, koff, kstep, vr, vi, d, keys_in, key_out, neg_im=False, eng="dve"):
                dsl = slice(16 * d, 16 * d + 16)
                if kstep == 1:
                    ksl = slice(koff, koff + 8)
                    pr = pw_re[:, ksl, dsl].rearrange("p j g -> p g j").unsqueeze(3).to_broadcast([128, 16, 8, 16])
                    pi = pw_im[:, ksl, dsl].rearrange("p j g -> p g j").unsqueeze(3).to_broadcast([128, 16, 8, 16])
                else:
                    pr = None
                vrb = vr[:, dsl, :].unsqueeze(2).to_broadcast([128, 16, 8, 16])
                vib = vi[:, dsl, :].unsqueeze(2).to_broadcast([128, 16, 8, 16])
                t1 = MT[2][:]
                t2 = MT[3][:]
                tt2(t1, vrb, pr, ALU.mult, keys_in, ["MT2"], eng)
                tt2(t2, vib, pi, ALU.mult, keys_in, ["MT3"], eng)
                tt2(out_re, t1, t2, ALU.subtract, ["MT2", "MT3"], [key_out[0]], eng)
                tt2(t1, vrb, pi, ALU.mult, keys_in, ["MT2"], eng)
                tt2(t2, vib, pr, ALU.mult, keys_in, ["MT3"], eng)
                tt2(out_im, t1, t2, (ALU.add), ["MT2", "MT3"], [key_out[1]], eng)
                if neg_im:
                    ts(out_im, out_im, -1.0, ALU.mult, r=[key_out[1]], w=[key_out[1]], eng=eng)

            dpr = sb(tt, "dpr", [128, 9, 32]); dpi = sb(tt, "dpi", [128, 9, 32])
            for k in range(9):
                P.op("pool", lambda e, k=k: e.tensor_copy(out=dpr[:, k, :], in_=pwr[:, 8 - k, :]), r=pwk, w=["dpr"])
                P.op("pool", lambda e, k=k: e.tensor_copy(out=dpi[:, k, :], in_=pwi[:, 8 - k, :]), r=pwk, w=["dpi"])
            tk = pwk + ["nwr", "nwi", "dpr", "dpi", "Bbr", "Bbi", "cre", "cim"]
            pTL = PS2[0]
            for d in range(2):
                if d == 0:
                    cprod(MX[0][:], MX[1][:], nwr, nwi, 0, 1, Bbr, Bbi, d, tk, ("MX0", "MX1"))
                    cprod(MX[2][:], MX[3][:], pwr, pwi, 0, 1, cre, cim, d, tk, ("MX2", "MX3"), neg_im=True)
                    cprod(MT[0][:], MT[1][:], dpr, dpi, 1, 1, Bbr, Bbi, d, tk, ("MT0", "MT1"))
                else:
                    cprod(MX[0][:], MX[1][:], pwr, pwi, 0, 1, Bbr, Bbi, d, tk, ("MX0", "MX1"))
                    cprod(MX[2][:], MX[3][:], nwr, nwi, 0, 1, cre, cim, d, tk, ("MX2", "MX3"), neg_im=True)
                    P.op("pool", lambda e: e.tensor_copy(out=MT[0][:], in_=MX[0][:]), r=["MX0"], w=["MT0"])
                    P.op("pool", lambda e: e.tensor_copy(out=MT[1][:], in_=MX[1][:]), r=["MX1"], w=["MT1"])
                for pl in range(2):
                    for g in range(32):
                        gh, gp = g // 16, g % 16
                        pb = PS2[2 + (g % 4)]

                        def trb(e, pl=pl, gh=gh, gp=gp, pb=pb):
                            return e.transpose(out=pb[:, 0:64], in_=MT[pl][64 * gh:64 * gh + 64, gp].rearrange("p j h -> p (j h)"),
                                               identity=ident[64 * gh:64 * gh + 64, 64 * gh:64 * gh + 64])
                        P.op("pe", trb, r=["MT0" if pl == 0 else "MT1", "ident"], w=[("p2", 2 + g % 4)])
                        P.op("act" if g % 2 == 0 else "dve",
                             (lambda e, pb=pb, d=d, g=g, pl=pl: e.copy(out=BendT[:, d, g, pl, :], in_=pb[:, 0:64])) if g % 2 == 0 else
                             (lambda e, pb=pb, d=d, g=g, pl=pl: e.tensor_copy(out=BendT[:, d, g, pl, :], in_=pb[:, 0:64])),
                             r=[("p2", 2 + g % 4)], w=["BendT"])
                if d == 0:
                    cprod(MT[0][:], MT[1][:], pwr, pwi, 1, 1, cre, cim, d, tk + ["BendT"], ("MT0", "MT1"), neg_im=True)
                else:
                    cprod(MT[0][:], MT[1][:], dpr, dpi, 0, 1, cre, cim, d, tk + ["BendT"], ("MT0", "MT1"), neg_im=True)
                P.op("act", lambda e, d=d: e.copy(out=Dm[:, d, :, 0, :], in_=MT[0][:].rearrange("p g j o -> p g (j o)")), r=["MT0"], w=["Dm"])
                P.op("act", lambda e, d=d: e.copy(out=Dm[:, d, :, 1, :], in_=MT[1][:].rearrange("p g j o -> p g (j o)")), r=["MT1"], w=["Dm"])
                for g in range(32):
                    gh, gp = g // 16, g % 16
                    rows = slice(64 * gh, 64 * gh + 64)
                    pq = PS2[6 + (g % 2)]

                    def mtl(e, rows=rows, gp=gp, pq=pq):
                        e.matmul(pq[:, 0:128], lhsT=MX[0][rows, gp].rearrange("p j h -> p (j h)"), rhs=MX[2][rows, gp].rearrange("p j h -> p (j h)"),
                                 start=True, stop=False)
                        return e.matmul(pq[:, 0:128], lhsT=MX[1][rows, gp].rearrange("p j h -> p (j h)"),
                                        rhs=MX[3][rows, gp].rearrange("p j h -> p (j h)"), start=False, stop=True)
                    P.op("pe", mtl, r=["MX0", "MX1", "MX2", "MX3"], w=[("p2", 6 + g % 2)])
                    if d == 0:
                        P.op("dve", lambda e, g=g, pq=pq: e.tensor_tensor(out=Tloc[:, g, :], in0=pq[:, 0:128], in1=cmk[:, 0:128], op=ALU.mult),
                             r=[("p2", 6 + g % 2), "cmk"], w=[("Tloc", g)])
                    else:
                        P.op("dve", lambda e, g=g, pq=pq: e.tensor_tensor(out=TL[:, g % 2, :], in0=pq[:, 0:128], in1=cmk[:, 128:256], op=ALU.mult),
                             r=[("p2", 6 + g % 2), "cmk"], w=[("TL", g % 2)])
                        P.op("pool", lambda e, g=g: e.tensor_tensor(out=Tloc[:, g, :], in0=Tloc[:, g, :], in1=TL[:, g % 2, :], op=ALU.add),
                             r=[("Tloc", g), ("TL", g % 2)], w=[("Tloc", g)])
                        P.op("pool", lambda e, g=g: e.scalar_tensor_tensor(out=Tloc[:, g, :], in0=ident[:], scalar=dsk[:, g:g + 1], in1=Tloc[:, g, :],
                                                                          op0=ALU.mult, op1=ALU.add) if False else
                             e.tensor_scalar(out=TL[:, g % 2, :], in0=ident[:], scalar1=dsk[:, g:g + 1], scalar2=None, op0=ALU.mult),
                             r=["ident", "dsk", ("Tloc", g)], w=[("TL", g % 2)])
                        P.op("pool", lambda e, g=g: e.tensor_tensor(out=Tloc[:, g, :], in0=Tloc[:, g, :], in1=TL[:, g % 2, :], op=ALU.add),
                             r=[("Tloc", g), ("TL", g % 2)], w=[("Tloc", g)])
            P.flush()
        U8 = sb(st, "U8", [128, 32, NCHe], BF16)
        Hall = [sb(st, "Hall%d" % d, [128, 16, 2, HW], BF16) for d in range(2)]
        with ExitStack() as tu:
            Uc = sb(tu, "Uc", [128, 1, 4096])
            Ucb = sb(tu, "Ucb", [128, 1, 4096], BF16)
            identb2 = sb(tu, "identb2", [128, 128], BF16)
            P.op("dve", lambda e: e.tensor_copy(out=identb2[:], in_=ident[:]), r=["ident"], w=["identb2"])
            u8v = u_s.rearrange("(c j) f -> c (j f)", j=8)
            nblk = (NCHe + 127) // 128
            for cb in range(nblk):
                c0 = cb * 128
                ncb = min(128, NCHe - c0)
                s = 0
                P.dmaop("sp", lambda e, s=s, c0=c0, ncb=ncb: e.dma_start(out=Uc[0:ncb, s, :], in_=u8v[c0:c0 + ncb, :]), r=["u_s"], w=[("Uc", s)])
                P.op("dve", lambda e, s=s, ncb=ncb: e.tensor_copy(
                    out=Ucb[0:ncb, s, :].rearrange("c (g j h) -> c g j h", g=32, j=8),
                    in_=Uc[0:ncb, s, :].rearrange("c (j g h) -> c g j h", j=8, g=32)), r=[("Uc", s)], w=[("Ucb", s)])
                for g4 in range(8):
                    pb = PS2[g4 % 4]
                    pbb = pb[:].bitcast(BF16)

                    def tru(e, g4=g4, s=s, ncb=ncb, pbb=pbb):
                        ins = None
                        for q in range(4):
                            g = g4 * 4 + q
                            ins = e.transpose(out=pbb[:, q * 128:q * 128 + ncb], in_=Ucb[0:ncb, s, g * 128:(g + 1) * 128],
                                              identity=identb2[0:ncb, 0:ncb])
                        return ins
                    P.op("pe", tru, r=[("Ucb", s), "identb2"], w=[("p2", g4 % 4)])
                    P.op("act" if g4 % 2 == 0 else "dve",
                         (lambda e, g4=g4, c0=c0, ncb=ncb, pbb=pbb: e.copy(
                             out=U8[:, g4 * 4:g4 * 4 + 4, c0:c0 + ncb], in_=pbb[:, 0:512].rearrange("p (q c) -> p q c", q=4)[:, :, 0:ncb]))
                         if g4 % 2 == 0 else
                         (lambda e, g4=g4, c0=c0, ncb=ncb, pbb=pbb: e.tensor_copy(
                             out=U8[:, g4 * 4:g4 * 4 + 4, c0:c0 + ncb], in_=pbb[:, 0:512].rearrange("p (q c) -> p q c", q=4)[:, :, 0:ncb])),
                         r=[("p2", g4 % 4)], w=["U8"])
            P.flush()
        if debug == 4:
            d1 = nc.dram_tensor("dbg_tloc", [128, 32 * 128], BF16, kind="ExternalOutput").ap()
            d2 = nc.dram_tensor("dbg_bendt", [128, 2 * 32 * 2 * 64], BF16, kind="ExternalOutput").ap()
            d3 = nc.dram_tensor("dbg_dm", [128, 2 * 16 * 2 * 128], BF16, kind="ExternalOutput").ap()
            d4 = nc.dram_tensor("dbg_u8", [128, 32 * NCHe], BF16, kind="ExternalOutput").ap()
            d5 = nc.dram_tensor("dbg_mu3", [128, 128], F32, kind="ExternalOutput").ap()
            P.dmaop("sp", lambda e: e.dma_start(out=d1, in_=Tloc[:].rearrange("p a b -> p (a b)")), w=["d1"])
            P.dmaop("sp", lambda e: e.dma_start(out=d2, in_=BendT[:].rearrange("p a b c d -> p (a b c d)")), w=["d2"])
            P.dmaop("sp", lambda e: e.dma_start(out=d3, in_=Dm[:].rearrange("p a b c d -> p (a b c d)")), w=["d3"])
            P.dmaop("sp", lambda e: e.dma_start(out=d4, in_=U8[:].rearrange("p a b -> p (a b)")), w=["d4"])
            P.dmaop("sp", lambda e: e.dma_start(out=d5, in_=mu3[:].rearrange("p a b c d -> p (a b c d)")), w=["d5"])
            P.flush()
            DONE.append(1)

        def hk(d, lo, hi):
            return [("Hall", d, q) for q in range(lo, hi)]
        P.op("pool", lambda e: e.memset(Hall[0][:, :, :, 0:1], 0.0), w=hk(0, 0, 1))
        P.op("pool", lambda e: e.memset(Hall[1][:, :, :, NCHe:NCHe + 1], 0.0), w=hk(1, NCHe, NCHe + 1))
        xlo = [NCC + 1, 0]
        clo = [1, NCX]
        ei = 0
        for d in range(2):
            for pl in range(2):
                pc = PS2[4 + pl]
                for gp in range(16):
                    px = PS2[(d * 32 + pl * 16 + gp) % 4]
                    pk = ("p2", (d * 32 + pl * 16 + gp) % 4)

                    def mms(e, d=d, pl=pl, gp=gp, px=px, pc=pc):
                        ins = None
                        for gh in range(2):
                            g = 16 * gh + gp
                            e.matmul(px[64 * gh:64 * gh + 64, 0:NCX], lhsT=BendT[:, d, g, pl, :], rhs=U8[:, g, NCC:NCC + NCX],
                                     start=True, stop=True, tile_position=(0, 64 * gh))
                            ins = e.matmul(pc[64 * gh:64 * gh + 64, gp * 32:gp * 32 + NCC], lhsT=BendT[:, d, g, pl, :], rhs=U8[:, g, 0:NCC],
                                           start=True, stop=True, tile_position=(0, 64 * gh))
                        return ins
                    P.op("pe", mms, r=["BendT", "U8"], w=[pk, ("p2c", 4 + pl, gp)])
                    dst = Hall[d][:, gp, pl, xlo[d]:xlo[d] + NCX]
                    if ei % 2 == 0:
                        P.op("act", lambda e, dst=dst, px=px: e.copy(out=dst, in_=px[:, 0:NCX]), r=[pk], w=hk(d, xlo[d], xlo[d] + NCX))
                    else:
                        P.op("dve", lambda e, dst=dst, px=px: e.tensor_copy(out=dst, in_=px[:, 0:NCX]), r=[pk], w=hk(d, xlo[d], xlo[d] + NCX))
                    ei += 1
                P.op("act", lambda e, d=d, pl=pl, pc=pc: e.copy(out=Hall[d][:, :, pl, clo[d]:clo[d] + NCC],
                                                                in_=pc[:, 0:512].rearrange("p (g c) -> p g c", g=16)[:, :, 0:NCC]),
                     r=[("p2c", 4 + pl, gp) for gp in range(16)], w=hk(d, clo[d], clo[d] + NCC))
        LB = 16
        NB = NCHe // LB
        assert NB * LB == NCHe
        NBd = [(NCC + NXO // 8) // LB, NB]
        assert NBd[0] * LB == NCC + NXO // 8
        with ExitStack() as tc:
            Rl = [sb(tc, "Rl_%d" % d, [128, 16, 3, NB + 1]) for d in range(2)]
            TA = [sb(tc, "TA_%d" % d, [128, 16, 2, NB]) for d in range(2)]
            TB = [sb(tc, "TB_%d" % d, [128, 16, 2, NB]) for d in range(2)]
            Ea = Rl
            ET = [sb(tc, "ET_%d" % d, [128, 2, 16, 2]) for d in range(2)]

            def hview(d, i):
                st0 = (1 + i) if d == 0 else (LB - 1 - i)
                return Hall[d][:, :, :, st0:st0 + LB * (NBd[d] - 1) + 1:LB]

            def hkeys(d, i):
                st0 = (1 + i) if d == 0 else (LB - 1 - i)
                return [("Hall", d, st0 + LB * m) for m in range(NBd[d])]

            def mub(d, k, which, n):
                return mu16[:, d, k, which].unsqueeze(3).to_broadcast([128, 16, 2, n])
            for d in range(2):
                eng = "dve"
                for i in range(LB):
                    hv = hview(d, i)
                    hk_i = hkeys(d, i)
                    if i == 0:
                        P.op(eng, lambda e, d=d, hv=hv: e.tensor_copy(out=Rl[d][:, :, 0:2, 0:NBd[d]], in_=hv), r=hk_i, w=[("Rl", d)])
                    else:
                        P.op(eng, lambda e, d=d: e.tensor_tensor(out=TA[d][:, :, :, 0:NBd[d]], in0=Rl[d][:, :, 0:2, 0:NBd[d]], in1=mub(d, 1, 0, NBd[d]), op=ALU.mult),
                             r=[("Rl", d), "mu16"], w=[("TA", d)])
                        P.op(eng, lambda e, d=d: e.tensor_tensor(out=TB[d][:, :, :, 0:NBd[d]], in0=Rl[d][:, :, 1:3, 0:NBd[d]], in1=mub(d, 1, 1, NBd[d]), op=ALU.mult),
                             r=[("Rl", d), "mu16"], w=[("TB", d)])
                        P.op(eng, lambda e, d=d: e.tensor_tensor(out=TA[d][:, :, :, 0:NBd[d]], in0=TA[d][:, :, :, 0:NBd[d]], in1=TB[d][:, :, :, 0:NBd[d]], op=ALU.add),
                             r=[("TA", d), ("TB", d)], w=[("TA", d)])
                        P.op(eng, lambda e, d=d, hv=hv: e.tensor_tensor(out=Rl[d][:, :, 0:2, 0:NBd[d]], in0=TA[d][:, :, :, 0:NBd[d]], in1=hv, op=ALU.add),
                             r=[("TA", d)] + hk_i, w=[("Rl", d)])
                        P.op("act", lambda e, d=d, hv=hv: e.copy(out=hv, in_=Rl[d][:, :, 0:2, 0:NBd[d]]), r=[("Rl", d)], w=hk_i)
                    if i < LB - 1:
                        P.op(eng, lambda e, d=d: e.tensor_copy(out=Rl[d][:, :, 2, 0:NBd[d]], in_=Rl[d][:, :, 0, 0:NBd[d]]), r=[("Rl", d)], w=[("Rl", d)])
            for d in range(2):
                eng = "dve" if d == 0 else "pool"
                P.op(eng, lambda e, d=d: e.memset(Ea[d][:], 0.0), w=[("Rl", d)])
                order = list(range(NBd[0] - 1)) if d == 0 else list(range(NB - 1, 0, -1))
                for m in order:
                    mn = m + 1 if d == 0 else m - 1
                    pend = (1 + 16 * m + 15) if d == 0 else (16 * m)
                    P.op(eng, lambda e, d=d, m=m: e.tensor_tensor(out=ET[d][:, 0], in0=Ea[d][:, :, 0:2, m], in1=mu16[:, d, 16, 0], op=ALU.mult),
                         r=[("Rl", d), "mu16"], w=[("ET", d, 0)])
                    P.op(eng, lambda e, d=d, m=m: e.tensor_tensor(out=ET[d][:, 1], in0=Ea[d][:, :, 1:3, m], in1=mu16[:, d, 16, 1], op=ALU.mult),
                         r=[("Rl", d), "mu16"], w=[("ET", d, 1)])
                    P.op(eng, lambda e, d=d: e.tensor_tensor(out=ET[d][:, 0], in0=ET[d][:, 0], in1=ET[d][:, 1], op=ALU.add),
                         r=[("ET", d, 0), ("ET", d, 1)], w=[("ET", d, 0)])
                    P.op(eng, lambda e, d=d, mn=mn, pend=pend: e.tensor_tensor(out=Ea[d][:, :, 0:2, mn], in0=ET[d][:, 0], in1=Hall[d][:, :, :, pend], op=ALU.add),
                         r=[("ET", d, 0), ("Hall", d, pend)], w=[("Rl", d)])
                    P.op(eng, lambda e, d=d, mn=mn: e.tensor_copy(out=Ea[d][:, :, 2, mn], in_=Ea[d][:, :, 0, mn]), r=[("Rl", d)], w=[("Rl", d)])
            for d in range(2):
                eng = "dve"
                for i in range(LB):
                    hv = hview(d, i)
                    hk_i = hkeys(d, i)
                    P.op(eng, lambda e, d=d, i=i: e.tensor_tensor(out=TA[d][:, :, :, 0:NBd[d]], in0=Ea[d][:, :, 0:2, 0:NBd[d]], in1=mub(d, i + 1, 0, NBd[d]), op=ALU.mult),
                         r=[("Rl", d), "mu16"], w=[("TA", d)])
                    P.op(eng, lambda e, d=d, i=i: e.tensor_tensor(out=TB[d][:, :, :, 0:NBd[d]], in0=Ea[d][:, :, 1:3, 0:NBd[d]], in1=mub(d, i + 1, 1, NBd[d]), op=ALU.mult),
                         r=[("Rl", d), "mu16"], w=[("TB", d)])
                    P.op(eng, lambda e, d=d: e.tensor_tensor(out=TA[d][:, :, :, 0:NBd[d]], in0=TA[d][:, :, :, 0:NBd[d]], in1=TB[d][:, :, :, 0:NBd[d]], op=ALU.add),
                         r=[("TA", d), ("TB", d)], w=[("TA", d)])
                    P.op(eng, lambda e, d=d, hv=hv: e.tensor_tensor(out=hv, in0=hv, in1=TA[d][:, :, :, 0:NBd[d]], op=ALU.add), r=[("TA", d)] + hk_i, w=hk_i)
            P.flush()
        NCXo = NXO // 8
        NCB = (NCXo + 127) // 128
        NPASS = 1
        CBP = NCB // NPASS
        NCP = NCXo // NPASS
        Yc = sb(st, "Yc", [128, CBP, 4096], BF16)
        Ysb = sb(st, "Ysb", [128, 2, NCP])
        allH = [hk(0, 0, HW), hk(1, 0, HW)]
        for pz in range(NPASS):
            c_lo = pz * NCP
            for g in range(32):
                gh, gp = g // 16, g % 16
                rows = slice(64 * gh, 64 * gh + 64)
                pr = PS2[g % 2]
                prk = ("p2", g % 2)

                def mmy(e, g=g, gp=gp, rows=rows, pr=pr, c_lo=c_lo):
                    e.matmul(pr[:, 0:NCP], lhsT=Tloc[:, g, :], rhs=U8[:, g, NCC + c_lo:NCC + c_lo + NCP], start=True, stop=False)
                    e.matmul(pr[:, 0:NCP], lhsT=Dm[rows, 0, gp, 0, :], rhs=Hall[0][rows, gp, 0, NCC + c_lo:NCC + c_lo + NCP], start=False, stop=False)
                    e.matmul(pr[:, 0:NCP], lhsT=Dm[rows, 0, gp, 1, :], rhs=Hall[0][rows, gp, 1, NCC + c_lo:NCC + c_lo + NCP], start=False, stop=False)
                    e.matmul(pr[:, 0:NCP], lhsT=Dm[rows, 1, gp, 0, :], rhs=Hall[1][rows, gp, 0, 1 + c_lo:1 + c_lo + NCP], start=False, stop=False)
                    return e.matmul(pr[:, 0:NCP], lhsT=Dm[rows, 1, gp, 1, :], rhs=Hall[1][rows, gp, 1, 1 + c_lo:1 + c_lo + NCP], start=False, stop=True)
                P.op("pe", mmy, r=[("Tloc", g), "U8", "Dm"] + allH[0] + allH[1], w=[prk])
                ys = g % 2
                if g % 2 == 0:
                    P.op("act", lambda e, ys=ys, pr=pr: e.copy(out=Ysb[:, ys, :], in_=pr[:, 0:NCP]), r=[prk], w=[("Ysb", ys)])
                else:
                    P.op("dve", lambda e, ys=ys, pr=pr: e.tensor_copy(out=Ysb[:, ys, :], in_=pr[:, 0:NCP]), r=[prk], w=[("Ysb", ys)])
                for cb in range(CBP):
                    ncb = min(128, NCP - cb * 128)
                    pt_ = PS2[2 + (g * CBP + cb) % 4]
                    ptk = ("p2", 2 + (g * CBP + cb) % 4)
                    P.op("pe", lambda e, ys=ys, cb=cb, ncb=ncb, pt_=pt_: e.transpose(out=pt_[0:ncb, 0:128], in_=Ysb[:, ys, cb * 128:cb * 128 + ncb],
                                                                                   identity=ident[:]), r=[("Ysb", ys), "ident"], w=[ptk])
                    oap = Yc[0:ncb, cb, :].rearrange("c (j g o) -> c g j o", j=8, g=32)[:, g]
                    iap = pt_[0:ncb, 0:128].rearrange("c (j o) -> c j o", j=8)
                    if (g + cb) % 2 == 0:
                        P.op("dve", lambda e, oap=oap, iap=iap: e.tensor_copy(out=oap, in_=iap), r=[ptk], w=[("Yc", cb)])
                    else:
                        P.op("act", lambda e, oap=oap, iap=iap: e.copy(out=oap, in_=iap), r=[ptk], w=[("Yc", cb)])
            for cb in range(CBP):
                ncb = min(128, NCP - cb * 128)
                r0 = c_lo + cb * 128
                P.dmaop("sp", lambda e, cb=cb, ncb=ncb, r0=r0: e.dma_start(out=y_s[r0:r0 + ncb, :], in_=Yc[0:ncb, cb, :]),
                        r=[("Yc", cb)], w=["y_s"])
        P.flush()
    if DONE:
        es.close()
        return nc
    if debug == 5:
        dbg = nc.dram_tensor("dbg_y", [NXe // 8, 4096], BF16, kind="ExternalOutput").ap()
        P.dmaop("sp", lambda e: e.dma_start(out=dbg, in_=y_s[0:NXe // 8, :]), w=["dbg"])
        P.flush()
        es.close()
        return nc

    if debug in (0, 3, 6):
      with ExitStack() as st:
        vt = sb(st, "vt", [128, NTe, 8, 80], BF16)
        kTh = sb(st, "kTh", [128, 2, NTe * 128], BF16)
        qTh = sb(st, "qTh", [128, 2, NXO], BF16)
        pT = sb(st, "pT", [128, 5, 512], BF16)
        osb = sb(st, "osb", [128, 2, 512])
        rr = sb(st, "rr", [128, 512])
        ao = sb(st, "ao", [64, 2, 512])
        ones1 = sb(st, "ones1", [128, 64])
        PSs = [ps(st, "ps_s%d" % i) for i in range(5)]
        PSo = [ps(st, "ps_o%d" % i) for i in range(2)]
        PSb = ps(st, "ps_b")
        P.op("pool", lambda e: e.memset(vt[:], 1.0), w=["vt"])
        P.op("pool", lambda e: e.memset(ones1[:], 1.0), w=["ones1"])
        for kt in range(NTe):
            P.dmaop("sp" if kt % 2 == 0 else "act",
                    lambda e, kt=kt: e.dma_start(out=vt[:, kt, :, 0:64],
                                                 in_=v_s[kt * 128:(kt + 1) * 128, :].rearrange("p (h d) -> p h d", h=8)),
                    r=["v_s"], w=["vt"])
        cnt = 0
        for h in range(H):
            hs = h % 2
            P.dmaop("sp", lambda e, h=h, hs=hs: e.dma_start(out=kTh[0:96, hs, :], in_=kT_s[h, :, 0:NTe * 128]), r=["kT_s"], w=[("kTh", hs)])
            P.dmaop("act", lambda e, h=h, hs=hs: e.dma_start(out=qTh[0:96, hs, :], in_=qT_s[h, :, 0:NXO]), r=["qT_s"], w=[("qTh", hs)])
            for g in range(NQG):
                og = (h * NQG + g) % 2

                def smm(e, kt, hs=hs, g=g):
                    return e.matmul(PSs[kt % 5][:, 0:QG], lhsT=kTh[0:96, hs, kt * 128:(kt + 1) * 128],
                                    rhs=qTh[0:96, hs, g * QG:(g + 1) * QG], start=True, stop=True)

                def pvm(e, kt, h=h, og=og):
                    return e.matmul(PSo[og][0:65, 0:QG], lhsT=vt[:, kt, h, 0:65], rhs=pT[:, kt % 5, 0:QG],
                                    start=(kt == 0), stop=(kt == NTe - 1))
                LOOK = 3
                for step in range(NTe + LOOK):
                    if step < NTe:
                        kt = step
                        P.op("pe", lambda e, kt=kt, f=smm: f(e, kt), r=[("kTh", hs), ("qTh", hs)], w=[("pss", kt % 5)])
                        P.op("act", lambda e, kt=kt: e.activation(out=pT[:, kt % 5, 0:QG], in_=PSs[kt % 5][:, 0:QG], func=AF.Exp),
                             r=[("pss", kt % 5)], w=[("pT", kt % 5)])
                    if step >= LOOK:
                        kt = step - LOOK
                        P.op("pe", lambda e, kt=kt, f=pvm: f(e, kt), r=[("pT", kt % 5), "vt"], w=[("pso", og)])
                P.op("dve", lambda e, og=og: e.tensor_copy(out=osb[0:65, og, 0:QG], in_=PSo[og][0:65, 0:QG]), r=[("pso", og)], w=[("osb", og)])
                P.op("dve", lambda e, og=og: e.reciprocal(out=rr[64:65, 0:QG], in_=osb[64:65, og, 0:QG]), r=[("osb", og)], w=["rr"])
                P.op("pe", lambda e: e.matmul(PSb[0:64, 0:QG], lhsT=ones1[64:65, 0:64], rhs=rr[64:65, 0:QG], start=True, stop=True),
                     r=["rr", "ones1"], w=["psb"])
                P.op("dve", lambda e, og=og: e.tensor_tensor(out=ao[:, og, 0:QG], in0=osb[0:64, og, 0:QG], in1=PSb[0:64, 0:QG], op=ALU.mult),
                     r=[("osb", og), "psb"], w=[("ao", og)])
                P.dmaop("sp", lambda e, h=h, g=g, og=og: e.dma_start(out=attnT_s[h, :, g * QG:(g + 1) * QG], in_=ao[:, og, 0:QG]),
                        r=[("ao", og)], w=["attnT_s"])
        P.flush()

    CAPe = 2 * NXe // NE
    SLT = min(128, CAPe)
    NRC = CAPe // SLT
    NXT = NXO // 128

    if debug in (0, 6):
      with ExitStack() as sp:
        idxT = sb(sp, "idxT", [128, NRC, 16], I32)
        gateT = sb(sp, "gateT", [128, NRC, 16])
        sp45 = ExitStack()
        affT = sb(sp45, "affT", [48, NXO])
        affo = sb(sp45, "affo", [16, NXO])
        with ExitStack() as st:
            wglu = sb(st, "wglu", [128, 4, 512], BF16)
            wsso = sb(st, "wsso", [128, 4, D], BF16)
            wmla = sb(st, "wmla", [64, 8, D], BF16)
            wout = sb(st, "wout", [128, 8, D], BF16)
            wrt = sb(st, "wrt", [128, 8, 16])
            bglu = sb(st, "bglu", [128, 512])
            identb = sb(st, "identb4", [128, 128], BF16)
            TL4 = [dict(), dict()]
            for _s in range(2):
                TL4[_s]['yt'] = sb(st, "yt_%d" % _s, [128, 512], BF16)
                TL4[_s]['yg'] = sb(st, "yg_%d" % _s, [128, 512])
                TL4[_s]['t1'] = sb(st, "t1_%d" % _s, [128, 512])
                TL4[_s]['ygb'] = sb(st, "ygb_%d" % _s, [128, 512], BF16)
                TL4[_s]['ygT'] = sb(st, "ygT_%d" % _s, [128, 4, 128], BF16)
                TL4[_s]['sg'] = sb(st, "sg_%d" % _s, [128, 512])
                TL4[_s]['zb'] = sb(st, "zb_%d" % _s, [128, 512], BF16)
                TL4[_s]['zT'] = sb(st, "zT_%d" % _s, [128, 4, 128], BF16)
                TL4[_s]['at32'] = sb(st, "at32_%d" % _s, [64, 8, 128])
                TL4[_s]['atb'] = sb(st, "atb_%d" % _s, [64, 8, 128], BF16)
                TL4[_s]['gt'] = sb(st, "gt_%d" % _s, [128, 2048])
                TL4[_s]['m1'] = sb(st, "m1_%d" % _s, [128, D])
                TL4[_s]['m2'] = sb(st, "m2_%d" % _s, [128, D])
                TL4[_s]['mb'] = sb(st, "mb_%d" % _s, [128, D], BF16)
                TL4[_s]['mT'] = sb(st, "mT_%d" % _s, [128, 8, 128], BF16)
                TL4[_s]['xt4'] = sb(st, "xt4_%d" % _s, [128, D])
                TL4[_s]['xm'] = sb(st, "xm_%d" % _s, [128, D])
                TL4[_s]['jk'] = sb(st, "jk_%d" % _s, [128, D])
                TL4[_s]['h2'] = sb(st, "h2_%d" % _s, [128, D])
                TL4[_s]['h2b'] = sb(st, "h2b_%d" % _s, [128, D], BF16)
                TL4[_s]['h2T'] = sb(st, "h2T_%d" % _s, [128, 8, 128])
                TL4[_s]['s4'] = sb(st, "s4_%d" % _s, [128, 8])
                TL4[_s]['lg'] = sb(st, "lg_%d" % _s, [128, 16])
                TL4[_s]['af'] = sb(st, "af_%d" % _s, [128, 48])
            BALL = [ps(st, "ph4_%d" % i) for i in range(8)]
            P.dmaop("pool", lambda e: e.dma_start(out=wglu[:], in_=w_glu.rearrange("(k p) n -> p k n", p=128)), w=["wglu"])
            P.dmaop("pool", lambda e: e.dma_start(out=wsso[:], in_=w_ssm_o.rearrange("(k p) n -> p k n", p=128)), w=["wsso"])
            P.dmaop("pool", lambda e: e.dma_start(out=wmla[:], in_=w_mla_o.rearrange("(h v) n -> v h n", v=64)), w=["wmla"])
            P.dmaop("pool", lambda e: e.dma_start(out=wout[:], in_=w_out.rearrange("(k p) n -> p k n", p=128)), w=["wout"])
            P.dmaop("sp", lambda e: e.dma_start(out=wrt[:], in_=w_router.rearrange("(k p) n -> p k n", p=128)), w=["wrt"])
            P.dmaop("sp", lambda e: e.dma_start(out=bglu[:], in_=b_glu.partition_broadcast(128)), w=["bglu"])
            P.op("dve", lambda e: e.tensor_copy(out=identb[:], in_=ident[:]), r=["ident"], w=["identb4"])
            ysv = y_s.rearrange("c (j f) -> (c j) f", j=8)
            for _s in range(2):
                P.op("pool", lambda e, _s=_s: e.memset(TL4[_s]['af'][:], 0.0), w=[("slot", _s, "af")])
            P.op("pool", lambda e: e.memset(TL4[0]['jk'][:], 0.0), w=[("slot", 0, "jk")])
            for r0 in range(0, 2 * NXO, 128):
                P.dmaop("sp" if (r0 // 128) % 2 == 0 else "act",
                        lambda e, r0=r0: e.dma_start(out=acc[r0:r0 + 128, :], in_=TL4[0]['jk'][:]), r=[("slot", 0, "jk")], w=["acc0"])
            P.op("pool", lambda e: e.memset(affT[:], 0.0), w=["affT"])
            SHARED4 = ["wglu", "wsso", "wmla", "wout", "wrt", "bglu", "identb4", "ident", "modx", "y_s", "gates_s", "attnT_s", "xm_s", "h2b_own", "affo"]
            ALIAS4 = {"b4": "b2", "b5": "b3", "b6": "b2", "b7": "b3"}

            def tile4(i, s):
                Pq = Keyed(P, s, SHARED4, ALIAS4)
                t0 = i * 128
                yt = TL4[s]['yt']
                yg = TL4[s]['yg']
                t1 = TL4[s]['t1']
                ygb = TL4[s]['ygb']
                ygT = TL4[s]['ygT']
                sg = TL4[s]['sg']
                zb = TL4[s]['zb']
                zT = TL4[s]['zT']
                at32 = TL4[s]['at32']
                atb = TL4[s]['atb']
                gt = TL4[s]['gt']
                m1 = TL4[s]['m1']
                m2 = TL4[s]['m2']
                mb = TL4[s]['mb']
                mT = TL4[s]['mT']
                xt4 = TL4[s]['xt4']
                xm = TL4[s]['xm']
                jk = TL4[s]['jk']
                h2 = TL4[s]['h2']
                h2b = TL4[s]['h2b']
                h2T = TL4[s]['h2T']
                s4 = TL4[s]['s4']
                lg = TL4[s]['lg']
                af = TL4[s]['af']
                bk = BALL[4 * s:4 * s + 4]
                B = [bk[0], bk[1], bk[2], bk[3], bk[2], bk[3], bk[2], bk[3]]
                B0b = B[0][:].bitcast(BF16)
                Pq.dmaop("sp", lambda e, t0=t0: e.dma_start(out=yt[:], in_=ysv[t0:t0 + 128, :]), r=["y_s"], w=["yt"])
                Pq.dmaop("act", lambda e, t0=t0: e.dma_start(out=gt[:], in_=gates_s[t0:t0 + 128, :]), r=["gates_s"], w=["gt"])
                Pq.dmaop("sp", lambda e, t0=t0: e.dma_start(out=at32[:], in_=attnT_s[:, :, t0:t0 + 128].rearrange("h v t -> v h t")),
                        r=["attnT_s"], w=["at32"])
                Pq.dmaop("act", lambda e, t0=t0: e.dma_start(out=xt4[:], in_=xc[NCTX + t0:NCTX + t0 + 128, :]), w=["xt4"])
                Pq.op("pool", lambda e: e.tensor_tensor(out=t1[:], in0=yt[:], in1=yt[:], op=ALU.mult), r=["yt"], w=["t1"])
                Pq.op("dve", lambda e: e.tensor_scalar(out=t1[:], in0=t1[:], scalar1=0.044715, scalar2=1.0, op0=ALU.mult, op1=ALU.add),
                     r=["t1"], w=["t1"])
                Pq.op("dve", lambda e: e.tensor_tensor(out=t1[:], in0=t1[:], in1=yt[:], op=ALU.mult), r=["t1", "yt"], w=["t1"])
                Pq.op("act", lambda e: e.activation(out=t1[:], in_=t1[:], func=AF.Tanh, scale=0.7978845608028654), r=["t1"], w=["t1"])
                Pq.op("dve", lambda e: e.tensor_scalar(out=t1[:], in0=t1[:], scalar1=1.0, scalar2=0.5, op0=ALU.add, op1=ALU.mult),
                     r=["t1"], w=["t1"])
                Pq.op("dve", lambda e: e.tensor_tensor(out=yg[:], in0=t1[:], in1=yt[:], op=ALU.mult), r=["t1", "yt"], w=["yg"])
                Pq.op("pool", lambda e: e.tensor_copy(out=ygb[:], in_=yg[:]), r=["yg"], w=["ygb"])

                def tr4(src, n):
                    def f(e):
                        ins = None
                        for k in range(n):
                            ins = e.transpose(out=B0b[:, k * 128:(k + 1) * 128], in_=src[:, k * 128:(k + 1) * 128], identity=identb[:])
                        return ins
                    return f
                Pq.op("pe", tr4(ygb, 4), r=["ygb", "identb4"], w=["b0"])
                Pq.op("act", lambda e: e.copy(out=ygT[:].rearrange("p a b -> p (a b)"), in_=B0b[:, 0:512]), r=["b0"], w=["ygT"])

                def mmglu(e):
                    ins = None
                    for k in range(4):
                        ins = e.matmul(B[1][:, 0:512], lhsT=ygT[:, k, :], rhs=wglu[:, k, :], start=(k == 0), stop=(k == 3))
                    return ins
                Pq.op("pe", mmglu, r=["ygT", "wglu"], w=["b1"])
                Pq.op("dve", lambda e: e.tensor_tensor(out=sg[:], in0=B[1][:, 0:512], in1=bglu[:], op=ALU.add), r=["b1", "bglu"], w=["sg"])
                Pq.op("act", lambda e: e.activation(out=sg[:], in_=sg[:], func=AF.Sigmoid), r=["sg"], w=["sg"])
                Pq.op("dve", lambda e: e.tensor_tensor(out=zb[:], in0=sg[:], in1=yg[:], op=ALU.mult), r=["sg", "yg"], w=["zb"])
                Pq.op("pe", tr4(zb, 4), r=["zb", "identb4"], w=["b0"])
                Pq.op("act", lambda e: e.copy(out=zT[:].rearrange("p a b -> p (a b)"), in_=B0b[:, 0:512]), r=["b0"], w=["zT"])

                def mmsso(e):
                    ins = None
                    for hf in range(2):
                        for k in range(4):
                            ins = e.matmul(B[2 + hf][:, 0:512], lhsT=zT[:, k, :], rhs=wsso[:, k, hf * 512:(hf + 1) * 512], start=(k == 0), stop=(k == 3))
                    return ins
                Pq.op("pe", mmsso, r=["zT", "wsso"], w=["b2", "b3"])
                for hf in range(2):
                    cs = slice(hf * 512, (hf + 1) * 512)
                    Pq.op("dve", lambda e, hf=hf, cs=cs: e.tensor_tensor(out=m1[:, cs], in0=B[2 + hf][:, 0:512], in1=gt[:, cs], op=ALU.mult),
                          r=["b%d" % (2 + hf), "gt"], w=[("m1", hf)])
                Pq.op("pool", lambda e: e.tensor_copy(out=atb[:], in_=at32[:]), r=["at32"], w=["atb"])

                def mmat(e):
                    ins = None
                    for hf in range(2):
                        for h in range(8):
                            ins = e.matmul(B[4 + hf][:, 0:512], lhsT=atb[:, h, :], rhs=wmla[:, h, hf * 512:(hf + 1) * 512], start=(h == 0), stop=(h == 7))
                    return ins
                Pq.op("pe", mmat, r=["atb", "wmla"], w=["b4", "b5"])
                for hf in range(2):
                    cs = slice(hf * 512, (hf + 1) * 512)
                    cs2 = slice(D + hf * 512, D + (hf + 1) * 512)
                    Pq.op("dve", lambda e, hf=hf, cs=cs, cs2=cs2: e.tensor_tensor(out=m2[:, cs], in0=B[4 + hf][:, 0:512], in1=gt[:, cs2], op=ALU.mult),
                          r=["b%d" % (4 + hf), "gt"], w=[("m2", hf)])
                Pq.op("pool", lambda e: e.tensor_tensor(out=mb[:], in0=m1[:], in1=m2[:], op=ALU.add),
                     r=[("m1", 0), ("m1", 1), ("m2", 0), ("m2", 1)], w=["mb"])
                Pq.op("pe", tr4(mb, 8), r=["mb", "identb4"], w=["b0"])
                Pq.op("act", lambda e: e.copy(out=mT[:].rearrange("p a b -> p (a b)"), in_=B0b[:, 0:1024]), r=["b0"], w=["mT"])

                def mmout(e):
                    ins = None
                    for hf in range(2):
                        for k in range(8):
                            ins = e.matmul(B[6 + hf][:, 0:512], lhsT=mT[:, k, :], rhs=wout[:, k, hf * 512:(hf + 1) * 512], start=(k == 0), stop=(k == 7))
                    return ins
                Pq.op("pe", mmout, r=["mT", "wout"], w=["b6", "b7"])
                for hf in range(2):
                    cs = slice(hf * 512, (hf + 1) * 512)
                    Pq.op("dve", lambda e, hf=hf, cs=cs: e.tensor_tensor(out=xm[:, cs], in0=B[6 + hf][:, 0:512], in1=modx[:, 2 * D + hf * 512:2 * D + (hf + 1) * 512],
                                                                    op=ALU.mult), r=["b%d" % (6 + hf), "modx"], w=[("xm", hf)])
                Pq.op("pool", lambda e: e.tensor_tensor(out=xm[:], in0=xm[:], in1=xt4[:], op=ALU.add), r=[("xm", 0), ("xm", 1), "xt4"], w=[("xm", 0), ("xm", 1)])
                Pq.dmaop("sp", lambda e, t0=t0: e.dma_start(out=xm_s[t0:t0 + 128, :], in_=xm[:]), r=[("xm", 0), ("xm", 1)], w=["xm_s"])
                Pq.op("act", lambda e: e.activation(out=jk[:], in_=xm[:], func=AF.Square, accum_out=s4[:, 0:1]), r=[("xm", 0), ("xm", 1)], w=["jk", "s4a"])
                Pq.op("dve", lambda e: e.tensor_scalar(out=s4[:, 1:2], in0=s4[:, 0:1], scalar1=1.0 / D, scalar2=EPS, op0=ALU.mult, op1=ALU.add), r=["s4a"], w=["s4b"])
                Pq.op("act", lambda e: e.activation(out=s4[:, 1:2], in_=s4[:, 1:2], func=AF.Sqrt), r=["s4b"], w=["s4b"])
                Pq.op("dve", lambda e: e.reciprocal(out=s4[:, 1:2], in_=s4[:, 1:2]), r=["s4b"], w=["s4b"])
                Pq.op("dve", lambda e: e.scalar_tensor_tensor(out=h2[:], in0=xm[:], scalar=s4[:, 1:2], in1=modx[:, 4 * D:5 * D], op0=ALU.mult, op1=ALU.mult),
                     r=[("xm", 0), ("xm", 1), "s4b", "modx"], w=["h2"])
                Pq.op("pool", lambda e: e.tensor_tensor(out=h2[:], in0=h2[:], in1=modx[:, 3 * D:4 * D], op=ALU.add), r=["h2", "modx"], w=["h2"])
                Pq.op("act", lambda e: e.copy(out=h2b[:], in_=h2[:]), r=["h2"], w=["h2b"])
                Pq.dmaop("act", lambda e, t0=t0: e.dma_start(out=h2b_own_c[t0 // RCH].ap()[t0 % RCH:t0 % RCH + 128, :], in_=h2b[:]), r=["h2b"], w=["h2b_own"])
                def trh2(e):
                    ins = None
                    for k in range(8):
                        ins = e.transpose(out=B[2 + k // 4][:, (k % 4) * 128:(k % 4 + 1) * 128], in_=h2[:, k * 128:(k + 1) * 128], identity=ident[:])
                    return ins
                Pq.op("pe", trh2, r=["h2", "ident", ("m1", 0), ("m1", 1)], w=["b2", "b3"])
                Pq.op("act", lambda e: e.copy(out=h2T[:, 0:4, :].rearrange("p a b -> p (a b)"), in_=B[2][:, 0:512]), r=["b2"], w=[("h2T", 0)])
                Pq.op("dve", lambda e: e.tensor_copy(out=h2T[:, 4:8, :].rearrange("p a b -> p (a b)"), in_=B[3][:, 0:512]), r=["b3"], w=[("h2T", 1)])

                def mmrt(e):
                    ins = None
                    for k in range(8):
                        ins = e.matmul(B[1][:, 0:16], lhsT=h2T[:, k, :], rhs=wrt[:, k, :], start=(k == 0), stop=(k == 7))
                    return ins
                Pq.op("pe", mmrt, r=[("h2T", 0), ("h2T", 1), "wrt", "sg"], w=["b1"])
                Pq.op("dve", lambda e: e.tensor_copy(out=lg[:], in_=B[1][:, 0:16]), r=["b1"], w=["lg"])
                Pq.op("dve", lambda e: e.tensor_reduce(out=s4[:, 2:3], in_=lg[:], axis=AX.X, op=ALU.max), r=["lg"], w=["s4c"])
                Pq.op("dve", lambda e: e.tensor_scalar(out=s4[:, 3:4], in0=s4[:, 2:3], scalar1=-1.0, scalar2=None, op0=ALU.mult), r=["s4c"], w=["s4d"])
                hb_ = 0
                afc = slice(0, 16)
                Pq.op("act", lambda e, afc=afc: e.activation(out=af[:, afc], in_=lg[:], func=AF.Exp, bias=s4[:, 3:4], accum_out=s4[:, 4:5]), r=["lg", "s4d"], w=["af", "s4e"])
                Pq.op("dve", lambda e: e.reciprocal(out=s4[:, 5:6], in_=s4[:, 4:5]), r=["s4e"], w=["s4f"])
                Pq.op("dve", lambda e, afc=afc: e.tensor_scalar(out=af[:, afc], in0=af[:, afc], scalar1=s4[:, 5:6], scalar2=None, op0=ALU.mult), r=["af", "s4f"], w=["af"])
                Pq.op("pe", lambda e: e.transpose(out=B[4][0:48, 0:128], in_=af[:], identity=ident[:]), r=["af", "ident", ("m2", 0), ("m2", 1)], w=["b4"])
                Pq.op("act", lambda e, t0=t0: e.copy(out=affo[:, t0:t0 + 128], in_=B[4][0:16, 0:128]), r=["b4"], w=["affo"])
                return Pq.cap
            for i in range(0, NXT, 2):
                caps = [tile4(i, 0)] + ([tile4(i + 1, 1)] if i + 1 < NXT else [])
                interleave(P, caps, chunk=int(os.environ.get("ILV", "2")))
            P.flush()
        with ExitStack() as st:
            NH = NXO
            wk = sb(st, "wk", [48, NH])
            vals = sb(st, "vals", [48, CAPe])
            idxu = sb(st, "idxu", [48, CAPe], U32)
            idxf = sb(st, "idxf", [48, CAPe])
            jrev = sb(st, "jrev", [128, 128])
            tA = sb(st, "tA", [128, 2, 16])
            tB = sb(st, "tB", [128, 2, 16])
            tM = sb(st, "tM", [128, 3, 16])
            B5 = [ps(st, "ph5_%d" % i) for i in range(4)]
            P.dmaop("sp", lambda e: e.dma_start(out=jrev[:], in_=jrev_d), w=["jrev"])
            a01 = sb(st, "a01", [16, 2, NXO])
            selt = sb(st, "selt", [16, 8])
            P.dmaop("sp", lambda e: e.dma_start(out=selt[:], in_=sel_d), w=["selt"])
            P.dmaop("sp", lambda e: e.dma_start(out=aff_own, in_=affo[:]), r=["affo"], w=["aff_own"])
            P.ccop(lambda e: e.collective_compute("AllGather", ALU.bypass, replica_groups=PAIRS, ins=[aff_own_t.ap().opt()], outs=[aff_all_t.ap().opt()]),
                   r=["aff_own"], w=["aff_all"])
            for c in range(NCHK):
                P.ccop(lambda e, c=c: e.collective_compute("AllGather", ALU.bypass, replica_groups=PAIRS, ins=[h2b_own_c[c].ap().opt()], outs=[h2b_ag_c[c].ap().opt()]),
                       r=["h2b_own"], w=[("h2b_ag", c)])
                for rk_ in range(2):
                    P.dmaop("act", lambda e, c=c, rk_=rk_: e.dma_start(out=h2b_all[rk_ * NXO + c * RCH:rk_ * NXO + (c + 1) * RCH, :],
                                                                      in_=h2b_ag_c[c].ap()[rk_ * RCH:(rk_ + 1) * RCH, :]), r=[("h2b_ag", c)], w=["h2b_all"])
            for rk_ in range(2):
                P.dmaop("sp", lambda e, rk_=rk_: e.dma_start(out=a01[:, rk_, :], in_=aff_all[16 * rk_:16 * rk_ + 16, :]), r=["aff_all"], w=[("a01", rk_)])
            for rk_ in range(2):
                for c0 in range(0, NXO, 512):
                    n = min(512, NXO - c0)
                    pb_ = B5[rk_]
                    P.op("pe", lambda e, rk_=rk_, c0=c0, n=n, pb_=pb_: e.matmul(pb_[32 * rk_:32 * rk_ + 8, 0:n], lhsT=selt[:, :], rhs=a01[:, rk_, c0:c0 + n],
                                                                             start=True, stop=True, tile_position=(0, 32 * rk_)),
                         r=["selt", ("a01", rk_)], w=[("b5", rk_)])
                    P.op("act", lambda e, rk_=rk_, c0=c0, n=n, pb_=pb_: e.copy(out=affT[32 * rk_:32 * rk_ + 8, c0:c0 + n], in_=pb_[32 * rk_:32 * rk_ + 8, 0:n]),
                         r=[("b5", rk_)], w=["affT"])
            P.op("dve", lambda e: e.tensor_copy(out=wk[:], in_=affT[:]), r=["affT"], w=["wk"])
            for r_ in range(CAPe // 8):
                sl = slice(r_ * 8, r_ * 8 + 8)
                P.op("dve", lambda e, sl=sl: e.max(out=vals[:, sl], in_=wk[:]), r=["wk"], w=[("vals", r_)])
                P.op("dve", lambda e, sl=sl: e.max_index(out=idxu[:, sl], in_max=vals[:, sl], in_values=wk[:]), r=["wk", ("vals", r_)], w=[("idxu", r_)])
                P.op("dve", lambda e, sl=sl: e.match_replace(out=wk[:], in_to_replace=vals[:, sl], in_values=wk[:], imm_value=-1.0),
                     r=["wk", ("vals", r_), ("idxu", r_)], w=["wk"])
            allv = [("vals", r_) for r_ in range(CAPe // 8)]
            alli = [("idxu", r_) for r_ in range(CAPe // 8)]
            P.op("dve", lambda e: e.tensor_copy(out=idxf[:], in_=idxu[:]), r=alli, w=["idxf"])
            P.op("dve", lambda e: e.tensor_scalar(out=idxf[32:48, :], in0=idxf[32:48, :], scalar1=float(NH), scalar2=None, op0=ALU.add), r=["idxf"], w=["idxf"])
            Jb = jrev[0:SLT, 128 - SLT:128]
            for rc in range(NRC):
                cs = slice(rc * SLT, (rc + 1) * SLT)
                rb = NRC - 1 - rc
                cb_ = slice(rb * SLT, (rb + 1) * SLT)
                for w_, src in ((0, vals), (1, idxf)):
                    rk = allv if w_ == 0 else ["idxf"]
                    P.op("pe", lambda e, cs=cs, src=src, w_=w_: e.transpose(out=B5[w_][0:SLT, 0:16], in_=src[0:16, cs], identity=ident[0:16, 0:16]),
                         r=rk + ["ident"], w=[("b5", w_)])
                    P.op("act", lambda e, w_=w_: e.copy(out=tA[0:SLT, w_, :], in_=B5[w_][0:SLT, 0:16]), r=[("b5", w_)], w=[("tA", w_)])
                    P.op("pe", lambda e, cb_=cb_, src=src, w_=w_: e.transpose(out=B5[2 + w_][0:SLT, 0:16], in_=src[32:48, cb_], identity=ident[32:48, 32:48]),
                         r=rk + ["ident"], w=[("b5", 2 + w_)])
                    P.op("act", lambda e, w_=w_: e.copy(out=tB[0:SLT, w_, :], in_=B5[2 + w_][0:SLT, 0:16]), r=[("b5", 2 + w_)], w=[("tB", w_)])
                    P.op("pe", lambda e, w_=w_: e.matmul(B5[2 + w_][0:SLT, 0:16], lhsT=Jb, rhs=tB[0:SLT, w_, :], start=True, stop=True),
                         r=[("tB", w_), "jrev"], w=[("b5", 2 + w_)])
                P.op("dve", lambda e: e.tensor_tensor(out=tM[0:SLT, 0, :], in0=tA[0:SLT, 0, :], in1=B5[2][0:SLT, 0:16], op=ALU.is_gt),
                     r=[("tA", 0), ("b5", 2)], w=[("tM", 0)])
                P.op("dve", lambda e, rc=rc: e.tensor_tensor(out=gateT[0:SLT, rc, :], in0=tA[0:SLT, 0, :], in1=B5[2][0:SLT, 0:16], op=ALU.max),
                     r=[("tA", 0), ("b5", 2)], w=["gateT"])
                P.op("dve", lambda e: e.tensor_tensor(out=tM[0:SLT, 1, :], in0=tA[0:SLT, 1, :], in1=B5[3][0:SLT, 0:16], op=ALU.subtract),
                     r=[("tA", 1), ("b5", 3)], w=[("tM", 1)])
                P.op("dve", lambda e: e.tensor_tensor(out=tM[0:SLT, 1, :], in0=tM[0:SLT, 1, :], in1=tM[0:SLT, 0, :], op=ALU.mult),
                     r=[("tM", 1), ("tM", 0)], w=[("tM", 1)])
                P.op("dve", lambda e: e.tensor_tensor(out=tM[0:SLT, 2, :], in0=tM[0:SLT, 1, :], in1=B5[3][0:SLT, 0:16], op=ALU.add),
                     r=[("tM", 1), ("b5", 3)], w=[("tM", 2)])
                P.op("dve", lambda e, rc=rc: e.tensor_copy(out=idxT[0:SLT, rc, :], in_=tM[0:SLT, 2, :]), r=[("tM", 2)], w=["idxT"])
            P.flush()
        sp45.close()
        NEe = NE // 2 if debug == 0 else int(os.environ.get("NEE", "8"))
        with ExitStack() as st:
            identb = sb(st, "identb6", [128, 128], BF16)
            xs = sb(st, "xs", [128, 1, NRC, D], BF16)
            xsT = sb(st, "xsT", [128, 2, 8, CAPe], BF16)
            wg = sb(st, "wg", [128, 3, 8, 512], BF16)
            wu = sb(st, "wu", [128, 3, 8, 512], BF16)
            wd = sb(st, "wd", [128, 2, 22, 512], BF16)
            sgt = sb(st, "sgt", [128, 2, CAPe])
            hidT = sb(st, "hidT", [128, 22, CAPe], BF16)
            ys = sb(st, "ys", [128, 1, NRC, D])
            B = [ps(st, "ph6_%d" % i) for i in range(8)]
            B0b = B[0][:].bitcast(BF16)
            P.op("dve", lambda e: e.tensor_copy(out=identb[:], in_=ident[:]), r=["ident"], w=["identb6"])
            wgi = 0
            wdi = 0
            def prep(ex):
                sl = ex % 2
                for rc in range(NRC):
                    P.dmaop("pool", lambda e, rc=rc, ex=ex, sl=sl: e.indirect_dma_start(
                        out=xs[0:SLT, 0, rc, :], out_offset=None, in_=h2b_all[0:2 * NXO, :],
                        in_offset=bass.IndirectOffsetOnAxis(ap=idxT[0:SLT, rc, ex:ex + 1], axis=0)),
                        r=["idxT", "h2b_all"], w=[("xs", 0, rc)])

                    def trx(e, rc=rc, sl=sl):
                        ins = None
                        for k in range(8):
                            ins = e.transpose(out=B0b[:, k * 128:k * 128 + SLT], in_=xs[0:SLT, 0, rc, k * 128:(k + 1) * 128], identity=identb[0:SLT, 0:SLT])
                        return ins
                    P.op("pe", trx, r=[("xs", 0, rc), "identb6"], w=["b0"])
                    P.op("act", lambda e, rc=rc, sl=sl: e.copy(out=xsT[:, sl, :, rc * SLT:(rc + 1) * SLT],
                                                               in_=B0b[:, 0:1024].rearrange("p (k c) -> p k c", k=8)[:, :, 0:SLT]), r=["b0"], w=[("xsT", sl)])
            prep(0)
            pending = []
            for ex in range(NEe):
                xsl = ex % 2
                wgv = w_e_gate[ex].rearrange("(dc p) f -> p dc f", p=128)
                wuv = w_e_up[ex].rearrange("(dc p) f -> p dc f", p=128)
                wdv = w_e_down[ex].rearrange("(fc p) d -> p fc d", p=128)
                for grp in range(6):
                    ws = wgi % 3
                    wgi += 1
                    f0 = grp * 512
                    fw = min(512, FF - f0)
                    P.dmaop("pool", lambda e, ws=ws, f0=f0, fw=fw, wgv=wgv: e.dma_start(out=wg[:, ws, :, 0:fw], in_=wgv[:, :, f0:f0 + fw]), w=[("wg", ws)])
                    P.dmaop("pool", lambda e, ws=ws, f0=f0, fw=fw, wuv=wuv: e.dma_start(out=wu[:, ws, :, 0:fw], in_=wuv[:, :, f0:f0 + fw]), w=[("wu", ws)])
                    if grp == 1:
                        for f_ in pending:
                            f_()
                        pending = []
                    for q in range(fw // 128):
                        fc = grp * 4 + q
                        pg = B[1 + fc % 2]
                        pu = B[3 + fc % 2]

                        def mmgu(e, ws=ws, q=q, pg=pg, pu=pu, xsl=xsl):
                            ins = None
                            for k in range(8):
                                e.matmul(pg[:, 0:CAPe], lhsT=wg[:, ws, k, q * 128:(q + 1) * 128], rhs=xsT[:, xsl, k, :], start=(k == 0), stop=(k == 7))
                            for k in range(8):
                                ins = e.matmul(pu[:, 0:CAPe], lhsT=wu[:, ws, k, q * 128:(q + 1) * 128], rhs=xsT[:, xsl, k, :], start=(k == 0), stop=(k == 7))
                            return ins
                        P.op("pe", mmgu, r=[("wg", ws), ("wu", ws), ("xsT", xsl)], w=[("b6", 1 + fc % 2), ("b6", 3 + fc % 2)])
                        P.op("act", lambda e, fc=fc, pg=pg: e.activation(out=sgt[:, fc % 2, :], in_=pg[:, 0:CAPe], func=AF.Silu),
                             r=[("b6", 1 + fc % 2)], w=[("sgt", fc % 2)])
                        P.op("dve", lambda e, fc=fc, pu=pu: e.tensor_tensor(out=hidT[:, fc, :], in0=sgt[:, fc % 2, :], in1=pu[:, 0:CAPe], op=ALU.mult),
                             r=[("sgt", fc % 2), ("b6", 3 + fc % 2)], w=[("hidT", fc)])
                hk_ = [("hidT", fc) for fc in range(22)]
                if ex + 1 < NEe:
                    prep(ex + 1)
                for dq in range(2):
                    ws = wdi % 2
                    wdi += 1
                    P.dmaop("pool", lambda e, ws=ws, dq=dq, wdv=wdv: e.dma_start(out=wd[:, ws], in_=wdv[:, :, dq * 512:(dq + 1) * 512]), w=[("wd", ws)])
                    for rc in range(NRC):
                        py = B[5 + (dq * NRC + rc) % 2]
                        pyk = ("b6", 5 + (dq * NRC + rc) % 2)

                        def mmd(e, ws=ws, rc=rc, py=py):
                            ins = None
                            for fc in range(22):
                                ins = e.matmul(py[0:SLT, 0:512], lhsT=hidT[:, fc, rc * SLT:(rc + 1) * SLT], rhs=wd[:, ws, fc, :], start=(fc == 0), stop=(fc == 21))
                            return ins
                        P.op("pe", mmd, r=hk_ + [("wd", ws)], w=[pyk])
                        P.op("dve", lambda e, rc=rc, dq=dq, py=py, ex=ex, xsl=xsl: e.scalar_tensor_tensor(
                            out=ys[0:SLT, 0, rc, dq * 512:(dq + 1) * 512], in0=py[0:SLT, 0:512], scalar=gateT[0:SLT, rc, ex:ex + 1],
                            in1=modx[0:SLT, 5 * D + dq * 512:5 * D + (dq + 1) * 512], op0=ALU.mult, op1=ALU.mult),
                            r=[pyk, "gateT", "modx"], w=[("ys", 0, rc, dq)])
                def scat(ex=ex):
                    prevk = [("outx", (ex - 1) % 2, rc2) for rc2 in range(NRC)] if ex > 0 else ["acc0"]
                    for rc in range(NRC):
                        P.dmaop("pool", lambda e, rc=rc, ex=ex: e.indirect_dma_start(
                            out=acc[0:2 * NXO, :], out_offset=bass.IndirectOffsetOnAxis(ap=idxT[0:SLT, rc, ex:ex + 1], axis=0),
                            in_=ys[0:SLT, 0, rc, :], in_offset=None, compute_op=ALU.add),
                            r=[("ys", 0, rc, dq) for dq in range(2)] + ["idxT"] + prevk, w=[("outx", ex % 2, rc)])
                pending.append(scat)
            for f_ in pending:
                f_()
            P.flush()
        with ExitStack() as st7:
            fa = sb(st7, "fa", [128, 4 * D])
            P.ccop(lambda e: e.collective_compute("ReduceScatter", ALU.add, replica_groups=PAIRS, ins=[acc_t.ap().opt()], outs=[rs_out_t.ap().opt()]),
                   w=["rs_out"])
            for i in range(NXT):
                t0 = i * 128
                k = i % 2
                P.dmaop("sp", lambda e, t0=t0, k=k: e.dma_start(out=fa[:, k * 2 * D:k * 2 * D + D], in_=xm_s[t0:t0 + 128, :]), w=[("fa", k, 0)])
                P.dmaop("act", lambda e, t0=t0, k=k: e.dma_start(out=fa[:, k * 2 * D + D:(k + 1) * 2 * D], in_=rs_out[t0:t0 + 128, :]), r=["rs_out"], w=[("fa", k, 1)])
                P.op("dve", lambda e, k=k: e.tensor_tensor(out=fa[:, k * 2 * D:k * 2 * D + D], in0=fa[:, k * 2 * D:k * 2 * D + D],
                                                          in1=fa[:, k * 2 * D + D:(k + 1) * 2 * D], op=ALU.add), r=[("fa", k, 0), ("fa", k, 1)], w=[("fa", k, 0)])
                P.dmaop("sp", lambda e, t0=t0, k=k: e.dma_start(out=out[t0:t0 + 128, :], in_=fa[:, k * 2 * D:k * 2 * D + D]), r=[("fa", k, 0)], w=["out"])
            P.flush()
        if debug == 6:
            DONE.append(1)
    if DONE:
        es.close()
        return nc

    if debug == 3:
        dbg = nc.dram_tensor("dbg", [H, 64, 256], F32, kind="ExternalOutput").ap()
        P.dmaop("sp", lambda e: e.dma_start(out=dbg, in_=attnT_s[:, :, 0:256]), w=["dbg"])
        P.flush()
        es.close()
        return nc

    if debug == 2:
        dbg = nc.dram_tensor("dbg", [H, QK, 512], BF16, kind="ExternalOutput").ap()
        dbg2 = nc.dram_tensor("dbg2", [512, 512], F32, kind="ExternalOutput").ap()
        dbg3 = nc.dram_tensor("dbg3", [H, QK, 256], BF16, kind="ExternalOutput").ap()
        P.dmaop("sp", lambda e: e.dma_start(out=dbg, in_=kT_s[:, :, 0:512]), w=["dbg"])
        P.dmaop("sp", lambda e: e.dma_start(out=dbg2, in_=u_s[0:512, :]), w=["dbg2"])
        P.dmaop("sp", lambda e: e.dma_start(out=dbg3, in_=qT_s[:, :, 0:256]), w=["dbg3"])
        P.flush()
        es.close()
        return nc

    if debug == 1:
        dbg = nc.dram_tensor("dbg", [128, 6 * D], F32, kind="ExternalOutput").ap()
        P.dmaop("sp", lambda e: e.dma_start(out=dbg, in_=modx[:]), r=["modx"], w=["dbg"])
        P.flush()
        es.close()
        return nc

    es.close()
    return nc


def _consts():
    ident = np.eye(128, dtype=np.float32)
    n = NX
    rows = n // 64
    row = np.repeat(np.arange(rows, dtype=np.float32), 64)
    col = np.tile(np.arange(64, dtype=np.float32), rows)
    inv = (10000.0 ** (-np.arange(8, dtype=np.float32) / 8)).astype(np.float32)
    ang = np.stack([row[:, None] * inv, col[:, None] * inv], axis=1).astype(np.float32)
    rope = np.zeros((NT, 32), np.float32)
    rope[:NCTX, :16] = 1.0
    rope[NCTX:, :16] = np.cos(ang).reshape(n, 16)
    rope[NCTX:, 16:] = np.sin(ang).reshape(n, 16)
    cm = np.zeros((128, 256), np.float32)
    for jp in range(8):
        for j in range(8):
            if jp <= j:
                cm[jp * 16:(jp + 1) * 16, j * 16:(j + 1) * 16] = 1.0
            if jp >= j:
                cm[jp * 16:(jp + 1) * 16, 128 + j * 16:128 + (j + 1) * 16] = 1.0
    return ident, rope, cm


def make_in_maps(inputs, nx=NX):
    ident, rope, cm = _consts()
    f = lambda a: np.ascontiguousarray(np.asarray(a, dtype=np.float32))
    maps = []
    dirk = ("ssm_lam_re", "ssm_lam_im", "ssm_log_dt", "ssm_b_re", "ssm_b_im", "ssm_c_re", "ssm_c_im")
    for b in range(NCORES // 2):
        for r in range(2):
            x_b = np.asarray(inputs["x"][b])[:nx]
            ctx_b = np.asarray(inputs["ctx"][b])
            rope_x = rope[NCTX:NCTX + nx]
            if r == 1:
                x_b, ctx_b, rope_x = x_b[::-1], ctx_b[::-1], rope_x[::-1]
            xc = np.zeros((NT, D), np.float32)
            xc[:NCTX] = ctx_b
            xc[NCTX:NCTX + nx] = x_b
            rope_r = rope.copy()
            rope_r[NCTX:NCTX + nx] = rope_x
            sel = np.zeros((NE, NE // 2), np.float32)
            sel[np.arange(NE // 2) + (NE // 2) * r, np.arange(NE // 2)] = 1.0
            m = {"xc": xc, "cb": f(inputs["c"][b]), "c_ctx": f(inputs["c_ctx"]), "ident": ident, "rope": rope_r, "cmask": cm,
                 "jrev": np.ascontiguousarray(ident[::-1]), "sel": sel}
            for k in ["w_ada", "b_ada", "norm1_g", "norm2_g", "w_in", "q_a_g", "w_qb", "kv_a_g", "w_kvb", "q_norm_g",
                      "k_norm_g", "w_mla_o", "ssm_d", "w_glu", "b_glu", "w_ssm_o", "w_out", "w_router"]:
                m[k] = f(np.asarray(inputs[k])[0])
            for k in dirk:
                a_ = np.asarray(inputs[k])[0]
                m[k] = f(a_[::-1] if r == 1 else a_)
            e0 = (NE // 2) * r
            for k in ("w_e_gate", "w_e_up", "w_e_down"):
                m[k] = f(np.asarray(inputs[k])[0][e0:e0 + NE // 2])
            maps.append(m)
    return maps


def assemble(results, nx=NX):
    h = nx // 2
    out = np.zeros((NCORES // 2, nx, D), np.float32)
    for b in range(NCORES // 2):
        out[b, :h] = np.asarray(results[2 * b]["out"], dtype=np.float32)[:h]
        out[b, h:] = np.asarray(results[2 * b + 1]["out"], dtype=np.float32)[:h][::-1]
    return out


def kernel(**inputs):
    nc = build()
    maps = make_in_maps(inputs)
    res = run_bass_kernel_spmd(nc, maps, core_ids=list(range(NCORES)))
    return assemble(res.results)
```

```python
import math
import os
from contextlib import ExitStack

import numpy as np
import concourse.bass as bass
import concourse.mybir as mybir
from concourse.bass_utils import run_bass_kernel_spmd

F32 = mybir.dt.float32
F32R = mybir.dt.float32r
BF16 = mybir.dt.bfloat16
U32 = mybir.dt.uint32
I32 = mybir.dt.int32
ALU = mybir.AluOpType
AF = mybir.ActivationFunctionType
AX = mybir.AxisListType

D = 1024
NX = 4096
NCTX = 256
NT = NX + NCTX
NTILE = NT // 128
NCH = NT // 8
NCH_C = NCTX // 8
H = 8
QK = 96
NE = 16
FF = 2816
CAP = 512
EPS = 1e-6
IN_COLS = 3232
NCORES = 8

ENG = {"pe": "tensor", "act": "scalar", "dve": "vector", "pool": "gpsimd", "sp": "sync"}


class Prog:
    def __init__(self, nc, sems, dsems):
        self.nc = nc
        self.ops = []
        self.lastw = {}
        self.readers = {}
        self.sems = sems
        self.dsems = dsems
        self.sig = {e: 0 for e in ENG}
        self.dcnt = {e: [0] * len(dsems[e]) for e in dsems}
        self.dnext = {e: 0 for e in dsems}
        self.seen = {e: {} for e in ENG}
        self.emitted = 0
        self.ccsem = None
        self.cccnt = 0

    def op(self, eng, fn, r=(), w=(), dma=False):
        i = len(self.ops)
        deps = set()
        for k in list(r) + list(w):
            if k in self.lastw:
                deps.add(self.lastw[k])
        for k in w:
            for j in self.readers.get(k, ()):
                deps.add(j)
        deps.discard(i)
        o = dict(eng=eng, fn=fn, deps=deps, dma=dma, need=False, val=None, sem=None, idx=i)
        self.ops.append(o)
        for k in w:
            self.lastw[k] = i
            self.readers[k] = []
        for k in r:
            if k not in w:
                self.readers.setdefault(k, []).append(i)
        return i

    def dmaop(self, eng, fn, r=(), w=()):
        return self.op(eng, fn, r, w, dma=True)

    def ccop(self, fn, r=(), w=()):
        return self.op("pool", fn, r, w, dma="cc")

    def flush(self, final_wait_eng="sp"):
        nc = self.nc
        ops = self.ops[self.emitted:]
        pos = {}
        cnt = {e: 0 for e in ENG}
        for o in self.ops[:self.emitted]:
            pass
        for o in ops:
            pos[o["idx"]] = cnt[o["eng"]]
            cnt[o["eng"]] += 1
        for o in ops:
            real = []
            for d in o["deps"]:
                if d < self.emitted:
                    continue
                p = self.ops[d]
                if not p["dma"] and p["eng"] == o["eng"]:
                    if o["eng"] == "pe":
                        continue
                    if o["dma"]:
                        continue
                real.append(d)
                p["need"] = True
            o["real"] = real
        for o in ops:
            if o["dma"]:
                for d in o["deps"]:
                    if d >= self.emitted:
                        p = self.ops[d]
                        if not p["dma"] and p["eng"] == o["eng"] and d not in o["real"]:
                            o["real"].append(d)
                            p["need"] = True
        for o in ops:
            if o["dma"] == "cc":
                o["prev"] = self.cccnt
                self.cccnt += 1
                o["sem"] = self.ccsem
                o["val"] = self.cccnt
            elif o["dma"]:
                e = o["eng"]
                k = self.dnext[e]
                self.dnext[e] = (k + 1) % len(self.dsems[e])
                o["prev"] = self.dcnt[e][k]
                self.dcnt[e][k] += 16
                o["sem"] = self.dsems[e][k]
                o["val"] = self.dcnt[e][k]
            elif o["need"]:
                self.sig[o["eng"]] += 1
                o["sem"] = self.sems[o["eng"]]
                o["val"] = self.sig[o["eng"]]
        byeng = {e: [o for o in ops if o["eng"] == e] for e in ENG}
        self_ = self
        seen = self.seen
        allops = self.ops
        dsems = self.dsems
        dcnt = self.dcnt

        def body(e):
            def f(engine):
                sn = seen[e]

                def wait(sem, val):
                    key = id(sem)
                    if sn.get(key, 0) >= val:
                        return
                    sn[key] = val
                    engine.wait_ge(sem, val)

                for o in byeng[e]:
                    for d in o["real"]:
                        p = allops[d]
                        wait(p["sem"], p["val"])
                    if o["dma"]:
                        if o["prev"] > 0:
                            wait(o["sem"], o["prev"])
                        ins = o["fn"](engine)
                        if o["dma"] == "cc":
                            ins.then_inc(o["sem"])
                        else:
                            ins.then_inc(o["sem"], 16)
                    else:
                        ins = o["fn"](engine)
                        if o["need"]:
                            ins.then_inc(o["sem"], 1)
                if e in dsems:
                    for k, s in enumerate(dsems[e]):
                        if dcnt[e][k] > 0:
                            wait(s, dcnt[e][k])
                if e == "pool" and self_.cccnt > 0:
                    wait(self_.ccsem, self_.cccnt)
            return f

        with nc.Block() as block:
            for e in ENG:
                getattr(block, ENG[e])(body(e))
        self.emitted = len(self.ops)
        self.lastw = {}
        self.readers = {}


class Keyed:
    def __init__(self, P, slot, shared, alias=None):
        self.P, self.slot, self.shared, self.alias = P, slot, set(shared), (alias or {})
        self.cap = []

    def _k(self, keys):
        out = []
        for k in keys:
            k = self.alias.get(k, k)
            base = k[0] if isinstance(k, tuple) else k
            out.append(k if base in self.shared else ("slot", self.slot, k))
        return out

    def op(self, eng, fn, r=(), w=(), dma=False):
        self.cap.append((eng, fn, self._k(r), self._k(w), dma))

    def dmaop(self, eng, fn, r=(), w=()):
        self.op(eng, fn, r, w, dma=True)


def interleave(P, caps, chunk=3):
    pos = [0] * len(caps)
    live = True
    while live:
        live = False
        for i, c in enumerate(caps):
            n = 0
            while pos[i] < len(c) and n < chunk:
                eng, fn, r, w, dma = c[pos[i]]
                P.op(eng, fn, r, w, dma=dma)
                pos[i] += 1
                n += 1
            if pos[i] < len(c):
                live = True


def r32(ap):
    return ap.bitcast(F32R)


def build(debug=0):
    nc = bass.Bass("TRN2", target_bir_lowering=False)
    es = ExitStack()
    DONE = []

    def din(name, shape, dt=F32):
        return nc.dram_tensor(name, list(shape), dt, kind="ExternalInput").ap()

    def dscr(name, shape, dt=F32):
        return nc.dram_tensor(name, list(shape), dt, kind="Internal").ap()

    xc = din("xc", [NT, D])
    cb = din("cb", [D])
    cctx = din("c_ctx", [D])
    w_ada = din("w_ada", [D, 6 * D])
    b_ada = din("b_ada", [6 * D])
    norm1_g = din("norm1_g", [D])
    norm2_g = din("norm2_g", [D])
    w_in = din("w_in", [D, IN_COLS])
    q_a_g = din("q_a_g", [384])
    w_qb = din("w_qb", [384, 768])
    kv_a_g = din("kv_a_g", [256])
    w_kvb = din("w_kvb", [256, 1024])
    q_norm_g = din("q_norm_g", [96])
    k_norm_g = din("k_norm_g", [96])
    w_mla_o = din("w_mla_o", [512, D])
    lam_re = din("ssm_lam_re", [2, 32, 64])
    lam_im = din("ssm_lam_im", [2, 32, 64])
    log_dt = din("ssm_log_dt", [2, 32])
    b_re = din("ssm_b_re", [2, 32, 64, 16])
    b_im = din("ssm_b_im", [2, 32, 64, 16])
    c_re = din("ssm_c_re", [2, 32, 16, 64])
    c_im = din("ssm_c_im", [2, 32, 16, 64])
    ssm_d = din("ssm_d", [512])
    w_glu = din("w_glu", [512, 512])
    b_glu = din("b_glu", [512])
    w_ssm_o = din("w_ssm_o", [512, D])
    w_out = din("w_out", [D, D])
    w_router = din("w_router", [D, NE])
    if debug in (0, 6):
        w_e_gate = din("w_e_gate", [NE // 2, D, FF])
        w_e_up = din("w_e_up", [NE // 2, D, FF])
        w_e_down = din("w_e_down", [NE // 2, FF, D])
    sel_d = din("sel", [NE, NE // 2])
    ident_d = din("ident", [128, 128])
    rope_d = din("rope", [NT, 32])
    jrev_d = din("jrev", [128, 128])
    cmask_d = din("cmask", [128, 256])
    small = debug in (2, 3, 4, 5, 6)
    NTe = 4 if small else NTILE
    NXe = (NTe - 2) * 128
    NXO = NXe // 2
    NTO = NXO // 128
    out = nc.dram_tensor("out", [NXO, D], F32, kind="ExternalOutput").ap()

    u_s = dscr("u_s", [NT, 512])
    gates_s = dscr("gates_s", [NXO, 2048])
    qT_s = dscr("qT_s", [H, QK, NXO], BF16)
    kT_s = dscr("kT_s", [H, QK, NT], BF16)
    v_s = dscr("v_s", [NT, 512], BF16)
    attnT_s = dscr("attnT_s", [H, 64, NXO])
    y_s = dscr("y_s", [NXO // 8, 8 * 512], BF16)
    xm_s = dscr("xm_s", [NXO, D])
    NCHK = 2 if NXO >= 2048 else 1
    RCH = NXO // NCHK
    h2b_own_c = [nc.dram_tensor("h2b_own%d" % c, [RCH, D], BF16) for c in range(NCHK)]
    h2b_ag_c = [nc.dram_tensor("h2b_ag%d" % c, [2 * RCH, D], BF16) for c in range(NCHK)]
    h2b_all_t = nc.dram_tensor("h2b_all", [2 * NXO, D], BF16)
    aff_own_t = nc.dram_tensor("aff_own", [NE, NXO], F32)
    aff_all_t = nc.dram_tensor("aff_all", [2 * NE, NXO], F32)
    acc_t = nc.dram_tensor("acc", [2 * NXO, D], F32)
    rs_out_t = nc.dram_tensor("rs_out", [NXO, D], F32)
    h2b_all, aff_own, aff_all, acc, rs_out = (t.ap() for t in (h2b_all_t, aff_own_t, aff_all_t, acc_t, rs_out_t))
    PAIRS = [[0, 1], [2, 3], [4, 5], [6, 7]]
    h2_s = dscr("h2_s", [NX, D])

    sems = {e: es.enter_context(nc.semaphore("s_" + e)) for e in ENG}
    dsems = {e: [es.enter_context(nc.semaphore("d_%s%d" % (e, k))) for k in range(8)]
             for e in ("sp", "act", "pool")}
    P = Prog(nc, sems, dsems)
    P.ccsem = es.enter_context(nc.semaphore("cc_sem"))

    def sb(stack, name, shape, dt=F32):
        return stack.enter_context(nc.sbuf_tensor("t_" + name, list(shape), dt))

    def ps(stack, name, shape=(128, 512), dt=F32):
        return stack.enter_context(nc.psum_tensor("p_" + name, list(shape), dt))

    ident = sb(es, "ident", [128, 128])
    modx = sb(es, "modx", [128, 6 * D])
    es1 = ExitStack()
    modc = sb(es1, "modc", [128, 2 * D])
    P.dmaop("sp", lambda e: e.dma_start(out=ident[:], in_=ident_d), w=["ident"])

    with ExitStack() as st:
        cT = sb(st, "cT", [128, 2, 8])
        sc = sb(st, "sc", [128, 2, 8])
        lbc = sb(st, "lbc", [128, 2, 8, 128], BF16)
        wa = sb(st, "wa", [128, 2, 8, 512], BF16)
        bb = sb(st, "bb", [128, 6 * D])
        g1b = sb(st, "g1b", [128, D])
        g2b = sb(st, "g2b", [128, D])
        pm = [ps(st, "pm%d" % i) for i in range(4)]
        P.dmaop("sp", lambda e: e.dma_start(out=cT[:, 0, :], in_=cb.rearrange("(dc p) -> p dc", p=128),
                                            allow_slow_non_contiguous=True), w=["cT0"])
        P.dmaop("sp", lambda e: e.dma_start(out=cT[:, 1, :], in_=cctx.rearrange("(dc p) -> p dc", p=128),
                                            allow_slow_non_contiguous=True), w=["cT1"])
        P.dmaop("act", lambda e: e.dma_start(out=bb[:], in_=b_ada.partition_broadcast(128)), w=["bb"])
        P.dmaop("act", lambda e: e.dma_start(out=g1b[:], in_=norm1_g.partition_broadcast(128)), w=["g1b"])
        P.dmaop("act", lambda e: e.dma_start(out=g2b[:], in_=norm2_g.partition_broadcast(128)), w=["g2b"])
        P.op("act", lambda e: e.activation(out=sc[:], in_=cT[:], func=AF.Silu), r=["cT0", "cT1"], w=["sc"])
        P.op("dve", lambda e: e.tensor_copy(out=lbc[:], in_=sc[:].unsqueeze(3).to_broadcast([128, 2, 8, 128])),
             r=["sc"], w=["lbc"])
        wv = w_ada.rearrange("(dc p) n -> p dc n", p=128)
        for ct in range(12):
            s = ct % 2
            P.dmaop("pool",
                    lambda e, ct=ct, s=s: e.dma_start(out=wa[:, s], in_=wv[:, :, ct * 512:(ct + 1) * 512]),
                    w=[("wa", s)])
            for which in range(2 if ct < 4 else 1):
                pt = pm[(ct * 2 + which) % 4]
                pk = ("pm", (ct * 2 + which) % 4)

                def mm(e, pt=pt, s=s, which=which):
                    ins = None
                    for dc in range(8):
                        ins = e.matmul(pt[:], lhsT=lbc[:, which, dc, :], rhs=wa[:, s, dc, :],
                                       start=(dc == 0), stop=(dc == 7))
                    return ins
                P.op("pe", mm, r=["lbc", ("wa", s)], w=[pk])
                dst = modx if which == 0 else modc
                P.op("dve", lambda e, pt=pt, dst=dst, ct=ct: e.tensor_tensor(
                    out=dst[:, ct * 512:(ct + 1) * 512], in0=pt[:], in1=bb[:, ct * 512:(ct + 1) * 512], op=ALU.add),
                    r=[pk, "bb"], w=["modx" if which == 0 else "modc"])
        P.op("dve", lambda e: e.scalar_tensor_tensor(out=modx[:, D:2 * D], in0=modx[:, D:2 * D], scalar=1.0, in1=g1b[:],
                                                     op0=ALU.add, op1=ALU.mult), r=["modx", "g1b"], w=["modx"])
        P.op("dve", lambda e: e.scalar_tensor_tensor(out=modx[:, 4 * D:5 * D], in0=modx[:, 4 * D:5 * D], scalar=1.0, in1=g2b[:],
                                                     op0=ALU.add, op1=ALU.mult), r=["modx", "g2b"], w=["modx"])
        P.op("dve", lambda e: e.scalar_tensor_tensor(out=modc[:, D:2 * D], in0=modc[:, D:2 * D], scalar=1.0, in1=g1b[:],
                                                     op0=ALU.add, op1=ALU.mult), r=["modc", "g1b"], w=["modc"])
        P.flush()


    with ExitStack() as st:
      if debug != 1:
          TL1 = [dict(), dict()]
          w_in_sb = sb(st, "w_in_sb", [128, 8, IN_COLS], BF16)
          w_qb_sb = sb(st, "w_qb_sb", [128, 3, 768], BF16)
          w_kvb_sb = sb(st, "w_kvb_sb", [128, 2, 1024], BF16)
          qag = sb(st, "qag", [128, 384])
          kvag = sb(st, "kvag", [128, 256])
          qng = sb(st, "qng", [128, 96])
          kng = sb(st, "kng", [128, 96])
          identb = sb(st, "identb", [128, 128], BF16)
          xt = sb(st, "xt", [128, 2, D])
          TL1[0]['junk'] = sb(st, "junk_0", [128, D]); TL1[1]['junk'] = sb(st, "junk_1", [128, D])
          TL1[0]['hh'] = sb(st, "hh_0", [128, D]); TL1[1]['hh'] = sb(st, "hh_1", [128, D])
          TL1[0]['hb'] = sb(st, "hb_0", [128, D], BF16); TL1[1]['hb'] = sb(st, "hb_1", [128, D], BF16)
          TL1[0]['hT'] = sb(st, "hT_0", [128, 8, 128], BF16); TL1[1]['hT'] = sb(st, "hT_1", [128, 8, 128], BF16)
          proj = sb(st, "proj", [128, 2, IN_COLS])
          st8 = sb(st, "st8", [128, 2, 64])
          TL1[0]['qn'] = sb(st, "qn_0", [128, 384], BF16); TL1[1]['qn'] = sb(st, "qn_1", [128, 384], BF16)
          TL1[0]['qnT'] = sb(st, "qnT_0", [128, 3, 128], BF16); TL1[1]['qnT'] = sb(st, "qnT_1", [128, 3, 128], BF16)
          TL1[0]['kvn'] = sb(st, "kvn_0", [128, 256], BF16); TL1[1]['kvn'] = sb(st, "kvn_1", [128, 256], BF16)
          TL1[0]['kvnT'] = sb(st, "kvnT_0", [128, 2, 128], BF16); TL1[1]['kvnT'] = sb(st, "kvnT_1", [128, 2, 128], BF16)
          TL1[0]['qsq'] = sb(st, "qsq_0", [128, 768]); TL1[1]['qsq'] = sb(st, "qsq_1", [128, 768])
          TL1[0]['qf'] = sb(st, "qf_0", [128, 8, 96]); TL1[1]['qf'] = sb(st, "qf_1", [128, 8, 96])
          TL1[0]['qb'] = sb(st, "qb_0", [128, 8, 96], BF16); TL1[1]['qb'] = sb(st, "qb_1", [128, 8, 96], BF16)
          TL1[0]['kf'] = sb(st, "kf_0", [128, 8, 96]); TL1[1]['kf'] = sb(st, "kf_1", [128, 8, 96])
          TL1[0]['kb'] = sb(st, "kb_0", [128, 8, 96], BF16); TL1[1]['kb'] = sb(st, "kb_1", [128, 8, 96], BF16)
          vb = sb(st, "vb", [128, 2, 512], BF16)
          TL1[0]['kvs'] = sb(st, "kvs_0", [128, 1024]); TL1[1]['kvs'] = sb(st, "kvs_1", [128, 1024])
          rp = sb(st, "rp", [128, 2, 32])
          TL1[0]['rt'] = sb(st, "rt_0", [128, 6, 128]); TL1[1]['rt'] = sb(st, "rt_1", [128, 6, 128])
          TL1[0]['krg'] = sb(st, "krg_0", [128, 32]); TL1[1]['krg'] = sb(st, "krg_1", [128, 32])
          TL1[0]['krr'] = sb(st, "krr_0", [128, 32]); TL1[1]['krr'] = sb(st, "krr_1", [128, 32])
          TL1[0]['qTt'] = sb(st, "qTt_0", [128, 8, 128], BF16); TL1[1]['qTt'] = sb(st, "qTt_1", [128, 8, 128], BF16)
          TL1[0]['kTt'] = sb(st, "kTt_0", [128, 8, 128], BF16); TL1[1]['kTt'] = sb(st, "kTt_1", [128, 8, 128], BF16)
          PSALL = [ps(st, "ph1_%d" % i) for i in range(8)]

          P.dmaop("pool", lambda e: e.dma_start(out=w_in_sb[:], in_=w_in.rearrange("(dc p) n -> p dc n", p=128)), w=["w_in_sb"])
          P.dmaop("pool", lambda e: e.dma_start(out=w_qb_sb[:], in_=w_qb.rearrange("(dc p) n -> p dc n", p=128)), w=["w_qb_sb"])
          P.dmaop("pool", lambda e: e.dma_start(out=w_kvb_sb[:], in_=w_kvb.rearrange("(dc p) n -> p dc n", p=128)), w=["w_kvb_sb"])
          P.dmaop("act", lambda e: e.dma_start(out=qag[:], in_=q_a_g.partition_broadcast(128)), w=["qag"])
          P.dmaop("act", lambda e: e.dma_start(out=kvag[:], in_=kv_a_g.partition_broadcast(128)), w=["kvag"])
          P.dmaop("act", lambda e: e.dma_start(out=qng[:], in_=q_norm_g.partition_broadcast(128)), w=["qng"])
          P.dmaop("act", lambda e: e.dma_start(out=kng[:], in_=k_norm_g.partition_broadcast(128)), w=["kng"])
          P.op("dve", lambda e: e.tensor_scalar(out=qng[:], in0=qng[:], scalar1=float(QK ** -0.5), scalar2=None, op0=ALU.mult),
               r=["qng"], w=["qng"])
          P.op("dve", lambda e: e.tensor_copy(out=identb[:], in_=ident[:]), r=["ident"], w=["identb"])

          def rstd(Pq, src_key, src_ap, n, dst_ap, dst_key):
              Pq.op("dve", lambda e: e.tensor_scalar(out=dst_ap, in0=src_ap, scalar1=1.0 / n, scalar2=EPS, op0=ALU.mult, op1=ALU.add),
                   r=[src_key], w=[dst_key])
              Pq.op("act", lambda e: e.activation(out=dst_ap, in_=dst_ap, func=AF.Sqrt), r=[dst_key], w=[dst_key])
              Pq.op("dve", lambda e: e.reciprocal(out=dst_ap, in_=dst_ap), r=[dst_key], w=[dst_key])

          def rope(Pq, rt, src, dst, tab, nh, tag, eng="pool"):
              sv = src.rearrange("p h (a t) -> p h a t", a=2)
              dv = dst.rearrange("p h (a t) -> p h a t", a=2)
              cosb = tab[:, 0:16].rearrange("p (a t) -> p a t", a=2).unsqueeze(1).to_broadcast([128, nh, 2, 8])
              sinb = tab[:, 16:32].rearrange("p (a t) -> p a t", a=2).unsqueeze(1).to_broadcast([128, nh, 2, 8])
              v0 = sv[:, :, :, 0:8]
              v1 = sv[:, :, :, 8:16]
              n = nh * 16
              T = [rt[:, k, 0:n].rearrange("p (h a t) -> p h a t", h=nh, a=2) for k in range(4)]
              rk = ("rt", tag)
              Pq.op(eng, lambda e: e.tensor_tensor(out=T[0], in0=v0, in1=cosb, op=ALU.mult), r=[tag + "_src", tag + "_tab"], w=[rk + (0,)])
              Pq.op(eng, lambda e: e.tensor_tensor(out=T[1], in0=v1, in1=sinb, op=ALU.mult), r=[tag + "_src", tag + "_tab"], w=[rk + (1,)])
              Pq.op(eng, lambda e: e.tensor_tensor(out=T[2], in0=v1, in1=cosb, op=ALU.mult), r=[tag + "_src", tag + "_tab"], w=[rk + (2,)])
              Pq.op(eng, lambda e: e.tensor_tensor(out=T[3], in0=v0, in1=sinb, op=ALU.mult), r=[tag + "_src", tag + "_tab"], w=[rk + (3,)])
              Pq.op(eng, lambda e: e.tensor_tensor(out=dv[:, :, :, 0:8], in0=T[0], in1=T[1], op=ALU.subtract),
                   r=[rk + (0,), rk + (1,)], w=[tag + "_dst"])
              Pq.op(eng, lambda e: e.tensor_tensor(out=dv[:, :, :, 8:16], in0=T[2], in1=T[3], op=ALU.add),
                   r=[rk + (2,), rk + (3,)], w=[tag + "_dst"])

          ntile1 = NTe
          import os
          STG = int(os.environ.get('PH1_STAGE', '9'))
          SHARED1 = ["w_in_sb", "w_qb_sb", "w_kvb_sb", "qag", "kvag", "qng", "kng", "identb", "ident", "modx", "modc", "u_s", "gates_s", "qT_s", "kT_s", "v_s"]
          ALIAS1 = {("ps", 1): ("ps", 0), "ps3": "ps2", ("ps", 6): ("ps", 4), ("ps", 7): ("ps", 5)}

          def tile1(i, s):
              Pq = Keyed(P, s, SHARED1, ALIAS1)
              junk = TL1[s]['junk']
              hh = TL1[s]['hh']
              hb = TL1[s]['hb']
              hT = TL1[s]['hT']
              qn = TL1[s]['qn']
              qnT = TL1[s]['qnT']
              kvn = TL1[s]['kvn']
              kvnT = TL1[s]['kvnT']
              qsq = TL1[s]['qsq']
              qf = TL1[s]['qf']
              qb = TL1[s]['qb']
              kf = TL1[s]['kf']
              kb = TL1[s]['kb']
              kvs = TL1[s]['kvs']
              rt = TL1[s]['rt']
              krg = TL1[s]['krg']
              krr = TL1[s]['krr']
              qTt = TL1[s]['qTt']
              kTt = TL1[s]['kTt']
              bk = PSALL[4 * s:4 * s + 4]
              PS = [bk[0], bk[0], bk[1], bk[1], bk[2], bk[3], bk[2], bk[3]]
              PSb2 = PS[2][:].bitcast(BF16)
              PSb3 = PS[3][:].bitcast(BF16)
              isx = i >= 2
              own = 2 <= i < 2 + NTO
              t0 = i * 128
              xq = t0 - NCTX
              G = (modx if isx else modc)[:, D:2 * D]
              SH = (modx if isx else modc)[:, 0:D]
              mk = "modx" if isx else "modc"
              Pq.dmaop("sp", lambda e, s=s, t0=t0: e.dma_start(out=xt[:, s, :], in_=xc[t0:t0 + 128, :]), w=[("xt", s)])
              Pq.dmaop("sp", lambda e, s=s, t0=t0: e.dma_start(out=rp[:, s, :], in_=rope_d[t0:t0 + 128, :]), w=[("rp", s)])
              Pq.op("act", lambda e, s=s: e.activation(out=junk[:], in_=xt[:, s, :], func=AF.Square, accum_out=st8[:, s, 0:1]),
                   r=[("xt", s)], w=["junk", ("st", s, 0)])
              rstd(Pq, ("st", s, 0), st8[:, s, 0:1], D, st8[:, s, 1:2], ("st", s, 1))
              Pq.op("dve", lambda e, s=s, G=G: e.scalar_tensor_tensor(out=hh[:], in0=xt[:, s, :], scalar=st8[:, s, 1:2], in1=G,
                                                                  op0=ALU.mult, op1=ALU.mult),
                   r=[("xt", s), ("st", s, 1), mk], w=["hh"])
              Pq.op("pool", lambda e, SH=SH: e.tensor_tensor(out=hb[:], in0=hh[:], in1=SH, op=ALU.add), r=["hh", mk], w=["hb"])

              def tr_h(e):
                  ins = None
                  for dc in range(8):
                      ins = e.transpose(out=PSb2[:, dc * 128:(dc + 1) * 128], in_=hb[:, dc * 128:(dc + 1) * 128], identity=identb[:])
                  return ins
              Pq.op("pe", tr_h, r=["hb", "identb"], w=["ps2"])
              Pq.op("act", lambda e: e.copy(out=hT[:].rearrange("p a b -> p (a b)"), in_=PSb2[:, 0:1024]), r=["ps2"], w=["hT"])
              segs = ([(c, c * 512, min(512, IN_COLS - c * 512), [c]) for c in range(7)] if own
                      else [(0, 0, 512, [0]), (1, 896, 288, [1, 2])])
              for (ctile, c0, n, wkeys) in segs:
                  pk = ctile % 2

                  def mmin(e, c0=c0, n=n, pk=pk):
                      ins = None
                      for dc in range(8):
                          ins = e.matmul(PS[pk][:, 0:n], lhsT=hT[:, dc, :], rhs=w_in_sb[:, dc, c0:c0 + n], start=(dc == 0), stop=(dc == 7))
                      return ins
                  Pq.op("pe", mmin, r=["hT", "w_in_sb"], w=[("ps", pk)])
                  if ctile % 2 == 0:
                      Pq.op("dve", lambda e, c0=c0, n=n, pk=pk, s=s: e.tensor_copy(out=proj[:, s, c0:c0 + n], in_=PS[pk][:, 0:n]),
                           r=[("ps", pk)], w=[("proj", s, c_) for c_ in wkeys])
                  else:
                      Pq.op("act", lambda e, c0=c0, n=n, pk=pk, s=s: e.copy(out=proj[:, s, c0:c0 + n], in_=PS[pk][:, 0:n]),
                           r=[("ps", pk)], w=[("proj", s, c_) for c_ in wkeys])
              pj = [("proj", s, c) for c in range(7)]
              Pq.dmaop("sp", lambda e, s=s, t0=t0: e.dma_start(out=u_s[t0:t0 + 128, :], in_=proj[:, s, 0:512]), r=[pj[0]], w=["u_s"])
              if own:
                  Pq.op("act", lambda e, s=s: e.activation(out=proj[:, s, 1184:3232], in_=proj[:, s, 1184:3232], func=AF.Sigmoid),
                        r=pj[2:], w=pj[2:])
                  Pq.dmaop("sp", lambda e, xq=xq, s=s: e.dma_start(out=gates_s[xq:xq + 128, :], in_=proj[:, s, 1184:3232]), r=pj[2:], w=["gates_s"])
                  Pq.op("act", lambda e, s=s: e.activation(out=junk[:, 0:384], in_=proj[:, s, 512:896], func=AF.Square,
                                                          accum_out=st8[:, s, 2:3]), r=[pj[1]], w=["junk", ("st", s, 2)])
                  rstd(Pq, ("st", s, 2), st8[:, s, 2:3], 384, st8[:, s, 3:4], ("st", s, 3))
                  Pq.op("dve", lambda e, s=s: e.scalar_tensor_tensor(out=qn[:], in0=proj[:, s, 512:896], scalar=st8[:, s, 3:4], in1=qag[:],
                                                                    op0=ALU.mult, op1=ALU.mult), r=[pj[1], ("st", s, 3), "qag"], w=["qn"])

                  def tr_q(e):
                      ins = None
                      for k in range(3):
                          ins = e.transpose(out=PSb2[:, k * 128:(k + 1) * 128], in_=qn[:, k * 128:(k + 1) * 128], identity=identb[:])
                      return ins
                  Pq.op("pe", tr_q, r=["qn", "identb"], w=["ps2"])
                  Pq.op("dve", lambda e: e.tensor_copy(out=qnT[:].rearrange("p a b -> p (a b)"), in_=PSb2[:, 0:384]), r=["ps2"], w=["qnT"])

                  def mmq(e):
                      ins = None
                      for (pi, c0, n) in ((4, 0, 512), (5, 512, 256)):
                          for k in range(3):
                              ins = e.matmul(PS[pi][:, 0:n], lhsT=qnT[:, k, :], rhs=w_qb_sb[:, k, c0:c0 + n], start=(k == 0), stop=(k == 2))
                      return ins
                  Pq.op("pe", mmq, r=["qnT", "w_qb_sb"], w=[("ps", 4), ("ps", 5)])
                  qfl = qf[:].rearrange("p h d -> p (h d)")
                  Pq.op("act", lambda e: e.copy(out=qfl[:, 0:512], in_=PS[4][:, 0:512]), r=[("ps", 4)], w=["qf"])
                  Pq.op("act", lambda e: e.copy(out=qfl[:, 512:768], in_=PS[5][:, 0:256]), r=[("ps", 5)], w=["qf"])
                  Pq.op("pool", lambda e: e.tensor_tensor(out=qsq[:], in0=qfl, in1=qfl, op=ALU.mult), r=["qf"], w=["qsq"])
                  Pq.op("dve", lambda e, s=s: e.tensor_reduce(out=st8[:, s, 8:16], in_=qsq[:].rearrange("p (h d) -> p h d", h=8),
                                                             axis=AX.X, op=ALU.add), r=["qsq"], w=[("st", s, 8)])
                  rstd(Pq, ("st", s, 8), st8[:, s, 8:16], QK, st8[:, s, 16:24], ("st", s, 16))
                  Pq.op("dve", lambda e, s=s: e.tensor_tensor(out=qf[:], in0=qf[:], in1=st8[:, s, 16:24].unsqueeze(2).to_broadcast([128, 8, 96]),
                                                             op=ALU.mult), r=["qf", ("st", s, 16)], w=["qf"])
                  Pq.op("dve", lambda e: e.tensor_tensor(out=qf[:], in0=qf[:], in1=qng[:].unsqueeze(1).to_broadcast([128, 8, 96]),
                                                        op=ALU.mult), r=["qf", "qng"], w=["qf", "q_src"])
                  Pq.op("act", lambda e: e.copy(out=qb[:, :, 0:64], in_=qf[:, :, 0:64]), r=["qf"], w=["qb"])
                  Pq.op("pool", lambda e, s=s: e.tensor_copy(out=rt[:, 5, 0:32], in_=rp[:, s, :]), r=[("rp", s)], w=["q_tab"])
                  rope(Pq, rt, qf[:, :, 64:96], qb[:, :, 64:96], rt[:, 5, 0:32], 8, "q")

                  def tr_qh(e):
                      ins = None
                      for h in range(8):
                          ins = e.transpose(out=PSb3[0:96, h * 128:(h + 1) * 128], in_=qb[:, h, :], identity=identb[:])
                      return ins
                  Pq.op("pe", tr_qh, r=["qb", "q_dst", "identb"], w=["ps3"])
                  Pq.op("act", lambda e: e.copy(out=qTt[0:96].rearrange("p a b -> p (a b)"), in_=PSb3[0:96, 0:1024]), r=["ps3"], w=["qTt"])
                  Pq.dmaop("act", lambda e, xq=xq: e.dma_start(out=qT_s[:, :, xq:xq + 128].rearrange("h d t -> d h t"), in_=qTt[0:96]),
                          r=["qTt"], w=["qT_s"])
              Pq.op("act", lambda e, s=s: e.activation(out=junk[:, 0:256], in_=proj[:, s, 896:1152], func=AF.Square,
                                                      accum_out=st8[:, s, 4:5]), r=[pj[1], pj[2]], w=["junk", ("st", s, 4)])
              rstd(Pq, ("st", s, 4), st8[:, s, 4:5], 256, st8[:, s, 5:6], ("st", s, 5))
              Pq.op("dve", lambda e, s=s: e.scalar_tensor_tensor(out=kvn[:], in0=proj[:, s, 896:1152], scalar=st8[:, s, 5:6], in1=kvag[:],
                                                                op0=ALU.mult, op1=ALU.mult), r=[pj[1], pj[2], ("st", s, 5), "kvag"], w=["kvn"])

              def tr_kv(e):
                  ins = None
                  for k in range(2):
                      ins = e.transpose(out=PSb2[:, k * 128:(k + 1) * 128], in_=kvn[:, k * 128:(k + 1) * 128], identity=identb[:])
                  return ins
              Pq.op("pe", tr_kv, r=["kvn", "identb"], w=["ps2"])
              Pq.op("dve", lambda e: e.tensor_copy(out=kvnT[:].rearrange("p a b -> p (a b)"), in_=PSb2[:, 0:256]), r=["ps2"], w=["kvnT"])

              def mmkv(e):
                  ins = None
                  for (pi, c0) in ((6, 0), (7, 512)):
                      for k in range(2):
                          ins = e.matmul(PS[pi][:, 0:512], lhsT=kvnT[:, k, :], rhs=w_kvb_sb[:, k, c0:c0 + 512], start=(k == 0), stop=(k == 1))
                  return ins
              Pq.op("pe", mmkv, r=["kvnT", "w_kvb_sb"], w=[("ps", 6), ("ps", 7)])
              for half in range(2):
                  (Pq.op("act", lambda e, half=half: e.copy(out=kvs[:, half * 512:(half + 1) * 512], in_=PS[6 + half][:, 0:512]),
                        r=[("ps", 6 + half)], w=[("kvs", half)]) if half == 0 else
                   Pq.op("dve", lambda e, half=half: e.tensor_copy(out=kvs[:, half * 512:(half + 1) * 512], in_=PS[6 + half][:, 0:512]),
                        r=[("ps", 6 + half)], w=[("kvs", half)]))
              kvs3 = kvs[:].rearrange("p (h d) -> p h d", h=8)
              kvk = [("kvs", 0), ("kvs", 1)]
              Pq.op("pool", lambda e, s=s: e.tensor_copy(out=vb[:, s].rearrange("p (h d) -> p h d", h=8), in_=kvs3[:, :, 64:128]),
                   r=kvk, w=[("vb", s)])
              Pq.dmaop("sp", lambda e, s=s, t0=t0: e.dma_start(out=v_s[t0:t0 + 128, :], in_=vb[:, s]),
                      r=[("vb", s)], w=["v_s"])
              Pq.op("act", lambda e: e.copy(out=kf[:, :, 0:64], in_=kvs3[:, :, 0:64]), r=kvk, w=[("kf", 0), ("kf", 1)])
              kfk = [("kf", 0), ("kf", 1)]
              Pq.op("pool", lambda e: e.tensor_tensor(out=qsq[:, 0:512].rearrange("p (h d) -> p h d", h=8), in0=kf[:, :, 0:64], in1=kf[:, :, 0:64],
                                                     op=ALU.mult), r=kfk, w=["qsq"])
              Pq.op("dve", lambda e, s=s: e.tensor_reduce(out=st8[:, s, 24:32], in_=qsq[:, 0:512].rearrange("p (h d) -> p h d", h=8),
                                                         axis=AX.X, op=ALU.add), r=["qsq"], w=[("st", s, 24)])
              Pq.op("act", lambda e, s=s: e.activation(out=junk[:, 0:32], in_=proj[:, s, 1152:1184], func=AF.Square,
                                                      accum_out=st8[:, s, 6:7]), r=[pj[2]], w=["junk", ("st", s, 6)])
              Pq.op("dve", lambda e, s=s: e.tensor_scalar(out=st8[:, s, 24:32], in0=st8[:, s, 24:32], scalar1=st8[:, s, 6:7], scalar2=None,
                                                         op0=ALU.add), r=[("st", s, 24), ("st", s, 6)], w=[("st", s, 24)])
              rstd(Pq, ("st", s, 24), st8[:, s, 24:32], QK, st8[:, s, 32:40], ("st", s, 32))
              Pq.op("dve", lambda e, s=s: e.tensor_tensor(out=kf[:, :, 0:64], in0=kf[:, :, 0:64],
                                                         in1=st8[:, s, 32:40].unsqueeze(2).to_broadcast([128, 8, 64]), op=ALU.mult),
                   r=kfk + [("st", s, 32)], w=kfk)
              Pq.op("dve", lambda e: e.tensor_tensor(out=kb[:, :, 0:64], in0=kf[:, :, 0:64],
                                                    in1=kng[:, 0:64].unsqueeze(1).to_broadcast([128, 8, 64]), op=ALU.mult),
                   r=kfk + ["kng"], w=["kb"])
              Pq.op("pool", lambda e, s=s: e.tensor_tensor(out=krg[:], in0=proj[:, s, 1152:1184], in1=kng[:, 64:96], op=ALU.mult),
                   r=[pj[2], "kng"], w=["krg", "k_src"])
              Pq.op("pool", lambda e, s=s: e.tensor_copy(out=rt[:, 4, 0:32], in_=rp[:, s, :]), r=[("rp", s)], w=["k_tab"])
              rope(Pq, rt, krg[:].unsqueeze(1), krr[:].unsqueeze(1), rt[:, 4, 0:32], 1, "k")
              Pq.op("dve", lambda e, s=s: e.tensor_tensor(out=kb[:, :, 64:96], in0=krr[:].unsqueeze(1).to_broadcast([128, 8, 32]),
                                                         in1=st8[:, s, 32:40].unsqueeze(2).to_broadcast([128, 8, 32]), op=ALU.mult),
                   r=["k_dst", ("st", s, 32)], w=["kb"])

              def tr_kh(e):
                  ins = None
                  for h in range(8):
                      ins = e.transpose(out=PSb3[0:96, h * 128:(h + 1) * 128], in_=kb[:, h, :], identity=identb[:])
                  return ins
              Pq.op("pe", tr_kh, r=["kb", "identb"], w=["ps3"])
              Pq.op("dve", lambda e: e.tensor_copy(out=kTt[0:96].rearrange("p a b -> p (a b)"), in_=PSb3[0:96, 0:1024]), r=["ps3"], w=["kTt"])
              Pq.dmaop("act", lambda e, t0=t0: e.dma_start(out=kT_s[:, :, t0:t0 + 128].rearrange("h d t -> d h t"), in_=kTt[0:96]),
                      r=["kTt"], w=["kT_s"])
              return Pq.cap
          for i in range(0, ntile1, 2):
              caps = [tile1(i, 0)] + ([tile1(i + 1, 1)] if i + 1 < ntile1 else [])
              interleave(P, caps, chunk=int(os.environ.get("ILV", "2")))
          P.flush()

    QG = min(512, NXO)
    NQG = NXO // QG

    es1.close()
    NCX = NXe // 8
    NCC = NCTX // 8
    NCHe = NCX + NCC
    HW = NCHe + 2
    if debug in (0, 4, 5, 6):
      with ExitStack() as st:
        BendT = sb(st, "BendT", [128, 2, 32, 2, 64], BF16)
        Dm = sb(st, "Dm", [128, 2, 16, 2, 128], BF16)
        Tloc = sb(st, "Tloc", [128, 32, 128], BF16)
        mu3 = sb(st, "mu3", [128, 2, 2, 16, 2])
        mu16 = sb(st, "mu16", [128, 2, 17, 2, 16, 2])
        PS2 = [ps(st, "ph2_%d" % i) for i in range(8)]
        with ExitStack() as tt:
            lre = sb(tt, "lre", [128, 32]); lim = sb(tt, "lim", [128, 32]); ldt = sb(tt, "ldt", [128, 32])
            bre = sb(tt, "bre", [128, 32, 16]); bim = sb(tt, "bim", [128, 32, 16])
            cre = sb(tt, "cre", [128, 32, 16]); cim = sb(tt, "cim", [128, 32, 16])
            dsk = sb(tt, "dsk", [128, 32])
            cmk = sb(tt, "cmk", [128, 256])
            tm = sb(tt, "tm", [128, 12, 32])
            ti = sb(tt, "ti", [128, 32], I32)
            pwr = sb(tt, "pwr", [128, 9, 32]); pwi = sb(tt, "pwi", [128, 9, 32])
            nwr = sb(tt, "nwr", [128, 9, 32]); nwi = sb(tt, "nwi", [128, 9, 32])
            Bbr = sb(tt, "Bbr", [128, 32, 16]); Bbi = sb(tt, "Bbi", [128, 32, 16])
            MX = [sb(tt, "MX%d" % i, [128, 16, 8, 16]) for i in range(4)]
            MT = [sb(tt, "MT%d" % i, [128, 16, 8, 16]) for i in range(4)]
            TL = sb(tt, "TL", [128, 2, 128])
            for gh in range(2):
                rows = slice(64 * gh, 64 * gh + 64)
                gs = slice(16 * gh, 16 * gh + 16)
                for d in range(2):
                    for (dst, src, nm) in ((lre, lam_re, "lre"), (lim, lam_im, "lim")):
                        P.dmaop("sp", lambda e, dst=dst, src=src, rows=rows, gs=gs, d=d: e.dma_start(
                            out=dst[rows, 16 * d:16 * d + 16], in_=src[d, gs, :].rearrange("g p -> p g"),
                            allow_slow_non_contiguous=True), w=[nm])
                    P.dmaop("sp", lambda e, rows=rows, gs=gs, d=d: e.dma_start(
                        out=ldt[rows, 16 * d:16 * d + 16], in_=log_dt[d, gs].partition_broadcast(64)), w=["ldt"])
                for (dst, src, nm) in ((bre, b_re, "bre"), (bim, b_im, "bim")):
                    for d in range(2):
                        P.dmaop("act", lambda e, dst=dst, src=src, rows=rows, gs=gs, d=d: e.dma_start(
                            out=dst[rows, 16 * d:16 * d + 16, :], in_=src[d, gs, :, :].rearrange("g p h -> p g h")), w=[nm])
                for (dst, src, nm) in ((cre, c_re, "cre"), (cim, c_im, "cim")):
                    for d in range(2):
                        P.dmaop("sp" if d == 0 else "act", lambda e, dst=dst, src=src, rows=rows, gs=gs, d=d: e.dma_start(
                            out=dst[rows, 16 * d:16 * d + 16, :], in_=src[d, gs, :, :].rearrange("g o p -> p g o"),
                            allow_slow_non_contiguous=True), w=[nm])
            for j in range(8):
                P.dmaop("sp", lambda e, j=j: e.dma_start(out=dsk[16 * j:16 * j + 16, :], in_=ssm_d.rearrange("(g h) -> h g", h=16),
                                                        allow_slow_non_contiguous=True), w=["dsk"])
            P.dmaop("sp", lambda e: e.dma_start(out=cmk[:], in_=cmask_d), w=["cmk"])

            K = [0]

            def T_(i):
                return tm[:, i, :]

            def dv(fn, r, w, eng="dve"):
                P.op(eng, fn, r=r, w=w)

            def tt2(out, a, b, op, r, w, eng="dve"):
                P.op(eng, lambda e: e.tensor_tensor(out=out, in0=a, in1=b, op=op), r=r, w=w)

            def ts(out, a, s1, op0, s2=None, op1=None, r=(), w=(), eng="dve"):
                if op1 is None:
                    P.op(eng, lambda e: e.tensor_scalar(out=out, in0=a, scalar1=s1, scalar2=None, op0=op0), r=r, w=w)
                else:
                    P.op(eng, lambda e: e.tensor_scalar(out=out, in0=a, scalar1=s1, scalar2=s2, op0=op0, op1=op1), r=r, w=w)
            PI = math.pi
            P.op("act", lambda e: e.activation(out=T_(0), in_=ldt[:], func=AF.Exp), r=["ldt"], w=["t0"])
            tt2(T_(1), lre[:], T_(0), ALU.mult, ["lre", "t0"], ["t1"])
            tt2(T_(2), lim[:], T_(0), ALU.mult, ["lim", "t0"], ["t2"])
            P.op("act", lambda e: e.activation(out=T_(3), in_=T_(1), func=AF.Exp), r=["t1"], w=["t3"])
            ts(T_(4), T_(2), 1.0 / (2 * PI), ALU.mult, r=["t2"], w=["t4"])
            P.op("dve", lambda e: e.tensor_copy(out=ti[:], in_=T_(4)), r=["t4"], w=["ti"])
            P.op("dve", lambda e: e.tensor_copy(out=T_(4), in_=ti[:]), r=["ti"], w=["t4"])
            P.op("dve", lambda e: e.scalar_tensor_tensor(out=T_(5), in0=T_(4), scalar=-2 * PI, in1=T_(2), op0=ALU.mult, op1=ALU.add),
                 r=["t4", "t2"], w=["t5"])
            for (src_i, dst_i) in ((5, 5),):
                ts(T_(6), T_(5), PI, ALU.is_gt, -2 * PI, ALU.mult, r=["t5"], w=["t6"])
                tt2(T_(5), T_(5), T_(6), ALU.add, ["t5", "t6"], ["t5"])
                ts(T_(6), T_(5), -PI, ALU.is_lt, 2 * PI, ALU.mult, r=["t5"], w=["t6"])
                tt2(T_(5), T_(5), T_(6), ALU.add, ["t5", "t6"], ["t5"])
            ts(T_(7), T_(5), PI / 2, ALU.add, r=["t5"], w=["t7"])
            ts(T_(6), T_(7), PI, ALU.is_gt, -2 * PI, ALU.mult, r=["t7"], w=["t6"])
            tt2(T_(7), T_(7), T_(6), ALU.add, ["t7", "t6"], ["t7"])
            P.op("act", lambda e: e.activation(out=T_(8), in_=T_(5), func=AF.Sin), r=["t5"], w=["t8"])
            P.op("act", lambda e: e.activation(out=T_(9), in_=T_(7), func=AF.Sin), r=["t7"], w=["t9"])
            P.op("pool", lambda e: e.memset(pwr[:, 0, :], 1.0), w=[("pw", 0)])
            P.op("pool", lambda e: e.memset(pwi[:, 0, :], 0.0), w=[("pw", 0)])
            tt2(pwr[:, 1, :], T_(3), T_(9), ALU.mult, ["t3", "t9"], [("pw", 1)])
            tt2(pwi[:, 1, :], T_(3), T_(8), ALU.mult, ["t3", "t8"], [("pw", 1)])
            for k in range(2, 9):
                a_r, a_i = pwr[:, k - 1, :], pwi[:, k - 1, :]
                tt2(T_(10), a_r, pwr[:, 1, :], ALU.mult, [("pw", k - 1), ("pw", 1)], ["t10"])
                tt2(T_(11), a_i, pwi[:, 1, :], ALU.mult, [("pw", k - 1), ("pw", 1)], ["t11"])
                tt2(pwr[:, k, :], T_(10), T_(11), ALU.subtract, ["t10", "t11"], [("pw", k)])
                tt2(T_(10), a_r, pwi[:, 1, :], ALU.mult, [("pw", k - 1), ("pw", 1)], ["t10"])
                tt2(T_(11), a_i, pwr[:, 1, :], ALU.mult, [("pw", k - 1), ("pw", 1)], ["t11"])
                tt2(pwi[:, k, :], T_(10), T_(11), ALU.add, ["t10", "t11"], [("pw", k)])
            pwk = [("pw", k) for k in range(9)]
            tt2(nwr[:], pwr[:], pwr[:], ALU.mult, pwk, ["nwr"])
            tt2(nwi[:], pwi[:], pwi[:], ALU.mult, pwk, ["nwi"])
            tt2(nwr[:], nwr[:], nwi[:], ALU.add, ["nwr", "nwi"], ["nwr"])
            P.op("dve", lambda e: e.reciprocal(out=nwr[:], in_=nwr[:]), r=["nwr"], w=["nwr"])
            P.op("dve", lambda e: e.scalar_tensor_tensor(out=nwi[:], in0=pwi[:], scalar=-1.0, in1=nwr[:], op0=ALU.mult, op1=ALU.mult),
                 r=pwk + ["nwr"], w=["nwi"])
            tt2(nwr[:], pwr[:], nwr[:], ALU.mult, pwk + ["nwr", "nwi"], ["nwr"])
            for d in range(2):
                dsl = slice(16 * d, 16 * d + 16)
                for pl in range(2):
                    P.op("dve", lambda e, d=d, pl=pl, dsl=dsl: e.tensor_copy(out=mu3[:, d, 0, :, pl], in_=pwr[:, 8, dsl]), r=pwk, w=["mu3"])
                P.op("dve", lambda e, d=d, dsl=dsl: e.tensor_scalar(out=mu3[:, d, 1, :, 0], in0=pwi[:, 8, dsl], scalar1=-1.0, scalar2=None, op0=ALU.mult),
                     r=pwk, w=["mu3"])
                P.op("dve", lambda e, d=d, dsl=dsl: e.tensor_copy(out=mu3[:, d, 1, :, 1], in_=pwi[:, 8, dsl]), r=pwk, w=["mu3"])
            q16r = sb(tt, "q16r", [128, 17, 32]); q16i = sb(tt, "q16i", [128, 17, 32])
            P.op("pool", lambda e: e.memset(q16r[:, 0, :], 1.0), w=[("q16", 0)])
            P.op("pool", lambda e: e.memset(q16i[:, 0, :], 0.0), w=[("q16", 0)])
            P.op("pool", lambda e: e.tensor_copy(out=q16r[:, 1, :], in_=pwr[:, 8, :]), r=pwk, w=[("q16", 1)])
            P.op("pool", lambda e: e.tensor_copy(out=q16i[:, 1, :], in_=pwi[:, 8, :]), r=pwk, w=[("q16", 1)])
            for k in range(2, 17):
                a_r, a_i = q16r[:, k - 1, :], q16i[:, k - 1, :]
                tt2(T_(10), a_r, q16r[:, 1, :], ALU.mult, [("q16", k - 1), ("q16", 1)], ["t10"])
                tt2(T_(11), a_i, q16i[:, 1, :], ALU.mult, [("q16", k - 1), ("q16", 1)], ["t11"])
                tt2(q16r[:, k, :], T_(10), T_(11), ALU.subtract, ["t10", "t11"], [("q16", k)])
                tt2(T_(10), a_r, q16i[:, 1, :], ALU.mult, [("q16", k - 1), ("q16", 1)], ["t10"])
                tt2(T_(11), a_i, q16r[:, 1, :], ALU.mult, [("q16", k - 1), ("q16", 1)], ["t11"])
                tt2(q16i[:, k, :], T_(10), T_(11), ALU.add, ["t10", "t11"], [("q16", k)])
            q16k = [("q16", k) for k in range(17)]
            for d in range(2):
                dsl = slice(16 * d, 16 * d + 16)
                for pl in range(2):
                    P.op("dve", lambda e, d=d, pl=pl, dsl=dsl: e.tensor_copy(out=mu16[:, d, :, 0, :, pl], in_=q16r[:, :, dsl]), r=q16k, w=["mu16"])
                P.op("dve", lambda e, d=d, dsl=dsl: e.tensor_scalar(out=mu16[:, d, :, 1, :, 0], in0=q16i[:, :, dsl], scalar1=-1.0, scalar2=None, op0=ALU.mult),
                     r=q16k, w=["mu16"])
                P.op("dve", lambda e, d=d, dsl=dsl: e.tensor_copy(out=mu16[:, d, :, 1, :, 1], in_=q16i[:, :, dsl]), r=q16k, w=["mu16"])
            tt2(T_(0), lre[:], lre[:], ALU.mult, ["lre"], ["t0"])
            tt2(T_(1), lim[:], lim[:], ALU.mult, ["lim"], ["t1"])
            tt2(T_(0), T_(0), T_(1), ALU.add, ["t0", "t1"], ["t0"])
            P.op("dve", lambda e: e.reciprocal(out=T_(0), in_=T_(0)), r=["t0"], w=["t0"])
            ts(T_(1), pwr[:, 1, :], -1.0, ALU.add, r=[("pw", 1)], w=["t1"])
            tt2(T_(2), T_(1), lre[:], ALU.mult, ["t1", "lre"], ["t2"])
            tt2(T_(3), pwi[:, 1, :], lim[:], ALU.mult, [("pw", 1), "lim"], ["t3"])
            tt2(T_(2), T_(2), T_(3), ALU.add, ["t2", "t3"], ["t2"])
            tt2(T_(2), T_(2), T_(0), ALU.mult, ["t2", "t0"], ["t2"])
            tt2(T_(3), pwi[:, 1, :], lre[:], ALU.mult, [("pw", 1), "lre"], ["t3"])
            tt2(T_(4), T_(1), lim[:], ALU.mult, ["t1", "lim"], ["t4"])
            tt2(T_(3), T_(3), T_(4), ALU.subtract, ["t3", "t4"], ["t3"])
            tt2(T_(3), T_(3), T_(0), ALU.mult, ["t3", "t0"], ["t3"])
            cfr = T_(2).unsqueeze(2).to_broadcast([128, 32, 16])
            cfi = T_(3).unsqueeze(2).to_broadcast([128, 32, 16])
            tmpA = MT[0][:].rearrange("p a b c -> p (a b c)")[:, 0:512].rearrange("p (g h) -> p g h", h=16)
            tt2(Bbr[:], bre[:], cfr, ALU.mult, ["bre", "t2"], ["Bbr"])
            tt2(tmpA, bim[:], cfi, ALU.mult, ["bim", "t3"], ["MT0"])
            tt2(Bbr[:], Bbr[:], tmpA, ALU.subtract, ["Bbr", "MT0"], ["Bbr"])
            tt2(Bbi[:], bim[:], cfr, ALU.mult, ["bim", "t2"], ["Bbi"])
            tt2(tmpA, bre[:], cfi, ALU.mult, ["bre", "t3"], ["MT0"])
            tt2(Bbi[:], Bbi[:], tmpA, ALU.add, ["Bbi", "MT0"], ["Bbi"])

            def cprod(out_re, out_im, pw_re, pw_im, koff, kstep, vr, vi, d, keys_in, key_out, neg_im=False, eng="dve"):
                dsl = slice(16 * d, 16 * d + 16)
                if kstep == 1:
                    ksl = slice(koff, koff + 8)
                    pr = pw_re[:, ksl, dsl].rearrange("p j g -> p g j").unsqueeze(3).to_broadcast([128, 16, 8, 16])
                    pi = pw_im[:, ksl, dsl].rearrange("p j g -> p g j").unsqueeze(3).to_broadcast([128, 16, 8, 16])
                else:
                    pr = None
                vrb = vr[:, dsl, :].unsqueeze(2).to_broadcast([128, 16, 8, 16])
                vib = vi[:, dsl, :].unsqueeze(2).to_broadcast([128, 16, 8, 16])
                t1 = MT[2][:]
                t2 = MT[3][:]
                tt2(t1, vrb, pr, ALU.mult, keys_in, ["MT2"], eng)
                tt2(t2, vib, pi, ALU.mult, keys_in, ["MT3"], eng)
                tt2(out_re, t1, t2, ALU.subtract, ["MT2", "MT3"], [key_out[0]], eng)
                tt2(t1, vrb, pi, ALU.mult, keys_in, ["MT2"], eng)
                tt2(t2, vib, pr, ALU.mult, keys_in, ["MT3"], eng)
                tt2(out_im, t1, t2, (ALU.add), ["MT2", "MT3"], [key_out[1]], eng)
                if neg_im:
                    ts(out_im, out_im, -1.0, ALU.mult, r=[key_out[1]], w=[key_out[1]], eng=eng)

            dpr = sb(tt, "dpr", [128, 9, 32]); dpi = sb(tt, "dpi", [128, 9, 32])
            for k in range(9):
                P.op("pool", lambda e, k=k: e.tensor_copy(out=dpr[:, k, :], in_=pwr[:, 8 - k, :]), r=pwk, w=["dpr"])
                P.op("pool", lambda e, k=k: e.tensor_copy(out=dpi[:, k, :], in_=pwi[:, 8 - k, :]), r=pwk, w=["dpi"])
            tk = pwk + ["nwr", "nwi", "dpr", "dpi", "Bbr", "Bbi", "cre", "cim"]
            pTL = PS2[0]
            for d in range(2):
                if d == 0:
                    cprod(MX[0][:], MX[1][:], nwr, nwi, 0, 1, Bbr, Bbi, d, tk, ("MX0", "MX1"))
                    cprod(MX[2][:], MX[3][:], pwr, pwi, 0, 1, cre, cim, d, tk, ("MX2", "MX3"), neg_im=True)
                    cprod(MT[0][:], MT[1][:], dpr, dpi, 1, 1, Bbr, Bbi, d, tk, ("MT0", "MT1"))
                else:
                    cprod(MX[0][:], MX[1][:], pwr, pwi, 0, 1, Bbr, Bbi, d, tk, ("MX0", "MX1"))
                    cprod(MX[2][:], MX[3][:], nwr, nwi, 0, 1, cre, cim, d, tk, ("MX2", "MX3"), neg_im=True)
                    P.op("pool", lambda e: e.tensor_copy(out=MT[0][:], in_=MX[0][:]), r=["MX0"], w=["MT0"])
                    P.op("pool", lambda e: e.tensor_copy(out=MT[1][:], in_=MX[1][:]), r=["MX1"], w=["MT1"])
                for pl in range(2):
                    for g in range(32):
                        gh, gp = g // 16, g % 16
                        pb = PS2[2 + (g % 4)]

                        def trb(e, pl=pl, gh=gh, gp=gp, pb=pb):
                            return e.transpose(out=pb[:, 0:64], in_=MT[pl][64 * gh:64 * gh + 64, gp].rearrange("p j h -> p (j h)"),
                                               identity=ident[64 * gh:64 * gh + 64, 64 * gh:64 * gh + 64])
                        P.op("pe", trb, r=["MT0" if pl == 0 else "MT1", "ident"], w=[("p2", 2 + g % 4)])
                        P.op("act" if g % 2 == 0 else "dve",
                             (lambda e, pb=pb, d=d, g=g, pl=pl: e.copy(out=BendT[:, d, g, pl, :], in_=pb[:, 0:64])) if g % 2 == 0 else
                             (lambda e, pb=pb, d=d, g=g, pl=pl: e.tensor_copy(out=BendT[:, d, g, pl, :], in_=pb[:, 0:64])),
                             r=[("p2", 2 + g % 4)], w=["BendT"])
                if d == 0:
                    cprod(MT[0][:], MT[1][:], pwr, pwi, 1, 1, cre, cim, d, tk + ["BendT"], ("MT0", "MT1"), neg_im=True)
                else:
                    cprod(MT[0][:], MT[1][:], dpr, dpi, 0, 1, cre, cim, d, tk + ["BendT"], ("MT0", "MT1"), neg_im=True)
                P.op("act", lambda e, d=d: e.copy(out=Dm[:, d, :, 0, :], in_=MT[0][:].rearrange("p g j o -> p g (j o)")), r=["MT0"], w=["Dm"])
                P.op("act", lambda e, d=d: e.copy(out=Dm[:, d, :, 1, :], in_=MT[1][:].rearrange("p g j o -> p g (j o)")), r=["MT1"], w=["Dm"])
                for g in range(32):
                    gh, gp = g // 16, g % 16
                    rows = slice(64 * gh, 64 * gh + 64)
                    pq = PS2[6 + (g % 2)]

                    def mtl(e, rows=rows, gp=gp, pq=pq):
                        e.matmul(pq[:, 0:128], lhsT=MX[0][rows, gp].rearrange("p j h -> p (j h)"), rhs=MX[2][rows, gp].rearrange("p j h -> p (j h)"),
                                 start=True, stop=False)
                        return e.matmul(pq[:, 0:128], lhsT=MX[1][rows, gp].rearrange("p j h -> p (j h)"),
                                        rhs=MX[3][rows, gp].rearrange("p j h -> p (j h)"), start=False, stop=True)
                    P.op("pe", mtl, r=["MX0", "MX1", "MX2", "MX3"], w=[("p2", 6 + g % 2)])
                    if d == 0:
                        P.op("dve", lambda e, g=g, pq=pq: e.tensor_tensor(out=Tloc[:, g, :], in0=pq[:, 0:128], in1=cmk[:, 0:128], op=ALU.mult),
                             r=[("p2", 6 + g % 2), "cmk"], w=[("Tloc", g)])
                    else:
                        P.op("dve", lambda e, g=g, pq=pq: e.tensor_tensor(out=TL[:, g % 2, :], in0=pq[:, 0:128], in1=cmk[:, 128:256], op=ALU.mult),
                             r=[("p2", 6 + g % 2), "cmk"], w=[("TL", g % 2)])
                        P.op("pool", lambda e, g=g: e.tensor_tensor(out=Tloc[:, g, :], in0=Tloc[:, g, :], in1=TL[:, g % 2, :], op=ALU.add),
                             r=[("Tloc", g), ("TL", g % 2)], w=[("Tloc", g)])
                        P.op("pool", lambda e, g=g: e.scalar_tensor_tensor(out=Tloc[:, g, :], in0=ident[:], scalar=dsk[:, g:g + 1], in1=Tloc[:, g, :],
                                                                          op0=ALU.mult, op1=ALU.add) if False else
                             e.tensor_scalar(out=TL[:, g % 2, :], in0=ident[:], scalar1=dsk[:, g:g + 1], scalar2=None, op0=ALU.mult),
                             r=["ident", "dsk", ("Tloc", g)], w=[("TL", g % 2)])
                        P.op("pool", lambda e, g=g: e.tensor_tensor(out=Tloc[:, g, :], in0=Tloc[:, g, :], in1=TL[:, g % 2, :], op=ALU.add),
                             r=[("Tloc", g), ("TL", g % 2)], w=[("Tloc", g)])
            P.flush()
        U8 = sb(st, "U8", [128, 32, NCHe], BF16)
        Hall = [sb(st, "Hall%d" % d, [128, 16, 2, HW], BF16) for d in range(2)]
        with ExitStack() as tu:
            Uc = sb(tu, "Uc", [128, 1, 4096])
            Ucb = sb(tu, "Ucb", [128, 1, 4096], BF16)
            identb2 = sb(tu, "identb2", [128, 128], BF16)
            P.op("dve", lambda e: e.tensor_copy(out=identb2[:], in_=ident[:]), r=["ident"], w=["identb2"])
            u8v = u_s.rearrange("(c j) f -> c (j f)", j=8)
            nblk = (NCHe + 127) // 128
            for cb in range(nblk):
                c0 = cb * 128
                ncb = min(128, NCHe - c0)
                s = 0
                P.dmaop("sp", lambda e, s=s, c0=c0, ncb=ncb: e.dma_start(out=Uc[0:ncb, s, :], in_=u8v[c0:c0 + ncb, :]), r=["u_s"], w=[("Uc", s)])
                P.op("dve", lambda e, s=s, ncb=ncb: e.tensor_copy(
                    out=Ucb[0:ncb, s, :].rearrange("c (g j h) -> c g j h", g=32, j=8),
                    in_=Uc[0:ncb, s, :].rearrange("c (j g h) -> c g j h", j=8, g=32)), r=[("Uc", s)], w=[("Ucb", s)])
                for g4 in range(8):
                    pb = PS2[g4 % 4]
                    pbb = pb[:].bitcast(BF16)

                    def tru(e, g4=g4, s=s, ncb=ncb, pbb=pbb):
                        ins = None
                        for q in range(4):
                            g = g4 * 4 + q
                            ins = e.transpose(out=pbb[:, q * 128:q * 128 + ncb], in_=Ucb[0:ncb, s, g * 128:(g + 1) * 128],
                                              identity=identb2[0:ncb, 0:ncb])
                        return ins
                    P.op("pe", tru, r=[("Ucb", s), "identb2"], w=[("p2", g4 % 4)])
                    P.op("act" if g4 % 2 == 0 else "dve",
                         (lambda e, g4=g4, c0=c0, ncb=ncb, pbb=pbb: e.copy(
                             out=U8[:, g4 * 4:g4 * 4 + 4, c0:c0 + ncb], in_=pbb[:, 0:512].rearrange("p (q c) -> p q c", q=4)[:, :, 0:ncb]))
                         if g4 % 2 == 0 else
                         (lambda e, g4=g4, c0=c0, ncb=ncb, pbb=pbb: e.tensor_copy(
                             out=U8[:, g4 * 4:g4 * 4 + 4, c0:c0 + ncb], in_=pbb[:, 0:512].rearrange("p (q c) -> p q c", q=4)[:, :, 0:ncb])),
                         r=[("p2", g4 % 4)], w=["U8"])
            P.flush()
        if debug == 4:
            d1 = nc.dram_tensor("dbg_tloc", [128, 32 * 128], BF16, kind="ExternalOutput").ap()
            d2 = nc.dram_tensor("dbg_bendt", [128, 2 * 32 * 2 * 64], BF16, kind="ExternalOutput").ap()
            d3 = nc.dram_tensor("dbg_dm", [128, 2 * 16 * 2 * 128], BF16, kind="ExternalOutput").ap()
            d4 = nc.dram_tensor("dbg_u8", [128, 32 * NCHe], BF16, kind="ExternalOutput").ap()
            d5 = nc.dram_tensor("dbg_mu3", [128, 128], F32, kind="ExternalOutput").ap()
            P.dmaop("sp", lambda e: e.dma_start(out=d1, in_=Tloc[:].rearrange("p a b -> p (a b)")), w=["d1"])
            P.dmaop("sp", lambda e: e.dma_start(out=d2, in_=BendT[:].rearrange("p a b c d -> p (a b c d)")), w=["d2"])
            P.dmaop("sp", lambda e: e.dma_start(out=d3, in_=Dm[:].rearrange("p a b c d -> p (a b c d)")), w=["d3"])
            P.dmaop("sp", lambda e: e.dma_start(out=d4, in_=U8[:].rearrange("p a b -> p (a b)")), w=["d4"])
            P.dmaop("sp", lambda e: e.dma_start(out=d5, in_=mu3[:].rearrange("p a b c d -> p (a b c d)")), w=["d5"])
            P.flush()
            DONE.append(1)

        def hk(d, lo, hi):
            return [("Hall", d, q) for q in range(lo, hi)]
        P.op("pool", lambda e: e.memset(Hall[0][:, :, :, 0:1], 0.0), w=hk(0, 0, 1))
        P.op("pool", lambda e: e.memset(Hall[1][:, :, :, NCHe:NCHe + 1], 0.0), w=hk(1, NCHe, NCHe + 1))
        xlo = [NCC + 1, 0]
        clo = [1, NCX]
        ei = 0
        for d in range(2):
            for pl in range(2):
                pc = PS2[4 + pl]
                for gp in range(16):
                    px = PS2[(d * 32 + pl * 16 + gp) % 4]
                    pk = ("p2", (d * 32 + pl * 16 + gp) % 4)

                    def mms(e, d=d, pl=pl, gp=gp, px=px, pc=pc):
                        ins = None
                        for gh in range(2):
                            g = 16 * gh + gp
                            e.matmul(px[64 * gh:64 * gh + 64, 0:NCX], lhsT=BendT[:, d, g, pl, :], rhs=U8[:, g, NCC:NCC + NCX],
                                     start=True, stop=True, tile_position=(0, 64 * gh))
                            ins = e.matmul(pc[64 * gh:64 * gh + 64, gp * 32:gp * 32 + NCC], lhsT=BendT[:, d, g, pl, :], rhs=U8[:, g, 0:NCC],
                                           start=True, stop=True, tile_position=(0, 64 * gh))
                        return ins
                    P.op("pe", mms, r=["BendT", "U8"], w=[pk, ("p2c", 4 + pl, gp)])
                    dst = Hall[d][:, gp, pl, xlo[d]:xlo[d] + NCX]
                    if ei % 2 == 0:
                        P.op("act", lambda e, dst=dst, px=px: e.copy(out=dst, in_=px[:, 0:NCX]), r=[pk], w=hk(d, xlo[d], xlo[d] + NCX))
                    else:
                        P.op("dve", lambda e, dst=dst, px=px: e.tensor_copy(out=dst, in_=px[:, 0:NCX]), r=[pk], w=hk(d, xlo[d], xlo[d] + NCX))
                    ei += 1
                P.op("act", lambda e, d=d, pl=pl, pc=pc: e.copy(out=Hall[d][:, :, pl, clo[d]:clo[d] + NCC],
                                                                in_=pc[:, 0:512].rearrange("p (g c) -> p g c", g=16)[:, :, 0:NCC]),
                     r=[("p2c", 4 + pl, gp) for gp in range(16)], w=hk(d, clo[d], clo[d] + NCC))
        LB = 16
        NB = NCHe // LB
        assert NB * LB == NCHe
        NBd = [(NCC + NXO // 8) // LB, NB]
        assert NBd[0] * LB == NCC + NXO // 8
        with ExitStack() as tc:
            Rl = [sb(tc, "Rl_%d" % d, [128, 16, 3, NB + 1]) for d in range(2)]
            TA = [sb(tc, "TA_%d" % d, [128, 16, 2, NB]) for d in range(2)]
            TB = [sb(tc, "TB_%d" % d, [128, 16, 2, NB]) for d in range(2)]
            Ea = Rl
            ET = [sb(tc, "ET_%d" % d, [128, 2, 16, 2]) for d in range(2)]

            def hview(d, i):
                st0 = (1 + i) if d == 0 else (LB - 1 - i)
                return Hall[d][:, :, :, st0:st0 + LB * (NBd[d] - 1) + 1:LB]

            def hkeys(d, i):
                st0 = (1 + i) if d == 0 else (LB - 1 - i)
                return [("Hall", d, st0 + LB * m) for m in range(NBd[d])]

            def mub(d, k, which, n):
                return mu16[:, d, k, which].unsqueeze(3).to_broadcast([128, 16, 2, n])
            for d in range(2):
                eng = "dve"
                for i in range(LB):
                    hv = hview(d, i)
                    hk_i = hkeys(d, i)
                    if i == 0:
                        P.op(eng, lambda e, d=d, hv=hv: e.tensor_copy(out=Rl[d][:, :, 0:2, 0:NBd[d]], in_=hv), r=hk_i, w=[("Rl", d)])
                    else:
                        P.op(eng, lambda e, d=d: e.tensor_tensor(out=TA[d][:, :, :, 0:NBd[d]], in0=Rl[d][:, :, 0:2, 0:NBd[d]], in1=mub(d, 1, 0, NBd[d]), op=ALU.mult),
                             r=[("Rl", d), "mu16"], w=[("TA", d)])
                        P.op(eng, lambda e, d=d: e.tensor_tensor(out=TB[d][:, :, :, 0:NBd[d]], in0=Rl[d][:, :, 1:3, 0:NBd[d]], in1=mub(d, 1, 1, NBd[d]), op=ALU.mult),
                             r=[("Rl", d), "mu16"], w=[("TB", d)])
                        P.op(eng, lambda e, d=d: e.tensor_tensor(out=TA[d][:, :, :, 0:NBd[d]], in0=TA[d][:, :, :, 0:NBd[d]], in1=TB[d][:, :, :, 0:NBd[d]], op=ALU.add),
                             r=[("TA", d), ("TB", d)], w=[("TA", d)])
                        P.op(eng, lambda e, d=d, hv=hv: e.tensor_tensor(out=Rl[d][:, :, 0:2, 0:NBd[d]], in0=TA[d][:, :, :, 0:NBd[d]], in1=hv, op=ALU.add),
                             r=[("TA", d)] + hk_i, w=[("Rl", d)])
                        P.op("act", lambda e, d=d, hv=hv: e.copy(out=hv, in_=Rl[d][:, :, 0:2, 0:NBd[d]]), r=[("Rl", d)], w=hk_i)
                    if i < LB - 1:
                        P.op(eng, lambda e, d=d: e.tensor_copy(out=Rl[d][:, :, 2, 0:NBd[d]], in_=Rl[d][:, :, 0, 0:NBd[d]]), r=[("Rl", d)], w=[("Rl", d)])
            for d in range(2):
                eng = "dve" if d == 0 else "pool"
                P.op(eng, lambda e, d=d: e.memset(Ea[d][:], 0.0), w=[("Rl", d)])
                order = list(range(NBd[0] - 1)) if d == 0 else list(range(NB - 1, 0, -1))
                for m in order:
                    mn = m + 1 if d == 0 else m - 1
                    pend = (1 + 16 * m + 15) if d == 0 else (16 * m)
                    P.op(eng, lambda e, d=d, m=m: e.tensor_tensor(out=ET[d][:, 0], in0=Ea[d][:, :, 0:2, m], in1=mu16[:, d, 16, 0], op=ALU.mult),
                         r=[("Rl", d), "mu16"], w=[("ET", d, 0)])
                    P.op(eng, lambda e, d=d, m=m: e.tensor_tensor(out=ET[d][:, 1], in0=Ea[d][:, :, 1:3, m], in1=mu16[:, d, 16, 1], op=ALU.mult),
                         r=[("Rl", d), "mu16"], w=[("ET", d, 1)])
                    P.op(eng, lambda e, d=d: e.tensor_tensor(out=ET[d][:, 0], in0=ET[d][:, 0], in1=ET[d][:, 1], op=ALU.add),
                         r=[("ET", d, 0), ("ET", d, 1)], w=[("ET", d, 0)])
                    P.op(eng, lambda e, d=d, mn=mn, pend=pend: e.tensor_tensor(out=Ea[d][:, :, 0:2, mn], in0=ET[d][:, 0], in1=Hall[d][:, :, :, pend], op=ALU.add),
                         r=[("ET", d, 0), ("Hall", d, pend)], w=[("Rl", d)])
                    P.op(eng, lambda e, d=d, mn=mn: e.tensor_copy(out=Ea[d][:, :, 2, mn], in_=Ea[d][:, :, 0, mn]), r=[("Rl", d)], w=[("Rl", d)])
            for d in range(2):
                eng = "dve"
                for i in range(LB):
                    hv = hview(d, i)
                    hk_i = hkeys(d, i)
                    P.op(eng, lambda e, d=d, i=i: e.tensor_tensor(out=TA[d][:, :, :, 0:NBd[d]], in0=Ea[d][:, :, 0:2, 0:NBd[d]], in1=mub(d, i + 1, 0, NBd[d]), op=ALU.mult),
                         r=[("Rl", d), "mu16"], w=[("TA", d)])
                    P.op(eng, lambda e, d=d, i=i: e.tensor_tensor(out=TB[d][:, :, :, 0:NBd[d]], in0=Ea[d][:, :, 1:3, 0:NBd[d]], in1=mub(d, i + 1, 1, NBd[d]), op=ALU.mult),
                         r=[("Rl", d), "mu16"], w=[("TB", d)])
                    P.op(eng, lambda e, d=d: e.tensor_tensor(out=TA[d][:, :, :, 0:NBd[d]], in0=TA[d][:, :, :, 0:NBd[d]], in1=TB[d][:, :, :, 0:NBd[d]], op=ALU.add),
                         r=[("TA", d), ("TB", d)], w=[("TA", d)])
                    P.op(eng, lambda e, d=d, hv=hv: e.tensor_tensor(out=hv, in0=hv, in1=TA[d][:, :, :, 0:NBd[d]], op=ALU.add), r=[("TA", d)] + hk_i, w=hk_i)
            P.flush()
        NCXo = NXO // 8
        NCB = (NCXo + 127) // 128
        NPASS = 1
        CBP = NCB // NPASS
        NCP = NCXo // NPASS
        Yc = sb(st, "Yc", [128, CBP, 4096], BF16)
        Ysb = sb(st, "Ysb", [128, 2, NCP])
        allH = [hk(0, 0, HW), hk(1, 0, HW)]
        for pz in range(NPASS):
            c_lo = pz * NCP
            for g in range(32):
                gh, gp = g // 16, g % 16
                rows = slice(64 * gh, 64 * gh + 64)
                pr = PS2[g % 2]
                prk = ("p2", g % 2)

                def mmy(e, g=g, gp=gp, rows=rows, pr=pr, c_lo=c_lo):
                    e.matmul(pr[:, 0:NCP], lhsT=Tloc[:, g, :], rhs=U8[:, g, NCC + c_lo:NCC + c_lo + NCP], start=True, stop=False)
                    e.matmul(pr[:, 0:NCP], lhsT=Dm[rows, 0, gp, 0, :], rhs=Hall[0][rows, gp, 0, NCC + c_lo:NCC + c_lo + NCP], start=False, stop=False)
                    e.matmul(pr[:, 0:NCP], lhsT=Dm[rows, 0, gp, 1, :], rhs=Hall[0][rows, gp, 1, NCC + c_lo:NCC + c_lo + NCP], start=False, stop=False)
                    e.matmul(pr[:, 0:NCP], lhsT=Dm[rows, 1, gp, 0, :], rhs=Hall[1][rows, gp, 0, 1 + c_lo:1 + c_lo + NCP], start=False, stop=False)
                    return e.matmul(pr[:, 0:NCP], lhsT=Dm[rows, 1, gp, 1, :], rhs=Hall[1][rows, gp, 1, 1 + c_lo:1 + c_lo + NCP], start=False, stop=True)
                P.op("pe", mmy, r=[("Tloc", g), "U8", "Dm"] + allH[0] + allH[1], w=[prk])
                ys = g % 2
                if g % 2 == 0:
                    P.op("act", lambda e, ys=ys, pr=pr: e.copy(out=Ysb[:, ys, :], in_=pr[:, 0:NCP]), r=[prk], w=[("Ysb", ys)])
                else:
                    P.op("dve", lambda e, ys=ys, pr=pr: e.tensor_copy(out=Ysb[:, ys, :], in_=pr[:, 0:NCP]), r=[prk], w=[("Ysb", ys)])
                for cb in range(CBP):
                    ncb = min(128, NCP - cb * 128)
                    pt_ = PS2[2 + (g * CBP + cb) % 4]
                    ptk = ("p2", 2 + (g * CBP + cb) % 4)
                    P.op("pe", lambda e, ys=ys, cb=cb, ncb=ncb, pt_=pt_: e.transpose(out=pt_[0:ncb, 0:128], in_=Ysb[:, ys, cb * 128:cb * 128 + ncb],
                                                                                   identity=ident[:]), r=[("Ysb", ys), "ident"], w=[ptk])
                    oap = Yc[0:ncb, cb, :].rearrange("c (j g o) -> c g j o", j=8, g=32)[:, g]
                    iap = pt_[0:ncb, 0:128].rearrange("c (j o) -> c j o", j=8)
                    if (g + cb) % 2 == 0:
                        P.op("dve", lambda e, oap=oap, iap=iap: e.tensor_copy(out=oap, in_=iap), r=[ptk], w=[("Yc", cb)])
                    else:
                        P.op("act", lambda e, oap=oap, iap=iap: e.copy(out=oap, in_=iap), r=[ptk], w=[("Yc", cb)])
            for cb in range(CBP):
                ncb = min(128, NCP - cb * 128)
                r0 = c_lo + cb * 128
                P.dmaop("sp", lambda e, cb=cb, ncb=ncb, r0=r0: e.dma_start(out=y_s[r0:r0 + ncb, :], in_=Yc[0:ncb, cb, :]),
                        r=[("Yc", cb)], w=["y_s"])
        P.flush()
    if DONE:
        es.close()
        return nc
    if debug == 5:
        dbg = nc.dram_tensor("dbg_y", [NXe // 8, 4096], BF16, kind="ExternalOutput").ap()
        P.dmaop("sp", lambda e: e.dma_start(out=dbg, in_=y_s[0:NXe // 8, :]), w=["dbg"])
        P.flush()
        es.close()
        return nc

    if debug in (0, 3, 6):
      with ExitStack() as st:
        vt = sb(st, "vt", [128, NTe, 8, 80], BF16)
        kTh = sb(st, "kTh", [128, 2, NTe * 128], BF16)
        qTh = sb(st, "qTh", [128, 2, NXO], BF16)
        pT = sb(st, "pT", [128, 5, 512], BF16)
        osb = sb(st, "osb", [128, 2, 512])
        rr = sb(st, "rr", [128, 512])
        ao = sb(st, "ao", [64, 2, 512])
        ones1 = sb(st, "ones1", [128, 64])
        PSs = [ps(st, "ps_s%d" % i) for i in range(5)]
        PSo = [ps(st, "ps_o%d" % i) for i in range(2)]
        PSb = ps(st, "ps_b")
        P.op("pool", lambda e: e.memset(vt[:], 1.0), w=["vt"])
        P.op("pool", lambda e: e.memset(ones1[:], 1.0), w=["ones1"])
        for kt in range(NTe):
            P.dmaop("sp" if kt % 2 == 0 else "act",
                    lambda e, kt=kt: e.dma_start(out=vt[:, kt, :, 0:64],
                                                 in_=v_s[kt * 128:(kt + 1) * 128, :].rearrange("p (h d) -> p h d", h=8)),
                    r=["v_s"], w=["vt"])
        cnt = 0
        for h in range(H):
            hs = h % 2
            P.dmaop("sp", lambda e, h=h, hs=hs: e.dma_start(out=kTh[0:96, hs, :], in_=kT_s[h, :, 0:NTe * 128]), r=["kT_s"], w=[("kTh", hs)])
            P.dmaop("act", lambda e, h=h, hs=hs: e.dma_start(out=qTh[0:96, hs, :], in_=qT_s[h, :, 0:NXO]), r=["qT_s"], w=[("qTh", hs)])
            for g in range(NQG):
                og = (h * NQG + g) % 2

                def smm(e, kt, hs=hs, g=g):
                    return e.matmul(PSs[kt % 5][:, 0:QG], lhsT=kTh[0:96, hs, kt * 128:(kt + 1) * 128],
                                    rhs=qTh[0:96, hs, g * QG:(g + 1) * QG], start=True, stop=True)

                def pvm(e, kt, h=h, og=og):
                    return e.matmul(PSo[og][0:65, 0:QG], lhsT=vt[:, kt, h, 0:65], rhs=pT[:, kt % 5, 0:QG],
                                    start=(kt == 0), stop=(kt == NTe - 1))
                LOOK = 3
                for step in range(NTe + LOOK):
                    if step < NTe:
                        kt = step
                        P.op("pe", lambda e, kt=kt, f=smm: f(e, kt), r=[("kTh", hs), ("qTh", hs)], w=[("pss", kt % 5)])
                        P.op("act", lambda e, kt=kt: e.activation(out=pT[:, kt % 5, 0:QG], in_=PSs[kt % 5][:, 0:QG], func=AF.Exp),
                             r=[("pss", kt % 5)], w=[("pT", kt % 5)])
                    if step >= LOOK:
                        kt = step - LOOK
                        P.op("pe", lambda e, kt=kt, f=pvm: f(e, kt), r=[("pT", kt % 5), "vt"], w=[("pso", og)])
                P.op("dve", lambda e, og=og: e.tensor_copy(out=osb[0:65, og, 0:QG], in_=PSo[og][0:65, 0:QG]), r=[("pso", og)], w=[("osb", og)])
                P.op("dve", lambda e, og=og: e.reciprocal(out=rr[64:65, 0:QG], in_=osb[64:65, og, 0:QG]), r=[("osb", og)], w=["rr"])
                P.op("pe", lambda e: e.matmul(PSb[0:64, 0:QG], lhsT=ones1[64:65, 0:64], rhs=rr[64:65, 0:QG], start=True, stop=True),
                     r=["rr", "ones1"], w=["psb"])
                P.op("dve", lambda e, og=og: e.tensor_tensor(out=ao[:, og, 0:QG], in0=osb[0:64, og, 0:QG], in1=PSb[0:64, 0:QG], op=ALU.mult),
                     r=[("osb", og), "psb"], w=[("ao", og)])
                P.dmaop("sp", lambda e, h=h, g=g, og=og: e.dma_start(out=attnT_s[h, :, g * QG:(g + 1) * QG], in_=ao[:, og, 0:QG]),
                        r=[("ao", og)], w=["attnT_s"])
        P.flush()

    CAPe = 2 * NXe // NE
    SLT = min(128, CAPe)
    NRC = CAPe // SLT
    NXT = NXO // 128

    if debug in (0, 6):
      with ExitStack() as sp:
        idxT = sb(sp, "idxT", [128, NRC, 16], I32)
        gateT = sb(sp, "gateT", [128, NRC, 16])
        sp45 = ExitStack()
        affT = sb(sp45, "affT", [48, NXO])
        affo = sb(sp45, "affo", [16, NXO])
        with ExitStack() as st:
            wglu = sb(st, "wglu", [128, 4, 512], BF16)
            wsso = sb(st, "wsso", [128, 4, D], BF16)
            wmla = sb(st, "wmla", [64, 8, D], BF16)
            wout = sb(st, "wout", [128, 8, D], BF16)
            wrt = sb(st, "wrt", [128, 8, 16])
            bglu = sb(st, "bglu", [128, 512])
            identb = sb(st, "identb4", [128, 128], BF16)
            TL4 = [dict(), dict()]
            for _s in range(2):
                TL4[_s]['yt'] = sb(st, "yt_%d" % _s, [128, 512], BF16)
                TL4[_s]['yg'] = sb(st, "yg_%d" % _s, [128, 512])
                TL4[_s]['t1'] = sb(st, "t1_%d" % _s, [128, 512])
                TL4[_s]['ygb'] = sb(st, "ygb_%d" % _s, [128, 512], BF16)
                TL4[_s]['ygT'] = sb(st, "ygT_%d" % _s, [128, 4, 128], BF16)
                TL4[_s]['sg'] = sb(st, "sg_%d" % _s, [128, 512])
                TL4[_s]['zb'] = sb(st, "zb_%d" % _s, [128, 512], BF16)
                TL4[_s]['zT'] = sb(st, "zT_%d" % _s, [128, 4, 128], BF16)
                TL4[_s]['at32'] = sb(st, "at32_%d" % _s, [64, 8, 128])
                TL4[_s]['atb'] = sb(st, "atb_%d" % _s, [64, 8, 128], BF16)
                TL4[_s]['gt'] = sb(st, "gt_%d" % _s, [128, 2048])
                TL4[_s]['m1'] = sb(st, "m1_%d" % _s, [128, D])
                TL4[_s]['m2'] = sb(st, "m2_%d" % _s, [128, D])
                TL4[_s]['mb'] = sb(st, "mb_%d" % _s, [128, D], BF16)
                TL4[_s]['mT'] = sb(st, "mT_%d" % _s, [128, 8, 128], BF16)
                TL4[_s]['xt4'] = sb(st, "xt4_%d" % _s, [128, D])
                TL4[_s]['xm'] = sb(st, "xm_%d" % _s, [128, D])
                TL4[_s]['jk'] = sb(st, "jk_%d" % _s, [128, D])
                TL4[_s]['h2'] = sb(st, "h2_%d" % _s, [128, D])
                TL4[_s]['h2b'] = sb(st, "h2b_%d" % _s, [128, D], BF16)
                TL4[_s]['h2T'] = sb(st, "h2T_%d" % _s, [128, 8, 128])
                TL4[_s]['s4'] = sb(st, "s4_%d" % _s, [128, 8])
                TL4[_s]['lg'] = sb(st, "lg_%d" % _s, [128, 16])
                TL4[_s]['af'] = sb(st, "af_%d" % _s, [128, 48])
            BALL = [ps(st, "ph4_%d" % i) for i in range(8)]
            P.dmaop("pool", lambda e: e.dma_start(out=wglu[:], in_=w_glu.rearrange("(k p) n -> p k n", p=128)), w=["wglu"])
            P.dmaop("pool", lambda e: e.dma_start(out=wsso[:], in_=w_ssm_o.rearrange("(k p) n -> p k n", p=128)), w=["wsso"])
            P.dmaop("pool", lambda e: e.dma_start(out=wmla[:], in_=w_mla_o.rearrange("(h v) n -> v h n", v=64)), w=["wmla"])
            P.dmaop("pool", lambda e: e.dma_start(out=wout[:], in_=w_out.rearrange("(k p) n -> p k n", p=128)), w=["wout"])
            P.dmaop("sp", lambda e: e.dma_start(out=wrt[:], in_=w_router.rearrange("(k p) n -> p k n", p=128)), w=["wrt"])
            P.dmaop("sp", lambda e: e.dma_start(out=bglu[:], in_=b_glu.partition_broadcast(128)), w=["bglu"])
            P.op("dve", lambda e: e.tensor_copy(out=identb[:], in_=ident[:]), r=["ident"], w=["identb4"])
            ysv = y_s.rearrange("c (j f) -> (c j) f", j=8)
            for _s in range(2):
                P.op("pool", lambda e, _s=_s: e.memset(TL4[_s]['af'][:], 0.0), w=[("slot", _s, "af")])
            P.op("pool", lambda e: e.memset(affT[:], 0.0), w=["affT"])
            SHARED4 = ["wglu", "wsso", "wmla", "wout", "wrt", "bglu", "identb4", "ident", "modx", "y_s", "gates_s", "attnT_s", "xm_s", "h2b_own", "affo"]
            ALIAS4 = {"b4": "b2", "b5": "b3", "b6": "b2", "b7": "b3"}

            def tile4(i, s):
                Pq = Keyed(P, s, SHARED4, ALIAS4)
                t0 = i * 128
                yt = TL4[s]['yt']
                yg = TL4[s]['yg']
                t1 = TL4[s]['t1']
                ygb = TL4[s]['ygb']
                ygT = TL4[s]['ygT']
                sg = TL4[s]['sg']
                zb = TL4[s]['zb']
                zT = TL4[s]['zT']
                at32 = TL4[s]['at32']
                atb = TL4[s]['atb']
                gt = TL4[s]['gt']
                m1 = TL4[s]['m1']
                m2 = TL4[s]['m2']
                mb = TL4[s]['mb']
                mT = TL4[s]['mT']
                xt4 = TL4[s]['xt4']
                xm = TL4[s]['xm']
                jk = TL4[s]['jk']
                h2 = TL4[s]['h2']
                h2b = TL4[s]['h2b']
                h2T = TL4[s]['h2T']
                s4 = TL4[s]['s4']
                lg = TL4[s]['lg']
                af = TL4[s]['af']
                bk = BALL[4 * s:4 * s + 4]
                B = [bk[0], bk[1], bk[2], bk[3], bk[2], bk[3], bk[2], bk[3]]
                B0b = B[0][:].bitcast(BF16)
                Pq.dmaop("sp", lambda e, t0=t0: e.dma_start(out=yt[:], in_=ysv[t0:t0 + 128, :]), r=["y_s"], w=["yt"])
                Pq.dmaop("act", lambda e, t0=t0: e.dma_start(out=gt[:], in_=gates_s[t0:t0 + 128, :]), r=["gates_s"], w=["gt"])
                Pq.dmaop("sp", lambda e, t0=t0: e.dma_start(out=at32[:], in_=attnT_s[:, :, t0:t0 + 128].rearrange("h v t -> v h t")),
                        r=["attnT_s"], w=["at32"])
                Pq.dmaop("act", lambda e, t0=t0: e.dma_start(out=xt4[:], in_=xc[NCTX + t0:NCTX + t0 + 128, :]), w=["xt4"])
                Pq.op("pool", lambda e: e.tensor_tensor(out=t1[:], in0=yt[:], in1=yt[:], op=ALU.mult), r=["yt"], w=["t1"])
                Pq.op("dve", lambda e: e.tensor_scalar(out=t1[:], in0=t1[:], scalar1=0.044715, scalar2=1.0, op0=ALU.mult, op1=ALU.add),
                     r=["t1"], w=["t1"])
                Pq.op("dve", lambda e: e.tensor_tensor(out=t1[:], in0=t1[:], in1=yt[:], op=ALU.mult), r=["t1", "yt"], w=["t1"])
                Pq.op("act", lambda e: e.activation(out=t1[:], in_=t1[:], func=AF.Tanh, scale=0.7978845608028654), r=["t1"], w=["t1"])
                Pq.op("dve", lambda e: e.tensor_scalar(out=t1[:], in0=t1[:], scalar1=1.0, scalar2=0.5, op0=ALU.add, op1=ALU.mult),
                     r=["t1"], w=["t1"])
                Pq.op("dve", lambda e: e.tensor_tensor(out=yg[:], in0=t1[:], in1=yt[:], op=ALU.mult), r=["t1", "yt"], w=["yg"])
                Pq.op("pool", lambda e: e.tensor_copy(out=ygb[:], in_=yg[:]), r=["yg"], w=["ygb"])

                def tr4(src, n):
                    def f(e):
                        ins = None
                        for k in range(n):
                            ins = e.transpose(out=B0b[:, k * 128:(k + 1) * 128], in_=src[:, k * 128:(k + 1) * 128], identity=identb[:])
                        return ins
                    return f
                Pq.op("pe", tr4(ygb, 4), r=["ygb", "identb4"], w=["b0"])
                Pq.op("act", lambda e: e.copy(out=ygT[:].rearrange("p a b -> p (a b)"), in_=B0b[:, 0:512]), r=["b0"], w=["ygT"])

                def mmglu(e):
                    ins = None
                    for k in range(4):
                        ins = e.matmul(B[1][:, 0:512], lhsT=ygT[:, k, :], rhs=wglu[:, k, :], start=(k == 0), stop=(k == 3))
                    return ins
                Pq.op("pe", mmglu, r=["ygT", "wglu"], w=["b1"])
                Pq.op("dve", lambda e: e.tensor_tensor(out=sg[:], in0=B[1][:, 0:512], in1=bglu[:], op=ALU.add), r=["b1", "bglu"], w=["sg"])
                Pq.op("act", lambda e: e.activation(out=sg[:], in_=sg[:], func=AF.Sigmoid), r=["sg"], w=["sg"])
                Pq.op("dve", lambda e: e.tensor_tensor(out=zb[:], in0=sg[:], in1=yg[:], op=ALU.mult), r=["sg", "yg"], w=["zb"])
                Pq.op("pe", tr4(zb, 4), r=["zb", "identb4"], w=["b0"])
                Pq.op("act", lambda e: e.copy(out=zT[:].rearrange("p a b -> p (a b)"), in_=B0b[:, 0:512]), r=["b0"], w=["zT"])

                def mmsso(e):
                    ins = None
                    for hf in range(2):
                        for k in range(4):
                            ins = e.matmul(B[2 + hf][:, 0:512], lhsT=zT[:, k, :], rhs=wsso[:, k, hf * 512:(hf + 1) * 512], start=(k == 0), stop=(k == 3))
                    return ins
                Pq.op("pe", mmsso, r=["zT", "wsso"], w=["b2", "b3"])
                for hf in range(2):
                    cs = slice(hf * 512, (hf + 1) * 512)
                    Pq.op("dve", lambda e, hf=hf, cs=cs: e.tensor_tensor(out=m1[:, cs], in0=B[2 + hf][:, 0:512], in1=gt[:, cs], op=ALU.mult),
                          r=["b%d" % (2 + hf), "gt"], w=[("m1", hf)])
                Pq.op("pool", lambda e: e.tensor_copy(out=atb[:], in_=at32[:]), r=["at32"], w=["atb"])

                def mmat(e):
                    ins = None
                    for hf in range(2):
                        for h in range(8):
                            ins = e.matmul(B[4 + hf][:, 0:512], lhsT=atb[:, h, :], rhs=wmla[:, h, hf * 512:(hf + 1) * 512], start=(h == 0), stop=(h == 7))
                    return ins
                Pq.op("pe", mmat, r=["atb", "wmla"], w=["b4", "b5"])
                for hf in range(2):
                    cs = slice(hf * 512, (hf + 1) * 512)
                    cs2 = slice(D + hf * 512, D + (hf + 1) * 512)
                    Pq.op("dve", lambda e, hf=hf, cs=cs, cs2=cs2: e.tensor_tensor(out=m2[:, cs], in0=B[4 + hf][:, 0:512], in1=gt[:, cs2], op=ALU.mult),
                          r=["b%d" % (4 + hf), "gt"], w=[("m2", hf)])
                Pq.op("pool", lambda e: e.tensor_tensor(out=mb[:], in0=m1[:], in1=m2[:], op=ALU.add),
                     r=[("m1", 0), ("m1", 1), ("m2", 0), ("m2", 1)], w=["mb"])
                Pq.op("pe", tr4(mb, 8), r=["mb", "identb4"], w=["b0"])
                Pq.op("act", lambda e: e.copy(out=mT[:].rearrange("p a b -> p (a b)"), in_=B0b[:, 0:1024]), r=["b0"], w=["mT"])

                def mmout(e):
                    ins = None
                    for hf in range(2):
                        for k in range(8):
                            ins = e.matmul(B[6 + hf][:, 0:512], lhsT=mT[:, k, :], rhs=wout[:, k, hf * 512:(hf + 1) * 512], start=(k == 0), stop=(k == 7))
                    return ins
                Pq.op("pe", mmout, r=["mT", "wout"], w=["b6", "b7"])
                for hf in range(2):
                    cs = slice(hf * 512, (hf + 1) * 512)
                    Pq.op("dve", lambda e, hf=hf, cs=cs: e.tensor_tensor(out=xm[:, cs], in0=B[6 + hf][:, 0:512], in1=modx[:, 2 * D + hf * 512:2 * D + (hf + 1) * 512],
                                                                    op=ALU.mult), r=["b%d" % (6 + hf), "modx"], w=[("xm", hf)])
                Pq.op("pool", lambda e: e.tensor_tensor(out=xm[:], in0=xm[:], in1=xt4[:], op=ALU.add), r=[("xm", 0), ("xm", 1), "xt4"], w=[("xm", 0), ("xm", 1)])
                Pq.dmaop("sp", lambda e, t0=t0: e.dma_start(out=xm_s[t0:t0 + 128, :], in_=xm[:]), r=[("xm", 0), ("xm", 1)], w=["xm_s"])
                Pq.op("act", lambda e: e.activation(out=jk[:], in_=xm[:], func=AF.Square, accum_out=s4[:, 0:1]), r=[("xm", 0), ("xm", 1)], w=["jk", "s4a"])
                Pq.op("dve", lambda e: e.tensor_scalar(out=s4[:, 1:2], in0=s4[:, 0:1], scalar1=1.0 / D, scalar2=EPS, op0=ALU.mult, op1=ALU.add), r=["s4a"], w=["s4b"])
                Pq.op("act", lambda e: e.activation(out=s4[:, 1:2], in_=s4[:, 1:2], func=AF.Sqrt), r=["s4b"], w=["s4b"])
                Pq.op("dve", lambda e: e.reciprocal(out=s4[:, 1:2], in_=s4[:, 1:2]), r=["s4b"], w=["s4b"])
                Pq.op("dve", lambda e: e.scalar_tensor_tensor(out=h2[:], in0=xm[:], scalar=s4[:, 1:2], in1=modx[:, 4 * D:5 * D], op0=ALU.mult, op1=ALU.mult),
                     r=[("xm", 0), ("xm", 1), "s4b", "modx"], w=["h2"])
                Pq.op("pool", lambda e: e.tensor_tensor(out=h2[:], in0=h2[:], in1=modx[:, 3 * D:4 * D], op=ALU.add), r=["h2", "modx"], w=["h2"])
                Pq.op("act", lambda e: e.copy(out=h2b[:], in_=h2[:]), r=["h2"], w=["h2b"])
                Pq.dmaop("act", lambda e, t0=t0: e.dma_start(out=h2b_own_c[t0 // RCH].ap()[t0 % RCH:t0 % RCH + 128, :], in_=h2b[:]), r=["h2b"], w=["h2b_own"])
                def trh2(e):
                    ins = None
                    for k in range(8):
                        ins = e.transpose(out=B[2 + k // 4][:, (k % 4) * 128:(k % 4 + 1) * 128], in_=h2[:, k * 128:(k + 1) * 128], identity=ident[:])
                    return ins
                Pq.op("pe", trh2, r=["h2", "ident", ("m1", 0), ("m1", 1)], w=["b2", "b3"])
                Pq.op("act", lambda e: e.copy(out=h2T[:, 0:4, :].rearrange("p a b -> p (a b)"), in_=B[2][:, 0:512]), r=["b2"], w=[("h2T", 0)])
                Pq.op("dve", lambda e: e.tensor_copy(out=h2T[:, 4:8, :].rearrange("p a b -> p (a b)"), in_=B[3][:, 0:512]), r=["b3"], w=[("h2T", 1)])

                def mmrt(e):
                    ins = None
                    for k in range(8):
                        ins = e.matmul(B[1][:, 0:16], lhsT=h2T[:, k, :], rhs=wrt[:, k, :], start=(k == 0), stop=(k == 7))
                    return ins
                Pq.op("pe", mmrt, r=[("h2T", 0), ("h2T", 1), "wrt", "sg"], w=["b1"])
                Pq.op("dve", lambda e: e.tensor_copy(out=lg[:], in_=B[1][:, 0:16]), r=["b1"], w=["lg"])
                Pq.op("dve", lambda e: e.tensor_reduce(out=s4[:, 2:3], in_=lg[:], axis=AX.X, op=ALU.max), r=["lg"], w=["s4c"])
                Pq.op("dve", lambda e: e.tensor_scalar(out=s4[:, 3:4], in0=s4[:, 2:3], scalar1=-1.0, scalar2=None, op0=ALU.mult), r=["s4c"], w=["s4d"])
                hb_ = 0
                afc = slice(0, 16)
                Pq.op("act", lambda e, afc=afc: e.activation(out=af[:, afc], in_=lg[:], func=AF.Exp, bias=s4[:, 3:4], accum_out=s4[:, 4:5]), r=["lg", "s4d"], w=["af", "s4e"])
                Pq.op("dve", lambda e: e.reciprocal(out=s4[:, 5:6], in_=s4[:, 4:5]), r=["s4e"], w=["s4f"])
                Pq.op("dve", lambda e, afc=afc: e.tensor_scalar(out=af[:, afc], in0=af[:, afc], scalar1=s4[:, 5:6], scalar2=None, op0=ALU.mult), r=["af", "s4f"], w=["af"])
                Pq.op("pe", lambda e: e.transpose(out=B[4][0:48, 0:128], in_=af[:], identity=ident[:]), r=["af", "ident", ("m2", 0), ("m2", 1)], w=["b4"])
                Pq.op("act", lambda e, t0=t0: e.copy(out=affo[:, t0:t0 + 128], in_=B[4][0:16, 0:128]), r=["b4"], w=["affo"])
                return Pq.cap
            for i in range(0, NXT, 2):
                caps = [tile4(i, 0)] + ([tile4(i + 1, 1)] if i + 1 < NXT else [])
                interleave(P, caps, chunk=int(os.environ.get("ILV", "2")))
            P.flush()
        with ExitStack() as st:
            NH = NXO
            wk = sb(st, "wk", [48, NH])
            vals = sb(st, "vals", [48, CAPe])
            idxu = sb(st, "idxu", [48, CAPe], U32)
            idxf = sb(st, "idxf", [48, CAPe])
            jrev = sb(st, "jrev", [128, 128])
            tA = sb(st, "tA", [128, 2, 16])
            tB = sb(st, "tB", [128, 2, 16])
            tM = sb(st, "tM", [128, 3, 16])
            B5 = [ps(st, "ph5_%d" % i) for i in range(4)]
            P.dmaop("sp", lambda e: e.dma_start(out=jrev[:], in_=jrev_d), w=["jrev"])
            a01 = sb(st, "a01", [16, 2, NXO])
            selt = sb(st, "selt", [16, 8])
            P.dmaop("sp", lambda e: e.dma_start(out=selt[:], in_=sel_d), w=["selt"])
            zt = sb(st, "zt", [128, D])
            P.op("pool", lambda e: e.memset(zt[:], 0.0), w=["zt"])
            for r0 in range(0, 2 * NXO, 128):
                P.dmaop("sp" if (r0 // 128) % 2 == 0 else "act", lambda e, r0=r0: e.dma_start(out=acc[r0:r0 + 128, :], in_=zt[:]), r=["zt"], w=["acc0"])
            P.dmaop("sp", lambda e: e.dma_start(out=aff_own, in_=affo[:]), r=["affo"], w=["aff_own"])
            P.ccop(lambda e: e.collective_compute("AllGather", ALU.bypass, replica_groups=PAIRS, ins=[aff_own_t.ap().opt()], outs=[aff_all_t.ap().opt()]),
                   r=["aff_own"], w=["aff_all"])
            for c in range(NCHK):
                P.ccop(lambda e, c=c: e.collective_compute("AllGather", ALU.bypass, replica_groups=PAIRS, ins=[h2b_own_c[c].ap().opt()], outs=[h2b_ag_c[c].ap().opt()]),
                       r=["h2b_own"], w=[("h2b_ag", c)])
                for rk_ in range(2):
                    P.dmaop("act", lambda e, c=c, rk_=rk_: e.dma_start(out=h2b_all[rk_ * NXO + c * RCH:rk_ * NXO + (c + 1) * RCH, :],
                                                                      in_=h2b_ag_c[c].ap()[rk_ * RCH:(rk_ + 1) * RCH, :]), r=[("h2b_ag", c)], w=["h2b_all"])
            for rk_ in range(2):
                P.dmaop("sp", lambda e, rk_=rk_: e.dma_start(out=a01[:, rk_, :], in_=aff_all[16 * rk_:16 * rk_ + 16, :]), r=["aff_all"], w=[("a01", rk_)])
            for rk_ in range(2):
                for c0 in range(0, NXO, 512):
                    n = min(512, NXO - c0)
                    pb_ = B5[rk_]
                    P.op("pe", lambda e, rk_=rk_, c0=c0, n=n, pb_=pb_: e.matmul(pb_[32 * rk_:32 * rk_ + 8, 0:n], lhsT=selt[:, :], rhs=a01[:, rk_, c0:c0 + n],
                                                                             start=True, stop=True, tile_position=(0, 32 * rk_)),
                         r=["selt", ("a01", rk_)], w=[("b5", rk_)])
                    P.op("act", lambda e, rk_=rk_, c0=c0, n=n, pb_=pb_: e.copy(out=affT[32 * rk_:32 * rk_ + 8, c0:c0 + n], in_=pb_[32 * rk_:32 * rk_ + 8, 0:n]),
                         r=[("b5", rk_)], w=["affT"])
            P.op("dve", lambda e: e.tensor_copy(out=wk[:], in_=affT[:]), r=["affT"], w=["wk"])
            for r_ in range(CAPe // 8):
                sl = slice(r_ * 8, r_ * 8 + 8)
                P.op("dve", lambda e, sl=sl: e.max(out=vals[:, sl], in_=wk[:]), r=["wk"], w=[("vals", r_)])
                P.op("dve", lambda e, sl=sl: e.max_index(out=idxu[:, sl], in_max=vals[:, sl], in_values=wk[:]), r=["wk", ("vals", r_)], w=[("idxu", r_)])
                P.op("dve", lambda e, sl=sl: e.match_replace(out=wk[:], in_to_replace=vals[:, sl], in_values=wk[:], imm_value=-1.0),
                     r=["wk", ("vals", r_), ("idxu", r_)], w=["wk"])
            allv = [("vals", r_) for r_ in range(CAPe // 8)]
            alli = [("idxu", r_) for r_ in range(CAPe // 8)]
            P.op("dve", lambda e: e.tensor_copy(out=idxf[:], in_=idxu[:]), r=alli, w=["idxf"])
            P.op("dve", lambda e: e.tensor_scalar(out=idxf[32:48, :], in0=idxf[32:48, :], scalar1=float(NH), scalar2=None, op0=ALU.add), r=["idxf"], w=["idxf"])
            Jb = jrev[0:SLT, 128 - SLT:128]
            for rc in range(NRC):
                cs = slice(rc * SLT, (rc + 1) * SLT)
                rb = NRC - 1 - rc
                cb_ = slice(rb * SLT, (rb + 1) * SLT)
                for w_, src in ((0, vals), (1, idxf)):
                    rk = allv if w_ == 0 else ["idxf"]
                    P.op("pe", lambda e, cs=cs, src=src, w_=w_: e.transpose(out=B5[w_][0:SLT, 0:16], in_=src[0:16, cs], identity=ident[0:16, 0:16]),
                         r=rk + ["ident"], w=[("b5", w_)])
                    P.op("act", lambda e, w_=w_: e.copy(out=tA[0:SLT, w_, :], in_=B5[w_][0:SLT, 0:16]), r=[("b5", w_)], w=[("tA", w_)])
                    P.op("pe", lambda e, cb_=cb_, src=src, w_=w_: e.transpose(out=B5[2 + w_][0:SLT, 0:16], in_=src[32:48, cb_], identity=ident[32:48, 32:48]),
                         r=rk + ["ident"], w=[("b5", 2 + w_)])
                    P.op("act", lambda e, w_=w_: e.copy(out=tB[0:SLT, w_, :], in_=B5[2 + w_][0:SLT, 0:16]), r=[("b5", 2 + w_)], w=[("tB", w_)])
                    P.op("pe", lambda e, w_=w_: e.matmul(B5[2 + w_][0:SLT, 0:16], lhsT=Jb, rhs=tB[0:SLT, w_, :], start=True, stop=True),
                         r=[("tB", w_), "jrev"], w=[("b5", 2 + w_)])
                P.op("dve", lambda e: e.tensor_tensor(out=tM[0:SLT, 0, :], in0=tA[0:SLT, 0, :], in1=B5[2][0:SLT, 0:16], op=ALU.is_gt),
                     r=[("tA", 0), ("b5", 2)], w=[("tM", 0)])
                P.op("dve", lambda e, rc=rc: e.tensor_tensor(out=gateT[0:SLT, rc, :], in0=tA[0:SLT, 0, :], in1=B5[2][0:SLT, 0:16], op=ALU.max),
                     r=[("tA", 0), ("b5", 2)], w=["gateT"])
                P.op("dve", lambda e: e.tensor_tensor(out=tM[0:SLT, 1, :], in0=tA[0:SLT, 1, :], in1=B5[3][0:SLT, 0:16], op=ALU.subtract),
                     r=[("tA", 1), ("b5", 3)], w=[("tM", 1)])
                P.op("dve", lambda e: e.tensor_tensor(out=tM[0:SLT, 1, :], in0=tM[0:SLT, 1, :], in1=tM[0:SLT, 0, :], op=ALU.mult),
                     r=[("tM", 1), ("tM", 0)], w=[("tM", 1)])
                P.op("dve", lambda e: e.tensor_tensor(out=tM[0:SLT, 2, :], in0=tM[0:SLT, 1, :], in1=B5[3][0:SLT, 0:16], op=ALU.add),
                     r=[("tM", 1), ("b5", 3)], w=[("tM", 2)])
                P.op("dve", lambda e, rc=rc: e.tensor_copy(out=idxT[0:SLT, rc, :], in_=tM[0:SLT, 2, :]), r=[("tM", 2)], w=["idxT"])
            P.flush()
        sp45.close()
        NEe = NE // 2 if debug == 0 else int(os.environ.get("NEE", "8"))
        with ExitStack() as st:
            identb = sb(st, "identb6", [128, 128], BF16)
            xs = sb(st, "xs", [128, 1, NRC, D], BF16)
            xsT = sb(st, "xsT", [128, 2, 8, CAPe], BF16)
            wg = sb(st, "wg", [128, 3, 8, 512], BF16)
            wu = sb(st, "wu", [128, 3, 8, 512], BF16)
            wd = sb(st, "wd", [128, 2, 22, 512], BF16)
            sgt = sb(st, "sgt", [128, 2, CAPe])
            hidT = sb(st, "hidT", [128, 22, CAPe], BF16)
            ys = sb(st, "ys", [128, 1, NRC, D])
            B = [ps(st, "ph6_%d" % i) for i in range(8)]
            B0b = B[0][:].bitcast(BF16)
            P.op("dve", lambda e: e.tensor_copy(out=identb[:], in_=ident[:]), r=["ident"], w=["identb6"])
            wgi = 0
            wdi = 0
            def prep(ex):
                sl = ex % 2
                for rc in range(NRC):
                    P.dmaop("pool", lambda e, rc=rc, ex=ex, sl=sl: e.indirect_dma_start(
                        out=xs[0:SLT, 0, rc, :], out_offset=None, in_=h2b_all[0:2 * NXO, :],
                        in_offset=bass.IndirectOffsetOnAxis(ap=idxT[0:SLT, rc, ex:ex + 1], axis=0)),
                        r=["idxT", "h2b_all"], w=[("xs", 0, rc)])

                    def trx(e, rc=rc, sl=sl):
                        ins = None
                        for k in range(8):
                            ins = e.transpose(out=B0b[:, k * 128:k * 128 + SLT], in_=xs[0:SLT, 0, rc, k * 128:(k + 1) * 128], identity=identb[0:SLT, 0:SLT])
                        return ins
                    P.op("pe", trx, r=[("xs", 0, rc), "identb6"], w=["b0"])
                    P.op("act", lambda e, rc=rc, sl=sl: e.copy(out=xsT[:, sl, :, rc * SLT:(rc + 1) * SLT],
                                                               in_=B0b[:, 0:1024].rearrange("p (k c) -> p k c", k=8)[:, :, 0:SLT]), r=["b0"], w=[("xsT", sl)])
            prep(0)
            pending = []
            for ex in range(NEe):
                xsl = ex % 2
                wgv = w_e_gate[ex].rearrange("(dc p) f -> p dc f", p=128)
                wuv = w_e_up[ex].rearrange("(dc p) f -> p dc f", p=128)
                wdv = w_e_down[ex].rearrange("(fc p) d -> p fc d", p=128)
                for grp in range(6):
                    ws = wgi % 3
                    wgi += 1
                    f0 = grp * 512
                    fw = min(512, FF - f0)
                    P.dmaop("pool", lambda e, ws=ws, f0=f0, fw=fw, wgv=wgv: e.dma_start(out=wg[:, ws, :, 0:fw], in_=wgv[:, :, f0:f0 + fw]), w=[("wg", ws)])
                    P.dmaop("pool", lambda e, ws=ws, f0=f0, fw=fw, wuv=wuv: e.dma_start(out=wu[:, ws, :, 0:fw], in_=wuv[:, :, f0:f0 + fw]), w=[("wu", ws)])
                    if grp == 1:
                        for f_ in pending:
                            f_()
                        pending = []
                    for q in range(fw // 128):
                        fc = grp * 4 + q
                        pg = B[1 + fc % 2]
                        pu = B[3 + fc % 2]

                        def mmgu(e, ws=ws, q=q, pg=pg, pu=pu, xsl=xsl):
                            ins = None
                            for k in range(8):
                                e.matmul(pg[:, 0:CAPe], lhsT=wg[:, ws, k, q * 128:(q + 1) * 128], rhs=xsT[:, xsl, k, :], start=(k == 0), stop=(k == 7))
                            for k in range(8):
                                ins = e.matmul(pu[:, 0:CAPe], lhsT=wu[:, ws, k, q * 128:(q + 1) * 128], rhs=xsT[:, xsl, k, :], start=(k == 0), stop=(k == 7))
                            return ins
                        P.op("pe", mmgu, r=[("wg", ws), ("wu", ws), ("xsT", xsl)], w=[("b6", 1 + fc % 2), ("b6", 3 + fc % 2)])
                        P.op("act", lambda e, fc=fc, pg=pg: e.activation(out=sgt[:, fc % 2, :], in_=pg[:, 0:CAPe], func=AF.Silu),
                             r=[("b6", 1 + fc % 2)], w=[("sgt", fc % 2)])
                        P.op("dve", lambda e, fc=fc, pu=pu: e.tensor_tensor(out=hidT[:, fc, :], in0=sgt[:, fc % 2, :], in1=pu[:, 0:CAPe], op=ALU.mult),
                             r=[("sgt", fc % 2), ("b6", 3 + fc % 2)], w=[("hidT", fc)])
                hk_ = [("hidT", fc) for fc in range(22)]
                if ex + 1 < NEe:
                    prep(ex + 1)
                for dq in range(2):
                    ws = wdi % 2
                    wdi += 1
                    P.dmaop("pool", lambda e, ws=ws, dq=dq, wdv=wdv: e.dma_start(out=wd[:, ws], in_=wdv[:, :, dq * 512:(dq + 1) * 512]), w=[("wd", ws)])
                    for rc in range(NRC):
                        py = B[5 + (dq * NRC + rc) % 2]
                        pyk = ("b6", 5 + (dq * NRC + rc) % 2)

                        def mmd(e, ws=ws, rc=rc, py=py):
                            ins = None
                            for fc in range(22):
                                ins = e.matmul(py[0:SLT, 0:512], lhsT=hidT[:, fc, rc * SLT:(rc + 1) * SLT], rhs=wd[:, ws, fc, :], start=(fc == 0), stop=(fc == 21))
                            return ins
                        P.op("pe", mmd, r=hk_ + [("wd", ws)], w=[pyk])
                        P.op("dve", lambda e, rc=rc, dq=dq, py=py, ex=ex, xsl=xsl: e.scalar_tensor_tensor(
                            out=ys[0:SLT, 0, rc, dq * 512:(dq + 1) * 512], in0=py[0:SLT, 0:512], scalar=gateT[0:SLT, rc, ex:ex + 1],
                            in1=modx[0:SLT, 5 * D + dq * 512:5 * D + (dq + 1) * 512], op0=ALU.mult, op1=ALU.mult),
                            r=[pyk, "gateT", "modx"], w=[("ys", 0, rc, dq)])
                def scat(ex=ex):
                    prevk = [("outx", (ex - 1) % 2, rc2) for rc2 in range(NRC)] if ex > 0 else ["acc0"]
                    for rc in range(NRC):
                        P.dmaop("pool", lambda e, rc=rc, ex=ex: e.indirect_dma_start(
                            out=acc[0:2 * NXO, :], out_offset=bass.IndirectOffsetOnAxis(ap=idxT[0:SLT, rc, ex:ex + 1], axis=0),
                            in_=ys[0:SLT, 0, rc, :], in_offset=None, compute_op=ALU.add),
                            r=[("ys", 0, rc, dq) for dq in range(2)] + ["idxT"] + prevk, w=[("outx", ex % 2, rc)])
                pending.append(scat)
            for f_ in pending:
                f_()
            P.flush()
        with ExitStack() as st7:
            fa = sb(st7, "fa", [128, 4 * D])
            P.ccop(lambda e: e.collective_compute("ReduceScatter", ALU.add, replica_groups=PAIRS, ins=[acc_t.ap().opt()], outs=[rs_out_t.ap().opt()]),
                   w=["rs_out"])
            for i in range(NXT):
                t0 = i * 128
                k = i % 2
                P.dmaop("sp", lambda e, t0=t0, k=k: e.dma_start(out=fa[:, k * 2 * D:k * 2 * D + D], in_=xm_s[t0:t0 + 128, :]), w=[("fa", k, 0)])
                P.dmaop("act", lambda e, t0=t0, k=k: e.dma_start(out=fa[:, k * 2 * D + D:(k + 1) * 2 * D], in_=rs_out[t0:t0 + 128, :]), r=["rs_out"], w=[("fa", k, 1)])
                P.op("dve", lambda e, k=k: e.tensor_tensor(out=fa[:, k * 2 * D:k * 2 * D + D], in0=fa[:, k * 2 * D:k * 2 * D + D],
                                                          in1=fa[:, k * 2 * D + D:(k + 1) * 2 * D], op=ALU.add), r=[("fa", k, 0), ("fa", k, 1)], w=[("fa", k, 0)])
                P.dmaop("sp", lambda e, t0=t0, k=k: e.dma_start(out=out[t0:t0 + 128, :], in_=fa[:, k * 2 * D:k * 2 * D + D]), r=[("fa", k, 0)], w=["out"])
            P.flush()
        if debug == 6:
            DONE.append(1)
    if DONE:
        es.close()
        return nc

    if debug == 3:
        dbg = nc.dram_tensor("dbg", [H, 64, 256], F32, kind="ExternalOutput").ap()
        P.dmaop("sp", lambda e: e.dma_start(out=dbg, in_=attnT_s[:, :, 0:256]), w=["dbg"])
        P.flush()
        es.close()
        return nc

    if debug == 2:
        dbg = nc.dram_tensor("dbg", [H, QK, 512], BF16, kind="ExternalOutput").ap()
        dbg2 = nc.dram_tensor("dbg2", [512, 512], F32, kind="ExternalOutput").ap()
        dbg3 = nc.dram_tensor("dbg3", [H, QK, 256], BF16, kind="ExternalOutput").ap()
        P.dmaop("sp", lambda e: e.dma_start(out=dbg, in_=kT_s[:, :, 0:512]), w=["dbg"])
        P.dmaop("sp", lambda e: e.dma_start(out=dbg2, in_=u_s[0:512, :]), w=["dbg2"])
        P.dmaop("sp", lambda e: e.dma_start(out=dbg3, in_=qT_s[:, :, 0:256]), w=["dbg3"])
        P.flush()
        es.close()
        return nc

    if debug == 1:
        dbg = nc.dram_tensor("dbg", [128, 6 * D], F32, kind="ExternalOutput").ap()
        P.dmaop("sp", lambda e: e.dma_start(out=dbg, in_=modx[:]), r=["modx"], w=["dbg"])
        P.flush()
        es.close()
        return nc

    es.close()
    return nc


def _consts():
    ident = np.eye(128, dtype=np.float32)
    n = NX
    rows = n // 64
    row = np.repeat(np.arange(rows, dtype=np.float32), 64)
    col = np.tile(np.arange(64, dtype=np.float32), rows)
    inv = (10000.0 ** (-np.arange(8, dtype=np.float32) / 8)).astype(np.float32)
    ang = np.stack([row[:, None] * inv, col[:, None] * inv], axis=1).astype(np.float32)
    rope = np.zeros((NT, 32), np.float32)
    rope[:NCTX, :16] = 1.0
    rope[NCTX:, :16] = np.cos(ang).reshape(n, 16)
    rope[NCTX:, 16:] = np.sin(ang).reshape(n, 16)
    cm = np.zeros((128, 256), np.float32)
    for jp in range(8):
        for j in range(8):
            if jp <= j:
                cm[jp * 16:(jp + 1) * 16, j * 16:(j + 1) * 16] = 1.0
            if jp >= j:
                cm[jp * 16:(jp + 1) * 16, 128 + j * 16:128 + (j + 1) * 16] = 1.0
    return ident, rope, cm


def make_in_maps(inputs, nx=NX):
    ident, rope, cm = _consts()
    f = lambda a: np.ascontiguousarray(np.asarray(a, dtype=np.float32))
    maps = []
    dirk = ("ssm_lam_re", "ssm_lam_im", "ssm_log_dt", "ssm_b_re", "ssm_b_im", "ssm_c_re", "ssm_c_im")
    for b in range(NCORES // 2):
        for r in range(2):
            x_b = np.asarray(inputs["x"][b])[:nx]
            ctx_b = np.asarray(inputs["ctx"][b])
            rope_x = rope[NCTX:NCTX + nx]
            if r == 1:
                x_b, ctx_b, rope_x = x_b[::-1], ctx_b[::-1], rope_x[::-1]
            xc = np.zeros((NT, D), np.float32)
            xc[:NCTX] = ctx_b
            xc[NCTX:NCTX + nx] = x_b
            rope_r = rope.copy()
            rope_r[NCTX:NCTX + nx] = rope_x
            sel = np.zeros((NE, NE // 2), np.float32)
            sel[np.arange(NE // 2) + (NE // 2) * r, np.arange(NE // 2)] = 1.0
            m = {"xc": xc, "cb": f(inputs["c"][b]), "c_ctx": f(inputs["c_ctx"]), "ident": ident, "rope": rope_r, "cmask": cm,
                 "jrev": np.ascontiguousarray(ident[::-1]), "sel": sel}
            for k in ["w_ada", "b_ada", "norm1_g", "norm2_g", "w_in", "q_a_g", "w_qb", "kv_a_g", "w_kvb", "q_norm_g",
                      "k_norm_g", "w_mla_o", "ssm_d", "w_glu", "b_glu", "w_ssm_o", "w_out", "w_router"]:
                m[k] = f(np.asarray(inputs[k])[0])
            for k in dirk:
                a_ = np.asarray(inputs[k])[0]
                m[k] = f(a_[::-1] if r == 1 else a_)
            e0 = (NE // 2) * r
            for k in ("w_e_gate", "w_e_up", "w_e_down"):
                m[k] = f(np.asarray(inputs[k])[0][e0:e0 + NE // 2])
            maps.append(m)
    return maps


def assemble(results, nx=NX):
    h = nx // 2
    out = np.zeros((NCORES // 2, nx, D), np.float32)
    for b in range(NCORES // 2):
        out[b, :h] = np.asarray(results[2 * b]["out"], dtype=np.float32)[:h]
        out[b, h:] = np.asarray(results[2 * b + 1]["out"], dtype=np.float32)[:h][::-1]
    return out


def kernel(**inputs):
    nc = build()
    maps = make_in_maps(inputs)
    res = run_bass_kernel_spmd(nc, maps, core_ids=list(range(NCORES)))
    return assemble(res.results)
```
